# Optimizing a Trainium2 kernel written in Bass

```python
import jax, jax.numpy as jnp
from jax import lax
import numpy as np

D_MODEL = 1024
BATCH = 8
SEQ = 2048
DEPTH = 4

CHUNK = 64
SB_BLOCK = 128
NORM_EPS = 1e-6
MASK_NEG = -1e30
LB_FLOOR = 1e-30
SB_HEADS = 8
SB_HEAD_DIM = 64
SB_WIDTH = SB_HEADS * SB_HEAD_DIM
HG_HEADS = 4
HG_HEAD_DIM = 128
HG_WIDTH = HG_HEADS * HG_HEAD_DIM
ML_HEADS = 4
ML_HEAD_DIM = 128
ML_WIDTH = ML_HEADS * ML_HEAD_DIM
ML_CONV = 4
N_BRANCH = 3
BRANCH_WIDTH = 512
IN_SPLITS = (SB_WIDTH, SB_WIDTH, SB_WIDTH,
             HG_WIDTH, HG_WIDTH, HG_WIDTH, HG_WIDTH,
             2 * ML_WIDTH, ML_WIDTH, ML_WIDTH, 2 * ML_HEADS,
             N_BRANCH * D_MODEL)
IN_WIDTH = 3 * SB_WIDTH + 4 * HG_WIDTH + 4 * ML_WIDTH + 2 * ML_HEADS + N_BRANCH * D_MODEL
PK_HEADS = 8
PK_NKEYS = 128
PK_NEXPERTS = PK_NKEYS * PK_NKEYS
PK_DKEY = 128
PK_TOPK = 16
PK_TOKEN_GROUP = 128

kernel_name = "hybrid_sb_hgrn2_mlstm_peer_adaln"


def rmsnorm(x, g):
    xf = x.astype(jnp.float32)
    y = xf * lax.rsqrt(jnp.mean(jnp.square(xf), axis=-1, keepdims=True) + NORM_EPS)
    return (y * g.astype(jnp.float32)).astype(x.dtype)


def _to_chunks(a):
    B, S, H, d = a.shape
    return a.reshape(B, S // CHUNK, CHUNK, H, d).transpose(1, 0, 3, 2, 4)


def _from_chunks(a):
    nC, B, H, L, d = a.shape
    return a.transpose(1, 0, 3, 2, 4).reshape(B, nC * L, H, d)


def stick_breaking_attention(q, k, v):
    B, S, H, dh = q.shape
    f32 = jnp.float32
    scale = dh ** -0.5
    outs = []
    for blk in range(S // SB_BLOCK):
        t0 = blk * SB_BLOCK
        kv_len = t0 + SB_BLOCK
        qb = q[:, t0:kv_len].astype(f32)
        kb = k[:, :kv_len].astype(f32)
        vb = v[:, :kv_len].astype(f32)
        z = jnp.einsum('bthd,bshd->bhts', qb, kb) * scale
        t_idx = t0 + jnp.arange(SB_BLOCK)[:, None]
        s_idx = jnp.arange(kv_len)[None, :]
        past = s_idx < t_idx
        log_keep = jnp.where(past, jax.nn.log_sigmoid(-z), 0.0)
        later = lax.cumsum(log_keep, axis=3, reverse=True) - log_keep
        w = jnp.where(past, jnp.exp(jax.nn.log_sigmoid(z) + later), 0.0)
        outs.append(jnp.einsum('bhts,bshd->bthd', w, vb))
    return jnp.concatenate(outs, axis=1)


def hgrn2_chunkwise(q, log_f, k, i):
    B, S, H, dk = q.shape
    dv = i.shape[-1]
    f32 = jnp.float32
    causal = jnp.tril(jnp.ones((CHUNK, CHUNK), dtype=bool))
    mask = causal[None, None, :, :, None]

    def step(state, xs):
        qb, fb, kb, ib = xs
        b = jnp.cumsum(fb, axis=2)
        diff = b[:, :, :, None, :] - b[:, :, None, :, :]
        decay = jnp.where(mask, jnp.exp(jnp.where(mask, diff, 0.0)), 0.0)
        scores = jnp.einsum('bhtsk,bhsk->bhts', decay * qb[:, :, :, None, :], kb)
        o = (jnp.einsum('bhts,bhsv->bhtv', scores, ib)
             + jnp.einsum('bhtk,bhkv->bhtv', qb * jnp.exp(b), state))
        b_last = b[:, :, -1, :]
        new_state = (jnp.exp(b_last)[..., None] * state
                     + jnp.einsum('bhsk,bhsv->bhkv', kb * jnp.exp(b_last[:, :, None, :] - b), ib))
        return new_state, o

    xs = tuple(_to_chunks(a.astype(f32)) for a in (q, log_f, k, i))
    s0 = jnp.zeros((B, H, dk, dv), f32)
    _, o = lax.scan(step, s0, xs)
    return _from_chunks(o)


def mlstm_chunkwise(q, k, v, log_i, log_f):
    B, S, H, d = q.shape
    f32 = jnp.float32
    k = k.astype(f32) * (d ** -0.5)
    causal = jnp.tril(jnp.ones((CHUNK, CHUNK), dtype=bool))

    def step(carry, xs):
        C, n, m = carry
        qb, kb, vb, ib, fb = xs
        b = jnp.cumsum(fb, axis=-1)
        log_d = jnp.where(causal, b[..., :, None] - b[..., None, :] + ib[..., None, :], MASK_NEG)
        log_inter = b + m[..., None]
        m_t = jnp.maximum(log_inter, jnp.max(log_d, axis=-1))
        dmat = jnp.where(causal, jnp.exp(log_d - m_t[..., None]), 0.0)
        inter = jnp.exp(log_inter - m_t)
        qk = jnp.einsum('bhtd,bhsd->bhts', qb, kb) * dmat
        num = (jnp.einsum('bhts,bhsv->bhtv', qk, vb)
               + inter[..., None] * jnp.einsum('bhvk,bhtk->bhtv', C, qb))
        den = jnp.sum(qk, axis=-1) + inter * jnp.einsum('bhk,bhtk->bht', n, qb)
        h = num / jnp.maximum(jnp.abs(den), jnp.exp(-m_t))[..., None]
        b_last = b[..., -1]
        log_w = b_last[..., None] - b + ib
        m_new = jnp.maximum(b_last + m, jnp.max(log_w, axis=-1))
        w = jnp.exp(log_w - m_new[..., None])
        dec = jnp.exp(b_last + m - m_new)
        C_new = dec[..., None, None] * C + jnp.einsum('bhsv,bhsk->bhvk', vb * w[..., None], kb)
        n_new = dec[..., None] * n + jnp.einsum('bhs,bhsk->bhk', w, kb)
        return (C_new, n_new, m_new), h

    qc, kc, vc = (_to_chunks(a.astype(f32)) for a in (q, k, v))
    ic = _to_chunks(log_i.astype(f32)[..., None])[..., 0]
    fc = _to_chunks(log_f.astype(f32)[..., None])[..., 0]
    carry0 = (jnp.zeros((B, H, d, d), f32), jnp.zeros((B, H, d), f32), jnp.zeros((B, H), f32))
    _, h = lax.scan(step, carry0, (qc, kc, vc, ic, fc))
    return _from_chunks(h)


def causal_short_conv(x, w, b):
    K, C = w.shape
    y = lax.conv_general_dilated(x, w[:, None, :].astype(x.dtype), window_strides=(1,),
                                 padding=[(K - 1, 0)], dimension_numbers=('NWC', 'WIO', 'NWC'),
                                 feature_group_count=C)
    return y + b


def hybrid_mixer(h, w_in, conv_w, conv_b, gate_b, lower_bound, hg_norm_g, ml_norm_g, w_branch, w_out):
    B, S, _ = h.shape
    proj = h @ w_in
    idx = np.cumsum(IN_SPLITS)[:-1].tolist()
    (sb_q, sb_k, sb_v, hg_q, hg_f, hg_i, hg_g,
     ml_qk, ml_v, ml_o, ml_if, br_g) = jnp.split(proj, idx, axis=-1)

    sb_shape = (B, S, SB_HEADS, SB_HEAD_DIM)
    y_a = stick_breaking_attention(sb_q.reshape(sb_shape), sb_k.reshape(sb_shape),
                                   sb_v.reshape(sb_shape)).reshape(B, S, SB_WIDTH)

    hg_shape = (B, S, HG_HEADS, HG_HEAD_DIM)
    lb = lower_bound.reshape(HG_HEADS, HG_HEAD_DIM)
    z = hg_f.astype(jnp.float32).reshape(hg_shape)
    log_lb = jnp.log(jnp.maximum(lb, LB_FLOOR))
    log_f = jnp.logaddexp(log_lb, jnp.log1p(-lb) + jax.nn.log_sigmoid(z))
    k_in = (1.0 - lb) * jax.nn.sigmoid(-z)
    o_b = hgrn2_chunkwise(hg_q.reshape(hg_shape), log_f, k_in, jax.nn.silu(hg_i).reshape(hg_shape))
    y_b = (rmsnorm(o_b, hg_norm_g.reshape(HG_HEADS, HG_HEAD_DIM)).reshape(B, S, HG_WIDTH)
           * jax.nn.silu(hg_g.astype(jnp.float32)))

    ml_shape = (B, S, ML_HEADS, ML_HEAD_DIM)
    qk = jax.nn.silu(causal_short_conv(ml_qk, conv_w, conv_b))
    ml_q, ml_k = jnp.split(qk, 2, axis=-1)
    gates = ml_if.astype(jnp.float32) + gate_b
    log_i = gates[..., :ML_HEADS]
    log_fc = jax.nn.log_sigmoid(gates[..., ML_HEADS:])
    h_c = mlstm_chunkwise(ml_q.reshape(ml_shape), ml_k.reshape(ml_shape), ml_v.reshape(ml_shape), log_i, log_fc)
    y_c = (rmsnorm(h_c, ml_norm_g.reshape(ML_HEADS, ML_HEAD_DIM)).reshape(B, S, ML_WIDTH)
           * jax.nn.sigmoid(ml_o.astype(jnp.float32)))

    branches = jnp.stack([y_a, y_b, y_c], axis=2)
    up = jnp.einsum('bsgm,gmd->bsgd', branches, w_branch.astype(jnp.float32))
    gate = jax.nn.sigmoid(br_g.astype(jnp.float32).reshape(B, S, N_BRANCH, D_MODEL))
    merged = jnp.sum(gate * up, axis=2)
    return (merged @ w_out.astype(jnp.float32)).astype(h.dtype)


def peer_ffn(h, w_q, keys, u, v):
    B, S, D = h.shape
    T = B * S
    tokens = h.reshape(T // PK_TOKEN_GROUP, PK_TOKEN_GROUP, D)

    def group(xb):
        G = xb.shape[0]
        q = (xb @ w_q).reshape(G, PK_HEADS, 2, PK_DKEY // 2)
        s = jnp.einsum('thpd,hpnd->thpn', q, keys, preferred_element_type=jnp.float32)
        sv, si = lax.top_k(s, PK_TOPK)
        cand = (sv[:, :, 0, :, None] + sv[:, :, 1, None, :]).reshape(G, PK_HEADS, PK_TOPK * PK_TOPK)
        cand_idx = (si[:, :, 0, :, None] * PK_NKEYS + si[:, :, 1, None, :]).reshape(G, PK_HEADS, PK_TOPK * PK_TOPK)
        top_s, pos = lax.top_k(cand, PK_TOPK)
        idx = jnp.take_along_axis(cand_idx, pos, axis=-1)
        g = jax.nn.softmax(top_s, axis=-1)
        u_sel = jnp.take(u, idx, axis=0)
        v_sel = jnp.take(v, idx, axis=0)
        a = jax.nn.gelu(jnp.einsum('td,thkd->thk', xb, u_sel, preferred_element_type=jnp.float32))
        return jnp.einsum('thk,thkd->td', g * a, v_sel.astype(jnp.float32)).astype(h.dtype)

    return lax.map(group, tokens).reshape(B, S, D)


def setup_inputs(seed: int = 0) -> dict:
    key = jax.random.key(seed)
    ks = jax.random.split(key, 24)
    f32 = jnp.float32

    def nrm(k, shape, s):
        return jax.random.normal(k, shape, f32) * s

    f_bias = jnp.broadcast_to(jnp.linspace(3.0, 6.0, ML_HEADS, dtype=f32), (DEPTH, ML_HEADS))
    ml_gate_b = jnp.concatenate([nrm(ks[9], (DEPTH, ML_HEADS), 0.1),
                                 f_bias + nrm(ks[10], (DEPTH, ML_HEADS), 0.1)], axis=-1)
    return {
        "x": nrm(ks[0], (BATCH, SEQ, D_MODEL), 1.0),
        "c": nrm(ks[1], (BATCH, D_MODEL), 1.0),
        "mod_w": nrm(ks[2], (DEPTH, D_MODEL, 6 * D_MODEL), 0.5 * D_MODEL ** -0.5),
        "mod_b": nrm(ks[3], (DEPTH, 6 * D_MODEL), 0.02),
        "norm_mix_g": 1.0 + nrm(ks[4], (DEPTH, D_MODEL), 0.02),
        "norm_ffn_g": 1.0 + nrm(ks[5], (DEPTH, D_MODEL), 0.02),
        "w_in": nrm(ks[6], (DEPTH, D_MODEL, IN_WIDTH), D_MODEL ** -0.5),
        "ml_conv_w": nrm(ks[7], (DEPTH, ML_CONV, 2 * ML_WIDTH), ML_CONV ** -0.5),
        "ml_conv_b": nrm(ks[8], (DEPTH, 2 * ML_WIDTH), 0.02),
        "ml_gate_b": ml_gate_b,
        "hg_lb_logits": nrm(ks[11], (DEPTH, HG_WIDTH), 0.1),
        "hg_norm_g": 1.0 + nrm(ks[12], (DEPTH, HG_WIDTH), 0.02),
        "ml_norm_g": 1.0 + nrm(ks[13], (DEPTH, ML_WIDTH), 0.02),
        "w_branch": nrm(ks[14], (DEPTH, N_BRANCH, BRANCH_WIDTH, D_MODEL), BRANCH_WIDTH ** -0.5),
        "w_out": nrm(ks[15], (DEPTH, D_MODEL, D_MODEL), D_MODEL ** -0.5),
        "pk_wq": nrm(ks[16], (DEPTH, D_MODEL, PK_HEADS * PK_DKEY), D_MODEL ** -0.5),
        "pk_keys": nrm(ks[17], (DEPTH, PK_HEADS, 2, PK_NKEYS, PK_DKEY // 2), (PK_DKEY // 2) ** -0.5),
        "pk_u": nrm(ks[18], (DEPTH, PK_NEXPERTS, D_MODEL), D_MODEL ** -0.5),
        "pk_v": nrm(ks[19], (DEPTH, PK_NEXPERTS, D_MODEL), PK_HEADS ** -0.5),
        "final_g": 1.0 + nrm(ks[20], (D_MODEL,), 0.02),
    }


def reference(x, c, mod_w, mod_b, norm_mix_g, norm_ffn_g, w_in, ml_conv_w, ml_conv_b, ml_gate_b,
              hg_lb_logits, hg_norm_g, ml_norm_g, w_branch, w_out, pk_wq, pk_keys, pk_u, pk_v, final_g):
    cond = jax.nn.silu(c.astype(jnp.float32))
    lb_soft = jax.nn.softmax(hg_lb_logits.astype(jnp.float32), axis=0)
    lower_bounds = jnp.cumsum(lb_soft, axis=0) - lb_soft[0]
    for l in range(DEPTH):
        mod = (cond @ mod_w[l].astype(jnp.float32) + mod_b[l]).astype(x.dtype)[:, None, :]
        shift1, scale1, gate1, shift2, scale2, gate2 = jnp.split(mod, 6, axis=-1)
        h = rmsnorm(x, norm_mix_g[l]) * (1 + scale1) + shift1
        y = hybrid_mixer(h, w_in[l], ml_conv_w[l], ml_conv_b[l], ml_gate_b[l], lower_bounds[l],
                         hg_norm_g[l], ml_norm_g[l], w_branch[l], w_out[l])
        x = x + gate1 * y
        h = rmsnorm(x, norm_ffn_g[l]) * (1 + scale2) + shift2
        x = x + gate2 * peer_ffn(h, pk_wq[l], pk_keys[l], pk_u[l], pk_v[l])
    return rmsnorm(x, final_g)
```

```python
from contextlib import ExitStack
import numpy as np
import concourse.bass as bass
import concourse.mybir as mybir
from concourse.bass_utils import run_bass_kernel_spmd

F32 = mybir.dt.float32
BF16 = mybir.dt.bfloat16
U32 = mybir.dt.uint32
I32 = mybir.dt.int32
AF = mybir.ActivationFunctionType
ALU = mybir.AluOpType
AX = mybir.AxisListType

D = 1024
S_LEN = 2048
DEPTH = 4
NT = S_LEN // 128
INW = 8712
EPS = 1e-6
PEER_IMPLEMENTED = True


class Sched:
    ENG = ("pe", "act", "dve", "pool", "sp")

    def __init__(self, nc, dma_slots=None, same_engine_sync=True):
        self.nc = nc
        self.ops = {e: [] for e in self.ENG}
        self.count = {e: 0 for e in self.ENG}
        self.sem = {}
        self.last_w = {}
        self.readers = {}
        self.same_engine_sync = same_engine_sync
        self.dma_slots_n = dma_slots or {"sp": 8, "pool": 12, "act": 4}
        self.dma_sems = {}
        self.dma_rr = {}
        self.seen = {e: {} for e in self.ENG}
        self._ctx = []
        self.n_ops = 0

    def open(self):
        nc = self.nc
        for e in self.ENG:
            cm = nc.semaphore("s_" + e)
            self.sem[e] = cm.__enter__()
            self._ctx.append(cm)
        for q, n in self.dma_slots_n.items():
            self.dma_sems[q] = []
            self.dma_rr[q] = 0
            for i in range(n):
                cm = nc.semaphore("sd_%s%d" % (q, i))
                s = cm.__enter__()
                self._ctx.append(cm)
                self.dma_sems[q].append(dict(sem=s, count=0, name="d_%s%d" % (q, i)))

    def close(self):
        for cm in reversed(self._ctx):
            cm.__exit__(None, None, None)

    def _tok_wait(self, tok):
        if tok[0] == "c":
            return (self.sem[tok[1]], tok[1], tok[2])
        d = self.dma_sems[tok[1]][tok[2]]
        return (d["sem"], d["name"], tok[3])

    def _collect(self, eng, reads, writes):
        toks = []
        for k in reads:
            t = self.last_w.get(k)
            if t is not None:
                toks.append(t)
        for k in writes:
            t = self.last_w.get(k)
            if t is not None:
                toks.append(t)
            toks.extend(self.readers.get(k, ()))
        waits = {}
        for t in toks:
            if t[0] == "c" and t[1] == eng and not self.same_engine_sync:
                continue
            s, name, v = self._tok_wait(t)
            if self.seen[eng].get(name, 0) >= v:
                continue
            if name not in waits or waits[name][1] < v:
                waits[name] = (s, v)
        for name, (s, v) in waits.items():
            self.seen[eng][name] = v
        return list(waits.values())

    def _commit(self, tok, reads, writes):
        for k in reads:
            self.readers.setdefault(k, []).append(tok)
        for k in writes:
            self.last_w[k] = tok
            self.readers[k] = []

    def op(self, eng, fn, reads=(), writes=()):
        waits = self._collect(eng, reads, writes)
        self.count[eng] += 1
        tok = ("c", eng, self.count[eng])
        self.ops[eng].append((fn, waits, "c", None))
        self._commit(tok, reads, writes)
        self.n_ops += 1
        return tok

    def dma(self, eng, fn, reads=(), writes=()):
        slot = self.dma_rr[eng]
        self.dma_rr[eng] = (slot + 1) % len(self.dma_sems[eng])
        d = self.dma_sems[eng][slot]
        waits = self._collect(eng, reads, writes)
        if d["count"] > 0 and self.seen[eng].get(d["name"], 0) < d["count"]:
            waits.append((d["sem"], d["count"]))
            self.seen[eng][d["name"]] = d["count"]
        d["count"] += 16
        tok = ("d", eng, slot, d["count"])
        self.ops[eng].append((fn, waits, "d", d["sem"]))
        self._commit(tok, reads, writes)
        self.n_ops += 1
        return tok

    def barrier(self):
        for e in self.ENG:
            waits = []
            for e2 in self.ENG:
                v = self.count[e2]
                if v > 0 and self.seen[e].get(e2, 0) < v and e2 != e:
                    waits.append((self.sem[e2], v))
                    self.seen[e][e2] = v
            for q in self.dma_sems:
                for d in self.dma_sems[q]:
                    if d["count"] > 0 and self.seen[e].get(d["name"], 0) < d["count"]:
                        waits.append((d["sem"], d["count"]))
                        self.seen[e][d["name"]] = d["count"]
            if self.count[e] > 0:
                waits.append((self.sem[e], self.count[e]))
                self.seen[e][e] = self.count[e]
            self.ops[e].append((None, waits, "w", None))

    def emit(self):
        nc = self.nc
        sched = self
        ops = self.ops
        self.ops = {e: [] for e in self.ENG}
        with nc.Block() as block:
            def run(engname, e):
                for fn, waits, kind, dsem in ops[engname]:
                    for s, v in waits:
                        e.wait_ge(s, v)
                    if fn is None:
                        continue
                    ins = fn(e)
                    if kind == "c":
                        ins.then_inc(sched.sem[engname], 1)
                    else:
                        ins.then_inc(dsem, 16)

            @block.sync
            def _(e):
                run("sp", e)

            @block.tensor
            def _(e):
                run("pe", e)

            @block.scalar
            def _(e):
                run("act", e)

            @block.vector
            def _(e):
                run("dve", e)

            @block.gpsimd
            def _(e):
                run("pool", e)


def _small_layout():
    off = {}
    o = 0
    for name, n in (("mod_b", DEPTH * 48), ("nmg", DEPTH * 8), ("nfg", DEPTH * 8),
                    ("convw", DEPTH * 32), ("convb", DEPTH * 8), ("gateb", DEPTH * 2),
                    ("lbl", DEPTH * 4), ("hgn", DEPTH * 4), ("mln", DEPTH * 4), ("fg", 8), ("cT", 8)):
        off[name] = (o, n)
        o += n
    return off, o


SM_OFF, NS = _small_layout()
C_ID, C_ONES, C_TRI, C_M64, C_SEL, C_IOTA, C_THR = 0, 128, 256, 384, 448, 960, 976
NCONST = 992


def make_consts():
    c = np.zeros((128, NCONST), np.float32)
    c[:, C_ID:C_ID + 128] = np.eye(128, dtype=np.float32)
    c[:, C_ONES:C_ONES + 128] = 1.0
    sp = np.arange(128)[:, None]
    s = np.arange(128)[None, :]
    c[:, C_TRI:C_TRI + 128] = (sp >= s).astype(np.float32)
    t = np.arange(64)[None, :]
    c[:, C_M64:C_M64 + 64] = (t >= (sp % 64)).astype(np.float32)
    for h in range(4):
        c[h, C_SEL + h * 128:C_SEL + (h + 1) * 128] = 1.0
    c[:, C_IOTA:C_IOTA + 16] = np.arange(16, dtype=np.float32)[None, :]
    c[:, C_THR:C_THR + 15] = 16.0 * np.arange(1, 16, dtype=np.float32)[None, :]
    return c


def make_small(inp, b):
    sm = np.zeros((128, NS), np.float32)

    def put(name, arr):
        o, n = SM_OFF[name]
        arr = np.asarray(arr, np.float32).reshape(128, n)
        sm[:, o:o + n] = arr

    put("mod_b", inp["mod_b"].reshape(DEPTH, 48, 128).transpose(2, 0, 1))
    put("nmg", inp["norm_mix_g"].reshape(DEPTH, 8, 128).transpose(2, 0, 1))
    put("nfg", inp["norm_ffn_g"].reshape(DEPTH, 8, 128).transpose(2, 0, 1))
    put("convw", inp["ml_conv_w"].reshape(DEPTH, 4, 8, 128).transpose(3, 0, 1, 2))
    put("convb", inp["ml_conv_b"].reshape(DEPTH, 8, 128).transpose(2, 0, 1))
    gb = np.zeros((128, DEPTH, 2), np.float32)
    gb[0:4, :, 0] = inp["ml_gate_b"][:, 0:4].T
    gb[0:4, :, 1] = inp["ml_gate_b"][:, 4:8].T
    put("gateb", gb)
    put("lbl", inp["hg_lb_logits"].reshape(DEPTH, 4, 128).transpose(2, 0, 1))
    put("hgn", inp["hg_norm_g"].reshape(DEPTH, 4, 128).transpose(2, 0, 1))
    put("mln", inp["ml_norm_g"].reshape(DEPTH, 4, 128).transpose(2, 0, 1))
    put("fg", inp["final_g"].reshape(8, 128).T)
    put("cT", inp["c"][b].reshape(8, 128).T)
    return sm


class Prog:
    def __init__(self, n_layers=DEPTH, stages=None, debug=()):
        self.n_layers = n_layers
        self.stages = stages
        self.debug = set(debug)
        self.nc = bass.Bass("TRN2", target_bir_lowering=False)
        self.S = Sched(self.nc)
        self.uid = 0

    def sbt(self, name, shape, dt):
        self.uid += 1
        return self.nc.sbuf_tensor("%s_u%d" % (name, self.uid), shape, dt)

    def pst(self, name, shape, dt):
        self.uid += 1
        return self.nc.psum_tensor("%s_u%d" % (name, self.uid), shape, dt)

    def want(self, st):
        return self.stages is None or st in self.stages

    def dram(self, name, shape, dt, kind=None):
        if kind is None:
            kind = "ExternalOutput" if name in self.debug else "Internal"
        return self.nc.dram_tensor(name, list(shape), dt, kind=kind).ap()

    def build(self):
        nc, S = self.nc, self.S
        L = self.n_layers
        ext = lambda n, s, dt=F32: nc.dram_tensor(n, list(s), dt, kind="ExternalInput").ap()
        self.x_in = ext("x", [S_LEN, D])
        self.small_d = ext("small", [128, NS])
        self.consts_d = ext("consts", [128, NCONST])
        self.mod_w = ext("mod_w", [L, D, 6 * D])
        self.w_in = ext("w_in", [L, D, INW])
        self.input_names = ["x", "small", "consts", "mod_w", "w_in"]
        if self.want("merge"):
            self.w_branch = ext("w_branch", [L, 3, 512, D])
            self.w_out = ext("w_out", [L, D, D])
            self.input_names += ["w_branch", "w_out"]
        if self.want("peer") and PEER_IMPLEMENTED:
            self.pk_wq = ext("pk_wq", [L, D, D])
            self.keysbd = ext("keysbd", [L, 8, 128, 256])
            self.pk_u = ext("pk_u", [L, 16384, D])
            self.pk_v = ext("pk_v", [L, 16384, D])
            self.input_names += ["pk_wq", "keysbd", "pk_u", "pk_v"]
        self.out_d = nc.dram_tensor("out", [S_LEN, D], F32, kind="ExternalOutput").ap()
        self.xres = self.dram("xres", [S_LEN, D], F32)
        self.hT_d = self.dram("hT", [8, 128, S_LEN], BF16)
        self.qT_d = self.dram("qT", [512, S_LEN], BF16)
        self.kT_d = self.dram("kT", [512, S_LEN], BF16)
        self.v_d = self.dram("v_tok", [S_LEN, 512], BF16)
        self.hqT_d = self.dram("hqT", [512, S_LEN], F32)
        self.hfT_d = self.dram("hfT", [512, S_LEN], F32)
        self.hgT_d = self.dram("hgT", [512, S_LEN], F32)
        self.hi_d = self.dram("hi_tok", [S_LEN, 512], BF16)
        self.mqkT_d = self.dram("mqkT", [1024, S_LEN], F32)
        self.mv_d = self.dram("mv_tok", [S_LEN, 512], BF16)
        self.moT_d = self.dram("moT", [512, S_LEN], F32)
        self.gT_d = self.dram("gT", [2, 4, S_LEN], F32)
        self.bgT_d = self.dram("bgT", [3072, S_LEN], F32)
        self.yT_d = self.dram("yT", [3, 512, S_LEN], BF16)
        self.h2_d = self.dram("h2_tok", [S_LEN, D], F32)
        S.open()
        with (self.sbt("consts", [128, NCONST], F32) as cst,
              self.sbt("constb", [128, 384], BF16) as cstb,
              self.sbt("small", [128, NS], F32) as sm,
              self.sbt("modT", [128, DEPTH * 48], F32) as modT,
              self.sbt("lbT", [128, DEPTH * 4], F32) as lbT):
            self.cst, self.cstb, self.sm, self.modT, self.lbT = cst, cstb, sm, modT, lbT
            S.dma("sp", lambda e: e.dma_start(out=cst[:], in_=self.consts_d[:, :]), writes=["cst"])
            S.dma("sp", lambda e: e.dma_start(out=sm[:], in_=self.small_d[:, :]), writes=["sm"])
            S.op("dve", lambda e: e.tensor_copy(out=cstb[:], in_=cst[:, 0:384]), reads=["cst"], writes=["cstb"])
            S.dma("sp", lambda e: e.dma_start(out=self.xres[:, :], in_=self.x_in[:, :]), writes=["xres"])
            self.stage_mod()
            S.barrier(); S.emit()
            for l in range(L):
                if self.want("norm1"):
                    self.stage_norm(l, 0)
                    S.barrier(); S.emit()
                if self.want("proj"):
                    self.stage_proj(l)
                    S.barrier(); S.emit()
                if self.want("attn"):
                    self.stage_attn(l)
                    S.barrier(); S.emit()
                if self.want("hgrn"):
                    self.stage_hgrn(l)
                    S.barrier(); S.emit()
                if self.want("mlstm"):
                    self.stage_mlstm(l)
                    S.barrier(); S.emit()
                if self.want("merge"):
                    self.stage_merge(l)
                    S.barrier(); S.emit()
                if self.want("peer") and PEER_IMPLEMENTED:
                    self.stage_norm(l, 1)
                    S.barrier(); S.emit()
                    self.stage_peer(l)
                    S.barrier(); S.emit()
            self.stage_final()
            S.barrier(); S.emit()
        S.close()
        return nc

    def ident_f(self):
        return self.cst[:, C_ID:C_ID + 128]

    def ones_f(self):
        return self.cst[:, C_ONES:C_ONES + 128]

    def tri_f(self):
        return self.cst[:, C_TRI:C_TRI + 128]

    def ident_b(self):
        return self.cstb[:, 0:128]

    def ones_b(self):
        return self.cstb[:, 128:256]

    def smc(self, name, l, j, n=1):
        o, tot = SM_OFF[name]
        per = tot // DEPTH
        return self.sm[:, o + l * per + j: o + l * per + j + n]

    def modc(self, l, part, c):
        j = l * 48 + part * 8 + c
        return self.modT[:, j:j + 1]

    def stage_mod(self):
        nc, S = self.nc, self.S
        L = self.n_layers
        sm = self.sm
        with (self.sbt("condT", [128, 8], F32) as condT,
              self.sbt("mw0", [128, 8, 768], F32) as mw0,
              self.sbt("mw1", [128, 8, 768], F32) as mw1,
              self.sbt("lbe", [128, DEPTH * 4], F32) as lbe,
              self.sbt("lbs", [128, 4], F32) as lbs,
              self.sbt("lbm", [128, 4], F32) as lbm,
              self.pst("ps_mod", [128, DEPTH * 48], F32) as psm):
            o, _ = SM_OFF["cT"]
            S.op("act", lambda e: e.activation(out=condT[:], in_=sm[:, o:o + 8], func=AF.Silu), reads=["sm"], writes=["condT"])
            mws = [mw0, mw1]
            gi = 0
            for l in range(L):
                for g in range(8):
                    mw = mws[gi % 2]
                    key = "mw%d" % (gi % 2)
                    gi += 1
                    src = self.mod_w[l, :, g * 768:(g + 1) * 768].rearrange("(kc p) c -> p kc c", p=128)
                    S.dma("sp", lambda e, mw=mw, src=src: e.dma_start(out=mw[:], in_=src), writes=[key])
                    for cc in range(6):
                        j = l * 48 + g * 6 + cc
                        for kc in range(8):
                            S.op("pe", lambda e, mw=mw, cc=cc, kc=kc, j=j: e.matmul(
                                psm[:, j:j + 1], lhsT=mw[:, kc, cc * 128:(cc + 1) * 128], rhs=condT[:, kc:kc + 1],
                                start=(kc == 0), stop=(kc == 7)), reads=[key, "condT"], writes=["psm"])
            ob, _ = SM_OFF["mod_b"]
            S.op("dve", lambda e: e.tensor_tensor(out=self.modT[:, 0:L * 48], in0=psm[:, 0:L * 48], in1=sm[:, ob:ob + L * 48], op=ALU.add),
                 reads=["psm", "sm"], writes=["modT"])
            ol, _ = SM_OFF["lbl"]
            lg = lambda l: sm[:, ol + l * 4: ol + l * 4 + 4]
            S.op("dve", lambda e: e.tensor_tensor(out=lbm[:], in0=lg(0), in1=lg(1), op=ALU.max), reads=["sm"], writes=["lbm"])
            for l in (2, 3):
                S.op("dve", lambda e, l=l: e.tensor_tensor(out=lbm[:], in0=lbm[:], in1=lg(l), op=ALU.max), reads=["sm", "lbm"], writes=["lbm"])
            for l in range(DEPTH):
                S.op("dve", lambda e, l=l: e.tensor_tensor(out=lbe[:, l * 4:l * 4 + 4], in0=lg(l), in1=lbm[:], op=ALU.subtract), reads=["sm", "lbm"], writes=["lbe"])
            S.op("act", lambda e: e.activation(out=lbe[:], in_=lbe[:], func=AF.Exp), reads=["lbe"], writes=["lbe"])
            S.op("dve", lambda e: e.tensor_tensor(out=lbs[:], in0=lbe[:, 0:4], in1=lbe[:, 4:8], op=ALU.add), reads=["lbe"], writes=["lbs"])
            for l in (2, 3):
                S.op("dve", lambda e, l=l: e.tensor_tensor(out=lbs[:], in0=lbs[:], in1=lbe[:, l * 4:l * 4 + 4], op=ALU.add), reads=["lbe", "lbs"], writes=["lbs"])
            S.op("dve", lambda e: e.reciprocal(out=lbs[:], in_=lbs[:]), reads=["lbs"], writes=["lbs"])
            lbT = self.lbT
            S.op("dve", lambda e: e.memset(lbT[:, 0:4], 0.0), writes=["lbT"])
            for l in range(1, DEPTH):
                S.op("dve", lambda e, l=l: e.tensor_tensor(out=lbe[:, l * 4:l * 4 + 4], in0=lbe[:, l * 4:l * 4 + 4], in1=lbs[:], op=ALU.mult), reads=["lbe", "lbs"], writes=["lbe"])
                S.op("dve", lambda e, l=l: e.tensor_tensor(out=lbT[:, l * 4:l * 4 + 4], in0=lbT[:, (l - 1) * 4:l * 4], in1=lbe[:, l * 4:l * 4 + 4], op=ALU.add), reads=["lbe", "lbT"], writes=["lbT"])

    def stage_norm(self, l, which):
        nc, S = self.nc, self.S
        gname = "nmg" if which == 0 else "nfg"
        p_shift, p_scale = (0, 1) if which == 0 else (3, 4)
        with (self.sbt("nx0", [128, D], F32) as nx0, self.sbt("nx1", [128, D], F32) as nx1,
              self.sbt("nsq", [128, D], F32) as nsq,
              self.sbt("nb0", [128, D], BF16) as nb0, self.sbt("nb1", [128, D], BF16) as nb1,
              self.sbt("nh0", [128, 8, 128], BF16) as nh0, self.sbt("nh1", [128, 8, 128], BF16) as nh1,
              self.sbt("nt0", [128, D], F32) as nt0, self.sbt("nt1", [128, D], F32) as nt1,
              self.sbt("nss", [128, 4], F32) as nss,
              self.sbt("nG", [128, 8], F32) as nG,
              self.pst("nps0", [128, 8, 128], BF16) as nps0, self.pst("nps1", [128, 8, 128], BF16) as nps1,
              self.pst("npt0", [128, 8, 128], F32) as npt0):
            j0 = l * 48 + p_scale * 8
            S.op("dve", lambda e: e.scalar_tensor_tensor(out=nG[:], in0=self.modT[:, j0:j0 + 8], scalar=1.0, in1=self.smc(gname, l, 0, 8),
                                                         op0=ALU.add, op1=ALU.mult), reads=["modT", "sm"], writes=["nG"])
            nx, nb, nh, nps, nt = [nx0, nx1], [nb0, nb1], [nh0, nh1], [nps0, nps1], [nt0, nt1]
            for t in range(NT):
                i = t % 2
                kx, kb, kh, kp, kt = "nx%d" % i, "nb%d" % i, "nh%d" % i, "nps%d" % i, "nt%d" % i
                S.dma("sp", lambda e, i=i, t=t: e.dma_start(out=nx[i][:], in_=self.xres[t * 128:(t + 1) * 128, :]), reads=["xres"], writes=[kx])
                S.op("act", lambda e, i=i: e.activation(out=nsq[:], in_=nx[i][:], func=AF.Square, accum_out=nss[:, 0:1]), reads=[kx], writes=["nsq", "nss"])
                S.op("dve", lambda e: e.tensor_scalar(out=nss[:, 1:2], in0=nss[:, 0:1], scalar1=1.0 / D, scalar2=EPS, op0=ALU.mult, op1=ALU.add), reads=["nss"], writes=["nss"])
                S.op("act", lambda e: e.activation(out=nss[:, 2:3], in_=nss[:, 1:2], func=AF.Sqrt), reads=["nss"], writes=["nss"])
                S.op("dve", lambda e: e.reciprocal(out=nss[:, 3:4], in_=nss[:, 2:3]), reads=["nss"], writes=["nss"])
                S.op("dve", lambda e, i=i: e.tensor_scalar(out=nb[i][:], in0=nx[i][:], scalar1=nss[:, 3:4], scalar2=None, op0=ALU.mult), reads=[kx, "nss"], writes=[kb])
                for c in range(8):
                    S.op("pe", lambda e, i=i, c=c: e.transpose(nps[i][:, c, :], nb[i][:, c * 128:(c + 1) * 128], self.ident_b()), reads=[kb, "cstb"], writes=[kp])
                for c in range(8):
                    S.op("act", lambda e, i=i, c=c: e.activation(out=nh[i][:, c, :], in_=nps[i][:, c, :], func=AF.Identity,
                                                                 scale=nG[:, c:c + 1], bias=self.modc(l, p_shift, c)), reads=[kp, "nG", "modT"], writes=[kh])
                S.dma("sp", lambda e, i=i, t=t: e.dma_start(out=self.hT_d[:, :, t * 128:(t + 1) * 128].rearrange("c p t -> p c t"), in_=nh[i][:]), reads=[kh], writes=["hT_d"])
                if which == 1:
                    pass
            if which == 1:
                self._h2_tokmajor(l, nx, nss, nG, nt, npt0, p_shift)

    def _h2_tokmajor(self, l, nx, nss, nG, nt, npt0, p_shift):
        nc, S = self.nc, self.S
        with (self.sbt("dg", [128, 128], F32) as dg,
              self.sbt("Gbc", [128, D], F32) as Gbc, self.sbt("Sbc", [128, D], F32) as Sbc):
            for which_v, dst in ((0, Gbc), (1, Sbc)):
                for c in range(8):
                    col = nG[:, c:c + 1] if which_v == 0 else self.modc(l, p_shift, c)
                    S.op("dve", lambda e, col=col: e.tensor_scalar(out=dg[:], in0=self.ident_f(), scalar1=col, scalar2=None, op0=ALU.mult), reads=["cst", "nG", "modT"], writes=["dg"])
                    S.op("pe", lambda e, c=c: e.matmul(npt0[:, c, :], lhsT=self.ones_f(), rhs=dg[:], start=True, stop=True), reads=["cst", "dg"], writes=["npt0"])
                S.op("act", lambda e, dst=dst: e.activation(out=dst[:], in_=npt0[:].rearrange("p c t -> p (c t)"), func=AF.Copy), reads=["npt0"], writes=["bc%d" % which_v])
            for t in range(NT):
                i = t % 2
                kx, kt = "nx%d" % i, "nt%d" % i
                S.dma("sp", lambda e, i=i, t=t: e.dma_start(out=nx[i][:], in_=self.xres[t * 128:(t + 1) * 128, :]), reads=["xres"], writes=[kx])
                S.op("act", lambda e, i=i: e.activation(out=nt[i][:], in_=nx[i][:], func=AF.Square, accum_out=nss[:, 0:1]), reads=[kx], writes=[kt, "nss"])
                S.op("dve", lambda e: e.tensor_scalar(out=nss[:, 1:2], in0=nss[:, 0:1], scalar1=1.0 / D, scalar2=EPS, op0=ALU.mult, op1=ALU.add), reads=["nss"], writes=["nss"])
                S.op("act", lambda e: e.activation(out=nss[:, 2:3], in_=nss[:, 1:2], func=AF.Sqrt), reads=["nss"], writes=["nss"])
                S.op("dve", lambda e: e.reciprocal(out=nss[:, 3:4], in_=nss[:, 2:3]), reads=["nss"], writes=["nss"])
                S.op("dve", lambda e, i=i: e.scalar_tensor_tensor(out=nt[i][:], in0=nx[i][:], scalar=nss[:, 3:4], in1=Gbc[:], op0=ALU.mult, op1=ALU.mult), reads=[kx, "nss", "bc0"], writes=[kt])
                S.op("dve", lambda e, i=i: e.tensor_tensor(out=nt[i][:], in0=nt[i][:], in1=Sbc[:], op=ALU.add), reads=[kt, "bc1"], writes=[kt])
                S.dma("sp", lambda e, i=i, t=t: e.dma_start(out=self.h2_d[t * 128:(t + 1) * 128, :], in_=nt[i][:]), reads=[kt], writes=["h2_d"])

    def stage_proj(self, l):
        nc, S = self.nc, self.S
        fm = []
        for c in range(4):
            fm.append((0 + c * 128, 128, self.qT_d[c * 128:(c + 1) * 128, :], AF.Copy, 0.125, BF16))
        for c in range(4):
            fm.append((512 + c * 128, 128, self.kT_d[c * 128:(c + 1) * 128, :], AF.Copy, 1.0, BF16))
        for c in range(4):
            fm.append((1536 + c * 128, 128, self.hqT_d[c * 128:(c + 1) * 128, :], AF.Copy, 1.0, F32))
        for c in range(4):
            fm.append((2048 + c * 128, 128, self.hfT_d[c * 128:(c + 1) * 128, :], AF.Copy, 1.0, F32))
        for c in range(4):
            fm.append((3072 + c * 128, 128, self.hgT_d[c * 128:(c + 1) * 128, :], AF.Silu, 1.0, F32))
        for c in range(8):
            fm.append((3584 + c * 128, 128, self.mqkT_d[c * 128:(c + 1) * 128, :], AF.Copy, 1.0, F32))
        for c in range(4):
            fm.append((5120 + c * 128, 128, self.moT_d[c * 128:(c + 1) * 128, :], AF.Sigmoid, 1.0, F32))
        fm.append((5632, 4, self.gT_d[0, :, :], AF.Copy, 1.0, F32))
        fm.append((5636, 4, self.gT_d[1, :, :], AF.Copy, 1.0, F32))
        for c in range(24):
            fm.append((5640 + c * 128, 128, self.bgT_d[c * 128:(c + 1) * 128, :], AF.Sigmoid, 1.0, F32))
        tm = [(1024, self.v_d, AF.Copy), (2560, self.hi_d, AF.Silu), (4608, self.mv_d, AF.Copy)]
        with (self.sbt("hT", [128, 8, S_LEN], BF16) as hT,
              self.sbt("pw0", [128, 8, 512], BF16) as pw0, self.sbt("pw1", [128, 8, 512], BF16) as pw1,
              self.sbt("pof0", [128, S_LEN], F32) as pof0, self.sbt("pof1", [128, S_LEN], F32) as pof1,
              self.sbt("pob0", [128, S_LEN], BF16) as pob0, self.sbt("pob1", [128, S_LEN], BF16) as pob1,
              self.sbt("pot0", [128, 512], BF16) as pot0, self.sbt("pot1", [128, 512], BF16) as pot1,
              self.pst("pp0", [128, 512], F32) as pp0, self.pst("pp1", [128, 512], F32) as pp1,
              self.pst("pp2", [128, 512], F32) as pp2, self.pst("pp3", [128, 512], F32) as pp3):
            for c in range(8):
                S.dma("sp", lambda e, c=c: e.dma_start(out=hT[:, c, :], in_=self.hT_d[c, :, :]), reads=["hT_d"], writes=["hT"])
            pw, pof, pob, pot, pp = [pw0, pw1], [pof0, pof1], [pob0, pob1], [pot0, pot1], [pp0, pp1, pp2, pp3]
            wi = 0
            pi = 0
            for ji, (col0, ncol, dest, func, scale, dt) in enumerate(fm):
                w = pw[wi % 2]; kw = "pw%d" % (wi % 2); wi += 1
                src = self.w_in[l, :, col0:col0 + ncol].rearrange("(kc p) c -> p kc c", p=128)
                S.dma("pool", lambda e, w=w, src=src, ncol=ncol: e.dma_start(out=w[:, :, 0:ncol], in_=src), writes=[kw])
                ob = (pof if dt == F32 else pob)[ji % 2]
                ko = ("pof%d" if dt == F32 else "pob%d") % (ji % 2)
                for tb in range(4):
                    ps = pp[pi % 4]; kp = "pp%d" % (pi % 4); pi += 1
                    for kc in range(8):
                        S.op("pe", lambda e, ps=ps, w=w, kc=kc, tb=tb, ncol=ncol: e.matmul(
                            ps[0:ncol, :], lhsT=w[:, kc, 0:ncol], rhs=hT[:, kc, tb * 512:(tb + 1) * 512],
                            start=(kc == 0), stop=(kc == 7)), reads=[kw, "hT"], writes=[kp])
                    S.op("act", lambda e, ps=ps, ob=ob, tb=tb, ncol=ncol, func=func, scale=scale: e.activation(
                        out=ob[0:ncol, tb * 512:(tb + 1) * 512], in_=ps[0:ncol, :], func=func, scale=scale), reads=[kp], writes=[ko])
                S.dma("sp", lambda e, ob=ob, dest=dest, ncol=ncol: e.dma_start(out=dest, in_=ob[0:ncol, :]), reads=[ko], writes=["projout"])
            for (col0, dest, func) in tm:
                w = pw[wi % 2]; kw = "pw%d" % (wi % 2); wi += 1
                src = self.w_in[l, :, col0:col0 + 512].rearrange("(kc p) c -> p kc c", p=128)
                S.dma("pool", lambda e, w=w, src=src: e.dma_start(out=w[:], in_=src), writes=[kw])
                for t in range(NT):
                    ps = pp[pi % 4]; kp = "pp%d" % (pi % 4); pi += 1
                    for kc in range(8):
                        S.op("pe", lambda e, ps=ps, w=w, kc=kc, t=t: e.matmul(
                            ps[:], lhsT=hT[:, kc, t * 128:(t + 1) * 128], rhs=w[:, kc, :],
                            start=(kc == 0), stop=(kc == 7)), reads=[kw, "hT"], writes=[kp])
                    ot = pot[t % 2]; kt = "pot%d" % (t % 2)
                    S.op("act", lambda e, ps=ps, ot=ot, func=func: e.activation(out=ot[:], in_=ps[:], func=func), reads=[kp], writes=[kt])
                    S.dma("sp", lambda e, ot=ot, dest=dest, t=t: e.dma_start(out=dest[t * 128:(t + 1) * 128, :], in_=ot[:]), reads=[kt], writes=["projout"])

    def stage_attn(self, l):
        nc, S = self.nc, self.S
        with ExitStack() as es:
            sb = lambda n, sh, dt: es.enter_context(self.sbt(n, sh, dt))
            pt = lambda n, sh, dt: es.enter_context(self.pst(n, sh, dt))
            aq0, aq1 = sb("aq0", [64, S_LEN], BF16), sb("aq1", [64, S_LEN], BF16)
            ak0, ak1 = sb("ak0", [64, S_LEN], BF16), sb("ak1", [64, S_LEN], BF16)
            av0, av1 = sb("av0", [128, NT, 64], BF16), sb("av1", [128, NT, 64], BF16)
            azs0, azs1 = sb("azs0", [128, 512], F32), sb("azs1", [128, 512], F32)
            asp0, asp1 = sb("asp0", [128, 512], F32), sb("asp1", [128, 512], F32)
            alw0, alw1 = sb("alw0", [128, 512], F32), sb("alw1", [128, 512], F32)
            awt0, awt1 = sb("awt0", [128, 512], BF16), sb("awt1", [128, 512], BF16)
            ars = sb("ars", [128, 512], F32)
            ayo0, ayo1 = sb("ayo0", [64, 512], BF16), sb("ayo1", [64, 512], BF16)
            apz0, apz1 = pt("apz0", [128, 512], F32), pt("apz1", [128, 512], F32)
            apc0, apc1 = pt("apc0", [128, 512], F32), pt("apc1", [128, 512], F32)
            apy0, apy1 = pt("apy0", [64, 512], F32), pt("apy1", [64, 512], F32)
            aq, ak, av = [aq0, aq1], [ak0, ak1], [av0, av1]
            azs, asp, alw, awt = [azs0, azs1], [asp0, asp1], [alw0, alw1], [awt0, awt1]
            ayo, apz, apc, apy = [ayo0, ayo1], [apz0, apz1], [apc0, apc1], [apy0, apy1]
            it = 0
            yi = 0
            for h in range(8):
                hb = h % 2
                q, k, v = aq[hb], ak[hb], av[hb]
                kq, kk, kv = "aq%d" % hb, "ak%d" % hb, "av%d" % hb
                S.dma("sp", lambda e, q=q, h=h: e.dma_start(out=q[:], in_=self.qT_d[h * 64:(h + 1) * 64, :]), reads=["projout"], writes=[kq])
                S.dma("sp", lambda e, k=k, h=h: e.dma_start(out=k[:], in_=self.kT_d[h * 64:(h + 1) * 64, :]), reads=["projout"], writes=[kk])
                S.dma("sp", lambda e, v=v, h=h: e.dma_start(out=v[:], in_=self.v_d[:, h * 64:(h + 1) * 64].rearrange("(j p) d -> p j d", p=128)), reads=["projout"], writes=[kv])
                for qb in range(4):
                    nkb = 4 * (qb + 1)
                    py = apy[yi % 2]; kpy = "apy%d" % (yi % 2)
                    yo = ayo[yi % 2]; kyo = "ayo%d" % (yi % 2)
                    yi += 1
                    for jn, j in enumerate(reversed(range(nkb))):
                        b = it % 2
                        it += 1
                        diag = j >= 4 * qb
                        base = qb * 512 - j * 128
                        pz, pc = apz[b], apc[b]
                        zs, sp, lw, wt = azs[b], asp[b], alw[b], awt[b]
                        kz, kc_, kzs, ksp, klw, kwt = "apz%d" % b, "apc%d" % b, "azs%d" % b, "asp%d" % b, "alw%d" % b, "awt%d" % b
                        S.op("pe", lambda e, pz=pz, k=k, q=q, j=j, qb=qb: e.matmul(pz[:], lhsT=k[:, j * 128:(j + 1) * 128], rhs=q[:, qb * 512:(qb + 1) * 512], start=True, stop=True),
                             reads=[kq, kk], writes=[kz])
                        S.op("dve", lambda e, zs=zs, pz=pz: e.tensor_copy(out=zs[:], in_=pz[:]), reads=[kz], writes=[kzs])
                        S.op("act", lambda e, sp=sp, zs=zs: e.activation(out=sp[:], in_=zs[:], func=AF.Exp), reads=[kzs], writes=[ksp])
                        S.op("act", lambda e, sp=sp: e.activation(out=sp[:], in_=sp[:], func=AF.Ln, bias=1.0), reads=[ksp], writes=[ksp])
                        if diag:
                            S.op("pool", lambda e, sp=sp, base=base: e.affine_select(out=sp[:], in_=sp[:], pattern=[[1, 512]], compare_op=ALU.is_gt, fill=0.0, base=base, channel_multiplier=-1),
                                 reads=[ksp], writes=[ksp])
                        S.op("pe", lambda e, pc=pc, sp=sp, jn=jn: e.matmul(pc[:], lhsT=self.tri_f(), rhs=sp[:], start=True, stop=(jn == 0)), reads=["cst", ksp], writes=[kc_])
                        if jn > 0:
                            S.op("pe", lambda e, pc=pc: e.matmul(pc[:], lhsT=self.ones_f(), rhs=ars[:], start=False, stop=True), reads=["cst", "ars"], writes=[kc_])
                        S.op("dve", lambda e, lw=lw, zs=zs, pc=pc: e.tensor_tensor(out=lw[:], in0=zs[:], in1=pc[:], op=ALU.subtract), reads=[kzs, kc_], writes=[klw])
                        S.op("act", lambda e, wt=wt, lw=lw: e.activation(out=wt[:], in_=lw[:], func=AF.Exp), reads=[klw], writes=[kwt])
                        if diag:
                            S.op("pool", lambda e, wt=wt, base=base: e.affine_select(out=wt[:], in_=wt[:], pattern=[[1, 512]], compare_op=ALU.is_gt, fill=0.0, base=base, channel_multiplier=-1),
                                 reads=[kwt], writes=[kwt])
                        S.op("pe", lambda e, py=py, v=v, wt=wt, j=j, jn=jn, nkb=nkb: e.matmul(py[:], lhsT=v[:, j, :], rhs=wt[:], start=(jn == 0), stop=(jn == nkb - 1)),
                             reads=[kv, kwt], writes=[kpy])
                        if jn == 0:
                            S.op("pool", lambda e, sp=sp: e.tensor_copy(out=ars[:], in_=sp[:]), reads=[ksp], writes=["ars"])
                        elif jn < nkb - 1:
                            S.op("pool", lambda e, sp=sp: e.tensor_tensor(out=ars[:], in0=ars[:], in1=sp[:], op=ALU.add), reads=[ksp, "ars"], writes=["ars"])
                    S.op("act", lambda e, yo=yo, py=py: e.activation(out=yo[:], in_=py[:], func=AF.Copy), reads=[kpy], writes=[kyo])
                    S.dma("sp", lambda e, yo=yo, h=h, qb=qb: e.dma_start(out=self.yT_d[0, h * 64:(h + 1) * 64, qb * 512:(qb + 1) * 512], in_=yo[:]), reads=[kyo], writes=["yT_d"])

    def _zero_branch(self, g):
        nc, S = self.nc, self.S
        with ExitStack() as es:
            z = es.enter_context(self.sbt("zb", [128, S_LEN], BF16))
            S.op("dve", lambda e: e.memset(z[:], 0.0), writes=["zb"])
            for kc in range(4):
                S.dma("sp", lambda e, kc=kc: e.dma_start(out=self.yT_d[g, kc * 128:(kc + 1) * 128, :], in_=z[:]), reads=["zb"], writes=["yT_d"])

    def stage_hgrn(self, l):
        nc, S = self.nc, self.S
        with ExitStack() as es:
            sb = lambda n, sh, dt: es.enter_context(self.sbt(n, sh, dt))
            pt = lambda n, sh, dt: es.enter_context(self.pst(n, sh, dt))
            q = sb("hq", [128, S_LEN], F32)
            f = sb("hf", [128, S_LEN], F32)
            lf = sb("hlf", [128, S_LEN], F32)
            kin = sb("hkin", [128, S_LEN], F32)
            Bg = sb("hBg", [128, S_LEN], F32)
            Dd = sb("hD", [128, S_LEN], F32)
            Ee = sb("hE", [128, S_LEN], F32)
            Q1, K1 = sb("hQ1", [128, S_LEN], BF16), sb("hK1", [128, S_LEN], BF16)
            Q2, K2 = sb("hQ2", [128, S_LEN], BF16), sb("hK2", [128, S_LEN], BF16)
            hi = sb("hhi", [128, NT, 128], BF16)
            k2t = sb("hk2t", [128, NT, 128], BF16)
            hg = sb("hhg", [128, S_LEN], F32)
            oT = sb("hoT", [128, S_LEN], F32)
            Bst, Bmid, Bend, dec = sb("hBst", [128, 32], F32), sb("hBmid", [128, 32], F32), sb("hBend", [128, 32], F32), sb("hdec", [128, 32], F32)
            oml = sb("homl", [128, 1], F32)
            St, Stb = sb("hSt", [128, 128], F32), sb("hStb", [128, 128], BF16)
            pTs = [sb("hpT%d" % i, [128, 64], BF16) for i in range(2)]
            sq = sb("hsq", [128, 512], F32)
            rs = sb("hrs", [128, 512], F32)
            yb = sb("hyb", [128, 512], F32)
            ybb = sb("hybb", [128, 512], BF16)
            pss = [pt("hpss%d" % i, [128, 512], F32) for i in range(2)]
            pso = [pt("hpso%d" % i, [128, 512], F32) for i in range(2)]
            psst = [pt("hpsst%d" % i, [128, 512], F32) for i in range(2)]
            ptr = pt("hptr", [128, 1024], BF16)
            psn = pt("hpsn", [128, 512], F32)
            mask = self.cst[:, C_M64:C_M64 + 64]
            Bg3 = Bg[:].rearrange("p (c t) -> p c t", t=64)
            for h in range(4):
                lb = self.lbT[:, l * 4 + h: l * 4 + h + 1]
                rows = slice(h * 128, (h + 1) * 128)
                S.dma("sp", lambda e, rows=rows: e.dma_start(out=q[:], in_=self.hqT_d[rows, :]), reads=["projout"], writes=["hq"])
                S.dma("sp", lambda e, rows=rows: e.dma_start(out=f[:], in_=self.hfT_d[rows, :]), reads=["projout"], writes=["hf"])
                S.dma("sp", lambda e, rows=rows: e.dma_start(out=hg[:], in_=self.hgT_d[rows, :]), reads=["projout"], writes=["hhg"])
                S.dma("sp", lambda e, rows=rows: e.dma_start(out=hi[:], in_=self.hi_d[:, rows].rearrange("(j p) v -> p j v", p=128)), reads=["projout"], writes=["hhi"])
                S.op("dve", lambda e, lb=lb: e.tensor_scalar(out=oml[:], in0=lb, scalar1=-1.0, scalar2=1.0, op0=ALU.mult, op1=ALU.add), reads=["lbT"], writes=["homl"])
                S.op("act", lambda e: e.activation(out=f[:], in_=f[:], func=AF.Sigmoid), reads=["hf"], writes=["hf"])
                S.op("dve", lambda e, lb=lb: e.tensor_scalar(out=f[:], in0=f[:], scalar1=oml[:, 0:1], scalar2=lb, op0=ALU.mult, op1=ALU.add), reads=["hf", "homl", "lbT"], writes=["hf"])
                S.op("act", lambda e: e.activation(out=lf[:], in_=f[:], func=AF.Ln), reads=["hf"], writes=["hlf"])
                S.op("dve", lambda e: e.tensor_scalar(out=kin[:], in0=f[:], scalar1=-1.0, scalar2=1.0, op0=ALU.mult, op1=ALU.add), reads=["hf"], writes=["hkin"])
                S.op("dve", lambda e: e.tensor_tensor_scan(out=Bg[:], data0=lf[:], data1=lf[:], initial=0.0, op0=ALU.add, op1=ALU.bypass), reads=["hlf"], writes=["hBg"])
                S.op("dve", lambda e: e.memset(Bst[:, 0:1], 0.0), writes=["hBst"])
                S.op("dve", lambda e: e.tensor_copy(out=Bst[:, 1:32], in_=Bg3[:, 0:31, 63]), reads=["hBg"], writes=["hBst"])
                S.op("dve", lambda e: e.tensor_copy(out=Bmid[:], in_=Bg3[:, :, 31]), reads=["hBg"], writes=["hBmid"])
                S.op("dve", lambda e: e.tensor_copy(out=Bend[:], in_=Bg3[:, :, 63]), reads=["hBg"], writes=["hBend"])
                S.op("dve", lambda e: e.tensor_tensor(out=dec[:], in0=Bend[:], in1=Bst[:], op=ALU.subtract), reads=["hBend", "hBst"], writes=["hdec"])
                S.op("act", lambda e: e.activation(out=dec[:], in_=dec[:], func=AF.Exp), reads=["hdec"], writes=["hdec"])

                def sub_cols(col, key):
                    for c in range(32):
                        S.op("dve", lambda e, c=c: e.tensor_scalar(out=Dd[:, c * 64:(c + 1) * 64], in0=Bg[:, c * 64:(c + 1) * 64], scalar1=col[:, c:c + 1], scalar2=None, op0=ALU.subtract),
                             reads=["hBg", key], writes=["hD"])
                sub_cols(Bmid, "hBmid")
                S.op("act", lambda e: e.activation(out=Ee[:], in_=Dd[:], func=AF.Exp), reads=["hD"], writes=["hE"])
                S.op("dve", lambda e: e.tensor_tensor(out=Q1[:], in0=q[:], in1=Ee[:], op=ALU.mult), reads=["hq", "hE"], writes=["hQ1"])
                S.op("act", lambda e: e.activation(out=Ee[:], in_=Dd[:], func=AF.Exp, scale=-1.0), reads=["hD", "hQ1"], writes=["hE"])
                S.op("dve", lambda e: e.tensor_tensor(out=K1[:], in0=kin[:], in1=Ee[:], op=ALU.mult), reads=["hkin", "hE"], writes=["hK1"])
                sub_cols(Bst, "hBst")
                S.op("act", lambda e: e.activation(out=Ee[:], in_=Dd[:], func=AF.Exp), reads=["hD", "hK1"], writes=["hE"])
                S.op("dve", lambda e: e.tensor_tensor(out=Q2[:], in0=q[:], in1=Ee[:], op=ALU.mult), reads=["hq", "hE"], writes=["hQ2"])
                sub_cols(Bend, "hBend")
                S.op("act", lambda e: e.activation(out=Ee[:], in_=Dd[:], func=AF.Exp, scale=-1.0), reads=["hD", "hQ2"], writes=["hE"])
                S.op("dve", lambda e: e.tensor_tensor(out=K2[:], in0=kin[:], in1=Ee[:], op=ALU.mult), reads=["hkin", "hE"], writes=["hK2"])
                for j in range(NT):
                    S.op("pe", lambda e, j=j: e.transpose(ptr[:, 0:128], K2[:, j * 128:(j + 1) * 128], self.ident_b()), reads=["hK2", "cstb"], writes=["hptr"])
                    S.op("act", lambda e, j=j: e.activation(out=k2t[:, j, :], in_=ptr[:, 0:128], func=AF.Copy), reads=["hptr"], writes=["hk2t"])
                for c in range(32):
                    j, half = c // 2, c % 2
                    r0 = half * 64
                    cs = slice(c * 64, (c + 1) * 64)
                    ps_s = pss[c % 2]; kps = "hpss%d" % (c % 2)
                    pT = pTs[c % 2]; kpT = "hpT%d" % (c % 2)
                    po = pso[(c // 8) % 2]; kpo = "hpso%d" % ((c // 8) % 2)
                    ocs = slice((c % 8) * 64, (c % 8 + 1) * 64)
                    pst_ = psst[c % 2]; kpst = "hpsst%d" % (c % 2)
                    S.op("pe", lambda e, ps_s=ps_s, r0=r0, cs=cs: e.matmul(ps_s[r0:r0 + 64, 0:64], lhsT=K1[:, cs], rhs=Q1[:, cs], start=True, stop=True), reads=["hK1", "hQ1"], writes=[kps])
                    S.op("dve", lambda e, ps_s=ps_s, pT=pT, r0=r0: e.tensor_copy(out=pT[r0:r0 + 64, :], in_=ps_s[r0:r0 + 64, 0:64]), reads=[kps], writes=[kpT])
                    S.op("pool", lambda e, pT=pT, r0=r0: e.affine_select(out=pT[r0:r0 + 64, :], in_=pT[r0:r0 + 64, :], pattern=[[1, 64]], compare_op=ALU.is_ge, fill=0.0, base=0, channel_multiplier=-1),
                         reads=[kpT], writes=[kpT])
                    S.op("pe", lambda e, po=po, pT=pT, r0=r0, j=j, ocs=ocs, c=c: e.matmul(po[:, ocs], lhsT=hi[r0:r0 + 64, j, :], rhs=pT[r0:r0 + 64, :], start=True, stop=(c == 0)), reads=["hhi", kpT], writes=[kpo])
                    if c > 0:
                        S.op("pe", lambda e, po=po, ocs=ocs, cs=cs: e.matmul(po[:, ocs], lhsT=Stb[:], rhs=Q2[:, cs], start=False, stop=True), reads=["hStb", "hQ2"], writes=[kpo])
                    if c < 31:
                        S.op("pe", lambda e, pst_=pst_, r0=r0, j=j: e.matmul(pst_[:, 0:128], lhsT=k2t[r0:r0 + 64, j, :], rhs=hi[r0:r0 + 64, j, :], start=True, stop=True), reads=["hk2t", "hhi"], writes=[kpst])
                        if c == 0:
                            S.op("dve", lambda e, pst_=pst_: e.tensor_copy(out=St[:], in_=pst_[:, 0:128]), reads=[kpst], writes=["hSt"])
                        else:
                            S.op("dve", lambda e, pst_=pst_, c=c: e.scalar_tensor_tensor(out=St[:], in0=St[:], scalar=dec[:, c:c + 1], in1=pst_[:, 0:128], op0=ALU.mult, op1=ALU.add), reads=[kpst, "hSt", "hdec"], writes=["hSt"])
                        S.op("act", lambda e: e.activation(out=Stb[:], in_=St[:], func=AF.Copy), reads=["hSt"], writes=["hStb"])
                    if c % 8 == 7:
                        tb = c // 8
                        S.op("act", lambda e, po=po, tb=tb: e.activation(out=oT[:, tb * 512:(tb + 1) * 512], in_=po[:], func=AF.Copy), reads=[kpo], writes=["hoT"])
                gcol = self.smc("hgn", l, h)
                for tb in range(4):
                    bs = slice(tb * 512, (tb + 1) * 512)
                    S.op("act", lambda e, bs=bs: e.activation(out=sq[:], in_=oT[:, bs], func=AF.Square), reads=["hoT"], writes=["hsq"])
                    S.op("pe", lambda e: e.matmul(psn[:], lhsT=self.ones_f(), rhs=sq[:], start=True, stop=True), reads=["cst", "hsq"], writes=["hpsn"])
                    S.op("dve", lambda e: e.tensor_scalar(out=rs[:], in0=psn[:], scalar1=1.0 / 128, scalar2=EPS, op0=ALU.mult, op1=ALU.add), reads=["hpsn"], writes=["hrs"])
                    S.op("act", lambda e: e.activation(out=rs[:], in_=rs[:], func=AF.Sqrt), reads=["hrs"], writes=["hrs"])
                    S.op("dve", lambda e: e.reciprocal(out=rs[:], in_=rs[:]), reads=["hrs"], writes=["hrs"])
                    S.op("dve", lambda e, bs=bs: e.tensor_tensor(out=yb[:], in0=oT[:, bs], in1=rs[:], op=ALU.mult), reads=["hoT", "hrs"], writes=["hyb"])
                    S.op("dve", lambda e, bs=bs, gcol=gcol: e.scalar_tensor_tensor(out=ybb[:], in0=yb[:], scalar=gcol, in1=hg[:, bs], op0=ALU.mult, op1=ALU.mult), reads=["hyb", "sm", "hhg"], writes=["hybb"])
                    S.dma("sp", lambda e, bs=bs, rows=rows: e.dma_start(out=self.yT_d[1, rows, bs], in_=ybb[:]), reads=["hybb"], writes=["yT_d"])

    def stage_mlstm(self, l):
        nc, S = self.nc, self.S
        with ExitStack() as es:
            sb = lambda n, sh, dt: es.enter_context(self.sbt(n, sh, dt))
            pt = lambda n, sh, dt: es.enter_context(self.pst(n, sh, dt))
            gi, gf = sb("mgi", [4, S_LEN], F32), sb("mgf", [4, S_LEN], F32)
            Bc, aa, AA = sb("mB", [4, S_LEN], F32), sb("ma", [4, S_LEN], F32), sb("mA", [4, S_LEN], F32)
            em, Dr = sb("mem", [4, S_LEN], F32), sb("mDr", [4, S_LEN], F32)
            uu, ww, it = sb("mu", [4, S_LEN], F32), sb("mw", [4, S_LEN], F32), sb("mit", [4, S_LEN], F32)
            Aend, Aprev, decr = sb("mAend", [4, 32], F32), sb("mAprev", [4, 32], F32), sb("mdecr", [4, 32], F32)
            nbf = sb("mnbf", [4, 1], F32)
            og, _ = SM_OFF["gateb"]
            bi = self.sm[0:4, og + l * 2: og + l * 2 + 1]
            bf = self.sm[0:4, og + l * 2 + 1: og + l * 2 + 2]
            S.dma("sp", lambda e: e.dma_start(out=gi[:], in_=self.gT_d[0, :, :]), reads=["projout"], writes=["mgi"])
            S.dma("sp", lambda e: e.dma_start(out=gf[:], in_=self.gT_d[1, :, :]), reads=["projout"], writes=["mgf"])
            S.op("dve", lambda e: e.tensor_scalar(out=gi[:], in0=gi[:], scalar1=bi, scalar2=None, op0=ALU.add), reads=["mgi", "sm"], writes=["mgi"])
            S.op("dve", lambda e: e.tensor_scalar(out=nbf[:], in0=bf, scalar1=-1.0, scalar2=None, op0=ALU.mult), reads=["sm"], writes=["mnbf"])
            S.op("act", lambda e: e.activation(out=gf[:], in_=gf[:], func=AF.Exp, scale=-1.0, bias=nbf[:, 0:1]), reads=["mgf", "mnbf"], writes=["mgf"])
            S.op("act", lambda e: e.activation(out=gf[:], in_=gf[:], func=AF.Ln, bias=1.0), reads=["mgf"], writes=["mgf"])
            S.op("dve", lambda e: e.tensor_scalar(out=gf[:], in0=gf[:], scalar1=-1.0, scalar2=None, op0=ALU.mult), reads=["mgf"], writes=["mgf"])
            S.op("dve", lambda e: e.tensor_tensor_scan(out=Bc[:], data0=gf[:], data1=gf[:], initial=0.0, op0=ALU.add, op1=ALU.bypass), reads=["mgf"], writes=["mB"])
            S.op("dve", lambda e: e.tensor_tensor(out=aa[:], in0=gi[:], in1=Bc[:], op=ALU.subtract), reads=["mgi", "mB"], writes=["ma"])
            S.op("dve", lambda e: e.tensor_tensor_scan(out=AA[:], data0=aa[:], data1=aa[:], initial=0.0, op0=ALU.max, op1=ALU.bypass), reads=["ma"], writes=["mA"])
            S.op("dve", lambda e: e.tensor_tensor(out=em[:], in0=Bc[:], in1=AA[:], op=ALU.add), reads=["mB", "mA"], writes=["mem"])
            S.op("act", lambda e: e.activation(out=em[:], in_=em[:], func=AF.Exp, scale=-1.0), reads=["mem"], writes=["mem"])
            A3 = AA[:].rearrange("p (c t) -> p c t", t=64)
            a3 = aa[:].rearrange("p (c t) -> p c t", t=64)
            D3 = Dr[:].rearrange("p (c t) -> p c t", t=64)
            S.op("dve", lambda e: e.tensor_copy(out=Aend[:], in_=A3[:, :, 63]), reads=["mA"], writes=["mAend"])
            S.op("dve", lambda e: e.memset(Aprev[:, 0:1], 0.0), writes=["mAprev"])
            S.op("dve", lambda e: e.tensor_copy(out=Aprev[:, 1:32], in_=A3[:, 0:31, 63]), reads=["mA"], writes=["mAprev"])
            S.op("dve", lambda e: e.tensor_tensor(out=decr[:], in0=Aprev[:], in1=Aend[:], op=ALU.subtract), reads=["mAprev", "mAend"], writes=["mdecr"])
            S.op("act", lambda e: e.activation(out=decr[:], in_=decr[:], func=AF.Exp), reads=["mdecr"], writes=["mdecr"])
            bc = lambda col: col[:].unsqueeze(2).to_broadcast([4, 32, 64])
            S.op("dve", lambda e: e.tensor_tensor(out=D3, in0=A3, in1=bc(Aend), op=ALU.subtract), reads=["mA", "mAend"], writes=["mDr"])
            S.op("act", lambda e: e.activation(out=uu[:], in_=Dr[:], func=AF.Exp, scale=-1.0), reads=["mDr"], writes=["mu"])
            S.op("dve", lambda e: e.tensor_tensor(out=D3, in0=a3, in1=bc(Aend), op=ALU.subtract), reads=["ma", "mAend", "mu"], writes=["mDr"])
            S.op("act", lambda e: e.activation(out=ww[:], in_=Dr[:], func=AF.Exp), reads=["mDr"], writes=["mw"])
            S.op("dve", lambda e: e.tensor_tensor(out=D3, in0=A3, in1=bc(Aprev), op=ALU.subtract), reads=["mA", "mAprev", "mw"], writes=["mDr"])
            S.op("act", lambda e: e.activation(out=it[:], in_=Dr[:], func=AF.Exp, scale=-1.0), reads=["mDr"], writes=["mit"])
            xr, acc = sb("mxr", [128, S_LEN], F32), sb("macc2", [128, S_LEN], F32)
            Q1, Q2, K1 = sb("mQ1", [128, S_LEN], BF16), sb("mQ2", [128, S_LEN], BF16), sb("mK1", [128, S_LEN], BF16)
            vv = sb("mvv", [128, NT, 128], BF16)
            k2t = sb("mk2t", [128, NT, 128], BF16)
            mo = sb("mmo", [128, S_LEN], F32)
            hT = sb("mhT", [128, S_LEN], F32)
            dec = sb("mdec", [128, 32], F32)
            Cs, Csb = sb("mCs", [128, 128], F32), sb("mCsb", [128, 128], BF16)
            Ns, Nsb = sb("mNs", [128, 128], F32), sb("mNsb", [128, 128], BF16)
            pTs = [sb("mpT%d" % i, [128, 64], BF16) for i in range(2)]
            numT, embc, dmx = sb("mnumT", [128, 512], F32), sb("membc", [128, 512], F32), sb("mdmx", [128, 512], F32)
            sq, rs, yb, ybb = sb("msq", [128, 512], F32), sb("mrs", [128, 512], F32), sb("myb", [128, 512], F32), sb("mybb", [128, 512], BF16)
            pss = pt("mpss", [128, 512], F32)
            pso = [pt("mpso%d" % i, [128, 512], F32) for i in range(2)]
            psd = [pt("mpsd%d" % i, [128, 512], F32) for i in range(2)]
            pstC, pstN = pt("mpstC", [128, 512], F32), pt("mpstN", [128, 512], F32)
            pmisc = pt("mpmisc", [128, 512], F32)
            pmisc_b = pmisc[:].bitcast(BF16)
            KM = "mpmisc"
            ones_b = self.ones_b()

            def bcast_rows(rows, h, tb):
                S.op("pe", lambda e: e.matmul(pmisc[:], lhsT=self.cst[0:4, C_SEL + h * 128: C_SEL + (h + 1) * 128], rhs=rows[0:4, tb * 512:(tb + 1) * 512], start=True, stop=True),
                     reads=["cst", "mu", "mw", "mit", "mem"], writes=[KM])

            def conv_silu(chunk):
                cw = lambda tap: self.smc("convw", l, tap * 8 + chunk)
                S.op("dve", lambda e: e.tensor_scalar(out=acc[:], in0=xr[:], scalar1=cw(3), scalar2=self.smc("convb", l, chunk), op0=ALU.mult, op1=ALU.add), reads=["mxr", "sm"], writes=["macc2"])
                for sh in (1, 2, 3):
                    S.op("dve", lambda e, sh=sh: e.scalar_tensor_tensor(out=acc[:, sh:S_LEN], in0=xr[:, 0:S_LEN - sh], scalar=cw(3 - sh), in1=acc[:, sh:S_LEN], op0=ALU.mult, op1=ALU.add),
                         reads=["mxr", "sm", "macc2"], writes=["macc2"])
                S.op("act", lambda e: e.activation(out=acc[:], in_=acc[:], func=AF.Silu), reads=["macc2"], writes=["macc2"])

            for h in range(4):
                rows = slice(h * 128, (h + 1) * 128)
                S.dma("sp", lambda e, rows=rows: e.dma_start(out=xr[:], in_=self.mqkT_d[rows, :]), reads=["projout"], writes=["mxr"])
                S.dma("sp", lambda e, rows=rows: e.dma_start(out=mo[:], in_=self.moT_d[rows, :]), reads=["projout"], writes=["mmo"])
                S.dma("sp", lambda e, rows=rows: e.dma_start(out=vv[:], in_=self.mv_d[:, rows].rearrange("(j p) v -> p j v", p=128)), reads=["projout"], writes=["mvv"])
                conv_silu(h)
                for tb in range(4):
                    bs = slice(tb * 512, (tb + 1) * 512)
                    bcast_rows(uu, h, tb)
                    S.op("dve", lambda e, bs=bs: e.tensor_tensor(out=Q1[:, bs], in0=acc[:, bs], in1=pmisc[:], op=ALU.mult), reads=["macc2", KM], writes=["mQ1"])
                    bcast_rows(it, h, tb)
                    S.op("dve", lambda e, bs=bs: e.tensor_tensor(out=Q2[:, bs], in0=acc[:, bs], in1=pmisc[:], op=ALU.mult), reads=["macc2", KM], writes=["mQ2"])
                S.dma("sp", lambda e, h=h: e.dma_start(out=xr[:], in_=self.mqkT_d[512 + h * 128: 512 + (h + 1) * 128, :]), reads=["projout"], writes=["mxr"])
                conv_silu(4 + h)
                for tb in range(4):
                    bs = slice(tb * 512, (tb + 1) * 512)
                    bcast_rows(ww, h, tb)
                    S.op("dve", lambda e, bs=bs: e.scalar_tensor_tensor(out=K1[:, bs], in0=acc[:, bs], scalar=128.0 ** -0.5, in1=pmisc[:], op0=ALU.mult, op1=ALU.mult), reads=["macc2", KM], writes=["mK1"])
                S.op("pe", lambda e, h=h: e.matmul(pmisc[:, 0:32], lhsT=self.cst[0:4, C_SEL + h * 128: C_SEL + (h + 1) * 128], rhs=decr[0:4, :], start=True, stop=True), reads=["cst", "mdecr"], writes=[KM])
                S.op("act", lambda e: e.activation(out=dec[:], in_=pmisc[:, 0:32], func=AF.Copy), reads=[KM], writes=["mdec"])
                for j in range(NT):
                    S.op("pe", lambda e, j=j: e.transpose(pmisc_b[:, 0:128], K1[:, j * 128:(j + 1) * 128], self.ident_b()), reads=["mK1", "cstb"], writes=[KM])
                    S.op("act", lambda e, j=j: e.activation(out=k2t[:, j, :], in_=pmisc_b[:, 0:128], func=AF.Copy), reads=[KM], writes=["mk2t"])
                for c in range(32):
                    j, half = c // 2, c % 2
                    r0 = half * 64
                    cs = slice(c * 64, (c + 1) * 64)
                    pT = pTs[c % 2]; kpT = "mpT%d" % (c % 2)
                    po = pso[(c // 8) % 2]; kpo = "mpso%d" % ((c // 8) % 2)
                    pd = psd[(c // 8) % 2]; kpd = "mpsd%d" % ((c // 8) % 2)
                    ocs = slice((c % 8) * 64, (c % 8 + 1) * 64)
                    S.op("pe", lambda e, r0=r0, cs=cs: e.matmul(pss[r0:r0 + 64, 0:64], lhsT=K1[:, cs], rhs=Q1[:, cs], start=True, stop=True), reads=["mK1", "mQ1"], writes=["mpss"])
                    S.op("dve", lambda e, pT=pT, r0=r0: e.tensor_copy(out=pT[r0:r0 + 64, :], in_=pss[r0:r0 + 64, 0:64]), reads=["mpss"], writes=[kpT])
                    S.op("pool", lambda e, pT=pT, r0=r0: e.affine_select(out=pT[r0:r0 + 64, :], in_=pT[r0:r0 + 64, :], pattern=[[1, 64]], compare_op=ALU.is_ge, fill=0.0, base=0, channel_multiplier=-1),
                         reads=[kpT], writes=[kpT])
                    S.op("pe", lambda e, po=po, pT=pT, r0=r0, j=j, ocs=ocs, c=c: e.matmul(po[:, ocs], lhsT=vv[r0:r0 + 64, j, :], rhs=pT[r0:r0 + 64, :], start=True, stop=(c == 0)), reads=["mvv", kpT], writes=[kpo])
                    if c > 0:
                        S.op("pe", lambda e, po=po, ocs=ocs, cs=cs: e.matmul(po[:, ocs], lhsT=Csb[:], rhs=Q2[:, cs], start=False, stop=True), reads=["mCsb", "mQ2"], writes=[kpo])
                    S.op("pe", lambda e, pd=pd, pT=pT, r0=r0, ocs=ocs, c=c: e.matmul(pd[:, ocs], lhsT=ones_b[r0:r0 + 64, :], rhs=pT[r0:r0 + 64, :], start=True, stop=(c == 0)), reads=["cstb", kpT], writes=[kpd])
                    if c > 0:
                        S.op("pe", lambda e, pd=pd, ocs=ocs, cs=cs: e.matmul(pd[:, ocs], lhsT=Nsb[:], rhs=Q2[:, cs], start=False, stop=True), reads=["mNsb", "mQ2"], writes=[kpd])
                    if c < 31:
                        S.op("pe", lambda e, r0=r0, j=j: e.matmul(pstC[:, 0:128], lhsT=k2t[r0:r0 + 64, j, :], rhs=vv[r0:r0 + 64, j, :], start=True, stop=True), reads=["mk2t", "mvv"], writes=["mpstC"])
                        S.op("pe", lambda e, r0=r0, j=j: e.matmul(pstN[:, 0:128], lhsT=k2t[r0:r0 + 64, j, :], rhs=ones_b[r0:r0 + 64, :], start=True, stop=True), reads=["mk2t", "cstb"], writes=["mpstN"])
                        if c == 0:
                            S.op("dve", lambda e: e.tensor_copy(out=Cs[:], in_=pstC[:, 0:128]), reads=["mpstC"], writes=["mCs"])
                            S.op("dve", lambda e: e.tensor_copy(out=Ns[:], in_=pstN[:, 0:128]), reads=["mpstN"], writes=["mNs"])
                        else:
                            S.op("dve", lambda e, c=c: e.scalar_tensor_tensor(out=Cs[:], in0=Cs[:], scalar=dec[:, c:c + 1], in1=pstC[:, 0:128], op0=ALU.mult, op1=ALU.add), reads=["mpstC", "mCs", "mdec"], writes=["mCs"])
                            S.op("dve", lambda e, c=c: e.scalar_tensor_tensor(out=Ns[:], in0=Ns[:], scalar=dec[:, c:c + 1], in1=pstN[:, 0:128], op0=ALU.mult, op1=ALU.add), reads=["mpstN", "mNs", "mdec"], writes=["mNs"])
                        S.op("act", lambda e: e.activation(out=Csb[:], in_=Cs[:], func=AF.Copy), reads=["mCs"], writes=["mCsb"])
                        S.op("act", lambda e: e.activation(out=Nsb[:], in_=Ns[:], func=AF.Copy), reads=["mNs"], writes=["mNsb"])
                    if c % 8 == 7:
                        tb = c // 8
                        bs = slice(tb * 512, (tb + 1) * 512)
                        S.op("act", lambda e, po=po: e.activation(out=numT[:], in_=po[:], func=AF.Copy), reads=[kpo], writes=["mnumT"])
                        bcast_rows(em, h, tb)
                        S.op("act", lambda e: e.activation(out=embc[:], in_=pmisc[:], func=AF.Copy), reads=[KM], writes=["membc"])
                        S.op("act", lambda e, pd=pd: e.activation(out=dmx[:], in_=pd[:], func=AF.Abs), reads=[kpd], writes=["mdmx"])
                        S.op("dve", lambda e: e.tensor_tensor(out=dmx[:], in0=dmx[:], in1=embc[:], op=ALU.max), reads=["mdmx", "membc"], writes=["mdmx"])
                        S.op("dve", lambda e: e.reciprocal(out=dmx[:], in_=dmx[:]), reads=["mdmx"], writes=["mdmx"])
                        S.op("dve", lambda e, bs=bs: e.tensor_tensor(out=hT[:, bs], in0=numT[:], in1=dmx[:], op=ALU.mult), reads=["mnumT", "mdmx"], writes=["mhT"])
                gcol = self.smc("mln", l, h)
                for tb in range(4):
                    bs = slice(tb * 512, (tb + 1) * 512)
                    S.op("act", lambda e, bs=bs: e.activation(out=sq[:], in_=hT[:, bs], func=AF.Square), reads=["mhT"], writes=["msq"])
                    S.op("pe", lambda e: e.matmul(pmisc[:], lhsT=self.ones_f(), rhs=sq[:], start=True, stop=True), reads=["cst", "msq"], writes=[KM])
                    S.op("dve", lambda e: e.tensor_scalar(out=rs[:], in0=pmisc[:], scalar1=1.0 / 128, scalar2=EPS, op0=ALU.mult, op1=ALU.add), reads=[KM], writes=["mrs"])
                    S.op("act", lambda e: e.activation(out=rs[:], in_=rs[:], func=AF.Sqrt), reads=["mrs"], writes=["mrs"])
                    S.op("dve", lambda e: e.reciprocal(out=rs[:], in_=rs[:]), reads=["mrs"], writes=["mrs"])
                    S.op("dve", lambda e, bs=bs: e.tensor_tensor(out=yb[:], in0=hT[:, bs], in1=rs[:], op=ALU.mult), reads=["mhT", "mrs"], writes=["myb"])
                    S.op("dve", lambda e, bs=bs, gcol=gcol: e.scalar_tensor_tensor(out=ybb[:], in0=yb[:], scalar=gcol, in1=mo[:, bs], op0=ALU.mult, op1=ALU.mult), reads=["myb", "sm", "mmo"], writes=["mybb"])
                    S.dma("sp", lambda e, bs=bs, rows=rows: e.dma_start(out=self.yT_d[2, rows, bs], in_=ybb[:]), reads=["mybb"], writes=["yT_d"])

    def _bcast_tile(self, es, name, colfn):
        nc, S = self.nc, self.S
        dg = es.enter_context(self.sbt(name + "dg", [128, 128], F32))
        dst = es.enter_context(self.sbt(name, [128, D], F32))
        ps = es.enter_context(self.pst(name + "ps", [128, 8, 128], F32))
        for c in range(8):
            S.op("dve", lambda e, c=c: e.tensor_scalar(out=dg[:], in0=self.ident_f(), scalar1=colfn(c), scalar2=None, op0=ALU.mult),
                 reads=["cst", "modT", "sm"], writes=[name + "dg"])
            S.op("pe", lambda e, c=c: e.matmul(ps[:, c, :], lhsT=self.ones_f(), rhs=dg[:], start=True, stop=True), reads=["cst", name + "dg"], writes=[name + "ps"])
        S.op("act", lambda e: e.activation(out=dst[:], in_=ps[:].rearrange("p c t -> p (c t)"), func=AF.Copy), reads=[name + "ps"], writes=[name])
        return dst

    def stage_merge(self, l):
        nc, S = self.nc, self.S
        with ExitStack() as es:
            sb = lambda n, sh, dt: es.enter_context(self.sbt(n, sh, dt))
            pt = lambda n, sh, dt: es.enter_context(self.pst(n, sh, dt))
            g1bc = self._bcast_tile(es, "g1bc", lambda c: self.modc(l, 2, c))
            wb = sb("mwb", [128, 3, 4, D], BF16)
            wo = sb("mwo", [128, 8, D], BF16)
            yt = sb("myt", [128, 3, 4, 512], BF16)
            gts = [sb("mgt%d" % i, [128, 512], F32) for i in range(3)]
            tmps = [sb("mtmp%d" % i, [128, 512], F32) for i in range(2)]
            macc = sb("macc", [128, 512], F32)
            mT = sb("mT", [128, 8, 512], BF16)
            xts = [sb("mxt%d" % i, [128, D], F32) for i in range(2)]
            ytmp = sb("mytmp", [128, 512], F32)
            psa = [pt("mpsa%d" % i, [128, 512], F32) for i in range(2)]
            psy = [pt("mpsy%d" % i, [128, 512], F32) for i in range(2)]
            for g in range(3):
                S.dma("pool", lambda e, g=g: e.dma_start(out=wb[:, g, :, :], in_=self.w_branch[l, g, :, :].rearrange("(kc p) d -> p kc d", p=128)), writes=["mwb"])
            S.dma("pool", lambda e: e.dma_start(out=wo[:], in_=self.w_out[l, :, :].rearrange("(c p) d -> p c d", p=128)), writes=["mwo"])
            ai = 0
            gi = 0
            yi = 0
            for tb in range(4):
                for g in range(3):
                    S.dma("sp", lambda e, g=g, tb=tb: e.dma_start(out=yt[:, g, :, :], in_=self.yT_d[g, :, tb * 512:(tb + 1) * 512].rearrange("(kc p) t -> p kc t", p=128)),
                          reads=["yT_d"], writes=["myt"])
                for dc in range(8):
                    for g in range(3):
                        ps = psa[ai % 2]; kps = "mpsa%d" % (ai % 2); ai += 1
                        gt = gts[gi % 3]; kgt = "mgt%d" % (gi % 3); gi += 1
                        S.dma("sp", lambda e, gt=gt, g=g, dc=dc, tb=tb: e.dma_start(out=gt[:], in_=self.bgT_d[g * 1024 + dc * 128: g * 1024 + (dc + 1) * 128, tb * 512:(tb + 1) * 512]),
                              reads=["projout"], writes=[kgt])
                        for kc in range(4):
                            S.op("pe", lambda e, ps=ps, g=g, kc=kc, dc=dc: e.matmul(ps[:], lhsT=wb[:, g, kc, dc * 128:(dc + 1) * 128], rhs=yt[:, g, kc, :], start=(kc == 0), stop=(kc == 3)),
                                 reads=["mwb", "myt"], writes=[kps])
                        if g == 0:
                            S.op("dve", lambda e, ps=ps, gt=gt: e.tensor_tensor(out=macc[:], in0=ps[:], in1=gt[:], op=ALU.mult), reads=[kps, kgt], writes=["macc"])
                        else:
                            tmp = tmps[g % 2]; ktmp = "mtmp%d" % (g % 2)
                            S.op("dve", lambda e, ps=ps, gt=gt, tmp=tmp: e.tensor_tensor(out=tmp[:], in0=ps[:], in1=gt[:], op=ALU.mult), reads=[kps, kgt], writes=[ktmp])
                            if g == 1:
                                S.op("pool", lambda e, tmp=tmp: e.tensor_tensor(out=macc[:], in0=macc[:], in1=tmp[:], op=ALU.add), reads=[ktmp, "macc"], writes=["macc"])
                            else:
                                S.op("pool", lambda e, tmp=tmp, dc=dc: e.tensor_tensor(out=mT[:, dc, :], in0=macc[:], in1=tmp[:], op=ALU.add), reads=[ktmp, "macc"], writes=["mT"])
                for tt in range(4):
                    t = tb * 4 + tt
                    xt = xts[t % 2]; kxt = "mxt%d" % (t % 2)
                    S.dma("sp", lambda e, xt=xt, t=t: e.dma_start(out=xt[:], in_=self.xres[t * 128:(t + 1) * 128, :]), reads=["xres"], writes=[kxt])
                    for dh in range(2):
                        ps = psy[yi % 2]; kps = "mpsy%d" % (yi % 2); yi += 1
                        for c in range(8):
                            S.op("pe", lambda e, ps=ps, c=c, tt=tt, dh=dh: e.matmul(ps[:], lhsT=mT[:, c, tt * 128:(tt + 1) * 128], rhs=wo[:, c, dh * 512:(dh + 1) * 512], start=(c == 0), stop=(c == 7)),
                                 reads=["mT", "mwo"], writes=[kps])
                        S.op("dve", lambda e, ps=ps, dh=dh: e.tensor_tensor(out=ytmp[:], in0=ps[:], in1=g1bc[:, dh * 512:(dh + 1) * 512], op=ALU.mult), reads=[kps, "g1bc"], writes=["mytmp"])
                        S.op("dve", lambda e, xt=xt, dh=dh: e.tensor_tensor(out=xt[:, dh * 512:(dh + 1) * 512], in0=xt[:, dh * 512:(dh + 1) * 512], in1=ytmp[:], op=ALU.add), reads=["mytmp", kxt], writes=[kxt])
                    S.dma("sp", lambda e, xt=xt, t=t: e.dma_start(out=self.xres[t * 128:(t + 1) * 128, :], in_=xt[:]), reads=[kxt], writes=["xres"])

    def stage_peer(self, l):
        nc, S = self.nc, self.S
        NB = 6
        with ExitStack() as es:
            sb = lambda n, sh, dt: es.enter_context(self.sbt(n, sh, dt))
            pt = lambda n, sh, dt: es.enter_context(self.pst(n, sh, dt))
            g2bc = self._bcast_tile(es, "g2bc", lambda c: self.modc(l, 5, c))
            hT = sb("phT", [128, 8, S_LEN], BF16)
            wq = sb("pwq", [128, 8, D], BF16)
            kbd = sb("pkbd", [128, 8, 256], F32)
            qTt = sb("pqTt", [128, 8, 128], F32)
            sc, s2, cand = sb("psc", [128, 2048], F32), sb("ps2", [128, 2048], F32), sb("pcand", [128, 2048], F32)
            mx, mi, sif = sb("pmx", [128, 256], F32), sb("pmi", [128, 256], U32), sb("psif", [128, 256], F32)
            tv, tp, tpf = sb("ptv", [128, 128], F32), sb("ptp", [128, 128], U32), sb("ptpf", [128, 128], F32)
            af, bf_ = sb("paf", [128, 128], F32), sb("pbf", [128, 128], F32)
            i1, i2 = sb("pi1", [128, 128], F32), sb("pi2", [128, 128], F32)
            idxi = sb("pidxi", [128, 128], I32)
            ee, gg, aa, ga = sb("pee", [128, 128], F32), sb("pgg", [128, 128], F32), sb("paa", [128, 128], F32), sb("pga", [128, 128], F32)
            ssum = sb("pssum", [128, 8], F32)
            h2ts = [sb("ph2t%d" % i, [128, D], F32) for i in range(2)]
            xts = [sb("pxt%d" % i, [128, D], F32) for i in range(2)]
            ubs = [sb("pub%d" % i, [128, D], F32) for i in range(NB)]
            junk, acc = sb("pjunk", [128, D], F32), sb("pacc", [128, D], F32)
            psq = pt("ppsq", [128, 8, 128], F32)
            pssc = pt("ppssc", [128, 2048], F32)
            U2 = self.pk_u.rearrange("l e d -> (l e) d")
            V2 = self.pk_v.rearrange("l e d -> (l e) d")
            iota16 = self.cst[:, C_IOTA:C_IOTA + 16]
            thr15 = self.cst[:, C_THR:C_THR + 15]
            for c in range(8):
                S.dma("sp", lambda e, c=c: e.dma_start(out=hT[:, c, :], in_=self.hT_d[c, :, :]), reads=["hT_d"], writes=["phT"])
            S.dma("pool", lambda e: e.dma_start(out=wq[:], in_=self.pk_wq[l, :, :].rearrange("(kc p) d -> p kc d", p=128)), writes=["pwq"])
            S.dma("sp", lambda e: e.dma_start(out=kbd[:], in_=self.keysbd[l, :, :, :].rearrange("h p n -> p h n")), writes=["pkbd"])
            sc3 = sc[:].rearrange("p (g n) -> p g n", n=128)
            s23 = s2[:].rearrange("p (g n) -> p g n", n=128)
            mx3 = mx[:].rearrange("p (g k) -> p g k", k=16)
            mi3 = mi[:].rearrange("p (g k) -> p g k", k=16)
            mx4 = mx[:].rearrange("p (h q k) -> p h q k", h=8, q=2)
            sif4 = sif[:].rearrange("p (h q k) -> p h q k", h=8, q=2)
            cand4 = cand[:].rearrange("p (h a b) -> p h a b", h=8, a=16)
            tv3 = tv[:].rearrange("p (h k) -> p h k", h=8)
            tp3 = tp[:].rearrange("p (h k) -> p h k", h=8)
            oh4 = s2[:].rearrange("p (h k a) -> p h k a", h=8, k=16)
            cmp3 = sc[:, 0:1920].rearrange("p (r j) -> p r j", j=15)
            B4 = [128, 8, 16, 16]
            ui = 0
            for t in range(NT):
                ts = slice(t * 128, (t + 1) * 128)
                h2t = h2ts[t % 2]; kh2 = "ph2t%d" % (t % 2)
                xt = xts[t % 2]; kxt = "pxt%d" % (t % 2)
                S.dma("sp", lambda e, h2t=h2t, ts=ts: e.dma_start(out=h2t[:], in_=self.h2_d[ts, :]), reads=["h2_d"], writes=[kh2])
                S.dma("sp", lambda e, xt=xt, ts=ts: e.dma_start(out=xt[:], in_=self.xres[ts, :]), reads=["xres"], writes=[kxt])
                for h in range(8):
                    for kc in range(8):
                        S.op("pe", lambda e, h=h, kc=kc, ts=ts: e.matmul(psq[:, h, :], lhsT=wq[:, kc, h * 128:(h + 1) * 128], rhs=hT[:, kc, ts], start=(kc == 0), stop=(kc == 7)),
                             reads=["pwq", "phT"], writes=["ppsq"])
                for hh in range(2):
                    S.op("act", lambda e, hh=hh: e.activation(out=qTt[:, hh * 4:(hh + 1) * 4, :], in_=psq[:, hh * 4:(hh + 1) * 4, :], func=AF.Copy), reads=["ppsq"], writes=["pqTt"])
                for h in range(8):
                    S.op("pe", lambda e, h=h: e.matmul(pssc[:, h * 256:(h + 1) * 256], lhsT=qTt[:, h, :], rhs=kbd[:, h, :], start=True, stop=True), reads=["pqTt", "pkbd"], writes=["ppssc"])
                for qd in range(4):
                    S.op("act", lambda e, qd=qd: e.activation(out=sc[:, qd * 512:(qd + 1) * 512], in_=pssc[:, qd * 512:(qd + 1) * 512], func=AF.Copy), reads=["ppssc"], writes=["psc"])
                for g in range(16):
                    S.op("dve", lambda e, g=g: e.max(out=mx3[:, g, 0:8], in_=sc3[:, g, :]), reads=["psc"], writes=["pmx"])
                    S.op("dve", lambda e, g=g: e.max_index(out=mi3[:, g, 0:8], in_max=mx3[:, g, 0:8], in_values=sc3[:, g, :]), reads=["psc", "pmx"], writes=["pmi"])
                    S.op("dve", lambda e, g=g: e.match_replace(out=s23[:, g, :], in_to_replace=mx3[:, g, 0:8], in_values=sc3[:, g, :], imm_value=-1e30), reads=["psc", "pmx"], writes=["ps2"])
                    S.op("dve", lambda e, g=g: e.max(out=mx3[:, g, 8:16], in_=s23[:, g, :]), reads=["ps2"], writes=["pmx"])
                    S.op("dve", lambda e, g=g: e.max_index(out=mi3[:, g, 8:16], in_max=mx3[:, g, 8:16], in_values=s23[:, g, :]), reads=["ps2", "pmx"], writes=["pmi"])
                S.op("dve", lambda e: e.tensor_copy(out=sif[:], in_=mi[:]), reads=["pmi"], writes=["psif"])
                S.op("dve", lambda e: e.tensor_tensor(out=cand4, in0=mx4[:, :, 0, :].unsqueeze(3).to_broadcast(B4), in1=mx4[:, :, 1, :].unsqueeze(2).to_broadcast(B4), op=ALU.add),
                     reads=["pmx"], writes=["pcand"])
                for h in range(8):
                    hs = slice(h * 256, (h + 1) * 256)
                    S.op("dve", lambda e, h=h, hs=hs: e.max(out=tv3[:, h, 0:8], in_=cand[:, hs]), reads=["pcand"], writes=["ptv"])
                    S.op("dve", lambda e, h=h, hs=hs: e.max_index(out=tp3[:, h, 0:8], in_max=tv3[:, h, 0:8], in_values=cand[:, hs]), reads=["pcand", "ptv"], writes=["ptp"])
                    S.op("dve", lambda e, h=h, hs=hs: e.match_replace(out=s2[:, hs], in_to_replace=tv3[:, h, 0:8], in_values=cand[:, hs], imm_value=-1e30), reads=["pcand", "ptv"], writes=["ps2"])
                    S.op("dve", lambda e, h=h, hs=hs: e.max(out=tv3[:, h, 8:16], in_=s2[:, hs]), reads=["ps2"], writes=["ptv"])
                    S.op("dve", lambda e, h=h, hs=hs: e.max_index(out=tp3[:, h, 8:16], in_max=tv3[:, h, 8:16], in_values=s2[:, hs]), reads=["ps2", "ptv"], writes=["ptp"])
                S.op("dve", lambda e: e.tensor_copy(out=tpf[:], in_=tp[:]), reads=["ptp"], writes=["ptpf"])
                S.op("dve", lambda e: e.tensor_tensor(out=cmp3, in0=tpf[:].unsqueeze(2).to_broadcast([128, 128, 15]), in1=thr15.unsqueeze(1).to_broadcast([128, 128, 15]), op=ALU.is_ge),
                     reads=["ptpf", "cst"], writes=["psc"])
                S.op("dve", lambda e: e.tensor_reduce(out=af[:], in_=cmp3, axis=AX.X, op=ALU.add), reads=["psc"], writes=["paf"])
                S.op("dve", lambda e: e.scalar_tensor_tensor(out=bf_[:], in0=af[:], scalar=-16.0, in1=tpf[:], op0=ALU.mult, op1=ALU.add), reads=["paf", "ptpf"], writes=["pbf"])
                for (src, q_, dst, kd) in ((af, 0, i1, "pi1"), (bf_, 1, i2, "pi2")):
                    ksrc = "paf" if q_ == 0 else "pbf"
                    S.op("dve", lambda e, src=src: e.tensor_tensor(out=oh4, in0=src[:].rearrange("p (h k) -> p h k", h=8).unsqueeze(3).to_broadcast(B4),
                                                                   in1=iota16.unsqueeze(1).unsqueeze(1).to_broadcast(B4), op=ALU.is_equal), reads=[ksrc, "cst"], writes=["ps2"])
                    S.op("dve", lambda e, q_=q_: e.tensor_tensor(out=oh4, in0=oh4, in1=sif4[:, :, q_, :].unsqueeze(2).to_broadcast(B4), op=ALU.mult), reads=["ps2", "psif"], writes=["ps2"])
                    S.op("dve", lambda e, dst=dst: e.tensor_reduce(out=dst[:], in_=oh4, axis=AX.X, op=ALU.add), reads=["ps2"], writes=[kd])
                S.op("dve", lambda e: e.tensor_scalar(out=i2[:], in0=i2[:], scalar1=float(l * 16384), scalar2=None, op0=ALU.add), reads=["pi2"], writes=["pi2"])
                S.op("dve", lambda e: e.scalar_tensor_tensor(out=i1[:], in0=i1[:], scalar=128.0, in1=i2[:], op0=ALU.mult, op1=ALU.add), reads=["pi1", "pi2"], writes=["pi1"])
                S.op("dve", lambda e: e.tensor_copy(out=idxi[:], in_=i1[:]), reads=["pi1"], writes=["pidxi"])
                S.op("dve", lambda e: e.tensor_tensor(out=ee[:].rearrange("p (h k) -> p h k", h=8), in0=tv3, in1=tv3[:, :, 0:1].to_broadcast([128, 8, 16]), op=ALU.subtract), reads=["ptv"], writes=["pee"])
                S.op("act", lambda e: e.activation(out=ee[:], in_=ee[:], func=AF.Exp), reads=["pee"], writes=["pee"])
                S.op("dve", lambda e: e.tensor_reduce(out=ssum[:], in_=ee[:].rearrange("p (h k) -> p h k", h=8), axis=AX.X, op=ALU.add), reads=["pee"], writes=["pssum"])
                S.op("dve", lambda e: e.reciprocal(out=ssum[:], in_=ssum[:]), reads=["pssum"], writes=["pssum"])
                S.op("dve", lambda e: e.tensor_tensor(out=gg[:].rearrange("p (h k) -> p h k", h=8), in0=ee[:].rearrange("p (h k) -> p h k", h=8), in1=ssum[:].unsqueeze(2).to_broadcast([128, 8, 16]), op=ALU.mult),
                     reads=["pee", "pssum"], writes=["pgg"])
                for r in range(128):
                    ub = ubs[ui % NB]; kub = "pub%d" % (ui % NB); ui += 1
                    S.dma("pool", lambda e, ub=ub, r=r: e.indirect_dma_start(out=ub[:], out_offset=None, in_=U2[:, :], in_offset=bass.IndirectOffsetOnAxis(ap=idxi[:, r:r + 1], axis=0)),
                          reads=["pidxi"], writes=[kub])
                    S.op("dve", lambda e, ub=ub, r=r, h2t=h2t: e.scalar_tensor_tensor(out=junk[:], in0=ub[:], scalar=1.0, in1=h2t[:], op0=ALU.mult, op1=ALU.mult, accum_out=aa[:, r:r + 1]),
                         reads=[kub, kh2], writes=["pjunk", "paa"])
                S.op("act", lambda e: e.activation(out=ga[:], in_=aa[:], func=AF.Gelu_apprx_tanh), reads=["paa"], writes=["pga"])
                S.op("dve", lambda e: e.tensor_tensor(out=ga[:], in0=ga[:], in1=gg[:], op=ALU.mult), reads=["pga", "pgg"], writes=["pga"])
                for r in range(128):
                    ub = ubs[ui % NB]; kub = "pub%d" % (ui % NB); ui += 1
                    S.dma("pool", lambda e, ub=ub, r=r: e.indirect_dma_start(out=ub[:], out_offset=None, in_=V2[:, :], in_offset=bass.IndirectOffsetOnAxis(ap=idxi[:, r:r + 1], axis=0)),
                          reads=["pidxi"], writes=[kub])
                    if r == 0:
                        S.op("dve", lambda e, ub=ub: e.tensor_scalar(out=acc[:], in0=ub[:], scalar1=ga[:, 0:1], scalar2=None, op0=ALU.mult), reads=[kub, "pga"], writes=["pacc"])
                    else:
                        S.op("dve", lambda e, ub=ub, r=r: e.scalar_tensor_tensor(out=acc[:], in0=ub[:], scalar=ga[:, r:r + 1], in1=acc[:], op0=ALU.mult, op1=ALU.add), reads=[kub, "pga", "pacc"], writes=["pacc"])
                S.op("dve", lambda e: e.tensor_tensor(out=acc[:], in0=acc[:], in1=g2bc[:], op=ALU.mult), reads=["pacc", "g2bc"], writes=["pacc"])
                S.op("dve", lambda e, xt=xt: e.tensor_tensor(out=xt[:], in0=xt[:], in1=acc[:], op=ALU.add), reads=["pacc", kxt], writes=[kxt])
                S.dma("sp", lambda e, xt=xt, ts=ts: e.dma_start(out=self.xres[ts, :], in_=xt[:]), reads=[kxt], writes=["xres"])

    def stage_final(self):
        nc, S = self.nc, self.S
        with ExitStack() as es:
            sb = lambda n, sh, dt: es.enter_context(self.sbt(n, sh, dt))
            o, _ = SM_OFF["fg"]
            fgbc = self._bcast_tile(es, "fgbc", lambda c: self.sm[:, o + c:o + c + 1])
            xts = [sb("fxt%d" % i, [128, D], F32) for i in range(2)]
            sq = sb("fsq", [128, D], F32)
            ss = sb("fss", [128, 4], F32)
            for t in range(NT):
                xt = xts[t % 2]; kxt = "fxt%d" % (t % 2)
                S.dma("sp", lambda e, xt=xt, t=t: e.dma_start(out=xt[:], in_=self.xres[t * 128:(t + 1) * 128, :]), reads=["xres"], writes=[kxt])
                S.op("act", lambda e, xt=xt: e.activation(out=sq[:], in_=xt[:], func=AF.Square, accum_out=ss[:, 0:1]), reads=[kxt], writes=["fsq", "fss"])
                S.op("dve", lambda e: e.tensor_scalar(out=ss[:, 1:2], in0=ss[:, 0:1], scalar1=1.0 / D, scalar2=EPS, op0=ALU.mult, op1=ALU.add), reads=["fss"], writes=["fss"])
                S.op("act", lambda e: e.activation(out=ss[:, 2:3], in_=ss[:, 1:2], func=AF.Sqrt), reads=["fss"], writes=["fss"])
                S.op("dve", lambda e: e.reciprocal(out=ss[:, 3:4], in_=ss[:, 2:3]), reads=["fss"], writes=["fss"])
                S.op("dve", lambda e, xt=xt: e.scalar_tensor_tensor(out=xt[:], in0=xt[:], scalar=ss[:, 3:4], in1=fgbc[:], op0=ALU.mult, op1=ALU.mult), reads=[kxt, "fss", "fgbc"], writes=[kxt])
                S.dma("sp", lambda e, xt=xt, t=t: e.dma_start(out=self.out_d[t * 128:(t + 1) * 128, :], in_=xt[:]), reads=[kxt], writes=["out_d"])


def prep_inputs(inputs):
    inp = {k: np.ascontiguousarray(np.asarray(v)) for k, v in inputs.items()}
    consts = make_consts()
    keys = inp["pk_keys"]
    kbd = np.zeros((DEPTH, 8, 128, 256), np.float32)
    for p in range(2):
        kbd[:, :, p * 64:(p + 1) * 64, p * 128:(p + 1) * 128] = keys[:, :, p].transpose(0, 1, 3, 2)
    shared = dict(consts=consts, mod_w=inp["mod_w"], w_in=inp["w_in"], w_branch=inp["w_branch"], w_out=inp["w_out"],
                  pk_wq=inp["pk_wq"], keysbd=kbd, pk_u=inp["pk_u"], pk_v=inp["pk_v"])
    in_maps = []
    for b in range(8):
        m = dict(shared)
        m["x"] = inp["x"][b]
        m["small"] = make_small(inp, b)
        in_maps.append(m)
    return in_maps


_PROG_CACHE = {}


def kernel(**inputs):
    in_maps = prep_inputs(inputs)
    if "nc" not in _PROG_CACHE:
        _PROG_CACHE["nc"] = Prog().build()
    res = run_bass_kernel_spmd(_PROG_CACHE["nc"], in_maps, core_ids=list(range(8)))
    return np.stack([np.asarray(r["out"]) for r in res.results], axis=0).astype(np.float32)
```

```python
from contextlib import ExitStack
import numpy as np
import concourse.bass as bass
import concourse.mybir as mybir
from concourse.bass_utils import run_bass_kernel_spmd

F32 = mybir.dt.float32
BF16 = mybir.dt.bfloat16
U32 = mybir.dt.uint32
I32 = mybir.dt.int32
AF = mybir.ActivationFunctionType
ALU = mybir.AluOpType
AX = mybir.AxisListType

D = 1024
S_LEN = 2048
DEPTH = 4
NT = S_LEN // 128
INW = 8712
EPS = 1e-6
PEER_IMPLEMENTED = True


class Sched:
    ENG = ("pe", "act", "dve", "pool", "sp")

    def __init__(self, nc, dma_slots=None, same_engine_sync=True):
        self.nc = nc
        self.ops = {e: [] for e in self.ENG}
        self.count = {e: 0 for e in self.ENG}
        self.sem = {}
        self.last_w = {}
        self.readers = {}
        self.same_engine_sync = same_engine_sync
        self.dma_slots_n = dma_slots or {"sp": 8, "pool": 12, "act": 4}
        self.dma_sems = {}
        self.dma_rr = {}
        self.seen = {e: {} for e in self.ENG}
        self._ctx = []
        self.n_ops = 0

    def open(self):
        nc = self.nc
        for e in self.ENG:
            cm = nc.semaphore("s_" + e)
            self.sem[e] = cm.__enter__()
            self._ctx.append(cm)
        for q, n in self.dma_slots_n.items():
            self.dma_sems[q] = []
            self.dma_rr[q] = 0
            for i in range(n):
                cm = nc.semaphore("sd_%s%d" % (q, i))
                s = cm.__enter__()
                self._ctx.append(cm)
                self.dma_sems[q].append(dict(sem=s, count=0, name="d_%s%d" % (q, i)))

    def close(self):
        for cm in reversed(self._ctx):
            cm.__exit__(None, None, None)

    def _tok_wait(self, tok):
        if tok[0] == "c":
            return (self.sem[tok[1]], tok[1], tok[2])
        d = self.dma_sems[tok[1]][tok[2]]
        return (d["sem"], d["name"], tok[3])

    def _collect(self, eng, reads, writes):
        toks = []
        for k in reads:
            t = self.last_w.get(k)
            if t is not None:
                toks.append(t)
        for k in writes:
            t = self.last_w.get(k)
            if t is not None:
                toks.append(t)
            toks.extend(self.readers.get(k, ()))
        waits = {}
        for t in toks:
            if t[0] == "c" and t[1] == eng and not self.same_engine_sync:
                continue
            s, name, v = self._tok_wait(t)
            if self.seen[eng].get(name, 0) >= v:
                continue
            if name not in waits or waits[name][1] < v:
                waits[name] = (s, v)
        for name, (s, v) in waits.items():
            self.seen[eng][name] = v
        return list(waits.values())

    def _commit(self, tok, reads, writes):
        for k in reads:
            self.readers.setdefault(k, []).append(tok)
        for k in writes:
            self.last_w[k] = tok
            self.readers[k] = []

    def op(self, eng, fn, reads=(), writes=()):
        waits = self._collect(eng, reads, writes)
        self.count[eng] += 1
        tok = ("c", eng, self.count[eng])
        self.ops[eng].append((fn, waits, "c", None))
        self._commit(tok, reads, writes)
        self.n_ops += 1
        return tok

    def dma(self, eng, fn, reads=(), writes=()):
        slot = self.dma_rr[eng]
        self.dma_rr[eng] = (slot + 1) % len(self.dma_sems[eng])
        d = self.dma_sems[eng][slot]
        waits = self._collect(eng, reads, writes)
        if d["count"] > 0 and self.seen[eng].get(d["name"], 0) < d["count"]:
            waits.append((d["sem"], d["count"]))
            self.seen[eng][d["name"]] = d["count"]
        d["count"] += 16
        tok = ("d", eng, slot, d["count"])
        self.ops[eng].append((fn, waits, "d", d["sem"]))
        self._commit(tok, reads, writes)
        self.n_ops += 1
        return tok

    def barrier(self):
        for e in self.ENG:
            waits = []
            for e2 in self.ENG:
                v = self.count[e2]
                if v > 0 and self.seen[e].get(e2, 0) < v and e2 != e:
                    waits.append((self.sem[e2], v))
                    self.seen[e][e2] = v
            for q in self.dma_sems:
                for d in self.dma_sems[q]:
                    if d["count"] > 0 and self.seen[e].get(d["name"], 0) < d["count"]:
                        waits.append((d["sem"], d["count"]))
                        self.seen[e][d["name"]] = d["count"]
            if self.count[e] > 0:
                waits.append((self.sem[e], self.count[e]))
                self.seen[e][e] = self.count[e]
            self.ops[e].append((None, waits, "w", None))

    def emit(self):
        nc = self.nc
        sched = self
        ops = self.ops
        self.ops = {e: [] for e in self.ENG}
        with nc.Block() as block:
            def run(engname, e):
                for fn, waits, kind, dsem in ops[engname]:
                    for s, v in waits:
                        e.wait_ge(s, v)
                    if fn is None:
                        continue
                    ins = fn(e)
                    if kind == "c":
                        ins.then_inc(sched.sem[engname], 1)
                    else:
                        ins.then_inc(dsem, 16)

            @block.sync
            def _(e):
                run("sp", e)

            @block.tensor
            def _(e):
                run("pe", e)

            @block.scalar
            def _(e):
                run("act", e)

            @block.vector
            def _(e):
                run("dve", e)

            @block.gpsimd
            def _(e):
                run("pool", e)


def _small_layout():
    off = {}
    o = 0
    for name, n in (("mod_b", DEPTH * 48), ("nmg", DEPTH * 8), ("nfg", DEPTH * 8),
                    ("convw", DEPTH * 32), ("convb", DEPTH * 8), ("gateb", DEPTH * 2),
                    ("lbl", DEPTH * 4), ("hgn", DEPTH * 4), ("mln", DEPTH * 4), ("fg", 8), ("cT", 8)):
        off[name] = (o, n)
        o += n
    return off, o


SM_OFF, NS = _small_layout()
C_ID, C_ONES, C_TRI, C_M64, C_SEL, C_IOTA, C_THR = 0, 128, 256, 384, 448, 960, 976
NCONST = 992


def make_consts():
    c = np.zeros((128, NCONST), np.float32)
    c[:, C_ID:C_ID + 128] = np.eye(128, dtype=np.float32)
    c[:, C_ONES:C_ONES + 128] = 1.0
    sp = np.arange(128)[:, None]
    s = np.arange(128)[None, :]
    c[:, C_TRI:C_TRI + 128] = (sp >= s).astype(np.float32)
    t = np.arange(64)[None, :]
    c[:, C_M64:C_M64 + 64] = (t >= (sp % 64)).astype(np.float32)
    for h in range(4):
        c[h, C_SEL + h * 128:C_SEL + (h + 1) * 128] = 1.0
    c[:, C_IOTA:C_IOTA + 16] = np.arange(16, dtype=np.float32)[None, :]
    c[:, C_THR:C_THR + 15] = 16.0 * np.arange(1, 16, dtype=np.float32)[None, :]
    return c


def make_small(inp, b):
    sm = np.zeros((128, NS), np.float32)

    def put(name, arr):
        o, n = SM_OFF[name]
        arr = np.asarray(arr, np.float32).reshape(128, n)
        sm[:, o:o + n] = arr

    put("mod_b", inp["mod_b"].reshape(DEPTH, 48, 128).transpose(2, 0, 1))
    put("nmg", inp["norm_mix_g"].reshape(DEPTH, 8, 128).transpose(2, 0, 1))
    put("nfg", inp["norm_ffn_g"].reshape(DEPTH, 8, 128).transpose(2, 0, 1))
    put("convw", inp["ml_conv_w"].reshape(DEPTH, 4, 8, 128).transpose(3, 0, 1, 2))
    put("convb", inp["ml_conv_b"].reshape(DEPTH, 8, 128).transpose(2, 0, 1))
    gb = np.zeros((128, DEPTH, 2), np.float32)
    gb[0:4, :, 0] = inp["ml_gate_b"][:, 0:4].T
    gb[0:4, :, 1] = inp["ml_gate_b"][:, 4:8].T
    put("gateb", gb)
    put("lbl", inp["hg_lb_logits"].reshape(DEPTH, 4, 128).transpose(2, 0, 1))
    put("hgn", inp["hg_norm_g"].reshape(DEPTH, 4, 128).transpose(2, 0, 1))
    put("mln", inp["ml_norm_g"].reshape(DEPTH, 4, 128).transpose(2, 0, 1))
    put("fg", inp["final_g"].reshape(8, 128).T)
    put("cT", inp["c"][b].reshape(8, 128).T)
    return sm


class Prog:
    def __init__(self, n_layers=DEPTH, stages=None, debug=()):
        self.n_layers = n_layers
        self.stages = stages
        self.debug = set(debug)
        self.nc = bass.Bass("TRN2", target_bir_lowering=False)
        self.S = Sched(self.nc)
        self.uid = 0

    def sbt(self, name, shape, dt):
        self.uid += 1
        return self.nc.sbuf_tensor("%s_u%d" % (name, self.uid), shape, dt)

    def pst(self, name, shape, dt):
        self.uid += 1
        return self.nc.psum_tensor("%s_u%d" % (name, self.uid), shape, dt)

    def want(self, st):
        return self.stages is None or st in self.stages

    def dram(self, name, shape, dt, kind=None):
        if kind is None:
            kind = "ExternalOutput" if name in self.debug else "Internal"
        return self.nc.dram_tensor(name, list(shape), dt, kind=kind).ap()

    def build(self):
        nc, S = self.nc, self.S
        L = self.n_layers
        ext = lambda n, s, dt=F32: nc.dram_tensor(n, list(s), dt, kind="ExternalInput").ap()
        self.x_in = ext("x", [S_LEN, D])
        self.small_d = ext("small", [128, NS])
        self.consts_d = ext("consts", [128, NCONST])
        self.mod_w = ext("mod_w", [L, D, 6 * D])
        self.w_in = ext("w_in", [L, D, INW])
        self.input_names = ["x", "small", "consts", "mod_w", "w_in"]
        if self.want("merge"):
            self.w_branch = ext("w_branch", [L, 3, 512, D])
            self.w_out = ext("w_out", [L, D, D])
            self.input_names += ["w_branch", "w_out"]
        if self.want("peer") and PEER_IMPLEMENTED:
            self.pk_wq = ext("pk_wq", [L, D, D])
            self.keysbd = ext("keysbd", [L, 8, 128, 256])
            self.pk_uv = ext("pk_uv", [L, 16384, 2 * D])
            self.input_names += ["pk_wq", "keysbd", "pk_uv"]
        self.out_d = nc.dram_tensor("out", [S_LEN, D], F32, kind="ExternalOutput").ap()
        self.xres = self.dram("xres", [S_LEN, D], F32)
        self.hT_d = self.dram("hT", [8, 128, S_LEN], BF16)
        self.qT_d = self.dram("qT", [512, S_LEN], BF16)
        self.kT_d = self.dram("kT", [512, S_LEN], BF16)
        self.v_d = self.dram("v_tok", [S_LEN, 512], BF16)
        self.hqT_d = self.dram("hqT", [512, S_LEN], F32)
        self.hfT_d = self.dram("hfT", [512, S_LEN], F32)
        self.hgT_d = self.dram("hgT", [512, S_LEN], F32)
        self.hi_d = self.dram("hi_tok", [S_LEN, 512], BF16)
        self.mqkT_d = self.dram("mqkT", [1024, S_LEN], F32)
        self.mv_d = self.dram("mv_tok", [S_LEN, 512], BF16)
        self.moT_d = self.dram("moT", [512, S_LEN], F32)
        self.gT_d = self.dram("gT", [2, 4, S_LEN], F32)
        self.bgT_d = self.dram("bgT", [3072, S_LEN], F32)
        self.yT_d = self.dram("yT", [3, 512, S_LEN], BF16)
        self.h2_d = self.dram("h2_tok", [S_LEN, D], F32)
        self.uvb = self.dram("uvb", [16384, 2 * D], BF16)
        S.open()
        with (self.sbt("consts", [128, NCONST], F32) as cst,
              self.sbt("constb", [128, 384], BF16) as cstb,
              self.sbt("small", [128, NS], F32) as sm,
              self.sbt("modT", [128, DEPTH * 48], F32) as modT,
              self.sbt("lbT", [128, DEPTH * 4], F32) as lbT):
            self.cst, self.cstb, self.sm, self.modT, self.lbT = cst, cstb, sm, modT, lbT
            S.dma("sp", lambda e: e.dma_start(out=cst[:], in_=self.consts_d[:, :]), writes=["cst"])
            S.dma("sp", lambda e: e.dma_start(out=sm[:], in_=self.small_d[:, :]), writes=["sm"])
            S.op("dve", lambda e: e.tensor_copy(out=cstb[:], in_=cst[:, 0:384]), reads=["cst"], writes=["cstb"])
            S.dma("sp", lambda e: e.dma_start(out=self.xres[:, :], in_=self.x_in[:, :]), writes=["xres"])
            self.stage_mod()
            S.barrier(); S.emit()
            for l in range(L):
                if self.want("norm1"):
                    self.stage_norm(l, 0)
                    S.barrier(); S.emit()
                if self.want("proj"):
                    self.stage_proj(l)
                    S.barrier(); S.emit()
                if self.want("attn"):
                    self.stage_attn(l)
                    S.barrier(); S.emit()
                if self.want("hgrn"):
                    self.stage_hgrn(l)
                    S.barrier(); S.emit()
                if self.want("mlstm"):
                    self.stage_mlstm(l)
                    S.barrier(); S.emit()
                if self.want("merge"):
                    self.stage_merge(l)
                    S.barrier(); S.emit()
                if self.want("peer") and PEER_IMPLEMENTED:
                    self.stage_uvcast(l)
                    S.barrier(); S.emit()
                    self.stage_norm(l, 1)
                    S.barrier(); S.emit()
                    self.stage_peer(l)
                    S.barrier(); S.emit()
            self.stage_final()
            S.barrier(); S.emit()
        S.close()
        return nc

    def ident_f(self):
        return self.cst[:, C_ID:C_ID + 128]

    def ones_f(self):
        return self.cst[:, C_ONES:C_ONES + 128]

    def tri_f(self):
        return self.cst[:, C_TRI:C_TRI + 128]

    def ident_b(self):
        return self.cstb[:, 0:128]

    def ones_b(self):
        return self.cstb[:, 128:256]

    def smc(self, name, l, j, n=1):
        o, tot = SM_OFF[name]
        per = tot // DEPTH
        return self.sm[:, o + l * per + j: o + l * per + j + n]

    def modc(self, l, part, c):
        j = l * 48 + part * 8 + c
        return self.modT[:, j:j + 1]

    def stage_mod(self):
        nc, S = self.nc, self.S
        L = self.n_layers
        sm = self.sm
        with (self.sbt("condT", [128, 8], F32) as condT,
              self.sbt("mw0", [128, 8, 768], F32) as mw0,
              self.sbt("mw1", [128, 8, 768], F32) as mw1,
              self.sbt("lbe", [128, DEPTH * 4], F32) as lbe,
              self.sbt("lbs", [128, 4], F32) as lbs,
              self.sbt("lbm", [128, 4], F32) as lbm,
              self.pst("ps_mod", [128, DEPTH * 48], F32) as psm):
            o, _ = SM_OFF["cT"]
            S.op("act", lambda e: e.activation(out=condT[:], in_=sm[:, o:o + 8], func=AF.Silu), reads=["sm"], writes=["condT"])
            mws = [mw0, mw1]
            gi = 0
            for l in range(L):
                for g in range(8):
                    mw = mws[gi % 2]
                    key = "mw%d" % (gi % 2)
                    gi += 1
                    src = self.mod_w[l, :, g * 768:(g + 1) * 768].rearrange("(kc p) c -> p kc c", p=128)
                    S.dma("sp", lambda e, mw=mw, src=src: e.dma_start(out=mw[:], in_=src), writes=[key])
                    for cc in range(6):
                        j = l * 48 + g * 6 + cc
                        for kc in range(8):
                            S.op("pe", lambda e, mw=mw, cc=cc, kc=kc, j=j: e.matmul(
                                psm[:, j:j + 1], lhsT=mw[:, kc, cc * 128:(cc + 1) * 128], rhs=condT[:, kc:kc + 1],
                                start=(kc == 0), stop=(kc == 7)), reads=[key, "condT"], writes=["psm"])
            ob, _ = SM_OFF["mod_b"]
            S.op("dve", lambda e: e.tensor_tensor(out=self.modT[:, 0:L * 48], in0=psm[:, 0:L * 48], in1=sm[:, ob:ob + L * 48], op=ALU.add),
                 reads=["psm", "sm"], writes=["modT"])
            ol, _ = SM_OFF["lbl"]
            lg = lambda l: sm[:, ol + l * 4: ol + l * 4 + 4]
            S.op("dve", lambda e: e.tensor_tensor(out=lbm[:], in0=lg(0), in1=lg(1), op=ALU.max), reads=["sm"], writes=["lbm"])
            for l in (2, 3):
                S.op("dve", lambda e, l=l: e.tensor_tensor(out=lbm[:], in0=lbm[:], in1=lg(l), op=ALU.max), reads=["sm", "lbm"], writes=["lbm"])
            for l in range(DEPTH):
                S.op("dve", lambda e, l=l: e.tensor_tensor(out=lbe[:, l * 4:l * 4 + 4], in0=lg(l), in1=lbm[:], op=ALU.subtract), reads=["sm", "lbm"], writes=["lbe"])
            S.op("act", lambda e: e.activation(out=lbe[:], in_=lbe[:], func=AF.Exp), reads=["lbe"], writes=["lbe"])
            S.op("dve", lambda e: e.tensor_tensor(out=lbs[:], in0=lbe[:, 0:4], in1=lbe[:, 4:8], op=ALU.add), reads=["lbe"], writes=["lbs"])
            for l in (2, 3):
                S.op("dve", lambda e, l=l: e.tensor_tensor(out=lbs[:], in0=lbs[:], in1=lbe[:, l * 4:l * 4 + 4], op=ALU.add), reads=["lbe", "lbs"], writes=["lbs"])
            S.op("dve", lambda e: e.reciprocal(out=lbs[:], in_=lbs[:]), reads=["lbs"], writes=["lbs"])
            lbT = self.lbT
            S.op("dve", lambda e: e.memset(lbT[:, 0:4], 0.0), writes=["lbT"])
            for l in range(1, DEPTH):
                S.op("dve", lambda e, l=l: e.tensor_tensor(out=lbe[:, l * 4:l * 4 + 4], in0=lbe[:, l * 4:l * 4 + 4], in1=lbs[:], op=ALU.mult), reads=["lbe", "lbs"], writes=["lbe"])
                S.op("dve", lambda e, l=l: e.tensor_tensor(out=lbT[:, l * 4:l * 4 + 4], in0=lbT[:, (l - 1) * 4:l * 4], in1=lbe[:, l * 4:l * 4 + 4], op=ALU.add), reads=["lbe", "lbT"], writes=["lbT"])

    def stage_norm(self, l, which):
        nc, S = self.nc, self.S
        gname = "nmg" if which == 0 else "nfg"
        p_shift, p_scale = (0, 1) if which == 0 else (3, 4)
        with (self.sbt("nx0", [128, D], F32) as nx0, self.sbt("nx1", [128, D], F32) as nx1,
              self.sbt("nsq", [128, D], F32) as nsq,
              self.sbt("nb0", [128, D], BF16) as nb0, self.sbt("nb1", [128, D], BF16) as nb1,
              self.sbt("nh0", [128, 8, 128], BF16) as nh0, self.sbt("nh1", [128, 8, 128], BF16) as nh1,
              self.sbt("nt0", [128, D], F32) as nt0, self.sbt("nt1", [128, D], F32) as nt1,
              self.sbt("nss", [128, 4], F32) as nss,
              self.sbt("nG", [128, 8], F32) as nG,
              self.pst("nps0", [128, 8, 128], BF16) as nps0, self.pst("nps1", [128, 8, 128], BF16) as nps1,
              self.pst("npt0", [128, 8, 128], F32) as npt0):
            j0 = l * 48 + p_scale * 8
            S.op("dve", lambda e: e.scalar_tensor_tensor(out=nG[:], in0=self.modT[:, j0:j0 + 8], scalar=1.0, in1=self.smc(gname, l, 0, 8),
                                                         op0=ALU.add, op1=ALU.mult), reads=["modT", "sm"], writes=["nG"])
            nx, nb, nh, nps, nt = [nx0, nx1], [nb0, nb1], [nh0, nh1], [nps0, nps1], [nt0, nt1]
            for t in range(NT):
                i = t % 2
                kx, kb, kh, kp, kt = "nx%d" % i, "nb%d" % i, "nh%d" % i, "nps%d" % i, "nt%d" % i
                S.dma("sp", lambda e, i=i, t=t: e.dma_start(out=nx[i][:], in_=self.xres[t * 128:(t + 1) * 128, :]), reads=["xres"], writes=[kx])
                S.op("act", lambda e, i=i: e.activation(out=nsq[:], in_=nx[i][:], func=AF.Square, accum_out=nss[:, 0:1]), reads=[kx], writes=["nsq", "nss"])
                S.op("dve", lambda e: e.tensor_scalar(out=nss[:, 1:2], in0=nss[:, 0:1], scalar1=1.0 / D, scalar2=EPS, op0=ALU.mult, op1=ALU.add), reads=["nss"], writes=["nss"])
                S.op("act", lambda e: e.activation(out=nss[:, 2:3], in_=nss[:, 1:2], func=AF.Sqrt), reads=["nss"], writes=["nss"])
                S.op("dve", lambda e: e.reciprocal(out=nss[:, 3:4], in_=nss[:, 2:3]), reads=["nss"], writes=["nss"])
                S.op("dve", lambda e, i=i: e.tensor_scalar(out=nb[i][:], in0=nx[i][:], scalar1=nss[:, 3:4], scalar2=None, op0=ALU.mult), reads=[kx, "nss"], writes=[kb])
                for c in range(8):
                    S.op("pe", lambda e, i=i, c=c: e.transpose(nps[i][:, c, :], nb[i][:, c * 128:(c + 1) * 128], self.ident_b()), reads=[kb, "cstb"], writes=[kp])
                for c in range(8):
                    S.op("act", lambda e, i=i, c=c: e.activation(out=nh[i][:, c, :], in_=nps[i][:, c, :], func=AF.Identity,
                                                                 scale=nG[:, c:c + 1], bias=self.modc(l, p_shift, c)), reads=[kp, "nG", "modT"], writes=[kh])
                S.dma("sp", lambda e, i=i, t=t: e.dma_start(out=self.hT_d[:, :, t * 128:(t + 1) * 128].rearrange("c p t -> p c t"), in_=nh[i][:]), reads=[kh], writes=["hT_d"])
                if which == 1:
                    pass
            if which == 1:
                self._h2_tokmajor(l, nx, nss, nG, nt, npt0, p_shift)

    def _h2_tokmajor(self, l, nx, nss, nG, nt, npt0, p_shift):
        nc, S = self.nc, self.S
        with (self.sbt("dg", [128, 128], F32) as dg,
              self.sbt("Gbc", [128, D], F32) as Gbc, self.sbt("Sbc", [128, D], F32) as Sbc):
            for which_v, dst in ((0, Gbc), (1, Sbc)):
                for c in range(8):
                    col = nG[:, c:c + 1] if which_v == 0 else self.modc(l, p_shift, c)
                    S.op("dve", lambda e, col=col: e.tensor_scalar(out=dg[:], in0=self.ident_f(), scalar1=col, scalar2=None, op0=ALU.mult), reads=["cst", "nG", "modT"], writes=["dg"])
                    S.op("pe", lambda e, c=c: e.matmul(npt0[:, c, :], lhsT=self.ones_f(), rhs=dg[:], start=True, stop=True), reads=["cst", "dg"], writes=["npt0"])
                S.op("act", lambda e, dst=dst: e.activation(out=dst[:], in_=npt0[:].rearrange("p c t -> p (c t)"), func=AF.Copy), reads=["npt0"], writes=["bc%d" % which_v])
            for t in range(NT):
                i = t % 2
                kx, kt = "nx%d" % i, "nt%d" % i
                S.dma("sp", lambda e, i=i, t=t: e.dma_start(out=nx[i][:], in_=self.xres[t * 128:(t + 1) * 128, :]), reads=["xres"], writes=[kx])
                S.op("act", lambda e, i=i: e.activation(out=nt[i][:], in_=nx[i][:], func=AF.Square, accum_out=nss[:, 0:1]), reads=[kx], writes=[kt, "nss"])
                S.op("dve", lambda e: e.tensor_scalar(out=nss[:, 1:2], in0=nss[:, 0:1], scalar1=1.0 / D, scalar2=EPS, op0=ALU.mult, op1=ALU.add), reads=["nss"], writes=["nss"])
                S.op("act", lambda e: e.activation(out=nss[:, 2:3], in_=nss[:, 1:2], func=AF.Sqrt), reads=["nss"], writes=["nss"])
                S.op("dve", lambda e: e.reciprocal(out=nss[:, 3:4], in_=nss[:, 2:3]), reads=["nss"], writes=["nss"])
                S.op("dve", lambda e, i=i: e.scalar_tensor_tensor(out=nt[i][:], in0=nx[i][:], scalar=nss[:, 3:4], in1=Gbc[:], op0=ALU.mult, op1=ALU.mult), reads=[kx, "nss", "bc0"], writes=[kt])
                S.op("dve", lambda e, i=i: e.tensor_tensor(out=nt[i][:], in0=nt[i][:], in1=Sbc[:], op=ALU.add), reads=[kt, "bc1"], writes=[kt])
                S.dma("sp", lambda e, i=i, t=t: e.dma_start(out=self.h2_d[t * 128:(t + 1) * 128, :], in_=nt[i][:]), reads=[kt], writes=["h2_d"])

    def stage_proj(self, l):
        nc, S = self.nc, self.S
        fm = []
        for c in range(4):
            fm.append((0 + c * 128, 128, self.qT_d[c * 128:(c + 1) * 128, :], AF.Copy, 0.125, BF16))
        for c in range(4):
            fm.append((512 + c * 128, 128, self.kT_d[c * 128:(c + 1) * 128, :], AF.Copy, 1.0, BF16))
        for c in range(4):
            fm.append((1536 + c * 128, 128, self.hqT_d[c * 128:(c + 1) * 128, :], AF.Copy, 1.0, F32))
        for c in range(4):
            fm.append((2048 + c * 128, 128, self.hfT_d[c * 128:(c + 1) * 128, :], AF.Copy, 1.0, F32))
        for c in range(4):
            fm.append((3072 + c * 128, 128, self.hgT_d[c * 128:(c + 1) * 128, :], AF.Silu, 1.0, F32))
        for c in range(8):
            fm.append((3584 + c * 128, 128, self.mqkT_d[c * 128:(c + 1) * 128, :], AF.Copy, 1.0, F32))
        for c in range(4):
            fm.append((5120 + c * 128, 128, self.moT_d[c * 128:(c + 1) * 128, :], AF.Sigmoid, 1.0, F32))
        fm.append((5632, 4, self.gT_d[0, :, :], AF.Copy, 1.0, F32))
        fm.append((5636, 4, self.gT_d[1, :, :], AF.Copy, 1.0, F32))
        for c in range(24):
            fm.append((5640 + c * 128, 128, self.bgT_d[c * 128:(c + 1) * 128, :], AF.Sigmoid, 1.0, F32))
        tm = [(1024, self.v_d, AF.Copy), (2560, self.hi_d, AF.Silu), (4608, self.mv_d, AF.Copy)]
        with (self.sbt("hT", [128, 8, S_LEN], BF16) as hT,
              self.sbt("pw0", [128, 8, 512], BF16) as pw0, self.sbt("pw1", [128, 8, 512], BF16) as pw1,
              self.sbt("pof0", [128, S_LEN], F32) as pof0, self.sbt("pof1", [128, S_LEN], F32) as pof1,
              self.sbt("pob0", [128, S_LEN], BF16) as pob0, self.sbt("pob1", [128, S_LEN], BF16) as pob1,
              self.sbt("pot0", [128, 512], BF16) as pot0, self.sbt("pot1", [128, 512], BF16) as pot1,
              self.pst("pp0", [128, 512], F32) as pp0, self.pst("pp1", [128, 512], F32) as pp1,
              self.pst("pp2", [128, 512], F32) as pp2, self.pst("pp3", [128, 512], F32) as pp3):
            for c in range(8):
                S.dma("sp", lambda e, c=c: e.dma_start(out=hT[:, c, :], in_=self.hT_d[c, :, :]), reads=["hT_d"], writes=["hT"])
            pw, pof, pob, pot, pp = [pw0, pw1], [pof0, pof1], [pob0, pob1], [pot0, pot1], [pp0, pp1, pp2, pp3]
            wi = 0
            pi = 0
            for ji, (col0, ncol, dest, func, scale, dt) in enumerate(fm):
                w = pw[wi % 2]; kw = "pw%d" % (wi % 2); wi += 1
                src = self.w_in[l, :, col0:col0 + ncol].rearrange("(kc p) c -> p kc c", p=128)
                S.dma("pool", lambda e, w=w, src=src, ncol=ncol: e.dma_start(out=w[:, :, 0:ncol], in_=src), writes=[kw])
                ob = (pof if dt == F32 else pob)[ji % 2]
                ko = ("pof%d" if dt == F32 else "pob%d") % (ji % 2)
                for tb in range(4):
                    ps = pp[pi % 4]; kp = "pp%d" % (pi % 4); pi += 1
                    for kc in range(8):
                        S.op("pe", lambda e, ps=ps, w=w, kc=kc, tb=tb, ncol=ncol: e.matmul(
                            ps[0:ncol, :], lhsT=w[:, kc, 0:ncol], rhs=hT[:, kc, tb * 512:(tb + 1) * 512],
                            start=(kc == 0), stop=(kc == 7)), reads=[kw, "hT"], writes=[kp])
                    S.op("act", lambda e, ps=ps, ob=ob, tb=tb, ncol=ncol, func=func, scale=scale: e.activation(
                        out=ob[0:ncol, tb * 512:(tb + 1) * 512], in_=ps[0:ncol, :], func=func, scale=scale), reads=[kp], writes=[ko])
                S.dma("sp", lambda e, ob=ob, dest=dest, ncol=ncol: e.dma_start(out=dest, in_=ob[0:ncol, :]), reads=[ko], writes=["projout"])
            for (col0, dest, func) in tm:
                w = pw[wi % 2]; kw = "pw%d" % (wi % 2); wi += 1
                src = self.w_in[l, :, col0:col0 + 512].rearrange("(kc p) c -> p kc c", p=128)
                S.dma("pool", lambda e, w=w, src=src: e.dma_start(out=w[:], in_=src), writes=[kw])
                for t in range(NT):
                    ps = pp[pi % 4]; kp = "pp%d" % (pi % 4); pi += 1
                    for kc in range(8):
                        S.op("pe", lambda e, ps=ps, w=w, kc=kc, t=t: e.matmul(
                            ps[:], lhsT=hT[:, kc, t * 128:(t + 1) * 128], rhs=w[:, kc, :],
                            start=(kc == 0), stop=(kc == 7)), reads=[kw, "hT"], writes=[kp])
                    ot = pot[t % 2]; kt = "pot%d" % (t % 2)
                    S.op("act", lambda e, ps=ps, ot=ot, func=func: e.activation(out=ot[:], in_=ps[:], func=func), reads=[kp], writes=[kt])
                    S.dma("sp", lambda e, ot=ot, dest=dest, t=t: e.dma_start(out=dest[t * 128:(t + 1) * 128, :], in_=ot[:]), reads=[kt], writes=["projout"])

    def stage_attn(self, l):
        nc, S = self.nc, self.S
        with ExitStack() as es:
            sb = lambda n, sh, dt: es.enter_context(self.sbt(n, sh, dt))
            pt = lambda n, sh, dt: es.enter_context(self.pst(n, sh, dt))
            aq0, aq1 = sb("aq0", [64, S_LEN], BF16), sb("aq1", [64, S_LEN], BF16)
            ak0, ak1 = sb("ak0", [64, S_LEN], BF16), sb("ak1", [64, S_LEN], BF16)
            av0, av1 = sb("av0", [128, NT, 64], BF16), sb("av1", [128, NT, 64], BF16)
            azs0, azs1 = sb("azs0", [128, 512], F32), sb("azs1", [128, 512], F32)
            asp0, asp1 = sb("asp0", [128, 512], F32), sb("asp1", [128, 512], F32)
            asb0, asb1 = sb("asb0", [128, 512], BF16), sb("asb1", [128, 512], BF16)
            alw0, alw1 = sb("alw0", [128, 512], F32), sb("alw1", [128, 512], F32)
            awt0, awt1 = sb("awt0", [128, 512], BF16), sb("awt1", [128, 512], BF16)
            ayo0, ayo1 = sb("ayo0", [64, 512], BF16), sb("ayo1", [64, 512], BF16)
            apz0, apz1 = pt("apz0", [128, 512], F32), pt("apz1", [128, 512], F32)
            apc0, apc1 = pt("apc0", [128, 512], F32), pt("apc1", [128, 512], F32)
            apy0, apy1 = pt("apy0", [64, 512], F32), pt("apy1", [64, 512], F32)
            apr0, apr1 = pt("apr0", [128, 512], F32), pt("apr1", [128, 512], F32)
            asb, apr = [asb0, asb1], [apr0, apr1]
            tri_b = self.cstb[:, 256:384]
            aq, ak, av = [aq0, aq1], [ak0, ak1], [av0, av1]
            azs, asp, alw, awt = [azs0, azs1], [asp0, asp1], [alw0, alw1], [awt0, awt1]
            ayo, apz, apc, apy = [ayo0, ayo1], [apz0, apz1], [apc0, apc1], [apy0, apy1]
            it = 0
            yi = 0
            for h in range(8):
                hb = h % 2
                q, k, v = aq[hb], ak[hb], av[hb]
                kq, kk, kv = "aq%d" % hb, "ak%d" % hb, "av%d" % hb
                S.dma("sp", lambda e, q=q, h=h: e.dma_start(out=q[:], in_=self.qT_d[h * 64:(h + 1) * 64, :]), reads=["projout"], writes=[kq])
                S.dma("sp", lambda e, k=k, h=h: e.dma_start(out=k[:], in_=self.kT_d[h * 64:(h + 1) * 64, :]), reads=["projout"], writes=[kk])
                S.dma("sp", lambda e, v=v, h=h: e.dma_start(out=v[:], in_=self.v_d[:, h * 64:(h + 1) * 64].rearrange("(j p) d -> p j d", p=128)), reads=["projout"], writes=[kv])
                for qb in range(4):
                    nkb = 4 * (qb + 1)
                    py = apy[yi % 2]; kpy = "apy%d" % (yi % 2)
                    yo = ayo[yi % 2]; kyo = "ayo%d" % (yi % 2)
                    pr = apr[yi % 2]; kpr = "apr%d" % (yi % 2)
                    yi += 1
                    for jn, j in enumerate(reversed(range(nkb))):
                        b = it % 2
                        it += 1
                        diag = j >= 4 * qb
                        base = qb * 512 - j * 128
                        pz, pc = apz[b], apc[b]
                        zs, sp, lw, wt, sbb = azs[b], asp[b], alw[b], awt[b], asb[b]
                        kz, kc_, kzs, ksp, klw, kwt, ksb = "apz%d" % b, "apc%d" % b, "azs%d" % b, "asp%d" % b, "alw%d" % b, "awt%d" % b, "asb%d" % b
                        S.op("pe", lambda e, pz=pz, k=k, q=q, j=j, qb=qb: e.matmul(pz[:], lhsT=k[:, j * 128:(j + 1) * 128], rhs=q[:, qb * 512:(qb + 1) * 512], start=True, stop=True),
                             reads=[kq, kk], writes=[kz])
                        S.op("dve", lambda e, zs=zs, pz=pz: e.tensor_copy(out=zs[:], in_=pz[:]), reads=[kz], writes=[kzs])
                        S.op("act", lambda e, sp=sp, zs=zs: e.activation(out=sp[:], in_=zs[:], func=AF.Exp), reads=[kzs], writes=[ksp])
                        S.op("act", lambda e, sp=sp, sbb=sbb: e.activation(out=sbb[:], in_=sp[:], func=AF.Ln, bias=1.0), reads=[ksp], writes=[ksb])
                        if diag:
                            S.op("pool", lambda e, sbb=sbb, base=base: e.affine_select(out=sbb[:], in_=sbb[:], pattern=[[1, 512]], compare_op=ALU.is_gt, fill=0.0, base=base, channel_multiplier=-1),
                                 reads=[ksb], writes=[ksb])
                        S.op("pe", lambda e, pc=pc, sbb=sbb: e.matmul(pc[:], lhsT=tri_b, rhs=sbb[:], start=True, stop=True), reads=["cstb", ksb], writes=[kc_])
                        S.op("dve", lambda e, lw=lw, zs=zs, pc=pc: e.tensor_tensor(out=lw[:], in0=zs[:], in1=pc[:], op=ALU.subtract), reads=[kzs, kc_], writes=[klw])
                        if jn > 0:
                            S.op("dve", lambda e, lw=lw, pr=pr: e.tensor_tensor(out=lw[:], in0=lw[:], in1=pr[:], op=ALU.subtract), reads=[klw, kpr], writes=[klw])
                        S.op("act", lambda e, wt=wt, lw=lw: e.activation(out=wt[:], in_=lw[:], func=AF.Exp), reads=[klw], writes=[kwt])
                        if diag:
                            S.op("pool", lambda e, wt=wt, base=base: e.affine_select(out=wt[:], in_=wt[:], pattern=[[1, 512]], compare_op=ALU.is_gt, fill=0.0, base=base, channel_multiplier=-1),
                                 reads=[kwt], writes=[kwt])
                        S.op("pe", lambda e, py=py, v=v, wt=wt, j=j, jn=jn, nkb=nkb: e.matmul(py[:], lhsT=v[:, j, :], rhs=wt[:], start=(jn == 0), stop=(jn == nkb - 1)),
                             reads=[kv, kwt], writes=[kpy])
                        if jn < nkb - 1:
                            S.op("pe", lambda e, pr=pr, sbb=sbb, jn=jn, nkb=nkb: e.matmul(pr[:], lhsT=self.ones_b(), rhs=sbb[:], start=(jn == 0), stop=(jn == nkb - 2)),
                                 reads=["cstb", ksb], writes=[kpr])
                    S.op("act", lambda e, yo=yo, py=py: e.activation(out=yo[:], in_=py[:], func=AF.Copy), reads=[kpy], writes=[kyo])
                    S.dma("sp", lambda e, yo=yo, h=h, qb=qb: e.dma_start(out=self.yT_d[0, h * 64:(h + 1) * 64, qb * 512:(qb + 1) * 512], in_=yo[:]), reads=[kyo], writes=["yT_d"])

    def _zero_branch(self, g):
        nc, S = self.nc, self.S
        with ExitStack() as es:
            z = es.enter_context(self.sbt("zb", [128, S_LEN], BF16))
            S.op("dve", lambda e: e.memset(z[:], 0.0), writes=["zb"])
            for kc in range(4):
                S.dma("sp", lambda e, kc=kc: e.dma_start(out=self.yT_d[g, kc * 128:(kc + 1) * 128, :], in_=z[:]), reads=["zb"], writes=["yT_d"])

    def stage_hgrn(self, l):
        nc, S = self.nc, self.S
        with ExitStack() as es:
            sb = lambda n, sh, dt: es.enter_context(self.sbt(n, sh, dt))
            pt = lambda n, sh, dt: es.enter_context(self.pst(n, sh, dt))
            q = sb("hq", [128, S_LEN], F32)
            f = sb("hf", [128, S_LEN], F32)
            lf = sb("hlf", [128, S_LEN], F32)
            kin = sb("hkin", [128, S_LEN], F32)
            Bg = sb("hBg", [128, S_LEN], F32)
            Dd = sb("hD", [128, S_LEN], F32)
            Ee = sb("hE", [128, S_LEN], F32)
            Q1, K1 = sb("hQ1", [128, S_LEN], BF16), sb("hK1", [128, S_LEN], BF16)
            Q2, K2 = sb("hQ2", [128, S_LEN], BF16), sb("hK2", [128, S_LEN], BF16)
            hi = sb("hhi", [128, NT, 128], BF16)
            k2t = sb("hk2t", [128, NT, 128], BF16)
            hg = sb("hhg", [128, S_LEN], F32)
            oT = sb("hoT", [128, S_LEN], F32)
            Bst, Bmid, Bend, dec = sb("hBst", [128, 32], F32), sb("hBmid", [128, 32], F32), sb("hBend", [128, 32], F32), sb("hdec", [128, 32], F32)
            oml = sb("homl", [128, 1], F32)
            St, Stb = sb("hSt", [128, 128], F32), sb("hStb", [128, 128], BF16)
            pTs = [sb("hpT%d" % i, [128, 64], BF16) for i in range(2)]
            sq = sb("hsq", [128, 512], F32)
            rs = sb("hrs", [128, 512], F32)
            yb = sb("hyb", [128, 512], F32)
            ybb = sb("hybb", [128, 512], BF16)
            pss = [pt("hpss%d" % i, [128, 512], F32) for i in range(2)]
            pso = [pt("hpso%d" % i, [128, 512], F32) for i in range(2)]
            psst = [pt("hpsst%d" % i, [128, 512], F32) for i in range(2)]
            ptr = pt("hptr", [128, 1024], BF16)
            psn = pt("hpsn", [128, 512], F32)
            mask = self.cst[:, C_M64:C_M64 + 64]
            Bg3 = Bg[:].rearrange("p (c t) -> p c t", t=64)
            for h in range(4):
                lb = self.lbT[:, l * 4 + h: l * 4 + h + 1]
                rows = slice(h * 128, (h + 1) * 128)
                S.dma("sp", lambda e, rows=rows: e.dma_start(out=q[:], in_=self.hqT_d[rows, :]), reads=["projout"], writes=["hq"])
                S.dma("sp", lambda e, rows=rows: e.dma_start(out=f[:], in_=self.hfT_d[rows, :]), reads=["projout"], writes=["hf"])
                S.dma("sp", lambda e, rows=rows: e.dma_start(out=hg[:], in_=self.hgT_d[rows, :]), reads=["projout"], writes=["hhg"])
                S.dma("sp", lambda e, rows=rows: e.dma_start(out=hi[:], in_=self.hi_d[:, rows].rearrange("(j p) v -> p j v", p=128)), reads=["projout"], writes=["hhi"])
                S.op("dve", lambda e, lb=lb: e.tensor_scalar(out=oml[:], in0=lb, scalar1=-1.0, scalar2=1.0, op0=ALU.mult, op1=ALU.add), reads=["lbT"], writes=["homl"])
                S.op("act", lambda e: e.activation(out=f[:], in_=f[:], func=AF.Sigmoid), reads=["hf"], writes=["hf"])
                S.op("dve", lambda e, lb=lb: e.tensor_scalar(out=f[:], in0=f[:], scalar1=oml[:, 0:1], scalar2=lb, op0=ALU.mult, op1=ALU.add), reads=["hf", "homl", "lbT"], writes=["hf"])
                S.op("act", lambda e: e.activation(out=lf[:], in_=f[:], func=AF.Ln), reads=["hf"], writes=["hlf"])
                S.op("dve", lambda e: e.tensor_scalar(out=kin[:], in0=f[:], scalar1=-1.0, scalar2=1.0, op0=ALU.mult, op1=ALU.add), reads=["hf"], writes=["hkin"])
                S.op("dve", lambda e: e.tensor_tensor_scan(out=Bg[:], data0=lf[:], data1=lf[:], initial=0.0, op0=ALU.add, op1=ALU.bypass), reads=["hlf"], writes=["hBg"])
                S.op("dve", lambda e: e.memset(Bst[:, 0:1], 0.0), writes=["hBst"])
                S.op("dve", lambda e: e.tensor_copy(out=Bst[:, 1:32], in_=Bg3[:, 0:31, 63]), reads=["hBg"], writes=["hBst"])
                S.op("dve", lambda e: e.tensor_copy(out=Bmid[:], in_=Bg3[:, :, 31]), reads=["hBg"], writes=["hBmid"])
                S.op("dve", lambda e: e.tensor_copy(out=Bend[:], in_=Bg3[:, :, 63]), reads=["hBg"], writes=["hBend"])
                S.op("dve", lambda e: e.tensor_tensor(out=dec[:], in0=Bend[:], in1=Bst[:], op=ALU.subtract), reads=["hBend", "hBst"], writes=["hdec"])
                S.op("act", lambda e: e.activation(out=dec[:], in_=dec[:], func=AF.Exp), reads=["hdec"], writes=["hdec"])

                def sub_cols(col, key):
                    for c in range(32):
                        S.op("dve", lambda e, c=c: e.tensor_scalar(out=Dd[:, c * 64:(c + 1) * 64], in0=Bg[:, c * 64:(c + 1) * 64], scalar1=col[:, c:c + 1], scalar2=None, op0=ALU.subtract),
                             reads=["hBg", key], writes=["hD"])
                sub_cols(Bmid, "hBmid")
                S.op("act", lambda e: e.activation(out=Ee[:], in_=Dd[:], func=AF.Exp), reads=["hD"], writes=["hE"])
                S.op("dve", lambda e: e.tensor_tensor(out=Q1[:], in0=q[:], in1=Ee[:], op=ALU.mult), reads=["hq", "hE"], writes=["hQ1"])
                S.op("act", lambda e: e.activation(out=Ee[:], in_=Dd[:], func=AF.Exp, scale=-1.0), reads=["hD", "hQ1"], writes=["hE"])
                S.op("dve", lambda e: e.tensor_tensor(out=K1[:], in0=kin[:], in1=Ee[:], op=ALU.mult), reads=["hkin", "hE"], writes=["hK1"])
                sub_cols(Bst, "hBst")
                S.op("act", lambda e: e.activation(out=Ee[:], in_=Dd[:], func=AF.Exp), reads=["hD", "hK1"], writes=["hE"])
                S.op("dve", lambda e: e.tensor_tensor(out=Q2[:], in0=q[:], in1=Ee[:], op=ALU.mult), reads=["hq", "hE"], writes=["hQ2"])
                sub_cols(Bend, "hBend")
                S.op("act", lambda e: e.activation(out=Ee[:], in_=Dd[:], func=AF.Exp, scale=-1.0), reads=["hD", "hQ2"], writes=["hE"])
                S.op("dve", lambda e: e.tensor_tensor(out=K2[:], in0=kin[:], in1=Ee[:], op=ALU.mult), reads=["hkin", "hE"], writes=["hK2"])
                for j in range(NT):
                    S.op("pe", lambda e, j=j: e.transpose(ptr[:, 0:128], K2[:, j * 128:(j + 1) * 128], self.ident_b()), reads=["hK2", "cstb"], writes=["hptr"])
                    S.op("act", lambda e, j=j: e.activation(out=k2t[:, j, :], in_=ptr[:, 0:128], func=AF.Copy), reads=["hptr"], writes=["hk2t"])
                for c in range(32):
                    j, half = c // 2, c % 2
                    r0 = half * 64
                    cs = slice(c * 64, (c + 1) * 64)
                    ps_s = pss[c % 2]; kps = "hpss%d" % (c % 2)
                    pT = pTs[c % 2]; kpT = "hpT%d" % (c % 2)
                    po = pso[(c // 8) % 2]; kpo = "hpso%d" % ((c // 8) % 2)
                    ocs = slice((c % 8) * 64, (c % 8 + 1) * 64)
                    pst_ = psst[c % 2]; kpst = "hpsst%d" % (c % 2)
                    S.op("pe", lambda e, ps_s=ps_s, r0=r0, cs=cs: e.matmul(ps_s[r0:r0 + 64, 0:64], lhsT=K1[:, cs], rhs=Q1[:, cs], start=True, stop=True), reads=["hK1", "hQ1"], writes=[kps])
                    S.op("dve", lambda e, ps_s=ps_s, pT=pT, r0=r0: e.tensor_copy(out=pT[r0:r0 + 64, :], in_=ps_s[r0:r0 + 64, 0:64]), reads=[kps], writes=[kpT])
                    S.op("pool", lambda e, pT=pT, r0=r0: e.affine_select(out=pT[r0:r0 + 64, :], in_=pT[r0:r0 + 64, :], pattern=[[1, 64]], compare_op=ALU.is_ge, fill=0.0, base=0, channel_multiplier=-1),
                         reads=[kpT], writes=[kpT])
                    S.op("pe", lambda e, po=po, pT=pT, r0=r0, j=j, ocs=ocs, c=c: e.matmul(po[:, ocs], lhsT=hi[r0:r0 + 64, j, :], rhs=pT[r0:r0 + 64, :], start=True, stop=(c == 0)), reads=["hhi", kpT], writes=[kpo])
                    if c > 0:
                        S.op("pe", lambda e, po=po, ocs=ocs, cs=cs: e.matmul(po[:, ocs], lhsT=Stb[:], rhs=Q2[:, cs], start=False, stop=True), reads=["hStb", "hQ2"], writes=[kpo])
                    if c < 31:
                        S.op("pe", lambda e, pst_=pst_, r0=r0, j=j: e.matmul(pst_[:, 0:128], lhsT=k2t[r0:r0 + 64, j, :], rhs=hi[r0:r0 + 64, j, :], start=True, stop=True), reads=["hk2t", "hhi"], writes=[kpst])
                        if c == 0:
                            S.op("dve", lambda e, pst_=pst_: e.tensor_copy(out=St[:], in_=pst_[:, 0:128]), reads=[kpst], writes=["hSt"])
                        else:
                            S.op("dve", lambda e, pst_=pst_, c=c: e.scalar_tensor_tensor(out=St[:], in0=St[:], scalar=dec[:, c:c + 1], in1=pst_[:, 0:128], op0=ALU.mult, op1=ALU.add), reads=[kpst, "hSt", "hdec"], writes=["hSt"])
                        S.op("act", lambda e: e.activation(out=Stb[:], in_=St[:], func=AF.Copy), reads=["hSt"], writes=["hStb"])
                    if c % 8 == 7:
                        tb = c // 8
                        S.op("act", lambda e, po=po, tb=tb: e.activation(out=oT[:, tb * 512:(tb + 1) * 512], in_=po[:], func=AF.Copy), reads=[kpo], writes=["hoT"])
                gcol = self.smc("hgn", l, h)
                for tb in range(4):
                    bs = slice(tb * 512, (tb + 1) * 512)
                    S.op("act", lambda e, bs=bs: e.activation(out=sq[:], in_=oT[:, bs], func=AF.Square), reads=["hoT"], writes=["hsq"])
                    S.op("pe", lambda e: e.matmul(psn[:], lhsT=self.ones_f(), rhs=sq[:], start=True, stop=True), reads=["cst", "hsq"], writes=["hpsn"])
                    S.op("dve", lambda e: e.tensor_scalar(out=rs[:], in0=psn[:], scalar1=1.0 / 128, scalar2=EPS, op0=ALU.mult, op1=ALU.add), reads=["hpsn"], writes=["hrs"])
                    S.op("act", lambda e: e.activation(out=rs[:], in_=rs[:], func=AF.Sqrt), reads=["hrs"], writes=["hrs"])
                    S.op("dve", lambda e: e.reciprocal(out=rs[:], in_=rs[:]), reads=["hrs"], writes=["hrs"])
                    S.op("dve", lambda e, bs=bs: e.tensor_tensor(out=yb[:], in0=oT[:, bs], in1=rs[:], op=ALU.mult), reads=["hoT", "hrs"], writes=["hyb"])
                    S.op("dve", lambda e, bs=bs, gcol=gcol: e.scalar_tensor_tensor(out=ybb[:], in0=yb[:], scalar=gcol, in1=hg[:, bs], op0=ALU.mult, op1=ALU.mult), reads=["hyb", "sm", "hhg"], writes=["hybb"])
                    S.dma("sp", lambda e, bs=bs, rows=rows: e.dma_start(out=self.yT_d[1, rows, bs], in_=ybb[:]), reads=["hybb"], writes=["yT_d"])

    def stage_mlstm(self, l):
        nc, S = self.nc, self.S
        with ExitStack() as es:
            sb = lambda n, sh, dt: es.enter_context(self.sbt(n, sh, dt))
            pt = lambda n, sh, dt: es.enter_context(self.pst(n, sh, dt))
            gi, gf = sb("mgi", [4, S_LEN], F32), sb("mgf", [4, S_LEN], F32)
            Bc, aa, AA = sb("mB", [4, S_LEN], F32), sb("ma", [4, S_LEN], F32), sb("mA", [4, S_LEN], F32)
            em, Dr = sb("mem", [4, S_LEN], F32), sb("mDr", [4, S_LEN], F32)
            uu, ww, it = sb("mu", [4, S_LEN], F32), sb("mw", [4, S_LEN], F32), sb("mit", [4, S_LEN], F32)
            Aend, Aprev, decr = sb("mAend", [4, 32], F32), sb("mAprev", [4, 32], F32), sb("mdecr", [4, 32], F32)
            nbf = sb("mnbf", [4, 1], F32)
            og, _ = SM_OFF["gateb"]
            bi = self.sm[0:4, og + l * 2: og + l * 2 + 1]
            bf = self.sm[0:4, og + l * 2 + 1: og + l * 2 + 2]
            S.dma("sp", lambda e: e.dma_start(out=gi[:], in_=self.gT_d[0, :, :]), reads=["projout"], writes=["mgi"])
            S.dma("sp", lambda e: e.dma_start(out=gf[:], in_=self.gT_d[1, :, :]), reads=["projout"], writes=["mgf"])
            S.op("dve", lambda e: e.tensor_scalar(out=gi[:], in0=gi[:], scalar1=bi, scalar2=None, op0=ALU.add), reads=["mgi", "sm"], writes=["mgi"])
            S.op("dve", lambda e: e.tensor_scalar(out=nbf[:], in0=bf, scalar1=-1.0, scalar2=None, op0=ALU.mult), reads=["sm"], writes=["mnbf"])
            S.op("act", lambda e: e.activation(out=gf[:], in_=gf[:], func=AF.Exp, scale=-1.0, bias=nbf[:, 0:1]), reads=["mgf", "mnbf"], writes=["mgf"])
            S.op("act", lambda e: e.activation(out=gf[:], in_=gf[:], func=AF.Ln, bias=1.0), reads=["mgf"], writes=["mgf"])
            S.op("dve", lambda e: e.tensor_scalar(out=gf[:], in0=gf[:], scalar1=-1.0, scalar2=None, op0=ALU.mult), reads=["mgf"], writes=["mgf"])
            S.op("dve", lambda e: e.tensor_tensor_scan(out=Bc[:], data0=gf[:], data1=gf[:], initial=0.0, op0=ALU.add, op1=ALU.bypass), reads=["mgf"], writes=["mB"])
            S.op("dve", lambda e: e.tensor_tensor(out=aa[:], in0=gi[:], in1=Bc[:], op=ALU.subtract), reads=["mgi", "mB"], writes=["ma"])
            S.op("dve", lambda e: e.tensor_tensor_scan(out=AA[:], data0=aa[:], data1=aa[:], initial=0.0, op0=ALU.max, op1=ALU.bypass), reads=["ma"], writes=["mA"])
            S.op("dve", lambda e: e.tensor_tensor(out=em[:], in0=Bc[:], in1=AA[:], op=ALU.add), reads=["mB", "mA"], writes=["mem"])
            S.op("act", lambda e: e.activation(out=em[:], in_=em[:], func=AF.Exp, scale=-1.0), reads=["mem"], writes=["mem"])
            A3 = AA[:].rearrange("p (c t) -> p c t", t=64)
            a3 = aa[:].rearrange("p (c t) -> p c t", t=64)
            D3 = Dr[:].rearrange("p (c t) -> p c t", t=64)
            S.op("dve", lambda e: e.tensor_copy(out=Aend[:], in_=A3[:, :, 63]), reads=["mA"], writes=["mAend"])
            S.op("dve", lambda e: e.memset(Aprev[:, 0:1], 0.0), writes=["mAprev"])
            S.op("dve", lambda e: e.tensor_copy(out=Aprev[:, 1:32], in_=A3[:, 0:31, 63]), reads=["mA"], writes=["mAprev"])
            S.op("dve", lambda e: e.tensor_tensor(out=decr[:], in0=Aprev[:], in1=Aend[:], op=ALU.subtract), reads=["mAprev", "mAend"], writes=["mdecr"])
            S.op("act", lambda e: e.activation(out=decr[:], in_=decr[:], func=AF.Exp), reads=["mdecr"], writes=["mdecr"])
            bc = lambda col: col[:].unsqueeze(2).to_broadcast([4, 32, 64])
            S.op("dve", lambda e: e.tensor_tensor(out=D3, in0=A3, in1=bc(Aend), op=ALU.subtract), reads=["mA", "mAend"], writes=["mDr"])
            S.op("act", lambda e: e.activation(out=uu[:], in_=Dr[:], func=AF.Exp, scale=-1.0), reads=["mDr"], writes=["mu"])
            S.op("dve", lambda e: e.tensor_tensor(out=D3, in0=a3, in1=bc(Aend), op=ALU.subtract), reads=["ma", "mAend", "mu"], writes=["mDr"])
            S.op("act", lambda e: e.activation(out=ww[:], in_=Dr[:], func=AF.Exp), reads=["mDr"], writes=["mw"])
            S.op("dve", lambda e: e.tensor_tensor(out=D3, in0=A3, in1=bc(Aprev), op=ALU.subtract), reads=["mA", "mAprev", "mw"], writes=["mDr"])
            S.op("act", lambda e: e.activation(out=it[:], in_=Dr[:], func=AF.Exp, scale=-1.0), reads=["mDr"], writes=["mit"])
            xr, acc = sb("mxr", [128, S_LEN], F32), sb("macc2", [128, S_LEN], F32)
            Q1, Q2, K1 = sb("mQ1", [128, S_LEN], BF16), sb("mQ2", [128, S_LEN], BF16), sb("mK1", [128, S_LEN], BF16)
            vv = sb("mvv", [128, NT, 128], BF16)
            k2t = sb("mk2t", [128, NT, 128], BF16)
            mo = sb("mmo", [128, S_LEN], F32)
            hT = sb("mhT", [128, S_LEN], F32)
            dec = sb("mdec", [128, 32], F32)
            Cs, Csb = sb("mCs", [128, 128], F32), sb("mCsb", [128, 128], BF16)
            Ns, Nsb = sb("mNs", [128, 128], F32), sb("mNsb", [128, 128], BF16)
            pTs = [sb("mpT%d" % i, [128, 64], BF16) for i in range(2)]
            numT, embc, dmx = sb("mnumT", [128, 512], F32), sb("membc", [128, 512], F32), sb("mdmx", [128, 512], F32)
            sq, rs, yb, ybb = sb("msq", [128, 512], F32), sb("mrs", [128, 512], F32), sb("myb", [128, 512], F32), sb("mybb", [128, 512], BF16)
            pss = pt("mpss", [128, 512], F32)
            pso = [pt("mpso%d" % i, [128, 512], F32) for i in range(2)]
            psd = [pt("mpsd%d" % i, [128, 512], F32) for i in range(2)]
            pstC, pstN = pt("mpstC", [128, 512], F32), pt("mpstN", [128, 512], F32)
            pmisc = pt("mpmisc", [128, 512], F32)
            pmisc_b = pmisc[:].bitcast(BF16)
            KM = "mpmisc"
            ones_b = self.ones_b()

            def bcast_rows(rows, h, tb):
                S.op("pe", lambda e: e.matmul(pmisc[:], lhsT=self.cst[0:4, C_SEL + h * 128: C_SEL + (h + 1) * 128], rhs=rows[0:4, tb * 512:(tb + 1) * 512], start=True, stop=True),
                     reads=["cst", "mu", "mw", "mit", "mem"], writes=[KM])

            def conv_silu(chunk):
                cw = lambda tap: self.smc("convw", l, tap * 8 + chunk)
                S.op("dve", lambda e: e.tensor_scalar(out=acc[:], in0=xr[:], scalar1=cw(3), scalar2=self.smc("convb", l, chunk), op0=ALU.mult, op1=ALU.add), reads=["mxr", "sm"], writes=["macc2"])
                for sh in (1, 2, 3):
                    S.op("dve", lambda e, sh=sh: e.scalar_tensor_tensor(out=acc[:, sh:S_LEN], in0=xr[:, 0:S_LEN - sh], scalar=cw(3 - sh), in1=acc[:, sh:S_LEN], op0=ALU.mult, op1=ALU.add),
                         reads=["mxr", "sm", "macc2"], writes=["macc2"])
                S.op("act", lambda e: e.activation(out=acc[:], in_=acc[:], func=AF.Silu), reads=["macc2"], writes=["macc2"])

            for h in range(4):
                rows = slice(h * 128, (h + 1) * 128)
                S.dma("sp", lambda e, rows=rows: e.dma_start(out=xr[:], in_=self.mqkT_d[rows, :]), reads=["projout"], writes=["mxr"])
                S.dma("sp", lambda e, rows=rows: e.dma_start(out=mo[:], in_=self.moT_d[rows, :]), reads=["projout"], writes=["mmo"])
                S.dma("sp", lambda e, rows=rows: e.dma_start(out=vv[:], in_=self.mv_d[:, rows].rearrange("(j p) v -> p j v", p=128)), reads=["projout"], writes=["mvv"])
                conv_silu(h)
                for tb in range(4):
                    bs = slice(tb * 512, (tb + 1) * 512)
                    bcast_rows(uu, h, tb)
                    S.op("dve", lambda e, bs=bs: e.tensor_tensor(out=Q1[:, bs], in0=acc[:, bs], in1=pmisc[:], op=ALU.mult), reads=["macc2", KM], writes=["mQ1"])
                    bcast_rows(it, h, tb)
                    S.op("dve", lambda e, bs=bs: e.tensor_tensor(out=Q2[:, bs], in0=acc[:, bs], in1=pmisc[:], op=ALU.mult), reads=["macc2", KM], writes=["mQ2"])
                S.dma("sp", lambda e, h=h: e.dma_start(out=xr[:], in_=self.mqkT_d[512 + h * 128: 512 + (h + 1) * 128, :]), reads=["projout"], writes=["mxr"])
                conv_silu(4 + h)
                for tb in range(4):
                    bs = slice(tb * 512, (tb + 1) * 512)
                    bcast_rows(ww, h, tb)
                    S.op("dve", lambda e, bs=bs: e.scalar_tensor_tensor(out=K1[:, bs], in0=acc[:, bs], scalar=128.0 ** -0.5, in1=pmisc[:], op0=ALU.mult, op1=ALU.mult), reads=["macc2", KM], writes=["mK1"])
                S.op("pe", lambda e, h=h: e.matmul(pmisc[:, 0:32], lhsT=self.cst[0:4, C_SEL + h * 128: C_SEL + (h + 1) * 128], rhs=decr[0:4, :], start=True, stop=True), reads=["cst", "mdecr"], writes=[KM])
                S.op("act", lambda e: e.activation(out=dec[:], in_=pmisc[:, 0:32], func=AF.Copy), reads=[KM], writes=["mdec"])
                for j in range(NT):
                    S.op("pe", lambda e, j=j: e.transpose(pmisc_b[:, 0:128], K1[:, j * 128:(j + 1) * 128], self.ident_b()), reads=["mK1", "cstb"], writes=[KM])
                    S.op("act", lambda e, j=j: e.activation(out=k2t[:, j, :], in_=pmisc_b[:, 0:128], func=AF.Copy), reads=[KM], writes=["mk2t"])
                for c in range(32):
                    j, half = c // 2, c % 2
                    r0 = half * 64
                    cs = slice(c * 64, (c + 1) * 64)
                    pT = pTs[c % 2]; kpT = "mpT%d" % (c % 2)
                    po = pso[(c // 8) % 2]; kpo = "mpso%d" % ((c // 8) % 2)
                    pd = psd[(c // 8) % 2]; kpd = "mpsd%d" % ((c // 8) % 2)
                    ocs = slice((c % 8) * 64, (c % 8 + 1) * 64)
                    S.op("pe", lambda e, r0=r0, cs=cs: e.matmul(pss[r0:r0 + 64, 0:64], lhsT=K1[:, cs], rhs=Q1[:, cs], start=True, stop=True), reads=["mK1", "mQ1"], writes=["mpss"])
                    S.op("dve", lambda e, pT=pT, r0=r0: e.tensor_copy(out=pT[r0:r0 + 64, :], in_=pss[r0:r0 + 64, 0:64]), reads=["mpss"], writes=[kpT])
                    S.op("pool", lambda e, pT=pT, r0=r0: e.affine_select(out=pT[r0:r0 + 64, :], in_=pT[r0:r0 + 64, :], pattern=[[1, 64]], compare_op=ALU.is_ge, fill=0.0, base=0, channel_multiplier=-1),
                         reads=[kpT], writes=[kpT])
                    S.op("pe", lambda e, po=po, pT=pT, r0=r0, j=j, ocs=ocs, c=c: e.matmul(po[:, ocs], lhsT=vv[r0:r0 + 64, j, :], rhs=pT[r0:r0 + 64, :], start=True, stop=(c == 0)), reads=["mvv", kpT], writes=[kpo])
                    if c > 0:
                        S.op("pe", lambda e, po=po, ocs=ocs, cs=cs: e.matmul(po[:, ocs], lhsT=Csb[:], rhs=Q2[:, cs], start=False, stop=True), reads=["mCsb", "mQ2"], writes=[kpo])
                    S.op("pe", lambda e, pd=pd, pT=pT, r0=r0, ocs=ocs, c=c: e.matmul(pd[:, ocs], lhsT=ones_b[r0:r0 + 64, :], rhs=pT[r0:r0 + 64, :], start=True, stop=(c == 0)), reads=["cstb", kpT], writes=[kpd])
                    if c > 0:
                        S.op("pe", lambda e, pd=pd, ocs=ocs, cs=cs: e.matmul(pd[:, ocs], lhsT=Nsb[:], rhs=Q2[:, cs], start=False, stop=True), reads=["mNsb", "mQ2"], writes=[kpd])
                    if c < 31:
                        S.op("pe", lambda e, r0=r0, j=j: e.matmul(pstC[:, 0:128], lhsT=k2t[r0:r0 + 64, j, :], rhs=vv[r0:r0 + 64, j, :], start=True, stop=True), reads=["mk2t", "mvv"], writes=["mpstC"])
                        S.op("pe", lambda e, r0=r0, j=j: e.matmul(pstN[:, 0:128], lhsT=k2t[r0:r0 + 64, j, :], rhs=ones_b[r0:r0 + 64, :], start=True, stop=True), reads=["mk2t", "cstb"], writes=["mpstN"])
                        if c == 0:
                            S.op("dve", lambda e: e.tensor_copy(out=Cs[:], in_=pstC[:, 0:128]), reads=["mpstC"], writes=["mCs"])
                            S.op("dve", lambda e: e.tensor_copy(out=Ns[:], in_=pstN[:, 0:128]), reads=["mpstN"], writes=["mNs"])
                        else:
                            S.op("dve", lambda e, c=c: e.scalar_tensor_tensor(out=Cs[:], in0=Cs[:], scalar=dec[:, c:c + 1], in1=pstC[:, 0:128], op0=ALU.mult, op1=ALU.add), reads=["mpstC", "mCs", "mdec"], writes=["mCs"])
                            S.op("dve", lambda e, c=c: e.scalar_tensor_tensor(out=Ns[:], in0=Ns[:], scalar=dec[:, c:c + 1], in1=pstN[:, 0:128], op0=ALU.mult, op1=ALU.add), reads=["mpstN", "mNs", "mdec"], writes=["mNs"])
                        S.op("act", lambda e: e.activation(out=Csb[:], in_=Cs[:], func=AF.Copy), reads=["mCs"], writes=["mCsb"])
                        S.op("act", lambda e: e.activation(out=Nsb[:], in_=Ns[:], func=AF.Copy), reads=["mNs"], writes=["mNsb"])
                    if c % 8 == 7:
                        tb = c // 8
                        bs = slice(tb * 512, (tb + 1) * 512)
                        S.op("act", lambda e, po=po: e.activation(out=numT[:], in_=po[:], func=AF.Copy), reads=[kpo], writes=["mnumT"])
                        bcast_rows(em, h, tb)
                        S.op("act", lambda e: e.activation(out=embc[:], in_=pmisc[:], func=AF.Copy), reads=[KM], writes=["membc"])
                        S.op("act", lambda e, pd=pd: e.activation(out=dmx[:], in_=pd[:], func=AF.Abs), reads=[kpd], writes=["mdmx"])
                        S.op("dve", lambda e: e.tensor_tensor(out=dmx[:], in0=dmx[:], in1=embc[:], op=ALU.max), reads=["mdmx", "membc"], writes=["mdmx"])
                        S.op("dve", lambda e: e.reciprocal(out=dmx[:], in_=dmx[:]), reads=["mdmx"], writes=["mdmx"])
                        S.op("dve", lambda e, bs=bs: e.tensor_tensor(out=hT[:, bs], in0=numT[:], in1=dmx[:], op=ALU.mult), reads=["mnumT", "mdmx"], writes=["mhT"])
                gcol = self.smc("mln", l, h)
                for tb in range(4):
                    bs = slice(tb * 512, (tb + 1) * 512)
                    S.op("act", lambda e, bs=bs: e.activation(out=sq[:], in_=hT[:, bs], func=AF.Square), reads=["mhT"], writes=["msq"])
                    S.op("pe", lambda e: e.matmul(pmisc[:], lhsT=self.ones_f(), rhs=sq[:], start=True, stop=True), reads=["cst", "msq"], writes=[KM])
                    S.op("dve", lambda e: e.tensor_scalar(out=rs[:], in0=pmisc[:], scalar1=1.0 / 128, scalar2=EPS, op0=ALU.mult, op1=ALU.add), reads=[KM], writes=["mrs"])
                    S.op("act", lambda e: e.activation(out=rs[:], in_=rs[:], func=AF.Sqrt), reads=["mrs"], writes=["mrs"])
                    S.op("dve", lambda e: e.reciprocal(out=rs[:], in_=rs[:]), reads=["mrs"], writes=["mrs"])
                    S.op("dve", lambda e, bs=bs: e.tensor_tensor(out=yb[:], in0=hT[:, bs], in1=rs[:], op=ALU.mult), reads=["mhT", "mrs"], writes=["myb"])
                    S.op("dve", lambda e, bs=bs, gcol=gcol: e.scalar_tensor_tensor(out=ybb[:], in0=yb[:], scalar=gcol, in1=mo[:, bs], op0=ALU.mult, op1=ALU.mult), reads=["myb", "sm", "mmo"], writes=["mybb"])
                    S.dma("sp", lambda e, bs=bs, rows=rows: e.dma_start(out=self.yT_d[2, rows, bs], in_=ybb[:]), reads=["mybb"], writes=["yT_d"])

    def _bcast_tile(self, es, name, colfn, ps_ap=None, ps_key=None):
        nc, S = self.nc, self.S
        dg = es.enter_context(self.sbt(name + "dg", [128, 128], F32))
        dst = es.enter_context(self.sbt(name, [128, D], F32))
        if ps_ap is None:
            ps = es.enter_context(self.pst(name + "ps", [128, D], F32))[:]
            ps_key = name + "ps"
        else:
            ps = ps_ap
        for c in range(8):
            S.op("dve", lambda e, c=c: e.tensor_scalar(out=dg[:], in0=self.ident_f(), scalar1=colfn(c), scalar2=None, op0=ALU.mult),
                 reads=["cst", "modT", "sm"], writes=[name + "dg"])
            S.op("pe", lambda e, c=c: e.matmul(ps[:, c * 128:(c + 1) * 128], lhsT=self.ones_f(), rhs=dg[:], start=True, stop=True), reads=["cst", name + "dg"], writes=[ps_key])
        for hh in range(2):
            S.op("act", lambda e, hh=hh: e.activation(out=dst[:, hh * 512:(hh + 1) * 512], in_=ps[:, hh * 512:(hh + 1) * 512], func=AF.Copy), reads=[ps_key], writes=[name])
        return dst

    def stage_merge(self, l):
        nc, S = self.nc, self.S
        with ExitStack() as es:
            sb = lambda n, sh, dt: es.enter_context(self.sbt(n, sh, dt))
            pt = lambda n, sh, dt: es.enter_context(self.pst(n, sh, dt))
            g1bc = self._bcast_tile(es, "g1bc", lambda c: self.modc(l, 2, c))
            wb = sb("mwb", [128, 3, 4, D], BF16)
            wo = sb("mwo", [128, 8, D], BF16)
            yt = sb("myt", [128, 3, 4, 512], BF16)
            gts = [sb("mgt%d" % i, [128, 512], F32) for i in range(3)]
            tmps = [sb("mtmp%d" % i, [128, 512], F32) for i in range(2)]
            macc = sb("macc", [128, 512], F32)
            mT = sb("mT", [128, 8, 512], BF16)
            xts = [sb("mxt%d" % i, [128, D], F32) for i in range(2)]
            ytmp = sb("mytmp", [128, 512], F32)
            psa = [pt("mpsa%d" % i, [128, 512], F32) for i in range(2)]
            psy = [pt("mpsy%d" % i, [128, 512], F32) for i in range(2)]
            for g in range(3):
                S.dma("pool", lambda e, g=g: e.dma_start(out=wb[:, g, :, :], in_=self.w_branch[l, g, :, :].rearrange("(kc p) d -> p kc d", p=128)), writes=["mwb"])
            S.dma("pool", lambda e: e.dma_start(out=wo[:], in_=self.w_out[l, :, :].rearrange("(c p) d -> p c d", p=128)), writes=["mwo"])
            ai = 0
            gi = 0
            yi = 0
            for tb in range(4):
                for g in range(3):
                    S.dma("sp", lambda e, g=g, tb=tb: e.dma_start(out=yt[:, g, :, :], in_=self.yT_d[g, :, tb * 512:(tb + 1) * 512].rearrange("(kc p) t -> p kc t", p=128)),
                          reads=["yT_d"], writes=["myt"])
                for dc in range(8):
                    for g in range(3):
                        ps = psa[ai % 2]; kps = "mpsa%d" % (ai % 2); ai += 1
                        gt = gts[gi % 3]; kgt = "mgt%d" % (gi % 3); gi += 1
                        S.dma("sp", lambda e, gt=gt, g=g, dc=dc, tb=tb: e.dma_start(out=gt[:], in_=self.bgT_d[g * 1024 + dc * 128: g * 1024 + (dc + 1) * 128, tb * 512:(tb + 1) * 512]),
                              reads=["projout"], writes=[kgt])
                        for kc in range(4):
                            S.op("pe", lambda e, ps=ps, g=g, kc=kc, dc=dc: e.matmul(ps[:], lhsT=wb[:, g, kc, dc * 128:(dc + 1) * 128], rhs=yt[:, g, kc, :], start=(kc == 0), stop=(kc == 3)),
                                 reads=["mwb", "myt"], writes=[kps])
                        if g == 0:
                            S.op("dve", lambda e, ps=ps, gt=gt: e.tensor_tensor(out=macc[:], in0=ps[:], in1=gt[:], op=ALU.mult), reads=[kps, kgt], writes=["macc"])
                        else:
                            tmp = tmps[g % 2]; ktmp = "mtmp%d" % (g % 2)
                            S.op("dve", lambda e, ps=ps, gt=gt, tmp=tmp: e.tensor_tensor(out=tmp[:], in0=ps[:], in1=gt[:], op=ALU.mult), reads=[kps, kgt], writes=[ktmp])
                            if g == 1:
                                S.op("pool", lambda e, tmp=tmp: e.tensor_tensor(out=macc[:], in0=macc[:], in1=tmp[:], op=ALU.add), reads=[ktmp, "macc"], writes=["macc"])
                            else:
                                S.op("pool", lambda e, tmp=tmp, dc=dc: e.tensor_tensor(out=mT[:, dc, :], in0=macc[:], in1=tmp[:], op=ALU.add), reads=[ktmp, "macc"], writes=["mT"])
                for tt in range(4):
                    t = tb * 4 + tt
                    xt = xts[t % 2]; kxt = "mxt%d" % (t % 2)
                    S.dma("sp", lambda e, xt=xt, t=t: e.dma_start(out=xt[:], in_=self.xres[t * 128:(t + 1) * 128, :]), reads=["xres"], writes=[kxt])
                    for dh in range(2):
                        ps = psy[yi % 2]; kps = "mpsy%d" % (yi % 2); yi += 1
                        for c in range(8):
                            S.op("pe", lambda e, ps=ps, c=c, tt=tt, dh=dh: e.matmul(ps[:], lhsT=mT[:, c, tt * 128:(tt + 1) * 128], rhs=wo[:, c, dh * 512:(dh + 1) * 512], start=(c == 0), stop=(c == 7)),
                                 reads=["mT", "mwo"], writes=[kps])
                        S.op("dve", lambda e, ps=ps, dh=dh: e.tensor_tensor(out=ytmp[:], in0=ps[:], in1=g1bc[:, dh * 512:(dh + 1) * 512], op=ALU.mult), reads=[kps, "g1bc"], writes=["mytmp"])
                        S.op("dve", lambda e, xt=xt, dh=dh: e.tensor_tensor(out=xt[:, dh * 512:(dh + 1) * 512], in0=xt[:, dh * 512:(dh + 1) * 512], in1=ytmp[:], op=ALU.add), reads=["mytmp", kxt], writes=[kxt])
                    S.dma("sp", lambda e, xt=xt, t=t: e.dma_start(out=self.xres[t * 128:(t + 1) * 128, :], in_=xt[:]), reads=[kxt], writes=["xres"])

    def stage_uvcast(self, l):
        nc, S = self.nc, self.S
        with ExitStack() as es:
            cbs = [es.enter_context(self.sbt("uvc%d" % i, [128, 4, 2 * D], BF16)) for i in range(3)]
            for k in range(32):
                cb = cbs[k % 3]; kcb = "uvc%d" % (k % 3)
                src = self.pk_uv[l, k * 512:(k + 1) * 512, :].rearrange("(p r) d -> p r d", p=128)
                dst = self.uvb[k * 512:(k + 1) * 512, :].rearrange("(p r) d -> p r d", p=128)
                S.dma("pool", lambda e, cb=cb, src=src: e.dma_start(out=cb[:], in_=src), writes=[kcb])
                S.dma("sp", lambda e, cb=cb, dst=dst: e.dma_start(out=dst, in_=cb[:]), reads=[kcb], writes=["uvb"])

    def stage_peer(self, l):
        nc, S = self.nc, self.S
        NB = 8
        with ExitStack() as es:
            sb = lambda n, sh, dt: es.enter_context(self.sbt(n, sh, dt))
            pt = lambda n, sh, dt: es.enter_context(self.pst(n, sh, dt))
            hTs = [sb("phT%d" % i, [128, 8, 128], BF16) for i in range(2)]
            wq = sb("pwq", [128, 8, D], BF16)
            kbd = sb("pkbd", [128, 8, 256], F32)
            qTt = sb("pqTt", [128, 8, 128], F32)
            sc, s2, cand = sb("psc", [128, 2048], F32), sb("ps2", [128, 2048], F32), sb("pcand", [128, 2048], F32)
            mx, mi, sif = sb("pmx", [128, 256], F32), sb("pmi", [128, 256], U32), sb("psif", [128, 256], F32)
            tv, tp, tpf = sb("ptv", [128, 128], F32), sb("ptp", [128, 128], U32), sb("ptpf", [128, 128], F32)
            af, bf_ = sb("paf", [128, 128], F32), sb("pbf", [128, 128], F32)
            i1, i2 = sb("pi1", [128, 128], F32), sb("pi2", [128, 128], F32)
            idxi = sb("pidxi", [128, 128], I32)
            ee, gg, aa, ga = sb("pee", [128, 128], F32), sb("pgg", [128, 128], F32), sb("paa", [128, 128], F32), sb("pga", [128, 128], F32)
            ssum = sb("pssum", [128, 8], F32)
            h2ts = [sb("ph2t%d" % i, [128, D], F32) for i in range(2)]
            xts = [sb("pxt%d" % i, [128, D], F32) for i in range(2)]
            ubs = [sb("pub%d" % i, [128, 2 * D], BF16) for i in range(NB)]
            junks = [sb("pjunk%d" % i, [128, D], F32) for i in range(2)]
            acc = sb("pacc", [128, D], F32)
            tmpbs = [sb("ptmpb%d" % i, [128, D], BF16) for i in range(3)]
            gl = sb("pgl", [128, 128], F32)
            psq = pt("ppsq", [128, 8, 128], F32)
            pssc = pt("ppssc", [128, 2048], F32)
            pacc = pt("ppacc", [128, D], F32)
            g2bc = self._bcast_tile(es, "g2bc", lambda c: self.modc(l, 5, c), ps_ap=pssc[:, 0:D], ps_key="ppssc")
            UV2 = self.uvb
            iota16 = self.cst[:, C_IOTA:C_IOTA + 16]
            thr15 = self.cst[:, C_THR:C_THR + 15]
            S.dma("pool", lambda e: e.dma_start(out=wq[:], in_=self.pk_wq[l, :, :].rearrange("(kc p) d -> p kc d", p=128)), writes=["pwq"])
            S.dma("sp", lambda e: e.dma_start(out=kbd[:], in_=self.keysbd[l, :, :, :].rearrange("h p n -> p h n")), writes=["pkbd"])
            sc3 = sc[:].rearrange("p (g n) -> p g n", n=128)
            s23 = s2[:].rearrange("p (g n) -> p g n", n=128)
            mx3 = mx[:].rearrange("p (g k) -> p g k", k=16)
            mi3 = mi[:].rearrange("p (g k) -> p g k", k=16)
            mx4 = mx[:].rearrange("p (h q k) -> p h q k", h=8, q=2)
            sif4 = sif[:].rearrange("p (h q k) -> p h q k", h=8, q=2)
            cand4 = cand[:].rearrange("p (h a b) -> p h a b", h=8, a=16)
            tv3 = tv[:].rearrange("p (h k) -> p h k", h=8)
            tp3 = tp[:].rearrange("p (h k) -> p h k", h=8)
            oh4 = s2[:].rearrange("p (h k a) -> p h k a", h=8, k=16)
            cmp3 = sc[:, 0:1920].rearrange("p (r j) -> p r j", j=15)
            B4 = [128, 8, 16, 16]
            ui = 0
            for t in range(NT):
                ts = slice(t * 128, (t + 1) * 128)
                h2t = h2ts[t % 2]; kh2 = "ph2t%d" % (t % 2)
                xt = xts[t % 2]; kxt = "pxt%d" % (t % 2)
                S.dma("sp", lambda e, h2t=h2t, ts=ts: e.dma_start(out=h2t[:], in_=self.h2_d[ts, :]), reads=["h2_d"], writes=[kh2])
                S.dma("sp", lambda e, xt=xt, ts=ts: e.dma_start(out=xt[:], in_=self.xres[ts, :]), reads=["xres"], writes=[kxt])
                hT = hTs[t % 2]; khT = "phT%d" % (t % 2)
                S.dma("sp", lambda e, hT=hT, ts=ts: e.dma_start(out=hT[:], in_=self.hT_d[:, :, ts].rearrange("c p t -> p c t")), reads=["hT_d"], writes=[khT])
                for h in range(8):
                    for kc in range(8):
                        S.op("pe", lambda e, h=h, kc=kc, hT=hT: e.matmul(psq[:, h, :], lhsT=wq[:, kc, h * 128:(h + 1) * 128], rhs=hT[:, kc, :], start=(kc == 0), stop=(kc == 7)),
                             reads=["pwq", khT], writes=["ppsq"])
                for hh in range(2):
                    S.op("act", lambda e, hh=hh: e.activation(out=qTt[:, hh * 4:(hh + 1) * 4, :], in_=psq[:, hh * 4:(hh + 1) * 4, :], func=AF.Copy), reads=["ppsq"], writes=["pqTt"])
                for h in range(8):
                    S.op("pe", lambda e, h=h: e.matmul(pssc[:, h * 256:(h + 1) * 256], lhsT=qTt[:, h, :], rhs=kbd[:, h, :], start=True, stop=True), reads=["pqTt", "pkbd"], writes=["ppssc"])
                for qd in range(4):
                    S.op("act", lambda e, qd=qd: e.activation(out=sc[:, qd * 512:(qd + 1) * 512], in_=pssc[:, qd * 512:(qd + 1) * 512], func=AF.Copy), reads=["ppssc"], writes=["psc"])
                for g in range(16):
                    S.op("dve", lambda e, g=g: e.max(out=mx3[:, g, 0:8], in_=sc3[:, g, :]), reads=["psc"], writes=["pmx"])
                    S.op("dve", lambda e, g=g: e.max_index(out=mi3[:, g, 0:8], in_max=mx3[:, g, 0:8], in_values=sc3[:, g, :]), reads=["psc", "pmx"], writes=["pmi"])
                    S.op("dve", lambda e, g=g: e.match_replace(out=s23[:, g, :], in_to_replace=mx3[:, g, 0:8], in_values=sc3[:, g, :], imm_value=-1e30), reads=["psc", "pmx"], writes=["ps2"])
                    S.op("dve", lambda e, g=g: e.max(out=mx3[:, g, 8:16], in_=s23[:, g, :]), reads=["ps2"], writes=["pmx"])
                    S.op("dve", lambda e, g=g: e.max_index(out=mi3[:, g, 8:16], in_max=mx3[:, g, 8:16], in_values=s23[:, g, :]), reads=["ps2", "pmx"], writes=["pmi"])
                S.op("dve", lambda e: e.tensor_copy(out=sif[:], in_=mi[:]), reads=["pmi"], writes=["psif"])
                S.op("dve", lambda e: e.tensor_tensor(out=cand4, in0=mx4[:, :, 0, :].unsqueeze(3).to_broadcast(B4), in1=mx4[:, :, 1, :].unsqueeze(2).to_broadcast(B4), op=ALU.add),
                     reads=["pmx"], writes=["pcand"])
                for h in range(8):
                    hs = slice(h * 256, (h + 1) * 256)
                    S.op("dve", lambda e, h=h, hs=hs: e.max(out=tv3[:, h, 0:8], in_=cand[:, hs]), reads=["pcand"], writes=["ptv"])
                    S.op("dve", lambda e, h=h, hs=hs: e.max_index(out=tp3[:, h, 0:8], in_max=tv3[:, h, 0:8], in_values=cand[:, hs]), reads=["pcand", "ptv"], writes=["ptp"])
                    S.op("dve", lambda e, h=h, hs=hs: e.match_replace(out=s2[:, hs], in_to_replace=tv3[:, h, 0:8], in_values=cand[:, hs], imm_value=-1e30), reads=["pcand", "ptv"], writes=["ps2"])
                    S.op("dve", lambda e, h=h, hs=hs: e.max(out=tv3[:, h, 8:16], in_=s2[:, hs]), reads=["ps2"], writes=["ptv"])
                    S.op("dve", lambda e, h=h, hs=hs: e.max_index(out=tp3[:, h, 8:16], in_max=tv3[:, h, 8:16], in_values=s2[:, hs]), reads=["ps2", "ptv"], writes=["ptp"])
                S.op("dve", lambda e: e.tensor_copy(out=tpf[:], in_=tp[:]), reads=["ptp"], writes=["ptpf"])
                S.op("dve", lambda e: e.tensor_tensor(out=cmp3, in0=tpf[:].unsqueeze(2).to_broadcast([128, 128, 15]), in1=thr15.unsqueeze(1).to_broadcast([128, 128, 15]), op=ALU.is_ge),
                     reads=["ptpf", "cst"], writes=["psc"])
                S.op("dve", lambda e: e.tensor_reduce(out=af[:], in_=cmp3, axis=AX.X, op=ALU.add), reads=["psc"], writes=["paf"])
                S.op("dve", lambda e: e.scalar_tensor_tensor(out=bf_[:], in0=af[:], scalar=-16.0, in1=tpf[:], op0=ALU.mult, op1=ALU.add), reads=["paf", "ptpf"], writes=["pbf"])
                for (src, q_, dst, kd) in ((af, 0, i1, "pi1"), (bf_, 1, i2, "pi2")):
                    ksrc = "paf" if q_ == 0 else "pbf"
                    S.op("dve", lambda e, src=src: e.tensor_tensor(out=oh4, in0=src[:].rearrange("p (h k) -> p h k", h=8).unsqueeze(3).to_broadcast(B4),
                                                                   in1=iota16.unsqueeze(1).unsqueeze(1).to_broadcast(B4), op=ALU.is_equal), reads=[ksrc, "cst"], writes=["ps2"])
                    S.op("dve", lambda e, q_=q_: e.tensor_tensor(out=oh4, in0=oh4, in1=sif4[:, :, q_, :].unsqueeze(2).to_broadcast(B4), op=ALU.mult), reads=["ps2", "psif"], writes=["ps2"])
                    S.op("dve", lambda e, dst=dst: e.tensor_reduce(out=dst[:], in_=oh4, axis=AX.X, op=ALU.add), reads=["ps2"], writes=[kd])
                S.op("dve", lambda e: e.tensor_scalar(out=i2[:], in0=i2[:], scalar1=0.0, scalar2=None, op0=ALU.add), reads=["pi2"], writes=["pi2"])
                S.op("dve", lambda e: e.scalar_tensor_tensor(out=i1[:], in0=i1[:], scalar=128.0, in1=i2[:], op0=ALU.mult, op1=ALU.add), reads=["pi1", "pi2"], writes=["pi1"])
                S.op("dve", lambda e: e.tensor_copy(out=idxi[:], in_=i1[:]), reads=["pi1"], writes=["pidxi"])
                S.op("dve", lambda e: e.tensor_tensor(out=ee[:].rearrange("p (h k) -> p h k", h=8), in0=tv3, in1=tv3[:, :, 0:1].to_broadcast([128, 8, 16]), op=ALU.subtract), reads=["ptv"], writes=["pee"])
                S.op("act", lambda e: e.activation(out=ee[:], in_=ee[:], func=AF.Exp), reads=["pee"], writes=["pee"])
                S.op("dve", lambda e: e.tensor_reduce(out=ssum[:], in_=ee[:].rearrange("p (h k) -> p h k", h=8), axis=AX.X, op=ALU.add), reads=["pee"], writes=["pssum"])
                S.op("dve", lambda e: e.reciprocal(out=ssum[:], in_=ssum[:]), reads=["pssum"], writes=["pssum"])
                S.op("dve", lambda e: e.tensor_tensor(out=gg[:].rearrange("p (h k) -> p h k", h=8), in0=ee[:].rearrange("p (h k) -> p h k", h=8), in1=ssum[:].unsqueeze(2).to_broadcast([128, 8, 16]), op=ALU.mult),
                     reads=["pee", "pssum"], writes=["pgg"])
                for r in range(128):
                    ub = ubs[ui % NB]; kub = "pub%d" % (ui % NB)
                    jk = junks[ui % 2]; kjk = "pjunk%d" % (ui % 2)
                    tb_ = tmpbs[ui % 3]; ktb = "ptmpb%d" % (ui % 3)
                    ui += 1
                    ka, kg, kga = "paa%d" % r, "pgl%d" % r, "pga%d" % r
                    S.dma("pool", lambda e, ub=ub, r=r: e.indirect_dma_start(out=ub[:], out_offset=None, in_=UV2[:, :], in_offset=bass.IndirectOffsetOnAxis(ap=idxi[:, r:r + 1], axis=0)),
                          reads=["pidxi", "uvb"], writes=[kub])
                    S.op("dve", lambda e, ub=ub, r=r, h2t=h2t, jk=jk: e.scalar_tensor_tensor(out=jk[:], in0=ub[:, 0:D], scalar=1.0, in1=h2t[:], op0=ALU.mult, op1=ALU.mult, accum_out=aa[:, r:r + 1]),
                         reads=[kub, kh2], writes=[kjk, ka])
                    S.op("act", lambda e, r=r: e.activation(out=gl[:, r:r + 1], in_=aa[:, r:r + 1], func=AF.Gelu_apprx_tanh), reads=[ka], writes=[kg])
                    S.op("act", lambda e, r=r: e.activation(out=ga[:, r:r + 1], in_=gl[:, r:r + 1], func=AF.Identity, scale=gg[:, r:r + 1]), reads=[kg, "pgg"], writes=[kga])
                    S.op("act", lambda e, ub=ub, r=r, tb_=tb_: e.activation(out=tb_[:], in_=ub[:, D:2 * D], func=AF.Identity, scale=ga[:, r:r + 1]), reads=[kub, kga], writes=[ktb])
                    for dh in range(2):
                        S.op("pe", lambda e, tb_=tb_, dh=dh, r=r: e.matmul(pacc[:, dh * 512:(dh + 1) * 512], lhsT=self.ident_b(), rhs=tb_[:, dh * 512:(dh + 1) * 512], start=(r == 0), stop=(r == 127)),
                             reads=["cstb", ktb], writes=["ppacc"])
                for dh in range(2):
                    S.op("dve", lambda e, dh=dh: e.tensor_tensor(out=acc[:, dh * 512:(dh + 1) * 512], in0=pacc[:, dh * 512:(dh + 1) * 512], in1=g2bc[:, dh * 512:(dh + 1) * 512], op=ALU.mult),
                         reads=["ppacc", "g2bc"], writes=["pacc"])
                S.op("dve", lambda e, xt=xt: e.tensor_tensor(out=xt[:], in0=xt[:], in1=acc[:], op=ALU.add), reads=["pacc", kxt], writes=[kxt])
                S.dma("sp", lambda e, xt=xt, ts=ts: e.dma_start(out=self.xres[ts, :], in_=xt[:]), reads=[kxt], writes=["xres"])

    def stage_final(self):
        nc, S = self.nc, self.S
        with ExitStack() as es:
            sb = lambda n, sh, dt: es.enter_context(self.sbt(n, sh, dt))
            o, _ = SM_OFF["fg"]
            fgbc = self._bcast_tile(es, "fgbc", lambda c: self.sm[:, o + c:o + c + 1])
            xts = [sb("fxt%d" % i, [128, D], F32) for i in range(2)]
            sq = sb("fsq", [128, D], F32)
            ss = sb("fss", [128, 4], F32)
            for t in range(NT):
                xt = xts[t % 2]; kxt = "fxt%d" % (t % 2)
                S.dma("sp", lambda e, xt=xt, t=t: e.dma_start(out=xt[:], in_=self.xres[t * 128:(t + 1) * 128, :]), reads=["xres"], writes=[kxt])
                S.op("act", lambda e, xt=xt: e.activation(out=sq[:], in_=xt[:], func=AF.Square, accum_out=ss[:, 0:1]), reads=[kxt], writes=["fsq", "fss"])
                S.op("dve", lambda e: e.tensor_scalar(out=ss[:, 1:2], in0=ss[:, 0:1], scalar1=1.0 / D, scalar2=EPS, op0=ALU.mult, op1=ALU.add), reads=["fss"], writes=["fss"])
                S.op("act", lambda e: e.activation(out=ss[:, 2:3], in_=ss[:, 1:2], func=AF.Sqrt), reads=["fss"], writes=["fss"])
                S.op("dve", lambda e: e.reciprocal(out=ss[:, 3:4], in_=ss[:, 2:3]), reads=["fss"], writes=["fss"])
                S.op("dve", lambda e, xt=xt: e.scalar_tensor_tensor(out=xt[:], in0=xt[:], scalar=ss[:, 3:4], in1=fgbc[:], op0=ALU.mult, op1=ALU.mult), reads=[kxt, "fss", "fgbc"], writes=[kxt])
                S.dma("sp", lambda e, xt=xt, t=t: e.dma_start(out=self.out_d[t * 128:(t + 1) * 128, :], in_=xt[:]), reads=[kxt], writes=["out_d"])


def prep_inputs(inputs):
    inp = {k: np.ascontiguousarray(np.asarray(v)) for k, v in inputs.items()}
    consts = make_consts()
    keys = inp["pk_keys"]
    kbd = np.zeros((DEPTH, 8, 128, 256), np.float32)
    for p in range(2):
        kbd[:, :, p * 64:(p + 1) * 64, p * 128:(p + 1) * 128] = keys[:, :, p].transpose(0, 1, 3, 2)
    shared = dict(consts=consts, mod_w=inp["mod_w"], w_in=inp["w_in"], w_branch=inp["w_branch"], w_out=inp["w_out"],
                  pk_wq=inp["pk_wq"], keysbd=kbd,
                  pk_uv=np.concatenate([inp["pk_u"], inp["pk_v"]], axis=2))
    in_maps = []
    for b in range(8):
        m = dict(shared)
        m["x"] = inp["x"][b]
        m["small"] = make_small(inp, b)
        in_maps.append(m)
    return in_maps


_PROG_CACHE = {}


def kernel(**inputs):
    in_maps = prep_inputs(inputs)
    if "nc" not in _PROG_CACHE:
        _PROG_CACHE["nc"] = Prog().build()
    res = run_bass_kernel_spmd(_PROG_CACHE["nc"], in_maps, core_ids=list(range(8)))
    return np.stack([np.asarray(r["out"]) for r in res.results], axis=0).astype(np.float32)
```

```python
from contextlib import ExitStack
import numpy as np
import concourse.bass as bass
import concourse.mybir as mybir
from concourse.bass_utils import run_bass_kernel_spmd

F32 = mybir.dt.float32
BF16 = mybir.dt.bfloat16
U32 = mybir.dt.uint32
I32 = mybir.dt.int32
AF = mybir.ActivationFunctionType
ALU = mybir.AluOpType
AX = mybir.AxisListType

D = 1024
S_LEN = 2048
DEPTH = 4
NT = S_LEN // 128
INW = 8712
EPS = 1e-6
PEER_IMPLEMENTED = True


class Sched:
    ENG = ("pe", "act", "dve", "pool", "sp")

    def __init__(self, nc, dma_slots=None, same_engine_sync=True):
        self.nc = nc
        self.ops = {e: [] for e in self.ENG}
        self.count = {e: 0 for e in self.ENG}
        self.sem = {}
        self.last_w = {}
        self.readers = {}
        self.same_engine_sync = same_engine_sync
        self.dma_slots_n = dma_slots or {"sp": 8, "pool": 12, "act": 4}
        self.dma_sems = {}
        self.dma_rr = {}
        self.seen = {e: {} for e in self.ENG}
        self._ctx = []
        self.n_ops = 0

    def open(self):
        nc = self.nc
        for e in self.ENG:
            cm = nc.semaphore("s_" + e)
            self.sem[e] = cm.__enter__()
            self._ctx.append(cm)
        for q, n in self.dma_slots_n.items():
            self.dma_sems[q] = []
            self.dma_rr[q] = 0
            for i in range(n):
                cm = nc.semaphore("sd_%s%d" % (q, i))
                s = cm.__enter__()
                self._ctx.append(cm)
                self.dma_sems[q].append(dict(sem=s, count=0, name="d_%s%d" % (q, i)))

    def close(self):
        for cm in reversed(self._ctx):
            cm.__exit__(None, None, None)

    def _tok_wait(self, tok):
        if tok[0] == "c":
            return (self.sem[tok[1]], tok[1], tok[2])
        d = self.dma_sems[tok[1]][tok[2]]
        return (d["sem"], d["name"], tok[3])

    def _collect(self, eng, reads, writes):
        toks = []
        for k in reads:
            t = self.last_w.get(k)
            if t is not None:
                toks.append(t)
        for k in writes:
            t = self.last_w.get(k)
            if t is not None:
                toks.append(t)
            toks.extend(self.readers.get(k, ()))
        waits = {}
        for t in toks:
            if t[0] == "c" and t[1] == eng and not self.same_engine_sync:
                continue
            s, name, v = self._tok_wait(t)
            if self.seen[eng].get(name, 0) >= v:
                continue
            if name not in waits or waits[name][1] < v:
                waits[name] = (s, v)
        for name, (s, v) in waits.items():
            self.seen[eng][name] = v
        return list(waits.values())

    def _commit(self, tok, reads, writes):
        for k in reads:
            self.readers.setdefault(k, []).append(tok)
        for k in writes:
            self.last_w[k] = tok
            self.readers[k] = []

    def op(self, eng, fn, reads=(), writes=()):
        waits = self._collect(eng, reads, writes)
        self.count[eng] += 1
        tok = ("c", eng, self.count[eng])
        self.ops[eng].append((fn, waits, "c", None))
        self._commit(tok, reads, writes)
        self.n_ops += 1
        return tok

    def dma(self, eng, fn, reads=(), writes=()):
        slot = self.dma_rr[eng]
        self.dma_rr[eng] = (slot + 1) % len(self.dma_sems[eng])
        d = self.dma_sems[eng][slot]
        waits = self._collect(eng, reads, writes)
        if d["count"] > 0 and self.seen[eng].get(d["name"], 0) < d["count"]:
            waits.append((d["sem"], d["count"]))
            self.seen[eng][d["name"]] = d["count"]
        d["count"] += 16
        tok = ("d", eng, slot, d["count"])
        self.ops[eng].append((fn, waits, "d", d["sem"]))
        self._commit(tok, reads, writes)
        self.n_ops += 1
        return tok

    def barrier(self):
        for e in self.ENG:
            waits = []
            for e2 in self.ENG:
                v = self.count[e2]
                if v > 0 and self.seen[e].get(e2, 0) < v and e2 != e:
                    waits.append((self.sem[e2], v))
                    self.seen[e][e2] = v
            for q in self.dma_sems:
                for d in self.dma_sems[q]:
                    if d["count"] > 0 and self.seen[e].get(d["name"], 0) < d["count"]:
                        waits.append((d["sem"], d["count"]))
                        self.seen[e][d["name"]] = d["count"]
            if self.count[e] > 0:
                waits.append((self.sem[e], self.count[e]))
                self.seen[e][e] = self.count[e]
            self.ops[e].append((None, waits, "w", None))

    def emit(self):
        nc = self.nc
        sched = self
        ops = self.ops
        self.ops = {e: [] for e in self.ENG}
        with nc.Block() as block:
            def run(engname, e):
                for fn, waits, kind, dsem in ops[engname]:
                    for s, v in waits:
                        e.wait_ge(s, v)
                    if fn is None:
                        continue
                    ins = fn(e)
                    if kind == "c":
                        ins.then_inc(sched.sem[engname], 1)
                    else:
                        ins.then_inc(dsem, 16)

            @block.sync
            def _(e):
                run("sp", e)

            @block.tensor
            def _(e):
                run("pe", e)

            @block.scalar
            def _(e):
                run("act", e)

            @block.vector
            def _(e):
                run("dve", e)

            @block.gpsimd
            def _(e):
                run("pool", e)


def _small_layout():
    off = {}
    o = 0
    for name, n in (("mod_b", DEPTH * 48), ("nmg", DEPTH * 8), ("nfg", DEPTH * 8),
                    ("convw", DEPTH * 32), ("convb", DEPTH * 8), ("gateb", DEPTH * 2),
                    ("lbl", DEPTH * 4), ("hgn", DEPTH * 4), ("mln", DEPTH * 4), ("fg", 8), ("cT", 8)):
        off[name] = (o, n)
        o += n
    return off, o


SM_OFF, NS = _small_layout()
C_ID, C_ONES, C_TRI, C_M64, C_SEL, C_IOTA, C_THR = 0, 128, 256, 384, 448, 960, 976
NCONST = 992


def make_consts():
    c = np.zeros((128, NCONST), np.float32)
    c[:, C_ID:C_ID + 128] = np.eye(128, dtype=np.float32)
    c[:, C_ONES:C_ONES + 128] = 1.0
    sp = np.arange(128)[:, None]
    s = np.arange(128)[None, :]
    c[:, C_TRI:C_TRI + 128] = (sp >= s).astype(np.float32)
    t = np.arange(64)[None, :]
    c[:, C_M64:C_M64 + 64] = (t >= (sp % 64)).astype(np.float32)
    for h in range(4):
        c[h, C_SEL + h * 128:C_SEL + (h + 1) * 128] = 1.0
    c[:, C_IOTA:C_IOTA + 16] = np.arange(16, dtype=np.float32)[None, :]
    c[:, C_THR:C_THR + 15] = 16.0 * np.arange(1, 16, dtype=np.float32)[None, :]
    return c


def make_small(inp, b):
    sm = np.zeros((128, NS), np.float32)

    def put(name, arr):
        o, n = SM_OFF[name]
        arr = np.asarray(arr, np.float32).reshape(128, n)
        sm[:, o:o + n] = arr

    put("mod_b", inp["mod_b"].reshape(DEPTH, 48, 128).transpose(2, 0, 1))
    put("nmg", inp["norm_mix_g"].reshape(DEPTH, 8, 128).transpose(2, 0, 1))
    put("nfg", inp["norm_ffn_g"].reshape(DEPTH, 8, 128).transpose(2, 0, 1))
    put("convw", inp["ml_conv_w"].reshape(DEPTH, 4, 8, 128).transpose(3, 0, 1, 2))
    put("convb", inp["ml_conv_b"].reshape(DEPTH, 8, 128).transpose(2, 0, 1))
    gb = np.zeros((128, DEPTH, 2), np.float32)
    gb[0:4, :, 0] = inp["ml_gate_b"][:, 0:4].T
    gb[0:4, :, 1] = inp["ml_gate_b"][:, 4:8].T
    put("gateb", gb)
    put("lbl", inp["hg_lb_logits"].reshape(DEPTH, 4, 128).transpose(2, 0, 1))
    put("hgn", inp["hg_norm_g"].reshape(DEPTH, 4, 128).transpose(2, 0, 1))
    put("mln", inp["ml_norm_g"].reshape(DEPTH, 4, 128).transpose(2, 0, 1))
    put("fg", inp["final_g"].reshape(8, 128).T)
    put("cT", inp["c"][b].reshape(8, 128).T)
    return sm


class Prog:
    def __init__(self, n_layers=DEPTH, stages=None, debug=()):
        self.n_layers = n_layers
        self.stages = stages
        self.debug = set(debug)
        self.nc = bass.Bass("TRN2", target_bir_lowering=False)
        self.S = Sched(self.nc)
        self.uid = 0

    def sbt(self, name, shape, dt):
        self.uid += 1
        return self.nc.sbuf_tensor("%s_u%d" % (name, self.uid), shape, dt)

    def pst(self, name, shape, dt):
        self.uid += 1
        return self.nc.psum_tensor("%s_u%d" % (name, self.uid), shape, dt)

    def want(self, st):
        return self.stages is None or st in self.stages

    def dram(self, name, shape, dt, kind=None):
        if kind is None:
            kind = "ExternalOutput" if name in self.debug else "Internal"
        return self.nc.dram_tensor(name, list(shape), dt, kind=kind).ap()

    def build(self):
        nc, S = self.nc, self.S
        L = self.n_layers
        ext = lambda n, s, dt=F32: nc.dram_tensor(n, list(s), dt, kind="ExternalInput").ap()
        self.x_in = ext("x", [S_LEN, D])
        self.small_d = ext("small", [128, NS])
        self.consts_d = ext("consts", [128, NCONST])
        self.mod_w = ext("mod_w", [L, D, 6 * D])
        self.w_in = ext("w_in", [L, D, INW])
        self.input_names = ["x", "small", "consts", "mod_w", "w_in"]
        if self.want("merge"):
            self.w_branch = ext("w_branch", [L, 3, 512, D])
            self.w_out = ext("w_out", [L, D, D])
            self.input_names += ["w_branch", "w_out"]
        if self.want("peer") and PEER_IMPLEMENTED:
            self.pk_wq = ext("pk_wq", [L, D, D])
            self.keysbd = ext("keysbd", [L, 8, 128, 256])
            self.pk_uv = ext("pk_uv", [L, 16384, 2 * D])
            self.input_names += ["pk_wq", "keysbd", "pk_uv"]
        self.out_d = nc.dram_tensor("out", [S_LEN, D], F32, kind="ExternalOutput").ap()
        self.xres = self.dram("xres", [S_LEN, D], F32)
        self.hT_d = self.dram("hT", [8, 128, S_LEN], BF16)
        self.qT_d = self.dram("qT", [512, S_LEN], BF16)
        self.kT_d = self.dram("kT", [512, S_LEN], BF16)
        self.v_d = self.dram("v_tok", [S_LEN, 512], BF16)
        self.hqT_d = self.dram("hqT", [512, S_LEN], F32)
        self.hfT_d = self.dram("hfT", [512, S_LEN], F32)
        self.hgT_d = self.dram("hgT", [512, S_LEN], F32)
        self.hi_d = self.dram("hi_tok", [S_LEN, 512], BF16)
        self.mqkT_d = self.dram("mqkT", [1024, S_LEN], F32)
        self.mv_d = self.dram("mv_tok", [S_LEN, 512], BF16)
        self.moT_d = self.dram("moT", [512, S_LEN], F32)
        self.gT_d = self.dram("gT", [2, 4, S_LEN], F32)
        self.bgT_d = self.dram("bgT", [3072, S_LEN], F32)
        self.yT_d = self.dram("yT", [3, 512, S_LEN], BF16)
        self.h2_d = self.dram("h2_tok", [S_LEN, D], F32)
        self.uvb = self.dram("uvb", [16384, 2 * D], BF16)
        S.open()
        with (self.sbt("consts", [128, NCONST], F32) as cst,
              self.sbt("constb", [128, 384], BF16) as cstb,
              self.sbt("small", [128, NS], F32) as sm,
              self.sbt("modT", [128, DEPTH * 48], F32) as modT,
              self.sbt("lbT", [128, DEPTH * 4], F32) as lbT):
            self.cst, self.cstb, self.sm, self.modT, self.lbT = cst, cstb, sm, modT, lbT
            S.dma("sp", lambda e: e.dma_start(out=cst[:], in_=self.consts_d[:, :]), writes=["cst"])
            S.dma("sp", lambda e: e.dma_start(out=sm[:], in_=self.small_d[:, :]), writes=["sm"])
            S.op("dve", lambda e: e.tensor_copy(out=cstb[:], in_=cst[:, 0:384]), reads=["cst"], writes=["cstb"])
            S.dma("sp", lambda e: e.dma_start(out=self.xres[:, :], in_=self.x_in[:, :]), writes=["xres"])
            self.stage_mod()
            S.barrier(); S.emit()
            for l in range(L):
                if self.want("norm1"):
                    self.stage_norm(l, 0)
                    S.barrier(); S.emit()
                if self.want("proj"):
                    self.stage_proj(l)
                    S.barrier(); S.emit()
                if self.want("attn"):
                    self.stage_attn(l)
                    S.barrier(); S.emit()
                if self.want("hgrn"):
                    self.stage_hgrn(l)
                    S.barrier(); S.emit()
                if self.want("mlstm"):
                    self.stage_mlstm(l)
                    S.barrier(); S.emit()
                if self.want("merge"):
                    self.stage_merge(l)
                    S.barrier(); S.emit()
                if self.want("peer") and PEER_IMPLEMENTED:
                    self.stage_uvcast(l)
                    S.barrier(); S.emit()
                    self.stage_norm(l, 1)
                    S.barrier(); S.emit()
                    self.stage_peer(l)
                    S.barrier(); S.emit()
            self.stage_final()
            S.barrier(); S.emit()
        S.close()
        return nc

    def ident_f(self):
        return self.cst[:, C_ID:C_ID + 128]

    def ones_f(self):
        return self.cst[:, C_ONES:C_ONES + 128]

    def tri_f(self):
        return self.cst[:, C_TRI:C_TRI + 128]

    def ident_b(self):
        return self.cstb[:, 0:128]

    def ones_b(self):
        return self.cstb[:, 128:256]

    def smc(self, name, l, j, n=1):
        o, tot = SM_OFF[name]
        per = tot // DEPTH
        return self.sm[:, o + l * per + j: o + l * per + j + n]

    def modc(self, l, part, c):
        j = l * 48 + part * 8 + c
        return self.modT[:, j:j + 1]

    def stage_mod(self):
        nc, S = self.nc, self.S
        L = self.n_layers
        sm = self.sm
        with (self.sbt("condT", [128, 8], F32) as condT,
              self.sbt("mw0", [128, 8, 768], F32) as mw0,
              self.sbt("mw1", [128, 8, 768], F32) as mw1,
              self.sbt("lbe", [128, DEPTH * 4], F32) as lbe,
              self.sbt("lbs", [128, 4], F32) as lbs,
              self.sbt("lbm", [128, 4], F32) as lbm,
              self.pst("ps_mod", [128, DEPTH * 48], F32) as psm):
            o, _ = SM_OFF["cT"]
            S.op("act", lambda e: e.activation(out=condT[:], in_=sm[:, o:o + 8], func=AF.Silu), reads=["sm"], writes=["condT"])
            mws = [mw0, mw1]
            gi = 0
            for l in range(L):
                for g in range(8):
                    mw = mws[gi % 2]
                    key = "mw%d" % (gi % 2)
                    gi += 1
                    src = self.mod_w[l, :, g * 768:(g + 1) * 768].rearrange("(kc p) c -> p kc c", p=128)
                    S.dma("sp", lambda e, mw=mw, src=src: e.dma_start(out=mw[:], in_=src), writes=[key])
                    for cc in range(6):
                        j = l * 48 + g * 6 + cc
                        for kc in range(8):
                            S.op("pe", lambda e, mw=mw, cc=cc, kc=kc, j=j: e.matmul(
                                psm[:, j:j + 1], lhsT=mw[:, kc, cc * 128:(cc + 1) * 128], rhs=condT[:, kc:kc + 1],
                                start=(kc == 0), stop=(kc == 7)), reads=[key, "condT"], writes=["psm"])
            ob, _ = SM_OFF["mod_b"]
            S.op("dve", lambda e: e.tensor_tensor(out=self.modT[:, 0:L * 48], in0=psm[:, 0:L * 48], in1=sm[:, ob:ob + L * 48], op=ALU.add),
                 reads=["psm", "sm"], writes=["modT"])
            ol, _ = SM_OFF["lbl"]
            lg = lambda l: sm[:, ol + l * 4: ol + l * 4 + 4]
            S.op("dve", lambda e: e.tensor_tensor(out=lbm[:], in0=lg(0), in1=lg(1), op=ALU.max), reads=["sm"], writes=["lbm"])
            for l in (2, 3):
                S.op("dve", lambda e, l=l: e.tensor_tensor(out=lbm[:], in0=lbm[:], in1=lg(l), op=ALU.max), reads=["sm", "lbm"], writes=["lbm"])
            for l in range(DEPTH):
                S.op("dve", lambda e, l=l: e.tensor_tensor(out=lbe[:, l * 4:l * 4 + 4], in0=lg(l), in1=lbm[:], op=ALU.subtract), reads=["sm", "lbm"], writes=["lbe"])
            S.op("act", lambda e: e.activation(out=lbe[:], in_=lbe[:], func=AF.Exp), reads=["lbe"], writes=["lbe"])
            S.op("dve", lambda e: e.tensor_tensor(out=lbs[:], in0=lbe[:, 0:4], in1=lbe[:, 4:8], op=ALU.add), reads=["lbe"], writes=["lbs"])
            for l in (2, 3):
                S.op("dve", lambda e, l=l: e.tensor_tensor(out=lbs[:], in0=lbs[:], in1=lbe[:, l * 4:l * 4 + 4], op=ALU.add), reads=["lbe", "lbs"], writes=["lbs"])
            S.op("dve", lambda e: e.reciprocal(out=lbs[:], in_=lbs[:]), reads=["lbs"], writes=["lbs"])
            lbT = self.lbT
            S.op("dve", lambda e: e.memset(lbT[:, 0:4], 0.0), writes=["lbT"])
            for l in range(1, DEPTH):
                S.op("dve", lambda e, l=l: e.tensor_tensor(out=lbe[:, l * 4:l * 4 + 4], in0=lbe[:, l * 4:l * 4 + 4], in1=lbs[:], op=ALU.mult), reads=["lbe", "lbs"], writes=["lbe"])
                S.op("dve", lambda e, l=l: e.tensor_tensor(out=lbT[:, l * 4:l * 4 + 4], in0=lbT[:, (l - 1) * 4:l * 4], in1=lbe[:, l * 4:l * 4 + 4], op=ALU.add), reads=["lbe", "lbT"], writes=["lbT"])

    def stage_norm(self, l, which):
        nc, S = self.nc, self.S
        gname = "nmg" if which == 0 else "nfg"
        p_shift, p_scale = (0, 1) if which == 0 else (3, 4)
        with (self.sbt("nx0", [128, D], F32) as nx0, self.sbt("nx1", [128, D], F32) as nx1,
              self.sbt("nsq", [128, D], F32) as nsq,
              self.sbt("nb0", [128, D], BF16) as nb0, self.sbt("nb1", [128, D], BF16) as nb1,
              self.sbt("nh0", [128, 8, 128], BF16) as nh0, self.sbt("nh1", [128, 8, 128], BF16) as nh1,
              self.sbt("nt0", [128, D], F32) as nt0, self.sbt("nt1", [128, D], F32) as nt1,
              self.sbt("nss", [128, 4], F32) as nss,
              self.sbt("nG", [128, 8], F32) as nG,
              self.pst("nps0", [128, 8, 128], BF16) as nps0, self.pst("nps1", [128, 8, 128], BF16) as nps1,
              self.pst("npt0", [128, 8, 128], F32) as npt0):
            j0 = l * 48 + p_scale * 8
            S.op("dve", lambda e: e.scalar_tensor_tensor(out=nG[:], in0=self.modT[:, j0:j0 + 8], scalar=1.0, in1=self.smc(gname, l, 0, 8),
                                                         op0=ALU.add, op1=ALU.mult), reads=["modT", "sm"], writes=["nG"])
            nx, nb, nh, nps, nt = [nx0, nx1], [nb0, nb1], [nh0, nh1], [nps0, nps1], [nt0, nt1]
            for t in range(NT):
                i = t % 2
                kx, kb, kh, kp, kt = "nx%d" % i, "nb%d" % i, "nh%d" % i, "nps%d" % i, "nt%d" % i
                S.dma("sp", lambda e, i=i, t=t: e.dma_start(out=nx[i][:], in_=self.xres[t * 128:(t + 1) * 128, :]), reads=["xres"], writes=[kx])
                S.op("act", lambda e, i=i: e.activation(out=nsq[:], in_=nx[i][:], func=AF.Square, accum_out=nss[:, 0:1]), reads=[kx], writes=["nsq", "nss"])
                S.op("dve", lambda e: e.tensor_scalar(out=nss[:, 1:2], in0=nss[:, 0:1], scalar1=1.0 / D, scalar2=EPS, op0=ALU.mult, op1=ALU.add), reads=["nss"], writes=["nss"])
                S.op("act", lambda e: e.activation(out=nss[:, 2:3], in_=nss[:, 1:2], func=AF.Sqrt), reads=["nss"], writes=["nss"])
                S.op("dve", lambda e: e.reciprocal(out=nss[:, 3:4], in_=nss[:, 2:3]), reads=["nss"], writes=["nss"])
                S.op("dve", lambda e, i=i: e.tensor_scalar(out=nb[i][:], in0=nx[i][:], scalar1=nss[:, 3:4], scalar2=None, op0=ALU.mult), reads=[kx, "nss"], writes=[kb])
                for c in range(8):
                    S.op("pe", lambda e, i=i, c=c: e.transpose(nps[i][:, c, :], nb[i][:, c * 128:(c + 1) * 128], self.ident_b()), reads=[kb, "cstb"], writes=[kp])
                for c in range(8):
                    S.op("act", lambda e, i=i, c=c: e.activation(out=nh[i][:, c, :], in_=nps[i][:, c, :], func=AF.Identity,
                                                                 scale=nG[:, c:c + 1], bias=self.modc(l, p_shift, c)), reads=[kp, "nG", "modT"], writes=[kh])
                S.dma("sp", lambda e, i=i, t=t: e.dma_start(out=self.hT_d[:, :, t * 128:(t + 1) * 128].rearrange("c p t -> p c t"), in_=nh[i][:]), reads=[kh], writes=["hT_d"])
                if which == 1:
                    pass
            if which == 1:
                self._h2_tokmajor(l, nx, nss, nG, nt, npt0, p_shift)

    def _h2_tokmajor(self, l, nx, nss, nG, nt, npt0, p_shift):
        nc, S = self.nc, self.S
        with (self.sbt("dg", [128, 128], F32) as dg,
              self.sbt("Gbc", [128, D], F32) as Gbc, self.sbt("Sbc", [128, D], F32) as Sbc):
            for which_v, dst in ((0, Gbc), (1, Sbc)):
                for c in range(8):
                    col = nG[:, c:c + 1] if which_v == 0 else self.modc(l, p_shift, c)
                    S.op("dve", lambda e, col=col: e.tensor_scalar(out=dg[:], in0=self.ident_f(), scalar1=col, scalar2=None, op0=ALU.mult), reads=["cst", "nG", "modT"], writes=["dg"])
                    S.op("pe", lambda e, c=c: e.matmul(npt0[:, c, :], lhsT=self.ones_f(), rhs=dg[:], start=True, stop=True), reads=["cst", "dg"], writes=["npt0"])
                S.op("act", lambda e, dst=dst: e.activation(out=dst[:], in_=npt0[:].rearrange("p c t -> p (c t)"), func=AF.Copy), reads=["npt0"], writes=["bc%d" % which_v])
            for t in range(NT):
                i = t % 2
                kx, kt = "nx%d" % i, "nt%d" % i
                S.dma("sp", lambda e, i=i, t=t: e.dma_start(out=nx[i][:], in_=self.xres[t * 128:(t + 1) * 128, :]), reads=["xres"], writes=[kx])
                S.op("act", lambda e, i=i: e.activation(out=nt[i][:], in_=nx[i][:], func=AF.Square, accum_out=nss[:, 0:1]), reads=[kx], writes=[kt, "nss"])
                S.op("dve", lambda e: e.tensor_scalar(out=nss[:, 1:2], in0=nss[:, 0:1], scalar1=1.0 / D, scalar2=EPS, op0=ALU.mult, op1=ALU.add), reads=["nss"], writes=["nss"])
                S.op("act", lambda e: e.activation(out=nss[:, 2:3], in_=nss[:, 1:2], func=AF.Sqrt), reads=["nss"], writes=["nss"])
                S.op("dve", lambda e: e.reciprocal(out=nss[:, 3:4], in_=nss[:, 2:3]), reads=["nss"], writes=["nss"])
                S.op("dve", lambda e, i=i: e.scalar_tensor_tensor(out=nt[i][:], in0=nx[i][:], scalar=nss[:, 3:4], in1=Gbc[:], op0=ALU.mult, op1=ALU.mult), reads=[kx, "nss", "bc0"], writes=[kt])
                S.op("dve", lambda e, i=i: e.tensor_tensor(out=nt[i][:], in0=nt[i][:], in1=Sbc[:], op=ALU.add), reads=[kt, "bc1"], writes=[kt])
                S.dma("sp", lambda e, i=i, t=t: e.dma_start(out=self.h2_d[t * 128:(t + 1) * 128, :], in_=nt[i][:]), reads=[kt], writes=["h2_d"])

    def stage_proj(self, l):
        nc, S = self.nc, self.S
        fm = []
        for c in range(4):
            fm.append((0 + c * 128, 128, self.qT_d[c * 128:(c + 1) * 128, :], AF.Copy, 0.125, BF16))
        for c in range(4):
            fm.append((512 + c * 128, 128, self.kT_d[c * 128:(c + 1) * 128, :], AF.Copy, 1.0, BF16))
        for c in range(4):
            fm.append((1536 + c * 128, 128, self.hqT_d[c * 128:(c + 1) * 128, :], AF.Copy, 1.0, F32))
        for c in range(4):
            fm.append((2048 + c * 128, 128, self.hfT_d[c * 128:(c + 1) * 128, :], AF.Copy, 1.0, F32))
        for c in range(4):
            fm.append((3072 + c * 128, 128, self.hgT_d[c * 128:(c + 1) * 128, :], AF.Silu, 1.0, F32))
        for c in range(8):
            fm.append((3584 + c * 128, 128, self.mqkT_d[c * 128:(c + 1) * 128, :], AF.Copy, 1.0, F32))
        for c in range(4):
            fm.append((5120 + c * 128, 128, self.moT_d[c * 128:(c + 1) * 128, :], AF.Sigmoid, 1.0, F32))
        fm.append((5632, 4, self.gT_d[0, :, :], AF.Copy, 1.0, F32))
        fm.append((5636, 4, self.gT_d[1, :, :], AF.Copy, 1.0, F32))
        for c in range(24):
            fm.append((5640 + c * 128, 128, self.bgT_d[c * 128:(c + 1) * 128, :], AF.Sigmoid, 1.0, F32))
        tm = [(1024, self.v_d, AF.Copy), (2560, self.hi_d, AF.Silu), (4608, self.mv_d, AF.Copy)]
        with (self.sbt("hT", [128, 8, S_LEN], BF16) as hT,
              self.sbt("pw0", [128, 8, 512], BF16) as pw0, self.sbt("pw1", [128, 8, 512], BF16) as pw1,
              self.sbt("pof0", [128, S_LEN], F32) as pof0, self.sbt("pof1", [128, S_LEN], F32) as pof1,
              self.sbt("pob0", [128, S_LEN], BF16) as pob0, self.sbt("pob1", [128, S_LEN], BF16) as pob1,
              self.sbt("pot0", [128, 512], BF16) as pot0, self.sbt("pot1", [128, 512], BF16) as pot1,
              self.pst("pp0", [128, 512], F32) as pp0, self.pst("pp1", [128, 512], F32) as pp1,
              self.pst("pp2", [128, 512], F32) as pp2, self.pst("pp3", [128, 512], F32) as pp3):
            for c in range(8):
                S.dma("sp", lambda e, c=c: e.dma_start(out=hT[:, c, :], in_=self.hT_d[c, :, :]), reads=["hT_d"], writes=["hT"])
            pw, pof, pob, pot, pp = [pw0, pw1], [pof0, pof1], [pob0, pob1], [pot0, pot1], [pp0, pp1, pp2, pp3]
            wi = 0
            pi = 0
            for ji, (col0, ncol, dest, func, scale, dt) in enumerate(fm):
                w = pw[wi % 2]; kw = "pw%d" % (wi % 2); wi += 1
                src = self.w_in[l, :, col0:col0 + ncol].rearrange("(kc p) c -> p kc c", p=128)
                S.dma("pool", lambda e, w=w, src=src, ncol=ncol: e.dma_start(out=w[:, :, 0:ncol], in_=src), writes=[kw])
                ob = (pof if dt == F32 else pob)[ji % 2]
                ko = ("pof%d" if dt == F32 else "pob%d") % (ji % 2)
                for tb in range(4):
                    ps = pp[pi % 4]; kp = "pp%d" % (pi % 4); pi += 1
                    for kc in range(8):
                        S.op("pe", lambda e, ps=ps, w=w, kc=kc, tb=tb, ncol=ncol: e.matmul(
                            ps[0:ncol, :], lhsT=w[:, kc, 0:ncol], rhs=hT[:, kc, tb * 512:(tb + 1) * 512],
                            start=(kc == 0), stop=(kc == 7)), reads=[kw, "hT"], writes=[kp])
                    S.op("act", lambda e, ps=ps, ob=ob, tb=tb, ncol=ncol, func=func, scale=scale: e.activation(
                        out=ob[0:ncol, tb * 512:(tb + 1) * 512], in_=ps[0:ncol, :], func=func, scale=scale), reads=[kp], writes=[ko])
                S.dma("sp", lambda e, ob=ob, dest=dest, ncol=ncol: e.dma_start(out=dest, in_=ob[0:ncol, :]), reads=[ko], writes=["projout"])
            for (col0, dest, func) in tm:
                w = pw[wi % 2]; kw = "pw%d" % (wi % 2); wi += 1
                src = self.w_in[l, :, col0:col0 + 512].rearrange("(kc p) c -> p kc c", p=128)
                S.dma("pool", lambda e, w=w, src=src: e.dma_start(out=w[:], in_=src), writes=[kw])
                for t in range(NT):
                    ps = pp[pi % 4]; kp = "pp%d" % (pi % 4); pi += 1
                    for kc in range(8):
                        S.op("pe", lambda e, ps=ps, w=w, kc=kc, t=t: e.matmul(
                            ps[:], lhsT=hT[:, kc, t * 128:(t + 1) * 128], rhs=w[:, kc, :],
                            start=(kc == 0), stop=(kc == 7)), reads=[kw, "hT"], writes=[kp])
                    ot = pot[t % 2]; kt = "pot%d" % (t % 2)
                    S.op("act", lambda e, ps=ps, ot=ot, func=func: e.activation(out=ot[:], in_=ps[:], func=func), reads=[kp], writes=[kt])
                    S.dma("sp", lambda e, ot=ot, dest=dest, t=t: e.dma_start(out=dest[t * 128:(t + 1) * 128, :], in_=ot[:]), reads=[kt], writes=["projout"])

    def stage_attn(self, l):
        nc, S = self.nc, self.S
        NBUF = 3
        with ExitStack() as es:
            sb = lambda n, sh, dt: es.enter_context(self.sbt(n, sh, dt))
            pt = lambda n, sh, dt: es.enter_context(self.pst(n, sh, dt))
            aq = [sb("aq%d" % i, [64, S_LEN], BF16) for i in range(2)]
            ak = [sb("ak%d" % i, [64, S_LEN], BF16) for i in range(2)]
            av = [sb("av%d" % i, [128, NT, 64], BF16) for i in range(2)]
            azs = [sb("azs%d" % i, [128, 512], F32) for i in range(NBUF)]
            asp = [sb("asp%d" % i, [128, 512], F32) for i in range(NBUF)]
            asb = [sb("asb%d" % i, [128, 512], BF16) for i in range(NBUF)]
            alw = [sb("alw%d" % i, [128, 512], F32) for i in range(NBUF)]
            awt = [sb("awt%d" % i, [128, 512], BF16) for i in range(NBUF)]
            ayo = [sb("ayo%d" % i, [64, 512], BF16) for i in range(2)]
            apz = [pt("apz%d" % i, [128, 512], F32) for i in range(2)]
            apc = [pt("apc%d" % i, [128, 512], F32) for i in range(2)]
            apy = [pt("apy%d" % i, [64, 512], F32) for i in range(2)]
            apr = [pt("apr%d" % i, [128, 512], F32) for i in range(2)]
            tri_b = self.cstb[:, 256:384]
            steps = []
            yi = 0
            for h in range(8):
                for qb in range(4):
                    nkb = 4 * (qb + 1)
                    for jn, j in enumerate(reversed(range(nkb))):
                        steps.append(dict(h=h, qb=qb, nkb=nkb, jn=jn, j=j, yi=yi, i=len(steps)))
                    yi += 1
            loaded = set()

            def load_head(h):
                if h in loaded or h >= 8:
                    return
                loaded.add(h)
                hb = h % 2
                S.dma("sp", lambda e: e.dma_start(out=aq[hb][:], in_=self.qT_d[h * 64:(h + 1) * 64, :]), reads=["projout"], writes=["aq%d" % hb])
                S.dma("sp", lambda e: e.dma_start(out=ak[hb][:], in_=self.kT_d[h * 64:(h + 1) * 64, :]), reads=["projout"], writes=["ak%d" % hb])
                S.dma("sp", lambda e: e.dma_start(out=av[hb][:], in_=self.v_d[:, h * 64:(h + 1) * 64].rearrange("(j p) d -> p j d", p=128)), reads=["projout"], writes=["av%d" % hb])

            def phaseA(st):
                h, qb, j, i = st["h"], st["qb"], st["j"], st["i"]
                load_head(h)
                hb = h % 2
                q, k = aq[hb], ak[hb]
                b2, b3 = i % 2, i % NBUF
                pz, zs, sp, sbb = apz[b2], azs[b3], asp[b3], asb[b3]
                kz, kzs, ksp, ksb = "apz%d" % b2, "azs%d" % b3, "asp%d" % b3, "asb%d" % b3
                diag = j >= 4 * qb
                base = qb * 512 - j * 128
                S.op("pe", lambda e: e.matmul(pz[:], lhsT=k[:, j * 128:(j + 1) * 128], rhs=q[:, qb * 512:(qb + 1) * 512], start=True, stop=True), reads=["aq%d" % hb, "ak%d" % hb], writes=[kz])
                S.op("dve", lambda e: e.tensor_copy(out=zs[:], in_=pz[:]), reads=[kz], writes=[kzs])
                S.op("act", lambda e: e.activation(out=sp[:], in_=zs[:], func=AF.Exp), reads=[kzs], writes=[ksp])
                S.op("act", lambda e: e.activation(out=sbb[:], in_=sp[:], func=AF.Ln, bias=1.0), reads=[ksp], writes=[ksb])
                if diag:
                    S.op("pool", lambda e: e.affine_select(out=sbb[:], in_=sbb[:], pattern=[[1, 512]], compare_op=ALU.is_gt, fill=0.0, base=base, channel_multiplier=-1), reads=[ksb], writes=[ksb])

            def phaseB(st):
                qb, j, jn, i = st["qb"], st["j"], st["jn"], st["i"]
                b2, b3 = i % 2, i % NBUF
                pc, zs, sbb, lw, wt = apc[b2], azs[b3], asb[b3], alw[b3], awt[b3]
                kc_, kzs, ksb, klw, kwt = "apc%d" % b2, "azs%d" % b3, "asb%d" % b3, "alw%d" % b3, "awt%d" % b3
                pr = apr[st["yi"] % 2]; kpr = "apr%d" % (st["yi"] % 2)
                diag = j >= 4 * qb
                base = qb * 512 - j * 128
                S.op("pe", lambda e: e.matmul(pc[:], lhsT=tri_b, rhs=sbb[:], start=True, stop=True), reads=["cstb", ksb], writes=[kc_])
                S.op("dve", lambda e: e.tensor_tensor(out=lw[:], in0=zs[:], in1=pc[:], op=ALU.subtract), reads=[kzs, kc_], writes=[klw])
                if jn > 0:
                    S.op("dve", lambda e: e.tensor_tensor(out=lw[:], in0=lw[:], in1=pr[:], op=ALU.subtract), reads=[klw, kpr], writes=[klw])
                S.op("act", lambda e: e.activation(out=wt[:], in_=lw[:], func=AF.Exp), reads=[klw], writes=[kwt])
                if diag:
                    S.op("pool", lambda e: e.affine_select(out=wt[:], in_=wt[:], pattern=[[1, 512]], compare_op=ALU.is_gt, fill=0.0, base=base, channel_multiplier=-1), reads=[kwt], writes=[kwt])

            def phaseC(st):
                h, qb, j, jn, nkb, i = st["h"], st["qb"], st["j"], st["jn"], st["nkb"], st["i"]
                hb = h % 2
                v = av[hb]
                b3 = i % NBUF
                sbb, wt = asb[b3], awt[b3]
                ksb, kwt = "asb%d" % b3, "awt%d" % b3
                y2 = st["yi"] % 2
                py, pr, yo = apy[y2], apr[y2], ayo[y2]
                kpy, kpr, kyo = "apy%d" % y2, "apr%d" % y2, "ayo%d" % y2
                S.op("pe", lambda e: e.matmul(py[:], lhsT=v[:, j, :], rhs=wt[:], start=(jn == 0), stop=(jn == nkb - 1)), reads=["av%d" % hb, kwt], writes=[kpy])
                if jn < nkb - 1:
                    S.op("pe", lambda e: e.matmul(pr[:], lhsT=self.ones_b(), rhs=sbb[:], start=(jn == 0), stop=(jn == nkb - 2)), reads=["cstb", ksb], writes=[kpr])
                else:
                    S.op("act", lambda e: e.activation(out=yo[:], in_=py[:], func=AF.Copy), reads=[kpy], writes=[kyo])
                    S.dma("sp", lambda e: e.dma_start(out=self.yT_d[0, h * 64:(h + 1) * 64, qb * 512:(qb + 1) * 512], in_=yo[:]), reads=[kyo], writes=["yT_d"])

            n = len(steps)
            for s_ in range(n + 2):
                if 0 <= s_ - 2 < n:
                    phaseC(steps[s_ - 2])
                if 0 <= s_ - 1 < n:
                    phaseB(steps[s_ - 1])
                if s_ < n:
                    phaseA(steps[s_])

    def _zero_branch(self, g):
        nc, S = self.nc, self.S
        with ExitStack() as es:
            z = es.enter_context(self.sbt("zb", [128, S_LEN], BF16))
            S.op("dve", lambda e: e.memset(z[:], 0.0), writes=["zb"])
            for kc in range(4):
                S.dma("sp", lambda e, kc=kc: e.dma_start(out=self.yT_d[g, kc * 128:(kc + 1) * 128, :], in_=z[:]), reads=["zb"], writes=["yT_d"])

    def stage_hgrn(self, l):
        nc, S = self.nc, self.S
        with ExitStack() as es:
            sb = lambda n, sh, dt: es.enter_context(self.sbt(n, sh, dt))
            pt = lambda n, sh, dt: es.enter_context(self.pst(n, sh, dt))
            q = sb("hq", [128, S_LEN], F32)
            f = sb("hf", [128, S_LEN], F32)
            lf = sb("hlf", [128, S_LEN], F32)
            kin = sb("hkin", [128, S_LEN], F32)
            Bg = sb("hBg", [128, S_LEN], F32)
            Dd = sb("hD", [128, S_LEN], F32)
            Ee = sb("hE", [128, S_LEN], F32)
            Q1, K1 = sb("hQ1", [128, S_LEN], BF16), sb("hK1", [128, S_LEN], BF16)
            Q2, K2 = sb("hQ2", [128, S_LEN], BF16), sb("hK2", [128, S_LEN], BF16)
            hi = sb("hhi", [128, NT, 128], BF16)
            k2t = sb("hk2t", [128, NT, 128], BF16)
            hg = sb("hhg", [128, S_LEN], F32)
            oT = sb("hoT", [128, S_LEN], F32)
            Bst, Bmid, Bend, dec = sb("hBst", [128, 32], F32), sb("hBmid", [128, 32], F32), sb("hBend", [128, 32], F32), sb("hdec", [128, 32], F32)
            oml = sb("homl", [128, 1], F32)
            St, Stb = sb("hSt", [128, 128], F32), sb("hStb", [128, 128], BF16)
            pTs = [sb("hpT%d" % i, [128, 64], BF16) for i in range(2)]
            sq = sb("hsq", [128, 512], F32)
            rs = sb("hrs", [128, 512], F32)
            yb = sb("hyb", [128, 512], F32)
            ybb = sb("hybb", [128, 512], BF16)
            pss = [pt("hpss%d" % i, [128, 512], F32) for i in range(2)]
            pso = [pt("hpso%d" % i, [128, 512], F32) for i in range(2)]
            psst = [pt("hpsst%d" % i, [128, 512], F32) for i in range(2)]
            ptr = pt("hptr", [128, 1024], BF16)
            psn = pt("hpsn", [128, 512], F32)
            mask = self.cst[:, C_M64:C_M64 + 64]
            Bg3 = Bg[:].rearrange("p (c t) -> p c t", t=64)
            for h in range(4):
                lb = self.lbT[:, l * 4 + h: l * 4 + h + 1]
                rows = slice(h * 128, (h + 1) * 128)
                S.dma("sp", lambda e, rows=rows: e.dma_start(out=q[:], in_=self.hqT_d[rows, :]), reads=["projout"], writes=["hq"])
                S.dma("sp", lambda e, rows=rows: e.dma_start(out=f[:], in_=self.hfT_d[rows, :]), reads=["projout"], writes=["hf"])
                S.dma("sp", lambda e, rows=rows: e.dma_start(out=hg[:], in_=self.hgT_d[rows, :]), reads=["projout"], writes=["hhg"])
                S.dma("sp", lambda e, rows=rows: e.dma_start(out=hi[:], in_=self.hi_d[:, rows].rearrange("(j p) v -> p j v", p=128)), reads=["projout"], writes=["hhi"])
                S.op("dve", lambda e, lb=lb: e.tensor_scalar(out=oml[:], in0=lb, scalar1=-1.0, scalar2=1.0, op0=ALU.mult, op1=ALU.add), reads=["lbT"], writes=["homl"])
                S.op("act", lambda e: e.activation(out=f[:], in_=f[:], func=AF.Sigmoid), reads=["hf"], writes=["hf"])
                S.op("dve", lambda e, lb=lb: e.tensor_scalar(out=f[:], in0=f[:], scalar1=oml[:, 0:1], scalar2=lb, op0=ALU.mult, op1=ALU.add), reads=["hf", "homl", "lbT"], writes=["hf"])
                S.op("act", lambda e: e.activation(out=lf[:], in_=f[:], func=AF.Ln), reads=["hf"], writes=["hlf"])
                S.op("dve", lambda e: e.tensor_scalar(out=kin[:], in0=f[:], scalar1=-1.0, scalar2=1.0, op0=ALU.mult, op1=ALU.add), reads=["hf"], writes=["hkin"])
                S.op("dve", lambda e: e.tensor_tensor_scan(out=Bg[:], data0=lf[:], data1=lf[:], initial=0.0, op0=ALU.add, op1=ALU.bypass), reads=["hlf"], writes=["hBg"])
                S.op("dve", lambda e: e.memset(Bst[:, 0:1], 0.0), writes=["hBst"])
                S.op("dve", lambda e: e.tensor_copy(out=Bst[:, 1:32], in_=Bg3[:, 0:31, 63]), reads=["hBg"], writes=["hBst"])
                S.op("dve", lambda e: e.tensor_copy(out=Bmid[:], in_=Bg3[:, :, 31]), reads=["hBg"], writes=["hBmid"])
                S.op("dve", lambda e: e.tensor_copy(out=Bend[:], in_=Bg3[:, :, 63]), reads=["hBg"], writes=["hBend"])
                S.op("dve", lambda e: e.tensor_tensor(out=dec[:], in0=Bend[:], in1=Bst[:], op=ALU.subtract), reads=["hBend", "hBst"], writes=["hdec"])
                S.op("act", lambda e: e.activation(out=dec[:], in_=dec[:], func=AF.Exp), reads=["hdec"], writes=["hdec"])

                def sub_cols(col, key):
                    for c in range(32):
                        S.op("dve", lambda e, c=c: e.tensor_scalar(out=Dd[:, c * 64:(c + 1) * 64], in0=Bg[:, c * 64:(c + 1) * 64], scalar1=col[:, c:c + 1], scalar2=None, op0=ALU.subtract),
                             reads=["hBg", key], writes=["hD"])
                sub_cols(Bmid, "hBmid")
                S.op("act", lambda e: e.activation(out=Ee[:], in_=Dd[:], func=AF.Exp), reads=["hD"], writes=["hE"])
                S.op("dve", lambda e: e.tensor_tensor(out=Q1[:], in0=q[:], in1=Ee[:], op=ALU.mult), reads=["hq", "hE"], writes=["hQ1"])
                S.op("act", lambda e: e.activation(out=Ee[:], in_=Dd[:], func=AF.Exp, scale=-1.0), reads=["hD", "hQ1"], writes=["hE"])
                S.op("dve", lambda e: e.tensor_tensor(out=K1[:], in0=kin[:], in1=Ee[:], op=ALU.mult), reads=["hkin", "hE"], writes=["hK1"])
                sub_cols(Bst, "hBst")
                S.op("act", lambda e: e.activation(out=Ee[:], in_=Dd[:], func=AF.Exp), reads=["hD", "hK1"], writes=["hE"])
                S.op("dve", lambda e: e.tensor_tensor(out=Q2[:], in0=q[:], in1=Ee[:], op=ALU.mult), reads=["hq", "hE"], writes=["hQ2"])
                sub_cols(Bend, "hBend")
                S.op("act", lambda e: e.activation(out=Ee[:], in_=Dd[:], func=AF.Exp, scale=-1.0), reads=["hD", "hQ2"], writes=["hE"])
                S.op("dve", lambda e: e.tensor_tensor(out=K2[:], in0=kin[:], in1=Ee[:], op=ALU.mult), reads=["hkin", "hE"], writes=["hK2"])
                for j in range(NT):
                    S.op("pe", lambda e, j=j: e.transpose(ptr[:, 0:128], K2[:, j * 128:(j + 1) * 128], self.ident_b()), reads=["hK2", "cstb"], writes=["hptr"])
                    S.op("act", lambda e, j=j: e.activation(out=k2t[:, j, :], in_=ptr[:, 0:128], func=AF.Copy), reads=["hptr"], writes=["hk2t"])
                for c in range(32):
                    j, half = c // 2, c % 2
                    r0 = half * 64
                    cs = slice(c * 64, (c + 1) * 64)
                    ps_s = pss[c % 2]; kps = "hpss%d" % (c % 2)
                    pT = pTs[c % 2]; kpT = "hpT%d" % (c % 2)
                    po = pso[(c // 8) % 2]; kpo = "hpso%d" % ((c // 8) % 2)
                    ocs = slice((c % 8) * 64, (c % 8 + 1) * 64)
                    pst_ = psst[c % 2]; kpst = "hpsst%d" % (c % 2)
                    S.op("pe", lambda e, ps_s=ps_s, r0=r0, cs=cs: e.matmul(ps_s[r0:r0 + 64, 0:64], lhsT=K1[:, cs], rhs=Q1[:, cs], start=True, stop=True), reads=["hK1", "hQ1"], writes=[kps])
                    S.op("dve", lambda e, ps_s=ps_s, pT=pT, r0=r0: e.tensor_copy(out=pT[r0:r0 + 64, :], in_=ps_s[r0:r0 + 64, 0:64]), reads=[kps], writes=[kpT])
                    S.op("pool", lambda e, pT=pT, r0=r0: e.affine_select(out=pT[r0:r0 + 64, :], in_=pT[r0:r0 + 64, :], pattern=[[1, 64]], compare_op=ALU.is_ge, fill=0.0, base=0, channel_multiplier=-1),
                         reads=[kpT], writes=[kpT])
                    S.op("pe", lambda e, po=po, pT=pT, r0=r0, j=j, ocs=ocs, c=c: e.matmul(po[:, ocs], lhsT=hi[r0:r0 + 64, j, :], rhs=pT[r0:r0 + 64, :], start=True, stop=(c == 0)), reads=["hhi", kpT], writes=[kpo])
                    if c > 0:
                        S.op("pe", lambda e, po=po, ocs=ocs, cs=cs: e.matmul(po[:, ocs], lhsT=Stb[:], rhs=Q2[:, cs], start=False, stop=True), reads=["hStb", "hQ2"], writes=[kpo])
                    if c < 31:
                        S.op("pe", lambda e, pst_=pst_, r0=r0, j=j: e.matmul(pst_[:, 0:128], lhsT=k2t[r0:r0 + 64, j, :], rhs=hi[r0:r0 + 64, j, :], start=True, stop=True), reads=["hk2t", "hhi"], writes=[kpst])
                        if c == 0:
                            S.op("dve", lambda e, pst_=pst_: e.tensor_copy(out=St[:], in_=pst_[:, 0:128]), reads=[kpst], writes=["hSt"])
                        else:
                            S.op("dve", lambda e, pst_=pst_, c=c: e.scalar_tensor_tensor(out=St[:], in0=St[:], scalar=dec[:, c:c + 1], in1=pst_[:, 0:128], op0=ALU.mult, op1=ALU.add), reads=[kpst, "hSt", "hdec"], writes=["hSt"])
                        S.op("act", lambda e: e.activation(out=Stb[:], in_=St[:], func=AF.Copy), reads=["hSt"], writes=["hStb"])
                    if c % 8 == 7:
                        tb = c // 8
                        S.op("act", lambda e, po=po, tb=tb: e.activation(out=oT[:, tb * 512:(tb + 1) * 512], in_=po[:], func=AF.Copy), reads=[kpo], writes=["hoT"])
                gcol = self.smc("hgn", l, h)
                for tb in range(4):
                    bs = slice(tb * 512, (tb + 1) * 512)
                    S.op("act", lambda e, bs=bs: e.activation(out=sq[:], in_=oT[:, bs], func=AF.Square), reads=["hoT"], writes=["hsq"])
                    S.op("pe", lambda e: e.matmul(psn[:], lhsT=self.ones_f(), rhs=sq[:], start=True, stop=True), reads=["cst", "hsq"], writes=["hpsn"])
                    S.op("dve", lambda e: e.tensor_scalar(out=rs[:], in0=psn[:], scalar1=1.0 / 128, scalar2=EPS, op0=ALU.mult, op1=ALU.add), reads=["hpsn"], writes=["hrs"])
                    S.op("act", lambda e: e.activation(out=rs[:], in_=rs[:], func=AF.Sqrt), reads=["hrs"], writes=["hrs"])
                    S.op("dve", lambda e: e.reciprocal(out=rs[:], in_=rs[:]), reads=["hrs"], writes=["hrs"])
                    S.op("dve", lambda e, bs=bs: e.tensor_tensor(out=yb[:], in0=oT[:, bs], in1=rs[:], op=ALU.mult), reads=["hoT", "hrs"], writes=["hyb"])
                    S.op("dve", lambda e, bs=bs, gcol=gcol: e.scalar_tensor_tensor(out=ybb[:], in0=yb[:], scalar=gcol, in1=hg[:, bs], op0=ALU.mult, op1=ALU.mult), reads=["hyb", "sm", "hhg"], writes=["hybb"])
                    S.dma("sp", lambda e, bs=bs, rows=rows: e.dma_start(out=self.yT_d[1, rows, bs], in_=ybb[:]), reads=["hybb"], writes=["yT_d"])

    def stage_mlstm(self, l):
        nc, S = self.nc, self.S
        with ExitStack() as es:
            sb = lambda n, sh, dt: es.enter_context(self.sbt(n, sh, dt))
            pt = lambda n, sh, dt: es.enter_context(self.pst(n, sh, dt))
            gi, gf = sb("mgi", [4, S_LEN], F32), sb("mgf", [4, S_LEN], F32)
            Bc, aa, AA = sb("mB", [4, S_LEN], F32), sb("ma", [4, S_LEN], F32), sb("mA", [4, S_LEN], F32)
            em, Dr = sb("mem", [4, S_LEN], F32), sb("mDr", [4, S_LEN], F32)
            uu, ww, it = sb("mu", [4, S_LEN], F32), sb("mw", [4, S_LEN], F32), sb("mit", [4, S_LEN], F32)
            Aend, Aprev, decr = sb("mAend", [4, 32], F32), sb("mAprev", [4, 32], F32), sb("mdecr", [4, 32], F32)
            nbf = sb("mnbf", [4, 1], F32)
            og, _ = SM_OFF["gateb"]
            bi = self.sm[0:4, og + l * 2: og + l * 2 + 1]
            bf = self.sm[0:4, og + l * 2 + 1: og + l * 2 + 2]
            S.dma("sp", lambda e: e.dma_start(out=gi[:], in_=self.gT_d[0, :, :]), reads=["projout"], writes=["mgi"])
            S.dma("sp", lambda e: e.dma_start(out=gf[:], in_=self.gT_d[1, :, :]), reads=["projout"], writes=["mgf"])
            S.op("dve", lambda e: e.tensor_scalar(out=gi[:], in0=gi[:], scalar1=bi, scalar2=None, op0=ALU.add), reads=["mgi", "sm"], writes=["mgi"])
            S.op("dve", lambda e: e.tensor_scalar(out=nbf[:], in0=bf, scalar1=-1.0, scalar2=None, op0=ALU.mult), reads=["sm"], writes=["mnbf"])
            S.op("act", lambda e: e.activation(out=gf[:], in_=gf[:], func=AF.Exp, scale=-1.0, bias=nbf[:, 0:1]), reads=["mgf", "mnbf"], writes=["mgf"])
            S.op("act", lambda e: e.activation(out=gf[:], in_=gf[:], func=AF.Ln, bias=1.0), reads=["mgf"], writes=["mgf"])
            S.op("dve", lambda e: e.tensor_scalar(out=gf[:], in0=gf[:], scalar1=-1.0, scalar2=None, op0=ALU.mult), reads=["mgf"], writes=["mgf"])
            S.op("dve", lambda e: e.tensor_tensor_scan(out=Bc[:], data0=gf[:], data1=gf[:], initial=0.0, op0=ALU.add, op1=ALU.bypass), reads=["mgf"], writes=["mB"])
            S.op("dve", lambda e: e.tensor_tensor(out=aa[:], in0=gi[:], in1=Bc[:], op=ALU.subtract), reads=["mgi", "mB"], writes=["ma"])
            S.op("dve", lambda e: e.tensor_tensor_scan(out=AA[:], data0=aa[:], data1=aa[:], initial=0.0, op0=ALU.max, op1=ALU.bypass), reads=["ma"], writes=["mA"])
            S.op("dve", lambda e: e.tensor_tensor(out=em[:], in0=Bc[:], in1=AA[:], op=ALU.add), reads=["mB", "mA"], writes=["mem"])
            S.op("act", lambda e: e.activation(out=em[:], in_=em[:], func=AF.Exp, scale=-1.0), reads=["mem"], writes=["mem"])
            A3 = AA[:].rearrange("p (c t) -> p c t", t=64)
            a3 = aa[:].rearrange("p (c t) -> p c t", t=64)
            D3 = Dr[:].rearrange("p (c t) -> p c t", t=64)
            S.op("dve", lambda e: e.tensor_copy(out=Aend[:], in_=A3[:, :, 63]), reads=["mA"], writes=["mAend"])
            S.op("dve", lambda e: e.memset(Aprev[:, 0:1], 0.0), writes=["mAprev"])
            S.op("dve", lambda e: e.tensor_copy(out=Aprev[:, 1:32], in_=A3[:, 0:31, 63]), reads=["mA"], writes=["mAprev"])
            S.op("dve", lambda e: e.tensor_tensor(out=decr[:], in0=Aprev[:], in1=Aend[:], op=ALU.subtract), reads=["mAprev", "mAend"], writes=["mdecr"])
            S.op("act", lambda e: e.activation(out=decr[:], in_=decr[:], func=AF.Exp), reads=["mdecr"], writes=["mdecr"])
            bc = lambda col: col[:].unsqueeze(2).to_broadcast([4, 32, 64])
            S.op("dve", lambda e: e.tensor_tensor(out=D3, in0=A3, in1=bc(Aend), op=ALU.subtract), reads=["mA", "mAend"], writes=["mDr"])
            S.op("act", lambda e: e.activation(out=uu[:], in_=Dr[:], func=AF.Exp, scale=-1.0), reads=["mDr"], writes=["mu"])
            S.op("dve", lambda e: e.tensor_tensor(out=D3, in0=a3, in1=bc(Aend), op=ALU.subtract), reads=["ma", "mAend", "mu"], writes=["mDr"])
            S.op("act", lambda e: e.activation(out=ww[:], in_=Dr[:], func=AF.Exp), reads=["mDr"], writes=["mw"])
            S.op("dve", lambda e: e.tensor_tensor(out=D3, in0=A3, in1=bc(Aprev), op=ALU.subtract), reads=["mA", "mAprev", "mw"], writes=["mDr"])
            S.op("act", lambda e: e.activation(out=it[:], in_=Dr[:], func=AF.Exp, scale=-1.0), reads=["mDr"], writes=["mit"])
            xr, acc = sb("mxr", [128, S_LEN], F32), sb("macc2", [128, S_LEN], F32)
            Q1, Q2, K1 = sb("mQ1", [128, S_LEN], BF16), sb("mQ2", [128, S_LEN], BF16), sb("mK1", [128, S_LEN], BF16)
            vv = sb("mvv", [128, NT, 128], BF16)
            k2t = sb("mk2t", [128, NT, 128], BF16)
            mo = sb("mmo", [128, S_LEN], F32)
            hT = sb("mhT", [128, S_LEN], F32)
            dec = sb("mdec", [128, 32], F32)
            Cs, Csb = sb("mCs", [128, 128], F32), sb("mCsb", [128, 128], BF16)
            Ns, Nsb = sb("mNs", [128, 128], F32), sb("mNsb", [128, 128], BF16)
            pTs = [sb("mpT%d" % i, [128, 64], BF16) for i in range(2)]
            numT, embc, dmx = sb("mnumT", [128, 512], F32), sb("membc", [128, 512], F32), sb("mdmx", [128, 512], F32)
            sq, rs, yb, ybb = sb("msq", [128, 512], F32), sb("mrs", [128, 512], F32), sb("myb", [128, 512], F32), sb("mybb", [128, 512], BF16)
            pss = pt("mpss", [128, 512], F32)
            pso = [pt("mpso%d" % i, [128, 512], F32) for i in range(2)]
            psd = [pt("mpsd%d" % i, [128, 512], F32) for i in range(2)]
            pstC, pstN = pt("mpstC", [128, 512], F32), pt("mpstN", [128, 512], F32)
            pmisc = pt("mpmisc", [128, 512], F32)
            pmisc_b = pmisc[:].bitcast(BF16)
            KM = "mpmisc"
            ones_b = self.ones_b()

            def bcast_rows(rows, h, tb):
                S.op("pe", lambda e: e.matmul(pmisc[:], lhsT=self.cst[0:4, C_SEL + h * 128: C_SEL + (h + 1) * 128], rhs=rows[0:4, tb * 512:(tb + 1) * 512], start=True, stop=True),
                     reads=["cst", "mu", "mw", "mit", "mem"], writes=[KM])

            def conv_silu(chunk):
                cw = lambda tap: self.smc("convw", l, tap * 8 + chunk)
                S.op("dve", lambda e: e.tensor_scalar(out=acc[:], in0=xr[:], scalar1=cw(3), scalar2=self.smc("convb", l, chunk), op0=ALU.mult, op1=ALU.add), reads=["mxr", "sm"], writes=["macc2"])
                for sh in (1, 2, 3):
                    S.op("dve", lambda e, sh=sh: e.scalar_tensor_tensor(out=acc[:, sh:S_LEN], in0=xr[:, 0:S_LEN - sh], scalar=cw(3 - sh), in1=acc[:, sh:S_LEN], op0=ALU.mult, op1=ALU.add),
                         reads=["mxr", "sm", "macc2"], writes=["macc2"])
                S.op("act", lambda e: e.activation(out=acc[:], in_=acc[:], func=AF.Silu), reads=["macc2"], writes=["macc2"])

            for h in range(4):
                rows = slice(h * 128, (h + 1) * 128)
                S.dma("sp", lambda e, rows=rows: e.dma_start(out=xr[:], in_=self.mqkT_d[rows, :]), reads=["projout"], writes=["mxr"])
                S.dma("sp", lambda e, rows=rows: e.dma_start(out=mo[:], in_=self.moT_d[rows, :]), reads=["projout"], writes=["mmo"])
                S.dma("sp", lambda e, rows=rows: e.dma_start(out=vv[:], in_=self.mv_d[:, rows].rearrange("(j p) v -> p j v", p=128)), reads=["projout"], writes=["mvv"])
                conv_silu(h)
                for tb in range(4):
                    bs = slice(tb * 512, (tb + 1) * 512)
                    bcast_rows(uu, h, tb)
                    S.op("dve", lambda e, bs=bs: e.tensor_tensor(out=Q1[:, bs], in0=acc[:, bs], in1=pmisc[:], op=ALU.mult), reads=["macc2", KM], writes=["mQ1"])
                    bcast_rows(it, h, tb)
                    S.op("dve", lambda e, bs=bs: e.tensor_tensor(out=Q2[:, bs], in0=acc[:, bs], in1=pmisc[:], op=ALU.mult), reads=["macc2", KM], writes=["mQ2"])
                S.dma("sp", lambda e, h=h: e.dma_start(out=xr[:], in_=self.mqkT_d[512 + h * 128: 512 + (h + 1) * 128, :]), reads=["projout"], writes=["mxr"])
                conv_silu(4 + h)
                for tb in range(4):
                    bs = slice(tb * 512, (tb + 1) * 512)
                    bcast_rows(ww, h, tb)
                    S.op("dve", lambda e, bs=bs: e.scalar_tensor_tensor(out=K1[:, bs], in0=acc[:, bs], scalar=128.0 ** -0.5, in1=pmisc[:], op0=ALU.mult, op1=ALU.mult), reads=["macc2", KM], writes=["mK1"])
                S.op("pe", lambda e, h=h: e.matmul(pmisc[:, 0:32], lhsT=self.cst[0:4, C_SEL + h * 128: C_SEL + (h + 1) * 128], rhs=decr[0:4, :], start=True, stop=True), reads=["cst", "mdecr"], writes=[KM])
                S.op("act", lambda e: e.activation(out=dec[:], in_=pmisc[:, 0:32], func=AF.Copy), reads=[KM], writes=["mdec"])
                for j in range(NT):
                    S.op("pe", lambda e, j=j: e.transpose(pmisc_b[:, 0:128], K1[:, j * 128:(j + 1) * 128], self.ident_b()), reads=["mK1", "cstb"], writes=[KM])
                    S.op("act", lambda e, j=j: e.activation(out=k2t[:, j, :], in_=pmisc_b[:, 0:128], func=AF.Copy), reads=[KM], writes=["mk2t"])
                for c in range(32):
                    j, half = c // 2, c % 2
                    r0 = half * 64
                    cs = slice(c * 64, (c + 1) * 64)
                    pT = pTs[c % 2]; kpT = "mpT%d" % (c % 2)
                    po = pso[(c // 8) % 2]; kpo = "mpso%d" % ((c // 8) % 2)
                    pd = psd[(c // 8) % 2]; kpd = "mpsd%d" % ((c // 8) % 2)
                    ocs = slice((c % 8) * 64, (c % 8 + 1) * 64)
                    S.op("pe", lambda e, r0=r0, cs=cs: e.matmul(pss[r0:r0 + 64, 0:64], lhsT=K1[:, cs], rhs=Q1[:, cs], start=True, stop=True), reads=["mK1", "mQ1"], writes=["mpss"])
                    S.op("dve", lambda e, pT=pT, r0=r0: e.tensor_copy(out=pT[r0:r0 + 64, :], in_=pss[r0:r0 + 64, 0:64]), reads=["mpss"], writes=[kpT])
                    S.op("pool", lambda e, pT=pT, r0=r0: e.affine_select(out=pT[r0:r0 + 64, :], in_=pT[r0:r0 + 64, :], pattern=[[1, 64]], compare_op=ALU.is_ge, fill=0.0, base=0, channel_multiplier=-1),
                         reads=[kpT], writes=[kpT])
                    S.op("pe", lambda e, po=po, pT=pT, r0=r0, j=j, ocs=ocs, c=c: e.matmul(po[:, ocs], lhsT=vv[r0:r0 + 64, j, :], rhs=pT[r0:r0 + 64, :], start=True, stop=(c == 0)), reads=["mvv", kpT], writes=[kpo])
                    if c > 0:
                        S.op("pe", lambda e, po=po, ocs=ocs, cs=cs: e.matmul(po[:, ocs], lhsT=Csb[:], rhs=Q2[:, cs], start=False, stop=True), reads=["mCsb", "mQ2"], writes=[kpo])
                    S.op("pe", lambda e, pd=pd, pT=pT, r0=r0, ocs=ocs, c=c: e.matmul(pd[:, ocs], lhsT=ones_b[r0:r0 + 64, :], rhs=pT[r0:r0 + 64, :], start=True, stop=(c == 0)), reads=["cstb", kpT], writes=[kpd])
                    if c > 0:
                        S.op("pe", lambda e, pd=pd, ocs=ocs, cs=cs: e.matmul(pd[:, ocs], lhsT=Nsb[:], rhs=Q2[:, cs], start=False, stop=True), reads=["mNsb", "mQ2"], writes=[kpd])
                    if c < 31:
                        S.op("pe", lambda e, r0=r0, j=j: e.matmul(pstC[:, 0:128], lhsT=k2t[r0:r0 + 64, j, :], rhs=vv[r0:r0 + 64, j, :], start=True, stop=True), reads=["mk2t", "mvv"], writes=["mpstC"])
                        S.op("pe", lambda e, r0=r0, j=j: e.matmul(pstN[:, 0:128], lhsT=k2t[r0:r0 + 64, j, :], rhs=ones_b[r0:r0 + 64, :], start=True, stop=True), reads=["mk2t", "cstb"], writes=["mpstN"])
                        if c == 0:
                            S.op("dve", lambda e: e.tensor_copy(out=Cs[:], in_=pstC[:, 0:128]), reads=["mpstC"], writes=["mCs"])
                            S.op("dve", lambda e: e.tensor_copy(out=Ns[:], in_=pstN[:, 0:128]), reads=["mpstN"], writes=["mNs"])
                        else:
                            S.op("dve", lambda e, c=c: e.scalar_tensor_tensor(out=Cs[:], in0=Cs[:], scalar=dec[:, c:c + 1], in1=pstC[:, 0:128], op0=ALU.mult, op1=ALU.add), reads=["mpstC", "mCs", "mdec"], writes=["mCs"])
                            S.op("dve", lambda e, c=c: e.scalar_tensor_tensor(out=Ns[:], in0=Ns[:], scalar=dec[:, c:c + 1], in1=pstN[:, 0:128], op0=ALU.mult, op1=ALU.add), reads=["mpstN", "mNs", "mdec"], writes=["mNs"])
                        S.op("act", lambda e: e.activation(out=Csb[:], in_=Cs[:], func=AF.Copy), reads=["mCs"], writes=["mCsb"])
                        S.op("act", lambda e: e.activation(out=Nsb[:], in_=Ns[:], func=AF.Copy), reads=["mNs"], writes=["mNsb"])
                    if c % 8 == 7:
                        tb = c // 8
                        bs = slice(tb * 512, (tb + 1) * 512)
                        S.op("act", lambda e, po=po: e.activation(out=numT[:], in_=po[:], func=AF.Copy), reads=[kpo], writes=["mnumT"])
                        bcast_rows(em, h, tb)
                        S.op("act", lambda e: e.activation(out=embc[:], in_=pmisc[:], func=AF.Copy), reads=[KM], writes=["membc"])
                        S.op("act", lambda e, pd=pd: e.activation(out=dmx[:], in_=pd[:], func=AF.Abs), reads=[kpd], writes=["mdmx"])
                        S.op("dve", lambda e: e.tensor_tensor(out=dmx[:], in0=dmx[:], in1=embc[:], op=ALU.max), reads=["mdmx", "membc"], writes=["mdmx"])
                        S.op("dve", lambda e: e.reciprocal(out=dmx[:], in_=dmx[:]), reads=["mdmx"], writes=["mdmx"])
                        S.op("dve", lambda e, bs=bs: e.tensor_tensor(out=hT[:, bs], in0=numT[:], in1=dmx[:], op=ALU.mult), reads=["mnumT", "mdmx"], writes=["mhT"])
                gcol = self.smc("mln", l, h)
                for tb in range(4):
                    bs = slice(tb * 512, (tb + 1) * 512)
                    S.op("act", lambda e, bs=bs: e.activation(out=sq[:], in_=hT[:, bs], func=AF.Square), reads=["mhT"], writes=["msq"])
                    S.op("pe", lambda e: e.matmul(pmisc[:], lhsT=self.ones_f(), rhs=sq[:], start=True, stop=True), reads=["cst", "msq"], writes=[KM])
                    S.op("dve", lambda e: e.tensor_scalar(out=rs[:], in0=pmisc[:], scalar1=1.0 / 128, scalar2=EPS, op0=ALU.mult, op1=ALU.add), reads=[KM], writes=["mrs"])
                    S.op("act", lambda e: e.activation(out=rs[:], in_=rs[:], func=AF.Sqrt), reads=["mrs"], writes=["mrs"])
                    S.op("dve", lambda e: e.reciprocal(out=rs[:], in_=rs[:]), reads=["mrs"], writes=["mrs"])
                    S.op("dve", lambda e, bs=bs: e.tensor_tensor(out=yb[:], in0=hT[:, bs], in1=rs[:], op=ALU.mult), reads=["mhT", "mrs"], writes=["myb"])
                    S.op("dve", lambda e, bs=bs, gcol=gcol: e.scalar_tensor_tensor(out=ybb[:], in0=yb[:], scalar=gcol, in1=mo[:, bs], op0=ALU.mult, op1=ALU.mult), reads=["myb", "sm", "mmo"], writes=["mybb"])
                    S.dma("sp", lambda e, bs=bs, rows=rows: e.dma_start(out=self.yT_d[2, rows, bs], in_=ybb[:]), reads=["mybb"], writes=["yT_d"])

    def _bcast_tile(self, es, name, colfn, ps_ap=None, ps_key=None):
        nc, S = self.nc, self.S
        dg = es.enter_context(self.sbt(name + "dg", [128, 128], F32))
        dst = es.enter_context(self.sbt(name, [128, D], F32))
        if ps_ap is None:
            ps = es.enter_context(self.pst(name + "ps", [128, D], F32))[:]
            ps_key = name + "ps"
        else:
            ps = ps_ap
        for c in range(8):
            S.op("dve", lambda e, c=c: e.tensor_scalar(out=dg[:], in0=self.ident_f(), scalar1=colfn(c), scalar2=None, op0=ALU.mult),
                 reads=["cst", "modT", "sm"], writes=[name + "dg"])
            S.op("pe", lambda e, c=c: e.matmul(ps[:, c * 128:(c + 1) * 128], lhsT=self.ones_f(), rhs=dg[:], start=True, stop=True), reads=["cst", name + "dg"], writes=[ps_key])
        for hh in range(2):
            S.op("act", lambda e, hh=hh: e.activation(out=dst[:, hh * 512:(hh + 1) * 512], in_=ps[:, hh * 512:(hh + 1) * 512], func=AF.Copy), reads=[ps_key], writes=[name])
        return dst

    def stage_merge(self, l):
        nc, S = self.nc, self.S
        with ExitStack() as es:
            sb = lambda n, sh, dt: es.enter_context(self.sbt(n, sh, dt))
            pt = lambda n, sh, dt: es.enter_context(self.pst(n, sh, dt))
            g1bc = self._bcast_tile(es, "g1bc", lambda c: self.modc(l, 2, c))
            wb = sb("mwb", [128, 3, 4, D], BF16)
            wo = sb("mwo", [128, 8, D], BF16)
            yt = sb("myt", [128, 3, 4, 512], BF16)
            gts = [sb("mgt%d" % i, [128, 512], F32) for i in range(3)]
            tmps = [sb("mtmp%d" % i, [128, 512], F32) for i in range(2)]
            macc = sb("macc", [128, 512], F32)
            mT = sb("mT", [128, 8, 512], BF16)
            xts = [sb("mxt%d" % i, [128, D], F32) for i in range(2)]
            ytmp = sb("mytmp", [128, 512], F32)
            psa = [pt("mpsa%d" % i, [128, 512], F32) for i in range(2)]
            psy = [pt("mpsy%d" % i, [128, 512], F32) for i in range(2)]
            for g in range(3):
                S.dma("pool", lambda e, g=g: e.dma_start(out=wb[:, g, :, :], in_=self.w_branch[l, g, :, :].rearrange("(kc p) d -> p kc d", p=128)), writes=["mwb"])
            S.dma("pool", lambda e: e.dma_start(out=wo[:], in_=self.w_out[l, :, :].rearrange("(c p) d -> p c d", p=128)), writes=["mwo"])
            ai = 0
            gi = 0
            yi = 0
            for tb in range(4):
                for g in range(3):
                    S.dma("sp", lambda e, g=g, tb=tb: e.dma_start(out=yt[:, g, :, :], in_=self.yT_d[g, :, tb * 512:(tb + 1) * 512].rearrange("(kc p) t -> p kc t", p=128)),
                          reads=["yT_d"], writes=["myt"])
                for dc in range(8):
                    for g in range(3):
                        ps = psa[ai % 2]; kps = "mpsa%d" % (ai % 2); ai += 1
                        gt = gts[gi % 3]; kgt = "mgt%d" % (gi % 3); gi += 1
                        S.dma("sp", lambda e, gt=gt, g=g, dc=dc, tb=tb: e.dma_start(out=gt[:], in_=self.bgT_d[g * 1024 + dc * 128: g * 1024 + (dc + 1) * 128, tb * 512:(tb + 1) * 512]),
                              reads=["projout"], writes=[kgt])
                        for kc in range(4):
                            S.op("pe", lambda e, ps=ps, g=g, kc=kc, dc=dc: e.matmul(ps[:], lhsT=wb[:, g, kc, dc * 128:(dc + 1) * 128], rhs=yt[:, g, kc, :], start=(kc == 0), stop=(kc == 3)),
                                 reads=["mwb", "myt"], writes=[kps])
                        if g == 0:
                            S.op("dve", lambda e, ps=ps, gt=gt: e.tensor_tensor(out=macc[:], in0=ps[:], in1=gt[:], op=ALU.mult), reads=[kps, kgt], writes=["macc"])
                        else:
                            tmp = tmps[g % 2]; ktmp = "mtmp%d" % (g % 2)
                            S.op("dve", lambda e, ps=ps, gt=gt, tmp=tmp: e.tensor_tensor(out=tmp[:], in0=ps[:], in1=gt[:], op=ALU.mult), reads=[kps, kgt], writes=[ktmp])
                            if g == 1:
                                S.op("pool", lambda e, tmp=tmp: e.tensor_tensor(out=macc[:], in0=macc[:], in1=tmp[:], op=ALU.add), reads=[ktmp, "macc"], writes=["macc"])
                            else:
                                S.op("pool", lambda e, tmp=tmp, dc=dc: e.tensor_tensor(out=mT[:, dc, :], in0=macc[:], in1=tmp[:], op=ALU.add), reads=[ktmp, "macc"], writes=["mT"])
                for tt in range(4):
                    t = tb * 4 + tt
                    xt = xts[t % 2]; kxt = "mxt%d" % (t % 2)
                    S.dma("sp", lambda e, xt=xt, t=t: e.dma_start(out=xt[:], in_=self.xres[t * 128:(t + 1) * 128, :]), reads=["xres"], writes=[kxt])
                    for dh in range(2):
                        ps = psy[yi % 2]; kps = "mpsy%d" % (yi % 2); yi += 1
                        for c in range(8):
                            S.op("pe", lambda e, ps=ps, c=c, tt=tt, dh=dh: e.matmul(ps[:], lhsT=mT[:, c, tt * 128:(tt + 1) * 128], rhs=wo[:, c, dh * 512:(dh + 1) * 512], start=(c == 0), stop=(c == 7)),
                                 reads=["mT", "mwo"], writes=[kps])
                        S.op("dve", lambda e, ps=ps, dh=dh: e.tensor_tensor(out=ytmp[:], in0=ps[:], in1=g1bc[:, dh * 512:(dh + 1) * 512], op=ALU.mult), reads=[kps, "g1bc"], writes=["mytmp"])
                        S.op("dve", lambda e, xt=xt, dh=dh: e.tensor_tensor(out=xt[:, dh * 512:(dh + 1) * 512], in0=xt[:, dh * 512:(dh + 1) * 512], in1=ytmp[:], op=ALU.add), reads=["mytmp", kxt], writes=[kxt])
                    S.dma("sp", lambda e, xt=xt, t=t: e.dma_start(out=self.xres[t * 128:(t + 1) * 128, :], in_=xt[:]), reads=[kxt], writes=["xres"])

    def stage_uvcast(self, l):
        nc, S = self.nc, self.S
        with ExitStack() as es:
            cbs = [es.enter_context(self.sbt("uvc%d" % i, [128, 4, 2 * D], BF16)) for i in range(3)]
            for k in range(32):
                cb = cbs[k % 3]; kcb = "uvc%d" % (k % 3)
                src = self.pk_uv[l, k * 512:(k + 1) * 512, :].rearrange("(p r) d -> p r d", p=128)
                dst = self.uvb[k * 512:(k + 1) * 512, :].rearrange("(p r) d -> p r d", p=128)
                S.dma("pool", lambda e, cb=cb, src=src: e.dma_start(out=cb[:], in_=src), writes=[kcb])
                S.dma("sp", lambda e, cb=cb, dst=dst: e.dma_start(out=dst, in_=cb[:]), reads=[kcb], writes=["uvb"])

    def stage_peer(self, l):
        nc, S = self.nc, self.S
        NB = 6
        with ExitStack() as es:
            sb = lambda n, sh, dt: es.enter_context(self.sbt(n, sh, dt))
            pt = lambda n, sh, dt: es.enter_context(self.pst(n, sh, dt))
            two = lambda n, sh, dt: [sb("%s%d" % (n, i), sh, dt) for i in range(2)]
            hTs = two("phT", [128, 8, 128], BF16)
            wq = sb("pwq", [128, 8, D], BF16)
            kbd = sb("pkbd", [128, 8, 256], F32)
            qTts = two("pqTt", [128, 8, 128], F32)
            scs, s2s, cands = two("psc", [128, 2048], F32), two("ps2", [128, 2048], F32), two("pcand", [128, 2048], F32)
            mxs, mis, sifs = two("pmx", [128, 256], F32), two("pmi", [128, 256], U32), two("psif", [128, 256], F32)
            tvs, tps, tpfs = two("ptv", [128, 128], F32), two("ptp", [128, 128], U32), two("ptpf", [128, 128], F32)
            afs, bfs = two("paf", [128, 128], F32), two("pbf", [128, 128], F32)
            i1s, i2s = two("pi1", [128, 128], F32), two("pi2", [128, 128], F32)
            idxis = two("pidxi", [128, 128], I32)
            ees, ggs = two("pee", [128, 128], F32), two("pgg", [128, 128], F32)
            ssums = two("pssum", [128, 8], F32)
            aa, ga, gl = sb("paa", [128, 128], F32), sb("pga", [128, 128], F32), sb("pgl", [128, 128], F32)
            h2ts = two("ph2t", [128, D], F32)
            xts = two("pxt", [128, D], F32)
            ubs = [sb("pub%d" % i, [128, 2 * D], BF16) for i in range(NB)]
            junks = two("pjunk", [128, D], F32)
            acc = sb("pacc", [128, D], F32)
            tmpbs = [sb("ptmpb%d" % i, [128, D], BF16) for i in range(3)]
            psq = pt("ppsq", [128, 8, 128], F32)
            pssc = pt("ppssc", [128, 2048], F32)
            pacc = pt("ppacc", [128, D], F32)
            g2bc = self._bcast_tile(es, "g2bc", lambda c: self.modc(l, 5, c), ps_ap=pssc[:, 0:D], ps_key="ppssc")
            UV2 = self.uvb
            iota16 = self.cst[:, C_IOTA:C_IOTA + 16]
            thr15 = self.cst[:, C_THR:C_THR + 15]
            S.dma("pool", lambda e: e.dma_start(out=wq[:], in_=self.pk_wq[l, :, :].rearrange("(kc p) d -> p kc d", p=128)), writes=["pwq"])
            S.dma("sp", lambda e: e.dma_start(out=kbd[:], in_=self.keysbd[l, :, :, :].rearrange("h p n -> p h n")), writes=["pkbd"])
            B4 = [128, 8, 16, 16]

            def prep(t):
                p = t % 2
                K = lambda n: "%s%d" % (n, p)
                ts = slice(t * 128, (t + 1) * 128)
                h2t, xt, hT, qTt = h2ts[p], xts[p], hTs[p], qTts[p]
                sc, s2, cand, mx, mi, sif = scs[p], s2s[p], cands[p], mxs[p], mis[p], sifs[p]
                tv, tp, tpf, af, bf_, i1, i2, idxi, ee, gg, ssum = tvs[p], tps[p], tpfs[p], afs[p], bfs[p], i1s[p], i2s[p], idxis[p], ees[p], ggs[p], ssums[p]
                sc3 = sc[:].rearrange("p (g n) -> p g n", n=128)
                s23 = s2[:].rearrange("p (g n) -> p g n", n=128)
                mx3 = mx[:].rearrange("p (g k) -> p g k", k=16)
                mi3 = mi[:].rearrange("p (g k) -> p g k", k=16)
                mx4 = mx[:].rearrange("p (h q k) -> p h q k", h=8, q=2)
                sif4 = sif[:].rearrange("p (h q k) -> p h q k", h=8, q=2)
                cand4 = cand[:].rearrange("p (h a b) -> p h a b", h=8, a=16)
                tv3 = tv[:].rearrange("p (h k) -> p h k", h=8)
                tp3 = tp[:].rearrange("p (h k) -> p h k", h=8)
                oh4 = s2[:].rearrange("p (h k a) -> p h k a", h=8, k=16)
                cmp3 = sc[:, 0:1920].rearrange("p (r j) -> p r j", j=15)
                S.dma("sp", lambda e: e.dma_start(out=h2t[:], in_=self.h2_d[ts, :]), reads=["h2_d"], writes=[K("ph2t")])
                S.dma("sp", lambda e: e.dma_start(out=xt[:], in_=self.xres[ts, :]), reads=["xres"], writes=[K("pxt")])
                S.dma("sp", lambda e: e.dma_start(out=hT[:], in_=self.hT_d[:, :, ts].rearrange("c p t -> p c t")), reads=["hT_d"], writes=[K("phT")])
                yield
                for h in range(8):
                    for kc in range(8):
                        S.op("pe", lambda e, h=h, kc=kc: e.matmul(psq[:, h, :], lhsT=wq[:, kc, h * 128:(h + 1) * 128], rhs=hT[:, kc, :], start=(kc == 0), stop=(kc == 7)),
                             reads=["pwq", K("phT")], writes=["ppsq"])
                    yield
                for hh in range(2):
                    S.op("act", lambda e, hh=hh: e.activation(out=qTt[:, hh * 4:(hh + 1) * 4, :], in_=psq[:, hh * 4:(hh + 1) * 4, :], func=AF.Copy), reads=["ppsq"], writes=[K("pqTt")])
                for h in range(8):
                    S.op("pe", lambda e, h=h: e.matmul(pssc[:, h * 256:(h + 1) * 256], lhsT=qTt[:, h, :], rhs=kbd[:, h, :], start=True, stop=True), reads=[K("pqTt"), "pkbd"], writes=["ppssc"])
                for qd in range(4):
                    S.op("act", lambda e, qd=qd: e.activation(out=sc[:, qd * 512:(qd + 1) * 512], in_=pssc[:, qd * 512:(qd + 1) * 512], func=AF.Copy), reads=["ppssc"], writes=[K("psc")])
                yield
                for g in range(16):
                    S.op("dve", lambda e, g=g: e.max(out=mx3[:, g, 0:8], in_=sc3[:, g, :]), reads=[K("psc")], writes=[K("pmx")])
                    S.op("dve", lambda e, g=g: e.max_index(out=mi3[:, g, 0:8], in_max=mx3[:, g, 0:8], in_values=sc3[:, g, :]), reads=[K("psc"), K("pmx")], writes=[K("pmi")])
                    yield
                    S.op("dve", lambda e, g=g: e.match_replace(out=s23[:, g, :], in_to_replace=mx3[:, g, 0:8], in_values=sc3[:, g, :], imm_value=-1e30), reads=[K("psc"), K("pmx")], writes=[K("ps2")])
                    S.op("dve", lambda e, g=g: e.max(out=mx3[:, g, 8:16], in_=s23[:, g, :]), reads=[K("ps2")], writes=[K("pmx")])
                    yield
                    S.op("dve", lambda e, g=g: e.max_index(out=mi3[:, g, 8:16], in_max=mx3[:, g, 8:16], in_values=s23[:, g, :]), reads=[K("ps2"), K("pmx")], writes=[K("pmi")])
                    yield
                S.op("dve", lambda e: e.tensor_copy(out=sif[:], in_=mi[:]), reads=[K("pmi")], writes=[K("psif")])
                S.op("dve", lambda e: e.tensor_tensor(out=cand4, in0=mx4[:, :, 0, :].unsqueeze(3).to_broadcast(B4), in1=mx4[:, :, 1, :].unsqueeze(2).to_broadcast(B4), op=ALU.add),
                     reads=[K("pmx")], writes=[K("pcand")])
                yield
                for h in range(8):
                    hs = slice(h * 256, (h + 1) * 256)
                    S.op("dve", lambda e, h=h, hs=hs: e.max(out=tv3[:, h, 0:8], in_=cand[:, hs]), reads=[K("pcand")], writes=[K("ptv")])
                    S.op("dve", lambda e, h=h, hs=hs: e.max_index(out=tp3[:, h, 0:8], in_max=tv3[:, h, 0:8], in_values=cand[:, hs]), reads=[K("pcand"), K("ptv")], writes=[K("ptp")])
                    yield
                    S.op("dve", lambda e, h=h, hs=hs: e.match_replace(out=s2[:, hs], in_to_replace=tv3[:, h, 0:8], in_values=cand[:, hs], imm_value=-1e30), reads=[K("pcand"), K("ptv")], writes=[K("ps2")])
                    S.op("dve", lambda e, h=h, hs=hs: e.max(out=tv3[:, h, 8:16], in_=s2[:, hs]), reads=[K("ps2")], writes=[K("ptv")])
                    yield
                    S.op("dve", lambda e, h=h, hs=hs: e.max_index(out=tp3[:, h, 8:16], in_max=tv3[:, h, 8:16], in_values=s2[:, hs]), reads=[K("ps2"), K("ptv")], writes=[K("ptp")])
                    yield
                S.op("dve", lambda e: e.tensor_copy(out=tpf[:], in_=tp[:]), reads=[K("ptp")], writes=[K("ptpf")])
                S.op("dve", lambda e: e.tensor_tensor(out=cmp3, in0=tpf[:].unsqueeze(2).to_broadcast([128, 128, 15]), in1=thr15.unsqueeze(1).to_broadcast([128, 128, 15]), op=ALU.is_ge),
                     reads=[K("ptpf"), "cst"], writes=[K("psc")])
                yield
                S.op("dve", lambda e: e.tensor_reduce(out=af[:], in_=cmp3, axis=AX.X, op=ALU.add), reads=[K("psc")], writes=[K("paf")])
                S.op("dve", lambda e: e.scalar_tensor_tensor(out=bf_[:], in0=af[:], scalar=-16.0, in1=tpf[:], op0=ALU.mult, op1=ALU.add), reads=[K("paf"), K("ptpf")], writes=[K("pbf")])
                yield
                for (src, q_, dst, kd) in ((af, 0, i1, K("pi1")), (bf_, 1, i2, K("pi2"))):
                    ksrc = K("paf") if q_ == 0 else K("pbf")
                    S.op("dve", lambda e, src=src: e.tensor_tensor(out=oh4, in0=src[:].rearrange("p (h k) -> p h k", h=8).unsqueeze(3).to_broadcast(B4),
                                                                   in1=iota16.unsqueeze(1).unsqueeze(1).to_broadcast(B4), op=ALU.is_equal), reads=[ksrc, "cst"], writes=[K("ps2")])
                    yield
                    S.op("dve", lambda e, q_=q_: e.tensor_tensor(out=oh4, in0=oh4, in1=sif4[:, :, q_, :].unsqueeze(2).to_broadcast(B4), op=ALU.mult), reads=[K("ps2"), K("psif")], writes=[K("ps2")])
                    yield
                    S.op("dve", lambda e, dst=dst: e.tensor_reduce(out=dst[:], in_=oh4, axis=AX.X, op=ALU.add), reads=[K("ps2")], writes=[kd])
                    yield
                S.op("dve", lambda e: e.scalar_tensor_tensor(out=i1[:], in0=i1[:], scalar=128.0, in1=i2[:], op0=ALU.mult, op1=ALU.add), reads=[K("pi1"), K("pi2")], writes=[K("pi1")])
                S.op("dve", lambda e: e.tensor_copy(out=idxi[:], in_=i1[:]), reads=[K("pi1")], writes=[K("pidxi")])
                yield
                ee3 = ee[:].rearrange("p (h k) -> p h k", h=8)
                S.op("dve", lambda e: e.tensor_tensor(out=ee3, in0=tv3, in1=tv3[:, :, 0:1].to_broadcast([128, 8, 16]), op=ALU.subtract), reads=[K("ptv")], writes=[K("pee")])
                S.op("act", lambda e: e.activation(out=ee[:], in_=ee[:], func=AF.Exp), reads=[K("pee")], writes=[K("pee")])
                S.op("dve", lambda e: e.tensor_reduce(out=ssum[:], in_=ee3, axis=AX.X, op=ALU.add), reads=[K("pee")], writes=[K("pssum")])
                yield
                S.op("dve", lambda e: e.reciprocal(out=ssum[:], in_=ssum[:]), reads=[K("pssum")], writes=[K("pssum")])
                S.op("dve", lambda e: e.tensor_tensor(out=gg[:].rearrange("p (h k) -> p h k", h=8), in0=ee3, in1=ssum[:].unsqueeze(2).to_broadcast([128, 8, 16]), op=ALU.mult),
                     reads=[K("pee"), K("pssum")], writes=[K("pgg")])
                yield

            def exhaust(g):
                if g is not None:
                    for _ in g:
                        pass

            def advance(g, n):
                if g is None:
                    return
                for _ in range(n):
                    try:
                        next(g)
                    except StopIteration:
                        return

            exhaust(prep(0))
            ui = 0
            for t in range(NT):
                p = t % 2
                K = lambda n: "%s%d" % (n, p)
                ts = slice(t * 128, (t + 1) * 128)
                h2t, xt, idxi, gg = h2ts[p], xts[p], idxis[p], ggs[p]
                nxt = prep(t + 1) if t + 1 < NT else None
                for r in range(128):
                    ub = ubs[ui % NB]; kub = "pub%d" % (ui % NB)
                    jk = junks[ui % 2]; kjk = "pjunk%d" % (ui % 2)
                    tb_ = tmpbs[ui % 3]; ktb = "ptmpb%d" % (ui % 3)
                    ui += 1
                    ka, kg, kga = "paa%d" % r, "pgl%d" % r, "pga%d" % r
                    S.dma("pool", lambda e, ub=ub, r=r, idxi=idxi: e.indirect_dma_start(out=ub[:], out_offset=None, in_=UV2[:, :], in_offset=bass.IndirectOffsetOnAxis(ap=idxi[:, r:r + 1], axis=0)),
                          reads=[K("pidxi"), "uvb"], writes=[kub])
                    S.op("dve", lambda e, ub=ub, r=r, h2t=h2t, jk=jk: e.scalar_tensor_tensor(out=jk[:], in0=ub[:, 0:D], scalar=1.0, in1=h2t[:], op0=ALU.mult, op1=ALU.mult, accum_out=aa[:, r:r + 1]),
                         reads=[kub, K("ph2t")], writes=[kjk, ka])
                    S.op("act", lambda e, r=r: e.activation(out=gl[:, r:r + 1], in_=aa[:, r:r + 1], func=AF.Gelu_apprx_tanh), reads=[ka], writes=[kg])
                    S.op("act", lambda e, r=r, gg=gg: e.activation(out=ga[:, r:r + 1], in_=gl[:, r:r + 1], func=AF.Identity, scale=gg[:, r:r + 1]), reads=[kg, K("pgg")], writes=[kga])
                    S.op("act", lambda e, ub=ub, r=r, tb_=tb_: e.activation(out=tb_[:], in_=ub[:, D:2 * D], func=AF.Identity, scale=ga[:, r:r + 1]), reads=[kub, kga], writes=[ktb])
                    for dh in range(2):
                        S.op("pe", lambda e, tb_=tb_, dh=dh, r=r: e.matmul(pacc[:, dh * 512:(dh + 1) * 512], lhsT=self.ident_b(), rhs=tb_[:, dh * 512:(dh + 1) * 512], start=(r == 0), stop=(r == 127)),
                             reads=["cstb", ktb], writes=["ppacc"])
                    advance(nxt, 1)
                for dh in range(2):
                    S.op("dve", lambda e, dh=dh: e.tensor_tensor(out=acc[:, dh * 512:(dh + 1) * 512], in0=pacc[:, dh * 512:(dh + 1) * 512], in1=g2bc[:, dh * 512:(dh + 1) * 512], op=ALU.mult),
                         reads=["ppacc", "g2bc"], writes=["pacc"])
                S.op("dve", lambda e, xt=xt: e.tensor_tensor(out=xt[:], in0=xt[:], in1=acc[:], op=ALU.add), reads=["pacc", K("pxt")], writes=[K("pxt")])
                S.dma("sp", lambda e, xt=xt, ts=ts: e.dma_start(out=self.xres[ts, :], in_=xt[:]), reads=[K("pxt")], writes=["xres"])
                exhaust(nxt)

    def stage_final(self):
        nc, S = self.nc, self.S
        with ExitStack() as es:
            sb = lambda n, sh, dt: es.enter_context(self.sbt(n, sh, dt))
            o, _ = SM_OFF["fg"]
            fgbc = self._bcast_tile(es, "fgbc", lambda c: self.sm[:, o + c:o + c + 1])
            xts = [sb("fxt%d" % i, [128, D], F32) for i in range(2)]
            sq = sb("fsq", [128, D], F32)
            ss = sb("fss", [128, 4], F32)
            for t in range(NT):
                xt = xts[t % 2]; kxt = "fxt%d" % (t % 2)
                S.dma("sp", lambda e, xt=xt, t=t: e.dma_start(out=xt[:], in_=self.xres[t * 128:(t + 1) * 128, :]), reads=["xres"], writes=[kxt])
                S.op("act", lambda e, xt=xt: e.activation(out=sq[:], in_=xt[:], func=AF.Square, accum_out=ss[:, 0:1]), reads=[kxt], writes=["fsq", "fss"])
                S.op("dve", lambda e: e.tensor_scalar(out=ss[:, 1:2], in0=ss[:, 0:1], scalar1=1.0 / D, scalar2=EPS, op0=ALU.mult, op1=ALU.add), reads=["fss"], writes=["fss"])
                S.op("act", lambda e: e.activation(out=ss[:, 2:3], in_=ss[:, 1:2], func=AF.Sqrt), reads=["fss"], writes=["fss"])
                S.op("dve", lambda e: e.reciprocal(out=ss[:, 3:4], in_=ss[:, 2:3]), reads=["fss"], writes=["fss"])
                S.op("dve", lambda e, xt=xt: e.scalar_tensor_tensor(out=xt[:], in0=xt[:], scalar=ss[:, 3:4], in1=fgbc[:], op0=ALU.mult, op1=ALU.mult), reads=[kxt, "fss", "fgbc"], writes=[kxt])
                S.dma("sp", lambda e, xt=xt, t=t: e.dma_start(out=self.out_d[t * 128:(t + 1) * 128, :], in_=xt[:]), reads=[kxt], writes=["out_d"])


def prep_inputs(inputs):
    inp = {k: np.ascontiguousarray(np.asarray(v)) for k, v in inputs.items()}
    consts = make_consts()
    keys = inp["pk_keys"]
    kbd = np.zeros((DEPTH, 8, 128, 256), np.float32)
    for p in range(2):
        kbd[:, :, p * 64:(p + 1) * 64, p * 128:(p + 1) * 128] = keys[:, :, p].transpose(0, 1, 3, 2)
    shared = dict(consts=consts, mod_w=inp["mod_w"], w_in=inp["w_in"], w_branch=inp["w_branch"], w_out=inp["w_out"],
                  pk_wq=inp["pk_wq"], keysbd=kbd,
                  pk_uv=np.concatenate([inp["pk_u"], inp["pk_v"]], axis=2))
    in_maps = []
    for b in range(8):
        m = dict(shared)
        m["x"] = inp["x"][b]
        m["small"] = make_small(inp, b)
        in_maps.append(m)
    return in_maps


_PROG_CACHE = {}


def kernel(**inputs):
    in_maps = prep_inputs(inputs)
    if "nc" not in _PROG_CACHE:
        _PROG_CACHE["nc"] = Prog().build()
    res = run_bass_kernel_spmd(_PROG_CACHE["nc"], in_maps, core_ids=list(range(8)))
    return np.stack([np.asarray(r["out"]) for r in res.results], axis=0).astype(np.float32)
```

```python
from contextlib import ExitStack
import numpy as np
import concourse.bass as bass
import concourse.mybir as mybir
from concourse.bass_utils import run_bass_kernel_spmd

F32 = mybir.dt.float32
BF16 = mybir.dt.bfloat16
U32 = mybir.dt.uint32
I32 = mybir.dt.int32
AF = mybir.ActivationFunctionType
ALU = mybir.AluOpType
AX = mybir.AxisListType

D = 1024
S_LEN = 2048
DEPTH = 4
NT = S_LEN // 128
INW = 8712
EPS = 1e-6
PEER_IMPLEMENTED = True


class Sched:
    ENG = ("pe", "act", "dve", "pool", "sp")

    def __init__(self, nc, dma_slots=None, same_engine_sync=True):
        self.nc = nc
        self.ops = {e: [] for e in self.ENG}
        self.count = {e: 0 for e in self.ENG}
        self.sem = {}
        self.last_w = {}
        self.readers = {}
        self.same_engine_sync = same_engine_sync
        self.dma_slots_n = dma_slots or {"sp": 8, "pool": 12, "act": 4}
        self.dma_sems = {}
        self.dma_rr = {}
        self.seen = {e: {} for e in self.ENG}
        self._ctx = []
        self.n_ops = 0

    def open(self):
        nc = self.nc
        for e in self.ENG:
            cm = nc.semaphore("s_" + e)
            self.sem[e] = cm.__enter__()
            self._ctx.append(cm)
        for q, n in self.dma_slots_n.items():
            self.dma_sems[q] = []
            self.dma_rr[q] = 0
            for i in range(n):
                cm = nc.semaphore("sd_%s%d" % (q, i))
                s = cm.__enter__()
                self._ctx.append(cm)
                self.dma_sems[q].append(dict(sem=s, count=0, name="d_%s%d" % (q, i)))

    def close(self):
        for cm in reversed(self._ctx):
            cm.__exit__(None, None, None)

    def _tok_wait(self, tok):
        if tok[0] == "c":
            return (self.sem[tok[1]], tok[1], tok[2])
        d = self.dma_sems[tok[1]][tok[2]]
        return (d["sem"], d["name"], tok[3])

    def _collect(self, eng, reads, writes):
        toks = []
        for k in reads:
            t = self.last_w.get(k)
            if t is not None:
                toks.append(t)
        for k in writes:
            t = self.last_w.get(k)
            if t is not None:
                toks.append(t)
            toks.extend(self.readers.get(k, ()))
        waits = {}
        for t in toks:
            if t[0] == "c" and t[1] == eng and not self.same_engine_sync:
                continue
            s, name, v = self._tok_wait(t)
            if self.seen[eng].get(name, 0) >= v:
                continue
            if name not in waits or waits[name][1] < v:
                waits[name] = (s, v)
        for name, (s, v) in waits.items():
            self.seen[eng][name] = v
        return list(waits.values())

    def _commit(self, tok, reads, writes):
        for k in reads:
            self.readers.setdefault(k, []).append(tok)
        for k in writes:
            self.last_w[k] = tok
            self.readers[k] = []

    def op(self, eng, fn, reads=(), writes=()):
        waits = self._collect(eng, reads, writes)
        self.count[eng] += 1
        tok = ("c", eng, self.count[eng])
        self.ops[eng].append((fn, waits, "c", None))
        self._commit(tok, reads, writes)
        self.n_ops += 1
        return tok

    def dma(self, eng, fn, reads=(), writes=()):
        slot = self.dma_rr[eng]
        self.dma_rr[eng] = (slot + 1) % len(self.dma_sems[eng])
        d = self.dma_sems[eng][slot]
        waits = self._collect(eng, reads, writes)
        if d["count"] > 0 and self.seen[eng].get(d["name"], 0) < d["count"]:
            waits.append((d["sem"], d["count"]))
            self.seen[eng][d["name"]] = d["count"]
        d["count"] += 16
        tok = ("d", eng, slot, d["count"])
        self.ops[eng].append((fn, waits, "d", d["sem"]))
        self._commit(tok, reads, writes)
        self.n_ops += 1
        return tok

    def barrier(self):
        for e in self.ENG:
            waits = []
            for e2 in self.ENG:
                v = self.count[e2]
                if v > 0 and self.seen[e].get(e2, 0) < v and e2 != e:
                    waits.append((self.sem[e2], v))
                    self.seen[e][e2] = v
            for q in self.dma_sems:
                for d in self.dma_sems[q]:
                    if d["count"] > 0 and self.seen[e].get(d["name"], 0) < d["count"]:
                        waits.append((d["sem"], d["count"]))
                        self.seen[e][d["name"]] = d["count"]
            if self.count[e] > 0:
                waits.append((self.sem[e], self.count[e]))
                self.seen[e][e] = self.count[e]
            self.ops[e].append((None, waits, "w", None))

    def emit(self):
        nc = self.nc
        sched = self
        ops = self.ops
        self.ops = {e: [] for e in self.ENG}
        with nc.Block() as block:
            def run(engname, e):
                for fn, waits, kind, dsem in ops[engname]:
                    for s, v in waits:
                        e.wait_ge(s, v)
                    if fn is None:
                        continue
                    ins = fn(e)
                    if kind == "c":
                        ins.then_inc(sched.sem[engname], 1)
                    else:
                        ins.then_inc(dsem, 16)

            @block.sync
            def _(e):
                run("sp", e)

            @block.tensor
            def _(e):
                run("pe", e)

            @block.scalar
            def _(e):
                run("act", e)

            @block.vector
            def _(e):
                run("dve", e)

            @block.gpsimd
            def _(e):
                run("pool", e)


def _small_layout():
    off = {}
    o = 0
    for name, n in (("mod_b", DEPTH * 48), ("nmg", DEPTH * 8), ("nfg", DEPTH * 8),
                    ("convw", DEPTH * 32), ("convb", DEPTH * 8), ("gateb", DEPTH * 2),
                    ("lbl", DEPTH * 4), ("hgn", DEPTH * 4), ("mln", DEPTH * 4), ("fg", 8), ("cT", 8)):
        off[name] = (o, n)
        o += n
    return off, o


SM_OFF, NS = _small_layout()
C_ID, C_ONES, C_TRI, C_M64, C_SEL, C_IOTA, C_THR = 0, 128, 256, 384, 448, 960, 976
NCONST = 992


def make_consts():
    c = np.zeros((128, NCONST), np.float32)
    c[:, C_ID:C_ID + 128] = np.eye(128, dtype=np.float32)
    c[:, C_ONES:C_ONES + 128] = 1.0
    sp = np.arange(128)[:, None]
    s = np.arange(128)[None, :]
    c[:, C_TRI:C_TRI + 128] = (sp >= s).astype(np.float32)
    t = np.arange(64)[None, :]
    c[:, C_M64:C_M64 + 64] = (t >= (sp % 64)).astype(np.float32)
    for h in range(4):
        c[h, C_SEL + h * 128:C_SEL + (h + 1) * 128] = 1.0
    c[:, C_IOTA:C_IOTA + 16] = np.arange(16, dtype=np.float32)[None, :]
    c[:, C_THR:C_THR + 15] = 16.0 * np.arange(1, 16, dtype=np.float32)[None, :]
    return c


def make_small(inp, b):
    sm = np.zeros((128, NS), np.float32)

    def put(name, arr):
        o, n = SM_OFF[name]
        arr = np.asarray(arr, np.float32).reshape(128, n)
        sm[:, o:o + n] = arr

    put("mod_b", inp["mod_b"].reshape(DEPTH, 48, 128).transpose(2, 0, 1))
    put("nmg", inp["norm_mix_g"].reshape(DEPTH, 8, 128).transpose(2, 0, 1))
    put("nfg", inp["norm_ffn_g"].reshape(DEPTH, 8, 128).transpose(2, 0, 1))
    put("convw", inp["ml_conv_w"].reshape(DEPTH, 4, 8, 128).transpose(3, 0, 1, 2))
    put("convb", inp["ml_conv_b"].reshape(DEPTH, 8, 128).transpose(2, 0, 1))
    gb = np.zeros((128, DEPTH, 2), np.float32)
    gb[0:4, :, 0] = inp["ml_gate_b"][:, 0:4].T
    gb[0:4, :, 1] = inp["ml_gate_b"][:, 4:8].T
    put("gateb", gb)
    put("lbl", inp["hg_lb_logits"].reshape(DEPTH, 4, 128).transpose(2, 0, 1))
    put("hgn", inp["hg_norm_g"].reshape(DEPTH, 4, 128).transpose(2, 0, 1))
    put("mln", inp["ml_norm_g"].reshape(DEPTH, 4, 128).transpose(2, 0, 1))
    put("fg", inp["final_g"].reshape(8, 128).T)
    put("cT", inp["c"][b].reshape(8, 128).T)
    return sm


class Prog:
    def __init__(self, n_layers=DEPTH, stages=None, debug=()):
        self.n_layers = n_layers
        self.stages = stages
        self.debug = set(debug)
        self.nc = bass.Bass("TRN2", target_bir_lowering=False)
        self.S = Sched(self.nc)
        self.uid = 0

    def sbt(self, name, shape, dt):
        self.uid += 1
        return self.nc.sbuf_tensor("%s_u%d" % (name, self.uid), shape, dt)

    def pst(self, name, shape, dt):
        self.uid += 1
        return self.nc.psum_tensor("%s_u%d" % (name, self.uid), shape, dt)

    def want(self, st):
        return self.stages is None or st in self.stages

    def dram(self, name, shape, dt, kind=None):
        if kind is None:
            kind = "ExternalOutput" if name in self.debug else "Internal"
        return self.nc.dram_tensor(name, list(shape), dt, kind=kind).ap()

    def build(self):
        nc, S = self.nc, self.S
        L = self.n_layers
        ext = lambda n, s, dt=F32: nc.dram_tensor(n, list(s), dt, kind="ExternalInput").ap()
        self.x_in = ext("x", [S_LEN, D])
        self.small_d = ext("small", [128, NS])
        self.consts_d = ext("consts", [128, NCONST])
        self.mod_w = ext("mod_w", [L, D, 6 * D])
        self.w_in = ext("w_in", [L, D, INW])
        self.input_names = ["x", "small", "consts", "mod_w", "w_in"]
        if self.want("merge"):
            self.w_branch = ext("w_branch", [L, 3, 512, D])
            self.w_out = ext("w_out", [L, D, D])
            self.input_names += ["w_branch", "w_out"]
        if self.want("peer") and PEER_IMPLEMENTED:
            self.pk_wq = ext("pk_wq", [L, D, D])
            self.keysbd = ext("keysbd", [L, 8, 128, 256])
            self.pk_uv = ext("pk_uv", [L, 16384, 2 * D])
            self.input_names += ["pk_wq", "keysbd", "pk_uv"]
        self.out_d = nc.dram_tensor("out", [S_LEN, D], F32, kind="ExternalOutput").ap()
        self.xres = self.dram("xres", [S_LEN, D], F32)
        self.hT_d = self.dram("hT", [8, 128, S_LEN], BF16)
        self.qT_d = self.dram("qT", [512, S_LEN], BF16)
        self.kT_d = self.dram("kT", [512, S_LEN], BF16)
        self.v_d = self.dram("v_tok", [S_LEN, 512], BF16)
        self.hqT_d = self.dram("hqT", [512, S_LEN], F32)
        self.hfT_d = self.dram("hfT", [512, S_LEN], F32)
        self.hgT_d = self.dram("hgT", [512, S_LEN], F32)
        self.hi_d = self.dram("hi_tok", [S_LEN, 512], BF16)
        self.mqkT_d = self.dram("mqkT", [1024, S_LEN], F32)
        self.mv_d = self.dram("mv_tok", [S_LEN, 512], BF16)
        self.moT_d = self.dram("moT", [512, S_LEN], F32)
        self.gT_d = self.dram("gT", [2, 4, S_LEN], F32)
        self.bgT_d = self.dram("bgT", [3072, S_LEN], F32)
        self.yT_d = self.dram("yT", [3, 512, S_LEN], BF16)
        self.h2_d = self.dram("h2_tok", [S_LEN, D], F32)
        self.uvb = self.dram("uvb", [16384, 2 * D], BF16)
        S.open()
        with (self.sbt("consts", [128, NCONST], F32) as cst,
              self.sbt("constb", [128, 384], BF16) as cstb,
              self.sbt("small", [128, NS], F32) as sm,
              self.sbt("modT", [128, DEPTH * 48], F32) as modT,
              self.sbt("lbT", [128, DEPTH * 4], F32) as lbT):
            self.cst, self.cstb, self.sm, self.modT, self.lbT = cst, cstb, sm, modT, lbT
            S.dma("sp", lambda e: e.dma_start(out=cst[:], in_=self.consts_d[:, :]), writes=["cst"])
            S.dma("sp", lambda e: e.dma_start(out=sm[:], in_=self.small_d[:, :]), writes=["sm"])
            S.op("dve", lambda e: e.tensor_copy(out=cstb[:], in_=cst[:, 0:384]), reads=["cst"], writes=["cstb"])
            S.dma("sp", lambda e: e.dma_start(out=self.xres[:, :], in_=self.x_in[:, :]), writes=["xres"])
            self.stage_mod()
            S.barrier(); S.emit()
            for l in range(L):
                if self.want("norm1"):
                    self.stage_norm(l, 0)
                    S.barrier(); S.emit()
                if self.want("proj"):
                    with ExitStack() as es2:
                        if self.want("peer") and PEER_IMPLEMENTED:
                            self._uvc_bufs = [es2.enter_context(self.sbt("uvc%d" % i, [128, 4, 2 * D], BF16)) for i in range(3)]
                        self.stage_proj(l)
                        S.barrier(); S.emit()
                if self.want("attn"):
                    self.stage_attn(l)
                    S.barrier(); S.emit()
                if self.want("hgrn"):
                    self.stage_hgrn(l)
                    S.barrier(); S.emit()
                if self.want("mlstm"):
                    self.stage_mlstm(l)
                    S.barrier(); S.emit()
                if self.want("merge"):
                    self.stage_merge(l)
                    S.barrier(); S.emit()
                if self.want("peer") and PEER_IMPLEMENTED:
                    self.stage_norm(l, 1)
                    S.barrier(); S.emit()
                    self.stage_peer(l)
                    S.barrier(); S.emit()
            self.stage_final()
            S.barrier(); S.emit()
        S.close()
        return nc

    def ident_f(self):
        return self.cst[:, C_ID:C_ID + 128]

    def ones_f(self):
        return self.cst[:, C_ONES:C_ONES + 128]

    def tri_f(self):
        return self.cst[:, C_TRI:C_TRI + 128]

    def ident_b(self):
        return self.cstb[:, 0:128]

    def ones_b(self):
        return self.cstb[:, 128:256]

    def smc(self, name, l, j, n=1):
        o, tot = SM_OFF[name]
        per = tot // DEPTH
        return self.sm[:, o + l * per + j: o + l * per + j + n]

    def modc(self, l, part, c):
        j = l * 48 + part * 8 + c
        return self.modT[:, j:j + 1]

    def stage_mod(self):
        nc, S = self.nc, self.S
        L = self.n_layers
        sm = self.sm
        with (self.sbt("condT", [128, 8], F32) as condT,
              self.sbt("mw0", [128, 8, 768], F32) as mw0,
              self.sbt("mw1", [128, 8, 768], F32) as mw1,
              self.sbt("lbe", [128, DEPTH * 4], F32) as lbe,
              self.sbt("lbs", [128, 4], F32) as lbs,
              self.sbt("lbm", [128, 4], F32) as lbm,
              self.pst("ps_mod", [128, DEPTH * 48], F32) as psm):
            o, _ = SM_OFF["cT"]
            S.op("act", lambda e: e.activation(out=condT[:], in_=sm[:, o:o + 8], func=AF.Silu), reads=["sm"], writes=["condT"])
            mws = [mw0, mw1]
            gi = 0
            for l in range(L):
                for g in range(8):
                    mw = mws[gi % 2]
                    key = "mw%d" % (gi % 2)
                    gi += 1
                    src = self.mod_w[l, :, g * 768:(g + 1) * 768].rearrange("(kc p) c -> p kc c", p=128)
                    S.dma("sp", lambda e, mw=mw, src=src: e.dma_start(out=mw[:], in_=src), writes=[key])
                    for cc in range(6):
                        j = l * 48 + g * 6 + cc
                        for kc in range(8):
                            S.op("pe", lambda e, mw=mw, cc=cc, kc=kc, j=j: e.matmul(
                                psm[:, j:j + 1], lhsT=mw[:, kc, cc * 128:(cc + 1) * 128], rhs=condT[:, kc:kc + 1],
                                start=(kc == 0), stop=(kc == 7)), reads=[key, "condT"], writes=["psm"])
            ob, _ = SM_OFF["mod_b"]
            S.op("dve", lambda e: e.tensor_tensor(out=self.modT[:, 0:L * 48], in0=psm[:, 0:L * 48], in1=sm[:, ob:ob + L * 48], op=ALU.add),
                 reads=["psm", "sm"], writes=["modT"])
            ol, _ = SM_OFF["lbl"]
            lg = lambda l: sm[:, ol + l * 4: ol + l * 4 + 4]
            S.op("dve", lambda e: e.tensor_tensor(out=lbm[:], in0=lg(0), in1=lg(1), op=ALU.max), reads=["sm"], writes=["lbm"])
            for l in (2, 3):
                S.op("dve", lambda e, l=l: e.tensor_tensor(out=lbm[:], in0=lbm[:], in1=lg(l), op=ALU.max), reads=["sm", "lbm"], writes=["lbm"])
            for l in range(DEPTH):
                S.op("dve", lambda e, l=l: e.tensor_tensor(out=lbe[:, l * 4:l * 4 + 4], in0=lg(l), in1=lbm[:], op=ALU.subtract), reads=["sm", "lbm"], writes=["lbe"])
            S.op("act", lambda e: e.activation(out=lbe[:], in_=lbe[:], func=AF.Exp), reads=["lbe"], writes=["lbe"])
            S.op("dve", lambda e: e.tensor_tensor(out=lbs[:], in0=lbe[:, 0:4], in1=lbe[:, 4:8], op=ALU.add), reads=["lbe"], writes=["lbs"])
            for l in (2, 3):
                S.op("dve", lambda e, l=l: e.tensor_tensor(out=lbs[:], in0=lbs[:], in1=lbe[:, l * 4:l * 4 + 4], op=ALU.add), reads=["lbe", "lbs"], writes=["lbs"])
            S.op("dve", lambda e: e.reciprocal(out=lbs[:], in_=lbs[:]), reads=["lbs"], writes=["lbs"])
            lbT = self.lbT
            S.op("dve", lambda e: e.memset(lbT[:, 0:4], 0.0), writes=["lbT"])
            for l in range(1, DEPTH):
                S.op("dve", lambda e, l=l: e.tensor_tensor(out=lbe[:, l * 4:l * 4 + 4], in0=lbe[:, l * 4:l * 4 + 4], in1=lbs[:], op=ALU.mult), reads=["lbe", "lbs"], writes=["lbe"])
                S.op("dve", lambda e, l=l: e.tensor_tensor(out=lbT[:, l * 4:l * 4 + 4], in0=lbT[:, (l - 1) * 4:l * 4], in1=lbe[:, l * 4:l * 4 + 4], op=ALU.add), reads=["lbe", "lbT"], writes=["lbT"])

    def stage_norm(self, l, which):
        nc, S = self.nc, self.S
        gname = "nmg" if which == 0 else "nfg"
        p_shift, p_scale = (0, 1) if which == 0 else (3, 4)
        with (self.sbt("nx0", [128, D], F32) as nx0, self.sbt("nx1", [128, D], F32) as nx1,
              self.sbt("nsq", [128, 2, D], BF16) as nsq,
              self.sbt("nb0", [128, D], BF16) as nb0, self.sbt("nb1", [128, D], BF16) as nb1,
              self.sbt("nh0", [128, 8, 128], BF16) as nh0, self.sbt("nh1", [128, 8, 128], BF16) as nh1,
              self.sbt("nt0", [128, D], F32) as nt0, self.sbt("nt1", [128, D], F32) as nt1,
              self.sbt("nss", [128, 8], F32) as nss_all,
              self.sbt("nG", [128, 8], F32) as nG,
              self.pst("nps0", [128, 8, 128], BF16) as nps0, self.pst("nps1", [128, 8, 128], BF16) as nps1,
              self.pst("npt0", [128, 8, 128], F32) as npt0):
            j0 = l * 48 + p_scale * 8
            S.op("dve", lambda e: e.scalar_tensor_tensor(out=nG[:], in0=self.modT[:, j0:j0 + 8], scalar=1.0, in1=self.smc(gname, l, 0, 8),
                                                         op0=ALU.add, op1=ALU.mult), reads=["modT", "sm"], writes=["nG"])
            nx, nb, nh, nps, nt = [nx0, nx1], [nb0, nb1], [nh0, nh1], [nps0, nps1], [nt0, nt1]
            for t in range(NT):
                i = t % 2
                kx, kb, kh, kp, kt = "nx%d" % i, "nb%d" % i, "nh%d" % i, "nps%d" % i, "nt%d" % i
                nss = nss_all[:, 4 * i:4 * i + 4]; kss = "nss%d" % i; ksq = "nsq%d" % i
                S.dma("sp", lambda e, i=i, t=t: e.dma_start(out=nx[i][:], in_=self.xres[t * 128:(t + 1) * 128, :]), reads=["xres"], writes=[kx])
                S.op("act", lambda e, i=i, nss=nss: e.activation(out=nsq[:, i, :], in_=nx[i][:], func=AF.Square, accum_out=nss[:, 0:1]), reads=[kx], writes=[ksq, kss])
                S.op("dve", lambda e, nss=nss: e.tensor_scalar(out=nss[:, 1:2], in0=nss[:, 0:1], scalar1=1.0 / D, scalar2=EPS, op0=ALU.mult, op1=ALU.add), reads=[kss], writes=[kss])
                S.op("act", lambda e, nss=nss: e.activation(out=nss[:, 2:3], in_=nss[:, 1:2], func=AF.Sqrt), reads=[kss], writes=[kss])
                S.op("dve", lambda e, nss=nss: e.reciprocal(out=nss[:, 3:4], in_=nss[:, 2:3]), reads=[kss], writes=[kss])
                S.op("dve", lambda e, i=i, nss=nss: e.tensor_scalar(out=nb[i][:], in0=nx[i][:], scalar1=nss[:, 3:4], scalar2=None, op0=ALU.mult), reads=[kx, kss], writes=[kb])
                for c in range(8):
                    S.op("pe", lambda e, i=i, c=c: e.transpose(nps[i][:, c, :], nb[i][:, c * 128:(c + 1) * 128], self.ident_b()), reads=[kb, "cstb"], writes=[kp])
                for c in range(8):
                    S.op("act", lambda e, i=i, c=c: e.activation(out=nh[i][:, c, :], in_=nps[i][:, c, :], func=AF.Identity,
                                                                 scale=nG[:, c:c + 1], bias=self.modc(l, p_shift, c)), reads=[kp, "nG", "modT"], writes=[kh])
                S.dma("sp", lambda e, i=i, t=t: e.dma_start(out=self.hT_d[:, :, t * 128:(t + 1) * 128].rearrange("c p t -> p c t"), in_=nh[i][:]), reads=[kh], writes=["hT_d"])
                if which == 1:
                    pass
            if which == 1:
                self._h2_tokmajor(l, nx, nss_all[:, 0:4], nG, nt, npt0, p_shift)

    def _h2_tokmajor(self, l, nx, nss, nG, nt, npt0, p_shift):
        nc, S = self.nc, self.S
        with (self.sbt("dg", [128, 128], F32) as dg,
              self.sbt("Gbc", [128, D], F32) as Gbc, self.sbt("Sbc", [128, D], F32) as Sbc):
            for which_v, dst in ((0, Gbc), (1, Sbc)):
                for c in range(8):
                    col = nG[:, c:c + 1] if which_v == 0 else self.modc(l, p_shift, c)
                    S.op("dve", lambda e, col=col: e.tensor_scalar(out=dg[:], in0=self.ident_f(), scalar1=col, scalar2=None, op0=ALU.mult), reads=["cst", "nG", "modT"], writes=["dg"])
                    S.op("pe", lambda e, c=c: e.matmul(npt0[:, c, :], lhsT=self.ones_f(), rhs=dg[:], start=True, stop=True), reads=["cst", "dg"], writes=["npt0"])
                S.op("act", lambda e, dst=dst: e.activation(out=dst[:], in_=npt0[:].rearrange("p c t -> p (c t)"), func=AF.Copy), reads=["npt0"], writes=["bc%d" % which_v])
            for t in range(NT):
                i = t % 2
                kx, kt = "nx%d" % i, "nt%d" % i
                S.dma("sp", lambda e, i=i, t=t: e.dma_start(out=nx[i][:], in_=self.xres[t * 128:(t + 1) * 128, :]), reads=["xres"], writes=[kx])
                S.op("act", lambda e, i=i: e.activation(out=nt[i][:], in_=nx[i][:], func=AF.Square, accum_out=nss[:, 0:1]), reads=[kx], writes=[kt, "nss0"])
                S.op("dve", lambda e: e.tensor_scalar(out=nss[:, 1:2], in0=nss[:, 0:1], scalar1=1.0 / D, scalar2=EPS, op0=ALU.mult, op1=ALU.add), reads=["nss0"], writes=["nss0"])
                S.op("act", lambda e: e.activation(out=nss[:, 2:3], in_=nss[:, 1:2], func=AF.Sqrt), reads=["nss0"], writes=["nss0"])
                S.op("dve", lambda e: e.reciprocal(out=nss[:, 3:4], in_=nss[:, 2:3]), reads=["nss0"], writes=["nss0"])
                S.op("dve", lambda e, i=i: e.scalar_tensor_tensor(out=nt[i][:], in0=nx[i][:], scalar=nss[:, 3:4], in1=Gbc[:], op0=ALU.mult, op1=ALU.mult), reads=[kx, "nss0", "bc0"], writes=[kt])
                S.op("dve", lambda e, i=i: e.tensor_tensor(out=nt[i][:], in0=nt[i][:], in1=Sbc[:], op=ALU.add), reads=[kt, "bc1"], writes=[kt])
                S.dma("sp", lambda e, i=i, t=t: e.dma_start(out=self.h2_d[t * 128:(t + 1) * 128, :], in_=nt[i][:]), reads=[kt], writes=["h2_d"])

    def stage_proj(self, l):
        nc, S = self.nc, self.S
        fm = []
        for c in range(4):
            fm.append((0 + c * 128, 128, self.qT_d[c * 128:(c + 1) * 128, :], AF.Copy, 0.125, BF16))
        for c in range(4):
            fm.append((512 + c * 128, 128, self.kT_d[c * 128:(c + 1) * 128, :], AF.Copy, 1.0, BF16))
        for c in range(4):
            fm.append((1536 + c * 128, 128, self.hqT_d[c * 128:(c + 1) * 128, :], AF.Copy, 1.0, F32))
        for c in range(4):
            fm.append((2048 + c * 128, 128, self.hfT_d[c * 128:(c + 1) * 128, :], AF.Copy, 1.0, F32))
        for c in range(4):
            fm.append((3072 + c * 128, 128, self.hgT_d[c * 128:(c + 1) * 128, :], AF.Silu, 1.0, F32))
        for c in range(8):
            fm.append((3584 + c * 128, 128, self.mqkT_d[c * 128:(c + 1) * 128, :], AF.Copy, 1.0, F32))
        for c in range(4):
            fm.append((5120 + c * 128, 128, self.moT_d[c * 128:(c + 1) * 128, :], AF.Sigmoid, 1.0, F32))
        fm.append((5632, 4, self.gT_d[0, :, :], AF.Copy, 1.0, F32))
        fm.append((5636, 4, self.gT_d[1, :, :], AF.Copy, 1.0, F32))
        for c in range(24):
            fm.append((5640 + c * 128, 128, self.bgT_d[c * 128:(c + 1) * 128, :], AF.Sigmoid, 1.0, F32))
        tm = [(1024, self.v_d, AF.Copy), (2560, self.hi_d, AF.Silu), (4608, self.mv_d, AF.Copy)]
        with (self.sbt("hT", [128, 8, S_LEN], BF16) as hT,
              self.sbt("pw0", [128, 8, 512], BF16) as pw0, self.sbt("pw1", [128, 8, 512], BF16) as pw1,
              self.sbt("pof0", [128, S_LEN], F32) as pof0, self.sbt("pof1", [128, S_LEN], F32) as pof1,
              self.sbt("pob0", [128, S_LEN], BF16) as pob0, self.sbt("pob1", [128, S_LEN], BF16) as pob1,
              self.sbt("pot0", [128, 512], BF16) as pot0, self.sbt("pot1", [128, 512], BF16) as pot1,
              self.pst("pp0", [128, 512], F32) as pp0, self.pst("pp1", [128, 512], F32) as pp1,
              self.pst("pp2", [128, 512], F32) as pp2, self.pst("pp3", [128, 512], F32) as pp3):
            for c in range(8):
                S.dma("sp", lambda e, c=c: e.dma_start(out=hT[:, c, :], in_=self.hT_d[c, :, :]), reads=["hT_d"], writes=["hT"])
            pw, pof, pob, pot, pp = [pw0, pw1], [pof0, pof1], [pob0, pob1], [pot0, pot1], [pp0, pp1, pp2, pp3]
            side = self.uvcast_gen(l, self._uvc_bufs) if (self.want("peer") and PEER_IMPLEMENTED) else None
            wi = 0
            pi = 0
            for ji, (col0, ncol, dest, func, scale, dt) in enumerate(fm):
                w = pw[wi % 2]; kw = "pw%d" % (wi % 2); wi += 1
                src = self.w_in[l, :, col0:col0 + ncol].rearrange("(kc p) c -> p kc c", p=128)
                S.dma("pool", lambda e, w=w, src=src, ncol=ncol: e.dma_start(out=w[:, :, 0:ncol], in_=src), writes=[kw])
                ob = (pof if dt == F32 else pob)[ji % 2]
                ko = ("pof%d" if dt == F32 else "pob%d") % (ji % 2)
                for tb in range(4):
                    ps = pp[pi % 4]; kp = "pp%d" % (pi % 4); pi += 1
                    for kc in range(8):
                        S.op("pe", lambda e, ps=ps, w=w, kc=kc, tb=tb, ncol=ncol: e.matmul(
                            ps[0:ncol, :], lhsT=w[:, kc, 0:ncol], rhs=hT[:, kc, tb * 512:(tb + 1) * 512],
                            start=(kc == 0), stop=(kc == 7)), reads=[kw, "hT"], writes=[kp])
                    S.op("act", lambda e, ps=ps, ob=ob, tb=tb, ncol=ncol, func=func, scale=scale: e.activation(
                        out=ob[0:ncol, tb * 512:(tb + 1) * 512], in_=ps[0:ncol, :], func=func, scale=scale), reads=[kp], writes=[ko])
                S.dma("sp", lambda e, ob=ob, dest=dest, ncol=ncol: e.dma_start(out=dest, in_=ob[0:ncol, :]), reads=[ko], writes=["projout"])
                if side is not None:
                    next(side, None)
            for (col0, dest, func) in tm:
                w = pw[wi % 2]; kw = "pw%d" % (wi % 2); wi += 1
                src = self.w_in[l, :, col0:col0 + 512].rearrange("(kc p) c -> p kc c", p=128)
                S.dma("pool", lambda e, w=w, src=src: e.dma_start(out=w[:], in_=src), writes=[kw])
                for t in range(NT):
                    ps = pp[pi % 4]; kp = "pp%d" % (pi % 4); pi += 1
                    for kc in range(8):
                        S.op("pe", lambda e, ps=ps, w=w, kc=kc, t=t: e.matmul(
                            ps[:], lhsT=hT[:, kc, t * 128:(t + 1) * 128], rhs=w[:, kc, :],
                            start=(kc == 0), stop=(kc == 7)), reads=[kw, "hT"], writes=[kp])
                    ot = pot[t % 2]; kt = "pot%d" % (t % 2)
                    S.op("act", lambda e, ps=ps, ot=ot, func=func: e.activation(out=ot[:], in_=ps[:], func=func), reads=[kp], writes=[kt])
                    S.dma("sp", lambda e, ot=ot, dest=dest, t=t: e.dma_start(out=dest[t * 128:(t + 1) * 128, :], in_=ot[:]), reads=[kt], writes=["projout"])
            if side is not None:
                for _ in side:
                    pass

    def stage_attn(self, l):
        nc, S = self.nc, self.S
        NBUF = 3
        with ExitStack() as es:
            sb = lambda n, sh, dt: es.enter_context(self.sbt(n, sh, dt))
            pt = lambda n, sh, dt: es.enter_context(self.pst(n, sh, dt))
            aq = [sb("aq%d" % i, [64, S_LEN], BF16) for i in range(2)]
            ak = [sb("ak%d" % i, [64, S_LEN], BF16) for i in range(2)]
            av = [sb("av%d" % i, [128, NT, 64], BF16) for i in range(2)]
            azs = [sb("azs%d" % i, [128, 512], F32) for i in range(NBUF)]
            asp = [sb("asp%d" % i, [128, 512], F32) for i in range(NBUF)]
            asb = [sb("asb%d" % i, [128, 512], BF16) for i in range(NBUF)]
            alw = [sb("alw%d" % i, [128, 512], F32) for i in range(NBUF)]
            awt = [sb("awt%d" % i, [128, 512], BF16) for i in range(NBUF)]
            ayo = [sb("ayo%d" % i, [64, 512], BF16) for i in range(2)]
            apz = [pt("apz%d" % i, [128, 512], F32) for i in range(2)]
            apc = [pt("apc%d" % i, [128, 512], F32) for i in range(2)]
            apy = [pt("apy%d" % i, [64, 512], F32) for i in range(2)]
            apr = [pt("apr%d" % i, [128, 512], F32) for i in range(2)]
            tri_b = self.cstb[:, 256:384]
            steps = []
            yi = 0
            for h in range(8):
                for qb in range(4):
                    nkb = 4 * (qb + 1)
                    for jn, j in enumerate(reversed(range(nkb))):
                        steps.append(dict(h=h, qb=qb, nkb=nkb, jn=jn, j=j, yi=yi, i=len(steps)))
                    yi += 1
            loaded = set()

            def load_head(h):
                if h in loaded or h >= 8:
                    return
                loaded.add(h)
                hb = h % 2
                S.dma("sp", lambda e: e.dma_start(out=aq[hb][:], in_=self.qT_d[h * 64:(h + 1) * 64, :]), reads=["projout"], writes=["aq%d" % hb])
                S.dma("sp", lambda e: e.dma_start(out=ak[hb][:], in_=self.kT_d[h * 64:(h + 1) * 64, :]), reads=["projout"], writes=["ak%d" % hb])
                S.dma("sp", lambda e: e.dma_start(out=av[hb][:], in_=self.v_d[:, h * 64:(h + 1) * 64].rearrange("(j p) d -> p j d", p=128)), reads=["projout"], writes=["av%d" % hb])

            def phaseA(st):
                h, qb, j, i = st["h"], st["qb"], st["j"], st["i"]
                load_head(h)
                hb = h % 2
                q, k = aq[hb], ak[hb]
                b2, b3 = i % 2, i % NBUF
                pz, zs, sp, sbb = apz[b2], azs[b3], asp[b3], asb[b3]
                kz, kzs, ksp, ksb = "apz%d" % b2, "azs%d" % b3, "asp%d" % b3, "asb%d" % b3
                diag = j >= 4 * qb
                base = qb * 512 - j * 128
                S.op("pe", lambda e: e.matmul(pz[:], lhsT=k[:, j * 128:(j + 1) * 128], rhs=q[:, qb * 512:(qb + 1) * 512], start=True, stop=True), reads=["aq%d" % hb, "ak%d" % hb], writes=[kz])
                S.op("dve", lambda e: e.tensor_copy(out=zs[:], in_=pz[:]), reads=[kz], writes=[kzs])
                S.op("act", lambda e: e.activation(out=sp[:], in_=zs[:], func=AF.Exp), reads=[kzs], writes=[ksp])
                S.op("act", lambda e: e.activation(out=sbb[:], in_=sp[:], func=AF.Ln, bias=1.0), reads=[ksp], writes=[ksb])
                if diag:
                    S.op("pool", lambda e: e.affine_select(out=sbb[:], in_=sbb[:], pattern=[[1, 512]], compare_op=ALU.is_gt, fill=0.0, base=base, channel_multiplier=-1), reads=[ksb], writes=[ksb])

            def phaseB(st):
                qb, j, jn, i = st["qb"], st["j"], st["jn"], st["i"]
                b2, b3 = i % 2, i % NBUF
                pc, zs, sbb, lw, wt = apc[b2], azs[b3], asb[b3], alw[b3], awt[b3]
                kc_, kzs, ksb, klw, kwt = "apc%d" % b2, "azs%d" % b3, "asb%d" % b3, "alw%d" % b3, "awt%d" % b3
                pr = apr[st["yi"] % 2]; kpr = "apr%d" % (st["yi"] % 2)
                diag = j >= 4 * qb
                base = qb * 512 - j * 128
                S.op("pe", lambda e: e.matmul(pc[:], lhsT=tri_b, rhs=sbb[:], start=True, stop=True), reads=["cstb", ksb], writes=[kc_])
                S.op("dve", lambda e: e.tensor_tensor(out=lw[:], in0=zs[:], in1=pc[:], op=ALU.subtract), reads=[kzs, kc_], writes=[klw])
                if jn > 0:
                    S.op("dve", lambda e: e.tensor_tensor(out=lw[:], in0=lw[:], in1=pr[:], op=ALU.subtract), reads=[klw, kpr], writes=[klw])
                S.op("act", lambda e: e.activation(out=wt[:], in_=lw[:], func=AF.Exp), reads=[klw], writes=[kwt])
                if diag:
                    S.op("pool", lambda e: e.affine_select(out=wt[:], in_=wt[:], pattern=[[1, 512]], compare_op=ALU.is_gt, fill=0.0, base=base, channel_multiplier=-1), reads=[kwt], writes=[kwt])

            def phaseC(st):
                h, qb, j, jn, nkb, i = st["h"], st["qb"], st["j"], st["jn"], st["nkb"], st["i"]
                hb = h % 2
                v = av[hb]
                b3 = i % NBUF
                sbb, wt = asb[b3], awt[b3]
                ksb, kwt = "asb%d" % b3, "awt%d" % b3
                y2 = st["yi"] % 2
                py, pr, yo = apy[y2], apr[y2], ayo[y2]
                kpy, kpr, kyo = "apy%d" % y2, "apr%d" % y2, "ayo%d" % y2
                S.op("pe", lambda e: e.matmul(py[:], lhsT=v[:, j, :], rhs=wt[:], start=(jn == 0), stop=(jn == nkb - 1)), reads=["av%d" % hb, kwt], writes=[kpy])
                if jn < nkb - 1:
                    S.op("pe", lambda e: e.matmul(pr[:], lhsT=self.ones_b(), rhs=sbb[:], start=(jn == 0), stop=(jn == nkb - 2)), reads=["cstb", ksb], writes=[kpr])
                else:
                    S.op("act", lambda e: e.activation(out=yo[:], in_=py[:], func=AF.Copy), reads=[kpy], writes=[kyo])
                    S.dma("sp", lambda e: e.dma_start(out=self.yT_d[0, h * 64:(h + 1) * 64, qb * 512:(qb + 1) * 512], in_=yo[:]), reads=[kyo], writes=["yT_d"])

            n = len(steps)
            for s_ in range(n + 2):
                if 0 <= s_ - 2 < n:
                    phaseC(steps[s_ - 2])
                if 0 <= s_ - 1 < n:
                    phaseB(steps[s_ - 1])
                if s_ < n:
                    phaseA(steps[s_])

    def _zero_branch(self, g):
        nc, S = self.nc, self.S
        with ExitStack() as es:
            z = es.enter_context(self.sbt("zb", [128, S_LEN], BF16))
            S.op("dve", lambda e: e.memset(z[:], 0.0), writes=["zb"])
            for kc in range(4):
                S.dma("sp", lambda e, kc=kc: e.dma_start(out=self.yT_d[g, kc * 128:(kc + 1) * 128, :], in_=z[:]), reads=["zb"], writes=["yT_d"])

    def stage_hgrn(self, l):
        nc, S = self.nc, self.S
        with ExitStack() as es:
            sb = lambda n, sh, dt: es.enter_context(self.sbt(n, sh, dt))
            pt = lambda n, sh, dt: es.enter_context(self.pst(n, sh, dt))
            q = sb("hq", [128, S_LEN], F32)
            f = sb("hf", [128, S_LEN], F32)
            lf = sb("hlf", [128, S_LEN], F32)
            kin = sb("hkin", [128, S_LEN], F32)
            Bg = sb("hBg", [128, S_LEN], F32)
            Dd = sb("hD", [128, S_LEN], F32)
            Ee = sb("hE", [128, S_LEN], F32)
            Q1, K1 = sb("hQ1", [128, S_LEN], BF16), sb("hK1", [128, S_LEN], BF16)
            Q2, K2 = sb("hQ2", [128, S_LEN], BF16), sb("hK2", [128, S_LEN], BF16)
            hi = sb("hhi", [128, NT, 128], BF16)
            k2t = sb("hk2t", [128, NT, 128], BF16)
            hg = sb("hhg", [128, S_LEN], F32)
            oT = sb("hoT", [128, S_LEN], F32)
            Bst, Bmid, Bend, dec = sb("hBst", [128, 32], F32), sb("hBmid", [128, 32], F32), sb("hBend", [128, 32], F32), sb("hdec", [128, 32], F32)
            oml = sb("homl", [128, 1], F32)
            St, Stb = sb("hSt", [128, 128], F32), sb("hStb", [128, 128], BF16)
            pTs = [sb("hpT%d" % i, [128, 64], BF16) for i in range(2)]
            sq = sb("hsq", [128, 512], F32)
            rs = sb("hrs", [128, 512], F32)
            yb = sb("hyb", [128, 512], F32)
            ybb = sb("hybb", [128, 512], BF16)
            pss = [pt("hpss%d" % i, [128, 512], F32) for i in range(2)]
            pso = [pt("hpso%d" % i, [128, 512], F32) for i in range(2)]
            psst = [pt("hpsst%d" % i, [128, 512], F32) for i in range(2)]
            ptr = pt("hptr", [128, 1024], BF16)
            psn = pt("hpsn", [128, 512], F32)
            mask = self.cst[:, C_M64:C_M64 + 64]
            Bg3 = Bg[:].rearrange("p (c t) -> p c t", t=64)
            for h in range(4):
                lb = self.lbT[:, l * 4 + h: l * 4 + h + 1]
                rows = slice(h * 128, (h + 1) * 128)
                S.dma("sp", lambda e, rows=rows: e.dma_start(out=q[:], in_=self.hqT_d[rows, :]), reads=["projout"], writes=["hq"])
                S.dma("sp", lambda e, rows=rows: e.dma_start(out=f[:], in_=self.hfT_d[rows, :]), reads=["projout"], writes=["hf"])
                S.dma("sp", lambda e, rows=rows: e.dma_start(out=hg[:], in_=self.hgT_d[rows, :]), reads=["projout"], writes=["hhg"])
                S.dma("sp", lambda e, rows=rows: e.dma_start(out=hi[:], in_=self.hi_d[:, rows].rearrange("(j p) v -> p j v", p=128)), reads=["projout"], writes=["hhi"])
                S.op("dve", lambda e, lb=lb: e.tensor_scalar(out=oml[:], in0=lb, scalar1=-1.0, scalar2=1.0, op0=ALU.mult, op1=ALU.add), reads=["lbT"], writes=["homl"])
                S.op("act", lambda e: e.activation(out=f[:], in_=f[:], func=AF.Sigmoid), reads=["hf"], writes=["hf"])
                S.op("dve", lambda e, lb=lb: e.tensor_scalar(out=f[:], in0=f[:], scalar1=oml[:, 0:1], scalar2=lb, op0=ALU.mult, op1=ALU.add), reads=["hf", "homl", "lbT"], writes=["hf"])
                S.op("act", lambda e: e.activation(out=lf[:], in_=f[:], func=AF.Ln), reads=["hf"], writes=["hlf"])
                S.op("dve", lambda e: e.tensor_scalar(out=kin[:], in0=f[:], scalar1=-1.0, scalar2=1.0, op0=ALU.mult, op1=ALU.add), reads=["hf"], writes=["hkin"])
                S.op("dve", lambda e: e.tensor_tensor_scan(out=Bg[:], data0=lf[:], data1=lf[:], initial=0.0, op0=ALU.add, op1=ALU.bypass), reads=["hlf"], writes=["hBg"])
                S.op("dve", lambda e: e.memset(Bst[:, 0:1], 0.0), writes=["hBst"])
                S.op("dve", lambda e: e.tensor_copy(out=Bst[:, 1:32], in_=Bg3[:, 0:31, 63]), reads=["hBg"], writes=["hBst"])
                S.op("dve", lambda e: e.tensor_copy(out=Bmid[:], in_=Bg3[:, :, 31]), reads=["hBg"], writes=["hBmid"])
                S.op("dve", lambda e: e.tensor_copy(out=Bend[:], in_=Bg3[:, :, 63]), reads=["hBg"], writes=["hBend"])
                S.op("dve", lambda e: e.tensor_tensor(out=dec[:], in0=Bend[:], in1=Bst[:], op=ALU.subtract), reads=["hBend", "hBst"], writes=["hdec"])
                S.op("act", lambda e: e.activation(out=dec[:], in_=dec[:], func=AF.Exp), reads=["hdec"], writes=["hdec"])

                def sub_cols(col, key):
                    for c in range(32):
                        S.op("dve", lambda e, c=c: e.tensor_scalar(out=Dd[:, c * 64:(c + 1) * 64], in0=Bg[:, c * 64:(c + 1) * 64], scalar1=col[:, c:c + 1], scalar2=None, op0=ALU.subtract),
                             reads=["hBg", key], writes=["hD"])
                sub_cols(Bmid, "hBmid")
                S.op("act", lambda e: e.activation(out=Ee[:], in_=Dd[:], func=AF.Exp), reads=["hD"], writes=["hE"])
                S.op("dve", lambda e: e.tensor_tensor(out=Q1[:], in0=q[:], in1=Ee[:], op=ALU.mult), reads=["hq", "hE"], writes=["hQ1"])
                S.op("act", lambda e: e.activation(out=Ee[:], in_=Dd[:], func=AF.Exp, scale=-1.0), reads=["hD", "hQ1"], writes=["hE"])
                S.op("dve", lambda e: e.tensor_tensor(out=K1[:], in0=kin[:], in1=Ee[:], op=ALU.mult), reads=["hkin", "hE"], writes=["hK1"])
                sub_cols(Bst, "hBst")
                S.op("act", lambda e: e.activation(out=Ee[:], in_=Dd[:], func=AF.Exp), reads=["hD", "hK1"], writes=["hE"])
                S.op("dve", lambda e: e.tensor_tensor(out=Q2[:], in0=q[:], in1=Ee[:], op=ALU.mult), reads=["hq", "hE"], writes=["hQ2"])
                sub_cols(Bend, "hBend")
                S.op("act", lambda e: e.activation(out=Ee[:], in_=Dd[:], func=AF.Exp, scale=-1.0), reads=["hD", "hQ2"], writes=["hE"])
                S.op("dve", lambda e: e.tensor_tensor(out=K2[:], in0=kin[:], in1=Ee[:], op=ALU.mult), reads=["hkin", "hE"], writes=["hK2"])
                for j in range(NT):
                    S.op("pe", lambda e, j=j: e.transpose(ptr[:, 0:128], K2[:, j * 128:(j + 1) * 128], self.ident_b()), reads=["hK2", "cstb"], writes=["hptr"])
                    S.op("act", lambda e, j=j: e.activation(out=k2t[:, j, :], in_=ptr[:, 0:128], func=AF.Copy), reads=["hptr"], writes=["hk2t"])
                for c in range(32):
                    j, half = c // 2, c % 2
                    r0 = half * 64
                    cs = slice(c * 64, (c + 1) * 64)
                    ps_s = pss[c % 2]; kps = "hpss%d" % (c % 2)
                    pT = pTs[c % 2]; kpT = "hpT%d" % (c % 2)
                    po = pso[(c // 8) % 2]; kpo = "hpso%d" % ((c // 8) % 2)
                    ocs = slice((c % 8) * 64, (c % 8 + 1) * 64)
                    pst_ = psst[c % 2]; kpst = "hpsst%d" % (c % 2)
                    S.op("pe", lambda e, ps_s=ps_s, r0=r0, cs=cs: e.matmul(ps_s[r0:r0 + 64, 0:64], lhsT=K1[:, cs], rhs=Q1[:, cs], start=True, stop=True), reads=["hK1", "hQ1"], writes=[kps])
                    S.op("dve", lambda e, ps_s=ps_s, pT=pT, r0=r0: e.tensor_copy(out=pT[r0:r0 + 64, :], in_=ps_s[r0:r0 + 64, 0:64]), reads=[kps], writes=[kpT])
                    S.op("pool", lambda e, pT=pT, r0=r0: e.affine_select(out=pT[r0:r0 + 64, :], in_=pT[r0:r0 + 64, :], pattern=[[1, 64]], compare_op=ALU.is_ge, fill=0.0, base=0, channel_multiplier=-1),
                         reads=[kpT], writes=[kpT])
                    S.op("pe", lambda e, po=po, pT=pT, r0=r0, j=j, ocs=ocs, c=c: e.matmul(po[:, ocs], lhsT=hi[r0:r0 + 64, j, :], rhs=pT[r0:r0 + 64, :], start=True, stop=(c == 0)), reads=["hhi", kpT], writes=[kpo])
                    if c > 0:
                        S.op("pe", lambda e, po=po, ocs=ocs, cs=cs: e.matmul(po[:, ocs], lhsT=Stb[:], rhs=Q2[:, cs], start=False, stop=True), reads=["hStb", "hQ2"], writes=[kpo])
                    if c < 31:
                        S.op("pe", lambda e, pst_=pst_, r0=r0, j=j: e.matmul(pst_[:, 0:128], lhsT=k2t[r0:r0 + 64, j, :], rhs=hi[r0:r0 + 64, j, :], start=True, stop=True), reads=["hk2t", "hhi"], writes=[kpst])
                        if c == 0:
                            S.op("dve", lambda e, pst_=pst_: e.tensor_copy(out=St[:], in_=pst_[:, 0:128]), reads=[kpst], writes=["hSt"])
                        else:
                            S.op("dve", lambda e, pst_=pst_, c=c: e.scalar_tensor_tensor(out=St[:], in0=St[:], scalar=dec[:, c:c + 1], in1=pst_[:, 0:128], op0=ALU.mult, op1=ALU.add), reads=[kpst, "hSt", "hdec"], writes=["hSt"])
                        S.op("act", lambda e: e.activation(out=Stb[:], in_=St[:], func=AF.Copy), reads=["hSt"], writes=["hStb"])
                    if c % 8 == 7:
                        tb = c // 8
                        S.op("act", lambda e, po=po, tb=tb: e.activation(out=oT[:, tb * 512:(tb + 1) * 512], in_=po[:], func=AF.Copy), reads=[kpo], writes=["hoT"])
                gcol = self.smc("hgn", l, h)
                for tb in range(4):
                    bs = slice(tb * 512, (tb + 1) * 512)
                    S.op("act", lambda e, bs=bs: e.activation(out=sq[:], in_=oT[:, bs], func=AF.Square), reads=["hoT"], writes=["hsq"])
                    S.op("pe", lambda e: e.matmul(psn[:], lhsT=self.ones_f(), rhs=sq[:], start=True, stop=True), reads=["cst", "hsq"], writes=["hpsn"])
                    S.op("dve", lambda e: e.tensor_scalar(out=rs[:], in0=psn[:], scalar1=1.0 / 128, scalar2=EPS, op0=ALU.mult, op1=ALU.add), reads=["hpsn"], writes=["hrs"])
                    S.op("act", lambda e: e.activation(out=rs[:], in_=rs[:], func=AF.Sqrt), reads=["hrs"], writes=["hrs"])
                    S.op("dve", lambda e: e.reciprocal(out=rs[:], in_=rs[:]), reads=["hrs"], writes=["hrs"])
                    S.op("dve", lambda e, bs=bs: e.tensor_tensor(out=yb[:], in0=oT[:, bs], in1=rs[:], op=ALU.mult), reads=["hoT", "hrs"], writes=["hyb"])
                    S.op("dve", lambda e, bs=bs, gcol=gcol: e.scalar_tensor_tensor(out=ybb[:], in0=yb[:], scalar=gcol, in1=hg[:, bs], op0=ALU.mult, op1=ALU.mult), reads=["hyb", "sm", "hhg"], writes=["hybb"])
                    S.dma("sp", lambda e, bs=bs, rows=rows: e.dma_start(out=self.yT_d[1, rows, bs], in_=ybb[:]), reads=["hybb"], writes=["yT_d"])

    def stage_mlstm(self, l):
        nc, S = self.nc, self.S
        with ExitStack() as es:
            sb = lambda n, sh, dt: es.enter_context(self.sbt(n, sh, dt))
            pt = lambda n, sh, dt: es.enter_context(self.pst(n, sh, dt))
            gi, gf = sb("mgi", [4, S_LEN], F32), sb("mgf", [4, S_LEN], F32)
            Bc, aa, AA = sb("mB", [4, S_LEN], F32), sb("ma", [4, S_LEN], F32), sb("mA", [4, S_LEN], F32)
            em, Dr = sb("mem", [4, S_LEN], F32), sb("mDr", [4, S_LEN], F32)
            uu, ww, it = sb("mu", [4, S_LEN], F32), sb("mw", [4, S_LEN], F32), sb("mit", [4, S_LEN], F32)
            Aend, Aprev, decr = sb("mAend", [4, 32], F32), sb("mAprev", [4, 32], F32), sb("mdecr", [4, 32], F32)
            nbf = sb("mnbf", [4, 1], F32)
            og, _ = SM_OFF["gateb"]
            bi = self.sm[0:4, og + l * 2: og + l * 2 + 1]
            bf = self.sm[0:4, og + l * 2 + 1: og + l * 2 + 2]
            S.dma("sp", lambda e: e.dma_start(out=gi[:], in_=self.gT_d[0, :, :]), reads=["projout"], writes=["mgi"])
            S.dma("sp", lambda e: e.dma_start(out=gf[:], in_=self.gT_d[1, :, :]), reads=["projout"], writes=["mgf"])
            S.op("dve", lambda e: e.tensor_scalar(out=gi[:], in0=gi[:], scalar1=bi, scalar2=None, op0=ALU.add), reads=["mgi", "sm"], writes=["mgi"])
            S.op("dve", lambda e: e.tensor_scalar(out=nbf[:], in0=bf, scalar1=-1.0, scalar2=None, op0=ALU.mult), reads=["sm"], writes=["mnbf"])
            S.op("act", lambda e: e.activation(out=gf[:], in_=gf[:], func=AF.Exp, scale=-1.0, bias=nbf[:, 0:1]), reads=["mgf", "mnbf"], writes=["mgf"])
            S.op("act", lambda e: e.activation(out=gf[:], in_=gf[:], func=AF.Ln, bias=1.0), reads=["mgf"], writes=["mgf"])
            S.op("dve", lambda e: e.tensor_scalar(out=gf[:], in0=gf[:], scalar1=-1.0, scalar2=None, op0=ALU.mult), reads=["mgf"], writes=["mgf"])
            S.op("dve", lambda e: e.tensor_tensor_scan(out=Bc[:], data0=gf[:], data1=gf[:], initial=0.0, op0=ALU.add, op1=ALU.bypass), reads=["mgf"], writes=["mB"])
            S.op("dve", lambda e: e.tensor_tensor(out=aa[:], in0=gi[:], in1=Bc[:], op=ALU.subtract), reads=["mgi", "mB"], writes=["ma"])
            S.op("dve", lambda e: e.tensor_tensor_scan(out=AA[:], data0=aa[:], data1=aa[:], initial=0.0, op0=ALU.max, op1=ALU.bypass), reads=["ma"], writes=["mA"])
            S.op("dve", lambda e: e.tensor_tensor(out=em[:], in0=Bc[:], in1=AA[:], op=ALU.add), reads=["mB", "mA"], writes=["mem"])
            S.op("act", lambda e: e.activation(out=em[:], in_=em[:], func=AF.Exp, scale=-1.0), reads=["mem"], writes=["mem"])
            A3 = AA[:].rearrange("p (c t) -> p c t", t=64)
            a3 = aa[:].rearrange("p (c t) -> p c t", t=64)
            D3 = Dr[:].rearrange("p (c t) -> p c t", t=64)
            S.op("dve", lambda e: e.tensor_copy(out=Aend[:], in_=A3[:, :, 63]), reads=["mA"], writes=["mAend"])
            S.op("dve", lambda e: e.memset(Aprev[:, 0:1], 0.0), writes=["mAprev"])
            S.op("dve", lambda e: e.tensor_copy(out=Aprev[:, 1:32], in_=A3[:, 0:31, 63]), reads=["mA"], writes=["mAprev"])
            S.op("dve", lambda e: e.tensor_tensor(out=decr[:], in0=Aprev[:], in1=Aend[:], op=ALU.subtract), reads=["mAprev", "mAend"], writes=["mdecr"])
            S.op("act", lambda e: e.activation(out=decr[:], in_=decr[:], func=AF.Exp), reads=["mdecr"], writes=["mdecr"])
            bc = lambda col: col[:].unsqueeze(2).to_broadcast([4, 32, 64])
            S.op("dve", lambda e: e.tensor_tensor(out=D3, in0=A3, in1=bc(Aend), op=ALU.subtract), reads=["mA", "mAend"], writes=["mDr"])
            S.op("act", lambda e: e.activation(out=uu[:], in_=Dr[:], func=AF.Exp, scale=-1.0), reads=["mDr"], writes=["mu"])
            S.op("dve", lambda e: e.tensor_tensor(out=D3, in0=a3, in1=bc(Aend), op=ALU.subtract), reads=["ma", "mAend", "mu"], writes=["mDr"])
            S.op("act", lambda e: e.activation(out=ww[:], in_=Dr[:], func=AF.Exp), reads=["mDr"], writes=["mw"])
            S.op("dve", lambda e: e.tensor_tensor(out=D3, in0=A3, in1=bc(Aprev), op=ALU.subtract), reads=["mA", "mAprev", "mw"], writes=["mDr"])
            S.op("act", lambda e: e.activation(out=it[:], in_=Dr[:], func=AF.Exp, scale=-1.0), reads=["mDr"], writes=["mit"])
            xr, acc = sb("mxr", [128, S_LEN], F32), sb("macc2", [128, S_LEN], F32)
            Q1, Q2, K1 = sb("mQ1", [128, S_LEN], BF16), sb("mQ2", [128, S_LEN], BF16), sb("mK1", [128, S_LEN], BF16)
            vv = sb("mvv", [128, NT, 128], BF16)
            k2t = sb("mk2t", [128, NT, 128], BF16)
            mo = sb("mmo", [128, S_LEN], F32)
            hT = sb("mhT", [128, S_LEN], F32)
            dec = sb("mdec", [128, 32], F32)
            Cs, Csb = sb("mCs", [128, 128], F32), sb("mCsb", [128, 128], BF16)
            Ns, Nsb = sb("mNs", [128, 128], F32), sb("mNsb", [128, 128], BF16)
            pTs = [sb("mpT%d" % i, [128, 64], BF16) for i in range(2)]
            numT, embc, dmx = sb("mnumT", [128, 512], F32), sb("membc", [128, 512], F32), sb("mdmx", [128, 512], F32)
            sq, rs, yb, ybb = sb("msq", [128, 512], F32), sb("mrs", [128, 512], F32), sb("myb", [128, 512], F32), sb("mybb", [128, 512], BF16)
            pss = pt("mpss", [128, 512], F32)
            pso = [pt("mpso%d" % i, [128, 512], F32) for i in range(2)]
            psd = [pt("mpsd%d" % i, [128, 512], F32) for i in range(2)]
            pstC, pstN = pt("mpstC", [128, 512], F32), pt("mpstN", [128, 512], F32)
            pmisc = pt("mpmisc", [128, 512], F32)
            pmisc_b = pmisc[:].bitcast(BF16)
            KM = "mpmisc"
            ones_b = self.ones_b()

            def bcast_rows(rows, h, tb):
                S.op("pe", lambda e: e.matmul(pmisc[:], lhsT=self.cst[0:4, C_SEL + h * 128: C_SEL + (h + 1) * 128], rhs=rows[0:4, tb * 512:(tb + 1) * 512], start=True, stop=True),
                     reads=["cst", "mu", "mw", "mit", "mem"], writes=[KM])

            def conv_silu(chunk):
                cw = lambda tap: self.smc("convw", l, tap * 8 + chunk)
                S.op("dve", lambda e: e.tensor_scalar(out=acc[:], in0=xr[:], scalar1=cw(3), scalar2=self.smc("convb", l, chunk), op0=ALU.mult, op1=ALU.add), reads=["mxr", "sm"], writes=["macc2"])
                for sh in (1, 2, 3):
                    S.op("dve", lambda e, sh=sh: e.scalar_tensor_tensor(out=acc[:, sh:S_LEN], in0=xr[:, 0:S_LEN - sh], scalar=cw(3 - sh), in1=acc[:, sh:S_LEN], op0=ALU.mult, op1=ALU.add),
                         reads=["mxr", "sm", "macc2"], writes=["macc2"])
                S.op("act", lambda e: e.activation(out=acc[:], in_=acc[:], func=AF.Silu), reads=["macc2"], writes=["macc2"])

            for h in range(4):
                rows = slice(h * 128, (h + 1) * 128)
                S.dma("sp", lambda e, rows=rows: e.dma_start(out=xr[:], in_=self.mqkT_d[rows, :]), reads=["projout"], writes=["mxr"])
                S.dma("sp", lambda e, rows=rows: e.dma_start(out=mo[:], in_=self.moT_d[rows, :]), reads=["projout"], writes=["mmo"])
                S.dma("sp", lambda e, rows=rows: e.dma_start(out=vv[:], in_=self.mv_d[:, rows].rearrange("(j p) v -> p j v", p=128)), reads=["projout"], writes=["mvv"])
                conv_silu(h)
                for tb in range(4):
                    bs = slice(tb * 512, (tb + 1) * 512)
                    bcast_rows(uu, h, tb)
                    S.op("dve", lambda e, bs=bs: e.tensor_tensor(out=Q1[:, bs], in0=acc[:, bs], in1=pmisc[:], op=ALU.mult), reads=["macc2", KM], writes=["mQ1"])
                    bcast_rows(it, h, tb)
                    S.op("dve", lambda e, bs=bs: e.tensor_tensor(out=Q2[:, bs], in0=acc[:, bs], in1=pmisc[:], op=ALU.mult), reads=["macc2", KM], writes=["mQ2"])
                S.dma("sp", lambda e, h=h: e.dma_start(out=xr[:], in_=self.mqkT_d[512 + h * 128: 512 + (h + 1) * 128, :]), reads=["projout"], writes=["mxr"])
                conv_silu(4 + h)
                for tb in range(4):
                    bs = slice(tb * 512, (tb + 1) * 512)
                    bcast_rows(ww, h, tb)
                    S.op("dve", lambda e, bs=bs: e.scalar_tensor_tensor(out=K1[:, bs], in0=acc[:, bs], scalar=128.0 ** -0.5, in1=pmisc[:], op0=ALU.mult, op1=ALU.mult), reads=["macc2", KM], writes=["mK1"])
                S.op("pe", lambda e, h=h: e.matmul(pmisc[:, 0:32], lhsT=self.cst[0:4, C_SEL + h * 128: C_SEL + (h + 1) * 128], rhs=decr[0:4, :], start=True, stop=True), reads=["cst", "mdecr"], writes=[KM])
                S.op("act", lambda e: e.activation(out=dec[:], in_=pmisc[:, 0:32], func=AF.Copy), reads=[KM], writes=["mdec"])
                for j in range(NT):
                    S.op("pe", lambda e, j=j: e.transpose(pmisc_b[:, 0:128], K1[:, j * 128:(j + 1) * 128], self.ident_b()), reads=["mK1", "cstb"], writes=[KM])
                    S.op("act", lambda e, j=j: e.activation(out=k2t[:, j, :], in_=pmisc_b[:, 0:128], func=AF.Copy), reads=[KM], writes=["mk2t"])
                for c in range(32):
                    j, half = c // 2, c % 2
                    r0 = half * 64
                    cs = slice(c * 64, (c + 1) * 64)
                    pT = pTs[c % 2]; kpT = "mpT%d" % (c % 2)
                    po = pso[(c // 8) % 2]; kpo = "mpso%d" % ((c // 8) % 2)
                    pd = psd[(c // 8) % 2]; kpd = "mpsd%d" % ((c // 8) % 2)
                    ocs = slice((c % 8) * 64, (c % 8 + 1) * 64)
                    S.op("pe", lambda e, r0=r0, cs=cs: e.matmul(pss[r0:r0 + 64, 0:64], lhsT=K1[:, cs], rhs=Q1[:, cs], start=True, stop=True), reads=["mK1", "mQ1"], writes=["mpss"])
                    S.op("dve", lambda e, pT=pT, r0=r0: e.tensor_copy(out=pT[r0:r0 + 64, :], in_=pss[r0:r0 + 64, 0:64]), reads=["mpss"], writes=[kpT])
                    S.op("pool", lambda e, pT=pT, r0=r0: e.affine_select(out=pT[r0:r0 + 64, :], in_=pT[r0:r0 + 64, :], pattern=[[1, 64]], compare_op=ALU.is_ge, fill=0.0, base=0, channel_multiplier=-1),
                         reads=[kpT], writes=[kpT])
                    S.op("pe", lambda e, po=po, pT=pT, r0=r0, j=j, ocs=ocs, c=c: e.matmul(po[:, ocs], lhsT=vv[r0:r0 + 64, j, :], rhs=pT[r0:r0 + 64, :], start=True, stop=(c == 0)), reads=["mvv", kpT], writes=[kpo])
                    if c > 0:
                        S.op("pe", lambda e, po=po, ocs=ocs, cs=cs: e.matmul(po[:, ocs], lhsT=Csb[:], rhs=Q2[:, cs], start=False, stop=True), reads=["mCsb", "mQ2"], writes=[kpo])
                    S.op("pe", lambda e, pd=pd, pT=pT, r0=r0, ocs=ocs, c=c: e.matmul(pd[:, ocs], lhsT=ones_b[r0:r0 + 64, :], rhs=pT[r0:r0 + 64, :], start=True, stop=(c == 0)), reads=["cstb", kpT], writes=[kpd])
                    if c > 0:
                        S.op("pe", lambda e, pd=pd, ocs=ocs, cs=cs: e.matmul(pd[:, ocs], lhsT=Nsb[:], rhs=Q2[:, cs], start=False, stop=True), reads=["mNsb", "mQ2"], writes=[kpd])
                    if c < 31:
                        S.op("pe", lambda e, r0=r0, j=j: e.matmul(pstC[:, 0:128], lhsT=k2t[r0:r0 + 64, j, :], rhs=vv[r0:r0 + 64, j, :], start=True, stop=True), reads=["mk2t", "mvv"], writes=["mpstC"])
                        S.op("pe", lambda e, r0=r0, j=j: e.matmul(pstN[:, 0:128], lhsT=k2t[r0:r0 + 64, j, :], rhs=ones_b[r0:r0 + 64, :], start=True, stop=True), reads=["mk2t", "cstb"], writes=["mpstN"])
                        if c == 0:
                            S.op("dve", lambda e: e.tensor_copy(out=Cs[:], in_=pstC[:, 0:128]), reads=["mpstC"], writes=["mCs"])
                            S.op("dve", lambda e: e.tensor_copy(out=Ns[:], in_=pstN[:, 0:128]), reads=["mpstN"], writes=["mNs"])
                        else:
                            S.op("dve", lambda e, c=c: e.scalar_tensor_tensor(out=Cs[:], in0=Cs[:], scalar=dec[:, c:c + 1], in1=pstC[:, 0:128], op0=ALU.mult, op1=ALU.add), reads=["mpstC", "mCs", "mdec"], writes=["mCs"])
                            S.op("dve", lambda e, c=c: e.scalar_tensor_tensor(out=Ns[:], in0=Ns[:], scalar=dec[:, c:c + 1], in1=pstN[:, 0:128], op0=ALU.mult, op1=ALU.add), reads=["mpstN", "mNs", "mdec"], writes=["mNs"])
                        S.op("act", lambda e: e.activation(out=Csb[:], in_=Cs[:], func=AF.Copy), reads=["mCs"], writes=["mCsb"])
                        S.op("act", lambda e: e.activation(out=Nsb[:], in_=Ns[:], func=AF.Copy), reads=["mNs"], writes=["mNsb"])
                    if c % 8 == 7:
                        tb = c // 8
                        bs = slice(tb * 512, (tb + 1) * 512)
                        S.op("act", lambda e, po=po: e.activation(out=numT[:], in_=po[:], func=AF.Copy), reads=[kpo], writes=["mnumT"])
                        bcast_rows(em, h, tb)
                        S.op("act", lambda e: e.activation(out=embc[:], in_=pmisc[:], func=AF.Copy), reads=[KM], writes=["membc"])
                        S.op("act", lambda e, pd=pd: e.activation(out=dmx[:], in_=pd[:], func=AF.Abs), reads=[kpd], writes=["mdmx"])
                        S.op("dve", lambda e: e.tensor_tensor(out=dmx[:], in0=dmx[:], in1=embc[:], op=ALU.max), reads=["mdmx", "membc"], writes=["mdmx"])
                        S.op("dve", lambda e: e.reciprocal(out=dmx[:], in_=dmx[:]), reads=["mdmx"], writes=["mdmx"])
                        S.op("dve", lambda e, bs=bs: e.tensor_tensor(out=hT[:, bs], in0=numT[:], in1=dmx[:], op=ALU.mult), reads=["mnumT", "mdmx"], writes=["mhT"])
                gcol = self.smc("mln", l, h)
                for tb in range(4):
                    bs = slice(tb * 512, (tb + 1) * 512)
                    S.op("act", lambda e, bs=bs: e.activation(out=sq[:], in_=hT[:, bs], func=AF.Square), reads=["mhT"], writes=["msq"])
                    S.op("pe", lambda e: e.matmul(pmisc[:], lhsT=self.ones_f(), rhs=sq[:], start=True, stop=True), reads=["cst", "msq"], writes=[KM])
                    S.op("dve", lambda e: e.tensor_scalar(out=rs[:], in0=pmisc[:], scalar1=1.0 / 128, scalar2=EPS, op0=ALU.mult, op1=ALU.add), reads=[KM], writes=["mrs"])
                    S.op("act", lambda e: e.activation(out=rs[:], in_=rs[:], func=AF.Sqrt), reads=["mrs"], writes=["mrs"])
                    S.op("dve", lambda e: e.reciprocal(out=rs[:], in_=rs[:]), reads=["mrs"], writes=["mrs"])
                    S.op("dve", lambda e, bs=bs: e.tensor_tensor(out=yb[:], in0=hT[:, bs], in1=rs[:], op=ALU.mult), reads=["mhT", "mrs"], writes=["myb"])
                    S.op("dve", lambda e, bs=bs, gcol=gcol: e.scalar_tensor_tensor(out=ybb[:], in0=yb[:], scalar=gcol, in1=mo[:, bs], op0=ALU.mult, op1=ALU.mult), reads=["myb", "sm", "mmo"], writes=["mybb"])
                    S.dma("sp", lambda e, bs=bs, rows=rows: e.dma_start(out=self.yT_d[2, rows, bs], in_=ybb[:]), reads=["mybb"], writes=["yT_d"])

    def _bcast_tile(self, es, name, colfn, ps_ap=None, ps_key=None):
        nc, S = self.nc, self.S
        dg = es.enter_context(self.sbt(name + "dg", [128, 128], F32))
        dst = es.enter_context(self.sbt(name, [128, D], F32))
        if ps_ap is None:
            ps = es.enter_context(self.pst(name + "ps", [128, D], F32))[:]
            ps_key = name + "ps"
        else:
            ps = ps_ap
        for c in range(8):
            S.op("dve", lambda e, c=c: e.tensor_scalar(out=dg[:], in0=self.ident_f(), scalar1=colfn(c), scalar2=None, op0=ALU.mult),
                 reads=["cst", "modT", "sm"], writes=[name + "dg"])
            S.op("pe", lambda e, c=c: e.matmul(ps[:, c * 128:(c + 1) * 128], lhsT=self.ones_f(), rhs=dg[:], start=True, stop=True), reads=["cst", name + "dg"], writes=[ps_key])
        for hh in range(2):
            S.op("act", lambda e, hh=hh: e.activation(out=dst[:, hh * 512:(hh + 1) * 512], in_=ps[:, hh * 512:(hh + 1) * 512], func=AF.Copy), reads=[ps_key], writes=[name])
        return dst

    def stage_merge(self, l):
        nc, S = self.nc, self.S
        with ExitStack() as es:
            sb = lambda n, sh, dt: es.enter_context(self.sbt(n, sh, dt))
            pt = lambda n, sh, dt: es.enter_context(self.pst(n, sh, dt))
            g1bc = self._bcast_tile(es, "g1bc", lambda c: self.modc(l, 2, c))
            wb = sb("mwb", [128, 3, 4, D], BF16)
            wo = sb("mwo", [128, 8, D], BF16)
            yt = sb("myt", [128, 3, 4, 512], BF16)
            gts = [sb("mgt%d" % i, [128, 512], F32) for i in range(3)]
            tmps = [sb("mtmp%d" % i, [128, 512], F32) for i in range(2)]
            macc = sb("macc", [128, 512], F32)
            mT = sb("mT", [128, 8, 512], BF16)
            xts = [sb("mxt%d" % i, [128, D], F32) for i in range(2)]
            ytmp = sb("mytmp", [128, 512], F32)
            psa = [pt("mpsa%d" % i, [128, 512], F32) for i in range(2)]
            psy = [pt("mpsy%d" % i, [128, 512], F32) for i in range(2)]
            for g in range(3):
                S.dma("pool", lambda e, g=g: e.dma_start(out=wb[:, g, :, :], in_=self.w_branch[l, g, :, :].rearrange("(kc p) d -> p kc d", p=128)), writes=["mwb"])
            S.dma("pool", lambda e: e.dma_start(out=wo[:], in_=self.w_out[l, :, :].rearrange("(c p) d -> p c d", p=128)), writes=["mwo"])
            ai = 0
            gi = 0
            yi = 0
            for tb in range(4):
                for g in range(3):
                    S.dma("sp", lambda e, g=g, tb=tb: e.dma_start(out=yt[:, g, :, :], in_=self.yT_d[g, :, tb * 512:(tb + 1) * 512].rearrange("(kc p) t -> p kc t", p=128)),
                          reads=["yT_d"], writes=["myt"])
                for dc in range(8):
                    for g in range(3):
                        ps = psa[ai % 2]; kps = "mpsa%d" % (ai % 2); ai += 1
                        gt = gts[gi % 3]; kgt = "mgt%d" % (gi % 3); gi += 1
                        S.dma("sp", lambda e, gt=gt, g=g, dc=dc, tb=tb: e.dma_start(out=gt[:], in_=self.bgT_d[g * 1024 + dc * 128: g * 1024 + (dc + 1) * 128, tb * 512:(tb + 1) * 512]),
                              reads=["projout"], writes=[kgt])
                        for kc in range(4):
                            S.op("pe", lambda e, ps=ps, g=g, kc=kc, dc=dc: e.matmul(ps[:], lhsT=wb[:, g, kc, dc * 128:(dc + 1) * 128], rhs=yt[:, g, kc, :], start=(kc == 0), stop=(kc == 3)),
                                 reads=["mwb", "myt"], writes=[kps])
                        if g == 0:
                            S.op("dve", lambda e, ps=ps, gt=gt: e.tensor_tensor(out=macc[:], in0=ps[:], in1=gt[:], op=ALU.mult), reads=[kps, kgt], writes=["macc"])
                        else:
                            tmp = tmps[g % 2]; ktmp = "mtmp%d" % (g % 2)
                            S.op("dve", lambda e, ps=ps, gt=gt, tmp=tmp: e.tensor_tensor(out=tmp[:], in0=ps[:], in1=gt[:], op=ALU.mult), reads=[kps, kgt], writes=[ktmp])
                            if g == 1:
                                S.op("pool", lambda e, tmp=tmp: e.tensor_tensor(out=macc[:], in0=macc[:], in1=tmp[:], op=ALU.add), reads=[ktmp, "macc"], writes=["macc"])
                            else:
                                S.op("pool", lambda e, tmp=tmp, dc=dc: e.tensor_tensor(out=mT[:, dc, :], in0=macc[:], in1=tmp[:], op=ALU.add), reads=[ktmp, "macc"], writes=["mT"])
                for tt in range(4):
                    t = tb * 4 + tt
                    xt = xts[t % 2]; kxt = "mxt%d" % (t % 2)
                    S.dma("sp", lambda e, xt=xt, t=t: e.dma_start(out=xt[:], in_=self.xres[t * 128:(t + 1) * 128, :]), reads=["xres"], writes=[kxt])
                    for dh in range(2):
                        ps = psy[yi % 2]; kps = "mpsy%d" % (yi % 2); yi += 1
                        for c in range(8):
                            S.op("pe", lambda e, ps=ps, c=c, tt=tt, dh=dh: e.matmul(ps[:], lhsT=mT[:, c, tt * 128:(tt + 1) * 128], rhs=wo[:, c, dh * 512:(dh + 1) * 512], start=(c == 0), stop=(c == 7)),
                                 reads=["mT", "mwo"], writes=[kps])
                        S.op("dve", lambda e, ps=ps, dh=dh: e.tensor_tensor(out=ytmp[:], in0=ps[:], in1=g1bc[:, dh * 512:(dh + 1) * 512], op=ALU.mult), reads=[kps, "g1bc"], writes=["mytmp"])
                        S.op("dve", lambda e, xt=xt, dh=dh: e.tensor_tensor(out=xt[:, dh * 512:(dh + 1) * 512], in0=xt[:, dh * 512:(dh + 1) * 512], in1=ytmp[:], op=ALU.add), reads=["mytmp", kxt], writes=[kxt])
                    S.dma("sp", lambda e, xt=xt, t=t: e.dma_start(out=self.xres[t * 128:(t + 1) * 128, :], in_=xt[:]), reads=[kxt], writes=["xres"])

    def uvcast_gen(self, l, cbs):
        nc, S = self.nc, self.S
        for k in range(32):
            cb = cbs[k % 3]; kcb = "uvc%d" % (k % 3)
            src = self.pk_uv[l, k * 512:(k + 1) * 512, :].rearrange("(p r) d -> p r d", p=128)
            dst = self.uvb[k * 512:(k + 1) * 512, :].rearrange("(p r) d -> p r d", p=128)
            S.dma("pool", lambda e, cb=cb, src=src: e.dma_start(out=cb[:], in_=src), writes=[kcb])
            S.dma("sp", lambda e, cb=cb, dst=dst: e.dma_start(out=dst, in_=cb[:]), reads=[kcb], writes=["uvb"])
            yield

    def stage_peer(self, l):
        nc, S = self.nc, self.S
        NB = 6
        with ExitStack() as es:
            sb = lambda n, sh, dt: es.enter_context(self.sbt(n, sh, dt))
            pt = lambda n, sh, dt: es.enter_context(self.pst(n, sh, dt))
            two = lambda n, sh, dt: [sb("%s%d" % (n, i), sh, dt) for i in range(2)]
            hTs = two("phT", [128, 8, 128], BF16)
            wq = sb("pwq", [128, 8, D], BF16)
            kbd = sb("pkbd", [128, 8, 256], F32)
            qTts = two("pqTt", [128, 8, 128], F32)
            scs, s2s, cands = two("psc", [128, 2048], F32), two("ps2", [128, 2048], F32), two("pcand", [128, 2048], F32)
            mxs, mis, sifs = two("pmx", [128, 256], F32), two("pmi", [128, 256], U32), two("psif", [128, 256], F32)
            tvs, tps, tpfs = two("ptv", [128, 128], F32), two("ptp", [128, 128], U32), two("ptpf", [128, 128], F32)
            afs, bfs = two("paf", [128, 128], F32), two("pbf", [128, 128], F32)
            i1s, i2s = two("pi1", [128, 128], F32), two("pi2", [128, 128], F32)
            idxis = two("pidxi", [128, 128], I32)
            ees, ggs = two("pee", [128, 128], F32), two("pgg", [128, 128], F32)
            ssums = two("pssum", [128, 8], F32)
            aa, ga, gl = sb("paa", [128, 128], F32), sb("pga", [128, 128], F32), sb("pgl", [128, 128], F32)
            h2ts = two("ph2t", [128, D], F32)
            xts = two("pxt", [128, D], F32)
            ubs = [sb("pub%d" % i, [128, 2 * D], BF16) for i in range(NB)]
            junks = two("pjunk", [128, D], F32)
            acc = sb("pacc", [128, D], F32)
            tmpbs = [sb("ptmpb%d" % i, [128, D], BF16) for i in range(3)]
            psq = pt("ppsq", [128, 8, 128], F32)
            pssc = pt("ppssc", [128, 2048], F32)
            pacc = pt("ppacc", [128, D], F32)
            g2bc = self._bcast_tile(es, "g2bc", lambda c: self.modc(l, 5, c), ps_ap=pssc[:, 0:D], ps_key="ppssc")
            UV2 = self.uvb
            iota16 = self.cst[:, C_IOTA:C_IOTA + 16]
            thr15 = self.cst[:, C_THR:C_THR + 15]
            S.dma("pool", lambda e: e.dma_start(out=wq[:], in_=self.pk_wq[l, :, :].rearrange("(kc p) d -> p kc d", p=128)), writes=["pwq"])
            S.dma("sp", lambda e: e.dma_start(out=kbd[:], in_=self.keysbd[l, :, :, :].rearrange("h p n -> p h n")), writes=["pkbd"])
            B4 = [128, 8, 16, 16]

            def prep(t):
                p = t % 2
                K = lambda n: "%s%d" % (n, p)
                ts = slice(t * 128, (t + 1) * 128)
                h2t, xt, hT, qTt = h2ts[p], xts[p], hTs[p], qTts[p]
                sc, s2, cand, mx, mi, sif = scs[p], s2s[p], cands[p], mxs[p], mis[p], sifs[p]
                tv, tp, tpf, af, bf_, i1, i2, idxi, ee, gg, ssum = tvs[p], tps[p], tpfs[p], afs[p], bfs[p], i1s[p], i2s[p], idxis[p], ees[p], ggs[p], ssums[p]
                sc3 = sc[:].rearrange("p (g n) -> p g n", n=128)
                s23 = s2[:].rearrange("p (g n) -> p g n", n=128)
                mx3 = mx[:].rearrange("p (g k) -> p g k", k=16)
                mi3 = mi[:].rearrange("p (g k) -> p g k", k=16)
                mx4 = mx[:].rearrange("p (h q k) -> p h q k", h=8, q=2)
                sif4 = sif[:].rearrange("p (h q k) -> p h q k", h=8, q=2)
                cand4 = cand[:].rearrange("p (h a b) -> p h a b", h=8, a=16)
                tv3 = tv[:].rearrange("p (h k) -> p h k", h=8)
                tp3 = tp[:].rearrange("p (h k) -> p h k", h=8)
                oh4 = s2[:].rearrange("p (h k a) -> p h k a", h=8, k=16)
                cmp3 = sc[:, 0:1920].rearrange("p (r j) -> p r j", j=15)
                S.dma("sp", lambda e: e.dma_start(out=h2t[:], in_=self.h2_d[ts, :]), reads=["h2_d"], writes=[K("ph2t")])
                S.dma("sp", lambda e: e.dma_start(out=xt[:], in_=self.xres[ts, :]), reads=["xres"], writes=[K("pxt")])
                S.dma("sp", lambda e: e.dma_start(out=hT[:], in_=self.hT_d[:, :, ts].rearrange("c p t -> p c t")), reads=["hT_d"], writes=[K("phT")])
                yield
                for h in range(8):
                    for kc in range(8):
                        S.op("pe", lambda e, h=h, kc=kc: e.matmul(psq[:, h, :], lhsT=wq[:, kc, h * 128:(h + 1) * 128], rhs=hT[:, kc, :], start=(kc == 0), stop=(kc == 7)),
                             reads=["pwq", K("phT")], writes=["ppsq"])
                    yield
                for hh in range(2):
                    S.op("act", lambda e, hh=hh: e.activation(out=qTt[:, hh * 4:(hh + 1) * 4, :], in_=psq[:, hh * 4:(hh + 1) * 4, :], func=AF.Copy), reads=["ppsq"], writes=[K("pqTt")])
                for h in range(8):
                    S.op("pe", lambda e, h=h: e.matmul(pssc[:, h * 256:(h + 1) * 256], lhsT=qTt[:, h, :], rhs=kbd[:, h, :], start=True, stop=True), reads=[K("pqTt"), "pkbd"], writes=["ppssc"])
                for qd in range(4):
                    S.op("act", lambda e, qd=qd: e.activation(out=sc[:, qd * 512:(qd + 1) * 512], in_=pssc[:, qd * 512:(qd + 1) * 512], func=AF.Copy), reads=["ppssc"], writes=[K("psc")])
                yield
                for g in range(16):
                    S.op("dve", lambda e, g=g: e.max(out=mx3[:, g, 0:8], in_=sc3[:, g, :]), reads=[K("psc")], writes=[K("pmx")])
                    S.op("dve", lambda e, g=g: e.max_index(out=mi3[:, g, 0:8], in_max=mx3[:, g, 0:8], in_values=sc3[:, g, :]), reads=[K("psc"), K("pmx")], writes=[K("pmi")])
                    yield
                    S.op("dve", lambda e, g=g: e.match_replace(out=s23[:, g, :], in_to_replace=mx3[:, g, 0:8], in_values=sc3[:, g, :], imm_value=-1e30), reads=[K("psc"), K("pmx")], writes=[K("ps2")])
                    S.op("dve", lambda e, g=g: e.max(out=mx3[:, g, 8:16], in_=s23[:, g, :]), reads=[K("ps2")], writes=[K("pmx")])
                    yield
                    S.op("dve", lambda e, g=g: e.max_index(out=mi3[:, g, 8:16], in_max=mx3[:, g, 8:16], in_values=s23[:, g, :]), reads=[K("ps2"), K("pmx")], writes=[K("pmi")])
                    yield
                S.op("dve", lambda e: e.tensor_copy(out=sif[:], in_=mi[:]), reads=[K("pmi")], writes=[K("psif")])
                S.op("dve", lambda e: e.tensor_tensor(out=cand4, in0=mx4[:, :, 0, :].unsqueeze(3).to_broadcast(B4), in1=mx4[:, :, 1, :].unsqueeze(2).to_broadcast(B4), op=ALU.add),
                     reads=[K("pmx")], writes=[K("pcand")])
                yield
                for h in range(8):
                    hs = slice(h * 256, (h + 1) * 256)
                    S.op("dve", lambda e, h=h, hs=hs: e.max(out=tv3[:, h, 0:8], in_=cand[:, hs]), reads=[K("pcand")], writes=[K("ptv")])
                    S.op("dve", lambda e, h=h, hs=hs: e.max_index(out=tp3[:, h, 0:8], in_max=tv3[:, h, 0:8], in_values=cand[:, hs]), reads=[K("pcand"), K("ptv")], writes=[K("ptp")])
                    yield
                    S.op("dve", lambda e, h=h, hs=hs: e.match_replace(out=s2[:, hs], in_to_replace=tv3[:, h, 0:8], in_values=cand[:, hs], imm_value=-1e30), reads=[K("pcand"), K("ptv")], writes=[K("ps2")])
                    S.op("dve", lambda e, h=h, hs=hs: e.max(out=tv3[:, h, 8:16], in_=s2[:, hs]), reads=[K("ps2")], writes=[K("ptv")])
                    yield
                    S.op("dve", lambda e, h=h, hs=hs: e.max_index(out=tp3[:, h, 8:16], in_max=tv3[:, h, 8:16], in_values=s2[:, hs]), reads=[K("ps2"), K("ptv")], writes=[K("ptp")])
                    yield
                S.op("dve", lambda e: e.tensor_copy(out=tpf[:], in_=tp[:]), reads=[K("ptp")], writes=[K("ptpf")])
                S.op("dve", lambda e: e.tensor_tensor(out=cmp3, in0=tpf[:].unsqueeze(2).to_broadcast([128, 128, 15]), in1=thr15.unsqueeze(1).to_broadcast([128, 128, 15]), op=ALU.is_ge),
                     reads=[K("ptpf"), "cst"], writes=[K("psc")])
                yield
                S.op("dve", lambda e: e.tensor_reduce(out=af[:], in_=cmp3, axis=AX.X, op=ALU.add), reads=[K("psc")], writes=[K("paf")])
                S.op("dve", lambda e: e.scalar_tensor_tensor(out=bf_[:], in0=af[:], scalar=-16.0, in1=tpf[:], op0=ALU.mult, op1=ALU.add), reads=[K("paf"), K("ptpf")], writes=[K("pbf")])
                yield
                for (src, q_, dst, kd) in ((af, 0, i1, K("pi1")), (bf_, 1, i2, K("pi2"))):
                    ksrc = K("paf") if q_ == 0 else K("pbf")
                    S.op("dve", lambda e, src=src: e.tensor_tensor(out=oh4, in0=src[:].rearrange("p (h k) -> p h k", h=8).unsqueeze(3).to_broadcast(B4),
                                                                   in1=iota16.unsqueeze(1).unsqueeze(1).to_broadcast(B4), op=ALU.is_equal), reads=[ksrc, "cst"], writes=[K("ps2")])
                    yield
                    S.op("dve", lambda e, q_=q_: e.tensor_tensor(out=oh4, in0=oh4, in1=sif4[:, :, q_, :].unsqueeze(2).to_broadcast(B4), op=ALU.mult), reads=[K("ps2"), K("psif")], writes=[K("ps2")])
                    yield
                    S.op("dve", lambda e, dst=dst: e.tensor_reduce(out=dst[:], in_=oh4, axis=AX.X, op=ALU.add), reads=[K("ps2")], writes=[kd])
                    yield
                S.op("dve", lambda e: e.scalar_tensor_tensor(out=i1[:], in0=i1[:], scalar=128.0, in1=i2[:], op0=ALU.mult, op1=ALU.add), reads=[K("pi1"), K("pi2")], writes=[K("pi1")])
                S.op("dve", lambda e: e.tensor_copy(out=idxi[:], in_=i1[:]), reads=[K("pi1")], writes=[K("pidxi")])
                yield
                ee3 = ee[:].rearrange("p (h k) -> p h k", h=8)
                S.op("dve", lambda e: e.tensor_tensor(out=ee3, in0=tv3, in1=tv3[:, :, 0:1].to_broadcast([128, 8, 16]), op=ALU.subtract), reads=[K("ptv")], writes=[K("pee")])
                S.op("act", lambda e: e.activation(out=ee[:], in_=ee[:], func=AF.Exp), reads=[K("pee")], writes=[K("pee")])
                S.op("dve", lambda e: e.tensor_reduce(out=ssum[:], in_=ee3, axis=AX.X, op=ALU.add), reads=[K("pee")], writes=[K("pssum")])
                yield
                S.op("dve", lambda e: e.reciprocal(out=ssum[:], in_=ssum[:]), reads=[K("pssum")], writes=[K("pssum")])
                S.op("dve", lambda e: e.tensor_tensor(out=gg[:].rearrange("p (h k) -> p h k", h=8), in0=ee3, in1=ssum[:].unsqueeze(2).to_broadcast([128, 8, 16]), op=ALU.mult),
                     reads=[K("pee"), K("pssum")], writes=[K("pgg")])
                yield

            def exhaust(g):
                if g is not None:
                    for _ in g:
                        pass

            def advance(g, n):
                if g is None:
                    return
                for _ in range(n):
                    try:
                        next(g)
                    except StopIteration:
                        return

            exhaust(prep(0))
            ui = 0
            for t in range(NT):
                p = t % 2
                K = lambda n: "%s%d" % (n, p)
                ts = slice(t * 128, (t + 1) * 128)
                h2t, xt, idxi, gg = h2ts[p], xts[p], idxis[p], ggs[p]
                nxt = prep(t + 1) if t + 1 < NT else None
                for r in range(128):
                    ub = ubs[ui % NB]; kub = "pub%d" % (ui % NB)
                    jk = junks[ui % 2]; kjk = "pjunk%d" % (ui % 2)
                    tb_ = tmpbs[ui % 3]; ktb = "ptmpb%d" % (ui % 3)
                    ui += 1
                    ka, kg, kga = "paa%d" % r, "pgl%d" % r, "pga%d" % r
                    S.dma("pool", lambda e, ub=ub, r=r, idxi=idxi: e.indirect_dma_start(out=ub[:], out_offset=None, in_=UV2[:, :], in_offset=bass.IndirectOffsetOnAxis(ap=idxi[:, r:r + 1], axis=0)),
                          reads=[K("pidxi"), "uvb"], writes=[kub])
                    S.op("dve", lambda e, ub=ub, r=r, h2t=h2t, jk=jk: e.scalar_tensor_tensor(out=jk[:], in0=ub[:, 0:D], scalar=1.0, in1=h2t[:], op0=ALU.mult, op1=ALU.mult, accum_out=aa[:, r:r + 1]),
                         reads=[kub, K("ph2t")], writes=[kjk, ka])
                    S.op("act", lambda e, r=r: e.activation(out=gl[:, r:r + 1], in_=aa[:, r:r + 1], func=AF.Gelu_apprx_tanh), reads=[ka], writes=[kg])
                    S.op("act", lambda e, r=r, gg=gg: e.activation(out=ga[:, r:r + 1], in_=gl[:, r:r + 1], func=AF.Identity, scale=gg[:, r:r + 1]), reads=[kg, K("pgg")], writes=[kga])
                    S.op("act", lambda e, ub=ub, r=r, tb_=tb_: e.activation(out=tb_[:], in_=ub[:, D:2 * D], func=AF.Identity, scale=ga[:, r:r + 1]), reads=[kub, kga], writes=[ktb])
                    for dh in range(2):
                        S.op("pe", lambda e, tb_=tb_, dh=dh, r=r: e.matmul(pacc[:, dh * 512:(dh + 1) * 512], lhsT=self.ident_b(), rhs=tb_[:, dh * 512:(dh + 1) * 512], start=(r == 0), stop=(r == 127)),
                             reads=["cstb", ktb], writes=["ppacc"])
                    advance(nxt, 1)
                for dh in range(2):
                    S.op("dve", lambda e, dh=dh: e.tensor_tensor(out=acc[:, dh * 512:(dh + 1) * 512], in0=pacc[:, dh * 512:(dh + 1) * 512], in1=g2bc[:, dh * 512:(dh + 1) * 512], op=ALU.mult),
                         reads=["ppacc", "g2bc"], writes=["pacc"])
                S.op("dve", lambda e, xt=xt: e.tensor_tensor(out=xt[:], in0=xt[:], in1=acc[:], op=ALU.add), reads=["pacc", K("pxt")], writes=[K("pxt")])
                S.dma("sp", lambda e, xt=xt, ts=ts: e.dma_start(out=self.xres[ts, :], in_=xt[:]), reads=[K("pxt")], writes=["xres"])
                exhaust(nxt)

    def stage_final(self):
        nc, S = self.nc, self.S
        with ExitStack() as es:
            sb = lambda n, sh, dt: es.enter_context(self.sbt(n, sh, dt))
            o, _ = SM_OFF["fg"]
            fgbc = self._bcast_tile(es, "fgbc", lambda c: self.sm[:, o + c:o + c + 1])
            xts = [sb("fxt%d" % i, [128, D], F32) for i in range(2)]
            sq = sb("fsq", [128, D], F32)
            ss = sb("fss", [128, 4], F32)
            for t in range(NT):
                xt = xts[t % 2]; kxt = "fxt%d" % (t % 2)
                S.dma("sp", lambda e, xt=xt, t=t: e.dma_start(out=xt[:], in_=self.xres[t * 128:(t + 1) * 128, :]), reads=["xres"], writes=[kxt])
                S.op("act", lambda e, xt=xt: e.activation(out=sq[:], in_=xt[:], func=AF.Square, accum_out=ss[:, 0:1]), reads=[kxt], writes=["fsq", "fss"])
                S.op("dve", lambda e: e.tensor_scalar(out=ss[:, 1:2], in0=ss[:, 0:1], scalar1=1.0 / D, scalar2=EPS, op0=ALU.mult, op1=ALU.add), reads=["fss"], writes=["fss"])
                S.op("act", lambda e: e.activation(out=ss[:, 2:3], in_=ss[:, 1:2], func=AF.Sqrt), reads=["fss"], writes=["fss"])
                S.op("dve", lambda e: e.reciprocal(out=ss[:, 3:4], in_=ss[:, 2:3]), reads=["fss"], writes=["fss"])
                S.op("dve", lambda e, xt=xt: e.scalar_tensor_tensor(out=xt[:], in0=xt[:], scalar=ss[:, 3:4], in1=fgbc[:], op0=ALU.mult, op1=ALU.mult), reads=[kxt, "fss", "fgbc"], writes=[kxt])
                S.dma("sp", lambda e, xt=xt, t=t: e.dma_start(out=self.out_d[t * 128:(t + 1) * 128, :], in_=xt[:]), reads=[kxt], writes=["out_d"])


def prep_inputs(inputs):
    inp = {k: np.ascontiguousarray(np.asarray(v)) for k, v in inputs.items()}
    consts = make_consts()
    keys = inp["pk_keys"]
    kbd = np.zeros((DEPTH, 8, 128, 256), np.float32)
    for p in range(2):
        kbd[:, :, p * 64:(p + 1) * 64, p * 128:(p + 1) * 128] = keys[:, :, p].transpose(0, 1, 3, 2)
    shared = dict(consts=consts, mod_w=inp["mod_w"], w_in=inp["w_in"], w_branch=inp["w_branch"], w_out=inp["w_out"],
                  pk_wq=inp["pk_wq"], keysbd=kbd,
                  pk_uv=np.concatenate([inp["pk_u"], inp["pk_v"]], axis=2))
    in_maps = []
    for b in range(8):
        m = dict(shared)
        m["x"] = inp["x"][b]
        m["small"] = make_small(inp, b)
        in_maps.append(m)
    return in_maps


_PROG_CACHE = {}


def kernel(**inputs):
    in_maps = prep_inputs(inputs)
    if "nc" not in _PROG_CACHE:
        _PROG_CACHE["nc"] = Prog().build()
    res = run_bass_kernel_spmd(_PROG_CACHE["nc"], in_maps, core_ids=list(range(8)))
    return np.stack([np.asarray(r["out"]) for r in res.results], axis=0).astype(np.float32)
```

```python
from contextlib import ExitStack
import numpy as np
import concourse.bass as bass
import concourse.mybir as mybir
from concourse.bass_utils import run_bass_kernel_spmd

F32 = mybir.dt.float32
BF16 = mybir.dt.bfloat16
U32 = mybir.dt.uint32
I32 = mybir.dt.int32
AF = mybir.ActivationFunctionType
ALU = mybir.AluOpType
AX = mybir.AxisListType

D = 1024
S_LEN = 2048
DEPTH = 4
NT = S_LEN // 128
INW = 8712
EPS = 1e-6
PEER_IMPLEMENTED = True


class Sched:
    ENG = ("pe", "act", "dve", "pool", "sp")

    def __init__(self, nc, dma_slots=None, same_engine_sync=True):
        self.nc = nc
        self.ops = {e: [] for e in self.ENG}
        self.count = {e: 0 for e in self.ENG}
        self.sem = {}
        self.last_w = {}
        self.readers = {}
        self.same_engine_sync = same_engine_sync
        self.dma_slots_n = dma_slots or {"sp": 8, "pool": 12, "act": 4}
        self.dma_sems = {}
        self.dma_rr = {}
        self.seen = {e: {} for e in self.ENG}
        self._ctx = []
        self.n_ops = 0

    def open(self):
        nc = self.nc
        for e in self.ENG:
            cm = nc.semaphore("s_" + e)
            self.sem[e] = cm.__enter__()
            self._ctx.append(cm)
        for q, n in self.dma_slots_n.items():
            self.dma_sems[q] = []
            self.dma_rr[q] = 0
            for i in range(n):
                cm = nc.semaphore("sd_%s%d" % (q, i))
                s = cm.__enter__()
                self._ctx.append(cm)
                self.dma_sems[q].append(dict(sem=s, count=0, name="d_%s%d" % (q, i)))

    def close(self):
        for cm in reversed(self._ctx):
            cm.__exit__(None, None, None)

    def _tok_wait(self, tok):
        if tok[0] == "c":
            return (self.sem[tok[1]], tok[1], tok[2])
        d = self.dma_sems[tok[1]][tok[2]]
        return (d["sem"], d["name"], tok[3])

    def _collect(self, eng, reads, writes):
        toks = []
        for k in reads:
            t = self.last_w.get(k)
            if t is not None:
                toks.append(t)
        for k in writes:
            t = self.last_w.get(k)
            if t is not None:
                toks.append(t)
            toks.extend(self.readers.get(k, ()))
        waits = {}
        for t in toks:
            if t[0] == "c" and t[1] == eng and not self.same_engine_sync:
                continue
            s, name, v = self._tok_wait(t)
            if self.seen[eng].get(name, 0) >= v:
                continue
            if name not in waits or waits[name][1] < v:
                waits[name] = (s, v)
        for name, (s, v) in waits.items():
            self.seen[eng][name] = v
        return list(waits.values())

    def _commit(self, tok, reads, writes):
        for k in reads:
            self.readers.setdefault(k, []).append(tok)
        for k in writes:
            self.last_w[k] = tok
            self.readers[k] = []

    def op(self, eng, fn, reads=(), writes=()):
        waits = self._collect(eng, reads, writes)
        self.count[eng] += 1
        tok = ("c", eng, self.count[eng])
        self.ops[eng].append((fn, waits, "c", None))
        self._commit(tok, reads, writes)
        self.n_ops += 1
        return tok

    def dma(self, eng, fn, reads=(), writes=()):
        slot = self.dma_rr[eng]
        self.dma_rr[eng] = (slot + 1) % len(self.dma_sems[eng])
        d = self.dma_sems[eng][slot]
        waits = self._collect(eng, reads, writes)
        if d["count"] > 0 and self.seen[eng].get(d["name"], 0) < d["count"]:
            waits.append((d["sem"], d["count"]))
            self.seen[eng][d["name"]] = d["count"]
        d["count"] += 16
        tok = ("d", eng, slot, d["count"])
        self.ops[eng].append((fn, waits, "d", d["sem"]))
        self._commit(tok, reads, writes)
        self.n_ops += 1
        return tok

    def barrier(self):
        for e in self.ENG:
            waits = []
            for e2 in self.ENG:
                v = self.count[e2]
                if v > 0 and self.seen[e].get(e2, 0) < v and e2 != e:
                    waits.append((self.sem[e2], v))
                    self.seen[e][e2] = v
            for q in self.dma_sems:
                for d in self.dma_sems[q]:
                    if d["count"] > 0 and self.seen[e].get(d["name"], 0) < d["count"]:
                        waits.append((d["sem"], d["count"]))
                        self.seen[e][d["name"]] = d["count"]
            if self.count[e] > 0:
                waits.append((self.sem[e], self.count[e]))
                self.seen[e][e] = self.count[e]
            self.ops[e].append((None, waits, "w", None))

    def emit(self):
        nc = self.nc
        sched = self
        ops = self.ops
        self.ops = {e: [] for e in self.ENG}
        with nc.Block() as block:
            def run(engname, e):
                for fn, waits, kind, dsem in ops[engname]:
                    for s, v in waits:
                        e.wait_ge(s, v)
                    if fn is None:
                        continue
                    ins = fn(e)
                    if kind == "c":
                        ins.then_inc(sched.sem[engname], 1)
                    else:
                        ins.then_inc(dsem, 16)

            @block.sync
            def _(e):
                run("sp", e)

            @block.tensor
            def _(e):
                run("pe", e)

            @block.scalar
            def _(e):
                run("act", e)

            @block.vector
            def _(e):
                run("dve", e)

            @block.gpsimd
            def _(e):
                run("pool", e)


def _small_layout():
    off = {}
    o = 0
    for name, n in (("mod_b", DEPTH * 48), ("nmg", DEPTH * 8), ("nfg", DEPTH * 8),
                    ("convw", DEPTH * 32), ("convb", DEPTH * 8), ("gateb", DEPTH * 2),
                    ("lbl", DEPTH * 4), ("hgn", DEPTH * 4), ("mln", DEPTH * 4), ("fg", 8), ("cT", 8)):
        off[name] = (o, n)
        o += n
    return off, o


SM_OFF, NS = _small_layout()
C_ID, C_ONES, C_TRI, C_M64, C_SEL, C_IOTA, C_THR = 0, 128, 256, 384, 448, 960, 976
NCONST = 992


def make_consts():
    c = np.zeros((128, NCONST), np.float32)
    c[:, C_ID:C_ID + 128] = np.eye(128, dtype=np.float32)
    c[:, C_ONES:C_ONES + 128] = 1.0
    sp = np.arange(128)[:, None]
    s = np.arange(128)[None, :]
    c[:, C_TRI:C_TRI + 128] = (sp >= s).astype(np.float32)
    t = np.arange(64)[None, :]
    c[:, C_M64:C_M64 + 64] = (t >= (sp % 64)).astype(np.float32)
    for h in range(4):
        c[h, C_SEL + h * 128:C_SEL + (h + 1) * 128] = 1.0
    c[:, C_IOTA:C_IOTA + 16] = np.arange(16, dtype=np.float32)[None, :]
    c[:, C_THR:C_THR + 15] = 16.0 * np.arange(1, 16, dtype=np.float32)[None, :]
    return c


def make_small(inp, b):
    sm = np.zeros((128, NS), np.float32)

    def put(name, arr):
        o, n = SM_OFF[name]
        arr = np.asarray(arr, np.float32).reshape(128, n)
        sm[:, o:o + n] = arr

    put("mod_b", inp["mod_b"].reshape(DEPTH, 48, 128).transpose(2, 0, 1))
    put("nmg", inp["norm_mix_g"].reshape(DEPTH, 8, 128).transpose(2, 0, 1))
    put("nfg", inp["norm_ffn_g"].reshape(DEPTH, 8, 128).transpose(2, 0, 1))
    put("convw", inp["ml_conv_w"].reshape(DEPTH, 4, 8, 128).transpose(3, 0, 1, 2))
    put("convb", inp["ml_conv_b"].reshape(DEPTH, 8, 128).transpose(2, 0, 1))
    gb = np.zeros((128, DEPTH, 2), np.float32)
    gb[0:4, :, 0] = inp["ml_gate_b"][:, 0:4].T
    gb[0:4, :, 1] = inp["ml_gate_b"][:, 4:8].T
    put("gateb", gb)
    put("lbl", inp["hg_lb_logits"].reshape(DEPTH, 4, 128).transpose(2, 0, 1))
    put("hgn", inp["hg_norm_g"].reshape(DEPTH, 4, 128).transpose(2, 0, 1))
    put("mln", inp["ml_norm_g"].reshape(DEPTH, 4, 128).transpose(2, 0, 1))
    put("fg", inp["final_g"].reshape(8, 128).T)
    put("cT", inp["c"][b].reshape(8, 128).T)
    return sm


class Prog:
    def __init__(self, n_layers=DEPTH, stages=None, debug=()):
        self.n_layers = n_layers
        self.stages = stages
        self.debug = set(debug)
        self.nc = bass.Bass("TRN2", target_bir_lowering=False)
        self.S = Sched(self.nc)
        self.uid = 0

    def sbt(self, name, shape, dt):
        self.uid += 1
        return self.nc.sbuf_tensor("%s_u%d" % (name, self.uid), shape, dt)

    def pst(self, name, shape, dt):
        self.uid += 1
        return self.nc.psum_tensor("%s_u%d" % (name, self.uid), shape, dt)

    def want(self, st):
        return self.stages is None or st in self.stages

    def dram(self, name, shape, dt, kind=None):
        if kind is None:
            kind = "ExternalOutput" if name in self.debug else "Internal"
        return self.nc.dram_tensor(name, list(shape), dt, kind=kind).ap()

    def build(self):
        nc, S = self.nc, self.S
        L = self.n_layers
        ext = lambda n, s, dt=F32: nc.dram_tensor(n, list(s), dt, kind="ExternalInput").ap()
        self.x_in = ext("x", [S_LEN, D])
        self.small_d = ext("small", [128, NS])
        self.consts_d = ext("consts", [128, NCONST])
        self.mod_w = ext("mod_w", [L, D, 6 * D])
        self.w_in = ext("w_in", [L, D, INW])
        self.input_names = ["x", "small", "consts", "mod_w", "w_in"]
        if self.want("merge"):
            self.w_branch = ext("w_branch", [L, 3, 512, D])
            self.w_out = ext("w_out", [L, D, D])
            self.input_names += ["w_branch", "w_out"]
        if self.want("peer") and PEER_IMPLEMENTED:
            self.pk_wq = ext("pk_wq", [L, D, D])
            self.keysbd = ext("keysbd", [L, 8, 128, 256])
            self.pk_uv = ext("pk_uv", [L, 16384, 2 * D])
            self.input_names += ["pk_wq", "keysbd", "pk_uv"]
        self.out_d = nc.dram_tensor("out", [S_LEN, D], F32, kind="ExternalOutput").ap()
        self.xres = self.dram("xres", [S_LEN, D], F32)
        self.hT_d = self.dram("hT", [8, 128, S_LEN], BF16)
        self.qT_d = self.dram("qT", [512, S_LEN], BF16)
        self.kT_d = self.dram("kT", [512, S_LEN], BF16)
        self.v_d = self.dram("v_tok", [S_LEN, 512], BF16)
        self.hqT_d = self.dram("hqT", [512, S_LEN], F32)
        self.hfT_d = self.dram("hfT", [512, S_LEN], F32)
        self.hgT_d = self.dram("hgT", [512, S_LEN], F32)
        self.hi_d = self.dram("hi_tok", [S_LEN, 512], BF16)
        self.mqkT_d = self.dram("mqkT", [1024, S_LEN], F32)
        self.mv_d = self.dram("mv_tok", [S_LEN, 512], BF16)
        self.moT_d = self.dram("moT", [512, S_LEN], F32)
        self.gT_d = self.dram("gT", [2, 4, S_LEN], F32)
        self.bgT_d = self.dram("bgT", [3072, S_LEN], F32)
        self.yT_d = self.dram("yT", [3, 512, S_LEN], BF16)
        self.h2_d = self.dram("h2_tok", [S_LEN, D], F32)
        self.uvb = self.dram("uvb", [16384, 2 * D], BF16)
        S.open()
        with (self.sbt("consts", [128, NCONST], F32) as cst,
              self.sbt("constb", [128, 384], BF16) as cstb,
              self.sbt("small", [128, NS], F32) as sm,
              self.sbt("modT", [128, DEPTH * 48], F32) as modT,
              self.sbt("lbT", [128, DEPTH * 4], F32) as lbT):
            self.cst, self.cstb, self.sm, self.modT, self.lbT = cst, cstb, sm, modT, lbT
            S.dma("sp", lambda e: e.dma_start(out=cst[:], in_=self.consts_d[:, :]), writes=["cst"])
            S.dma("sp", lambda e: e.dma_start(out=sm[:], in_=self.small_d[:, :]), writes=["sm"])
            S.op("dve", lambda e: e.tensor_copy(out=cstb[:], in_=cst[:, 0:384]), reads=["cst"], writes=["cstb"])
            S.dma("sp", lambda e: e.dma_start(out=self.xres[:, :], in_=self.x_in[:, :]), writes=["xres"])
            self.stage_mod()
            S.barrier(); S.emit()
            for l in range(L):
                if self.want("norm1"):
                    self.stage_norm(l, 0)
                    S.barrier(); S.emit()
                if self.want("proj"):
                    with ExitStack() as es2:
                        if self.want("peer") and PEER_IMPLEMENTED:
                            self._uvc_bufs = [es2.enter_context(self.sbt("uvc%d" % i, [128, 4, 2 * D], BF16)) for i in range(3)]
                        self.stage_proj(l)
                        S.barrier(); S.emit()
                if self.want("attn"):
                    self.stage_attn(l)
                    S.barrier(); S.emit()
                if self.want("hgrn"):
                    self.stage_hgrn(l)
                    S.barrier(); S.emit()
                if self.want("mlstm"):
                    self.stage_mlstm(l)
                    S.barrier(); S.emit()
                if self.want("merge"):
                    self.stage_merge(l)
                    S.barrier(); S.emit()
                if self.want("peer") and PEER_IMPLEMENTED:
                    self.stage_norm(l, 1)
                    S.barrier(); S.emit()
                    self.stage_peer(l)
                    S.barrier(); S.emit()
            self.stage_final()
            S.barrier(); S.emit()
        S.close()
        return nc

    def ident_f(self):
        return self.cst[:, C_ID:C_ID + 128]

    def ones_f(self):
        return self.cst[:, C_ONES:C_ONES + 128]

    def tri_f(self):
        return self.cst[:, C_TRI:C_TRI + 128]

    def ident_b(self):
        return self.cstb[:, 0:128]

    def ones_b(self):
        return self.cstb[:, 128:256]

    def smc(self, name, l, j, n=1):
        o, tot = SM_OFF[name]
        per = tot // DEPTH
        return self.sm[:, o + l * per + j: o + l * per + j + n]

    def modc(self, l, part, c):
        j = l * 48 + part * 8 + c
        return self.modT[:, j:j + 1]

    def stage_mod(self):
        nc, S = self.nc, self.S
        L = self.n_layers
        sm = self.sm
        with (self.sbt("condT", [128, 8], F32) as condT,
              self.sbt("mw0", [128, 8, 768], F32) as mw0,
              self.sbt("mw1", [128, 8, 768], F32) as mw1,
              self.sbt("lbe", [128, DEPTH * 4], F32) as lbe,
              self.sbt("lbs", [128, 4], F32) as lbs,
              self.sbt("lbm", [128, 4], F32) as lbm,
              self.pst("ps_mod", [128, DEPTH * 48], F32) as psm):
            o, _ = SM_OFF["cT"]
            S.op("act", lambda e: e.activation(out=condT[:], in_=sm[:, o:o + 8], func=AF.Silu), reads=["sm"], writes=["condT"])
            mws = [mw0, mw1]
            gi = 0
            for l in range(L):
                for g in range(8):
                    mw = mws[gi % 2]
                    key = "mw%d" % (gi % 2)
                    gi += 1
                    src = self.mod_w[l, :, g * 768:(g + 1) * 768].rearrange("(kc p) c -> p kc c", p=128)
                    S.dma("sp", lambda e, mw=mw, src=src: e.dma_start(out=mw[:], in_=src), writes=[key])
                    for cc in range(6):
                        j = l * 48 + g * 6 + cc
                        for kc in range(8):
                            S.op("pe", lambda e, mw=mw, cc=cc, kc=kc, j=j: e.matmul(
                                psm[:, j:j + 1], lhsT=mw[:, kc, cc * 128:(cc + 1) * 128], rhs=condT[:, kc:kc + 1],
                                start=(kc == 0), stop=(kc == 7)), reads=[key, "condT"], writes=["psm"])
            ob, _ = SM_OFF["mod_b"]
            S.op("dve", lambda e: e.tensor_tensor(out=self.modT[:, 0:L * 48], in0=psm[:, 0:L * 48], in1=sm[:, ob:ob + L * 48], op=ALU.add),
                 reads=["psm", "sm"], writes=["modT"])
            ol, _ = SM_OFF["lbl"]
            lg = lambda l: sm[:, ol + l * 4: ol + l * 4 + 4]
            S.op("dve", lambda e: e.tensor_tensor(out=lbm[:], in0=lg(0), in1=lg(1), op=ALU.max), reads=["sm"], writes=["lbm"])
            for l in (2, 3):
                S.op("dve", lambda e, l=l: e.tensor_tensor(out=lbm[:], in0=lbm[:], in1=lg(l), op=ALU.max), reads=["sm", "lbm"], writes=["lbm"])
            for l in range(DEPTH):
                S.op("dve", lambda e, l=l: e.tensor_tensor(out=lbe[:, l * 4:l * 4 + 4], in0=lg(l), in1=lbm[:], op=ALU.subtract), reads=["sm", "lbm"], writes=["lbe"])
            S.op("act", lambda e: e.activation(out=lbe[:], in_=lbe[:], func=AF.Exp), reads=["lbe"], writes=["lbe"])
            S.op("dve", lambda e: e.tensor_tensor(out=lbs[:], in0=lbe[:, 0:4], in1=lbe[:, 4:8], op=ALU.add), reads=["lbe"], writes=["lbs"])
            for l in (2, 3):
                S.op("dve", lambda e, l=l: e.tensor_tensor(out=lbs[:], in0=lbs[:], in1=lbe[:, l * 4:l * 4 + 4], op=ALU.add), reads=["lbe", "lbs"], writes=["lbs"])
            S.op("dve", lambda e: e.reciprocal(out=lbs[:], in_=lbs[:]), reads=["lbs"], writes=["lbs"])
            lbT = self.lbT
            S.op("dve", lambda e: e.memset(lbT[:, 0:4], 0.0), writes=["lbT"])
            for l in range(1, DEPTH):
                S.op("dve", lambda e, l=l: e.tensor_tensor(out=lbe[:, l * 4:l * 4 + 4], in0=lbe[:, l * 4:l * 4 + 4], in1=lbs[:], op=ALU.mult), reads=["lbe", "lbs"], writes=["lbe"])
                S.op("dve", lambda e, l=l: e.tensor_tensor(out=lbT[:, l * 4:l * 4 + 4], in0=lbT[:, (l - 1) * 4:l * 4], in1=lbe[:, l * 4:l * 4 + 4], op=ALU.add), reads=["lbe", "lbT"], writes=["lbT"])

    def stage_norm(self, l, which):
        nc, S = self.nc, self.S
        gname = "nmg" if which == 0 else "nfg"
        p_shift, p_scale = (0, 1) if which == 0 else (3, 4)
        with (self.sbt("nx0", [128, D], F32) as nx0, self.sbt("nx1", [128, D], F32) as nx1,
              self.sbt("nsq", [128, 2, D], BF16) as nsq,
              self.sbt("nb0", [128, D], BF16) as nb0, self.sbt("nb1", [128, D], BF16) as nb1,
              self.sbt("nh0", [128, 8, 128], BF16) as nh0, self.sbt("nh1", [128, 8, 128], BF16) as nh1,
              self.sbt("nt0", [128, D], F32) as nt0, self.sbt("nt1", [128, D], F32) as nt1,
              self.sbt("nss", [128, 8], F32) as nss_all,
              self.sbt("nG", [128, 8], F32) as nG,
              self.pst("nps0", [128, 8, 128], BF16) as nps0, self.pst("nps1", [128, 8, 128], BF16) as nps1,
              self.pst("npt0", [128, 8, 128], F32) as npt0):
            j0 = l * 48 + p_scale * 8
            S.op("dve", lambda e: e.scalar_tensor_tensor(out=nG[:], in0=self.modT[:, j0:j0 + 8], scalar=1.0, in1=self.smc(gname, l, 0, 8),
                                                         op0=ALU.add, op1=ALU.mult), reads=["modT", "sm"], writes=["nG"])
            nx, nb, nh, nps, nt = [nx0, nx1], [nb0, nb1], [nh0, nh1], [nps0, nps1], [nt0, nt1]
            for t in range(NT):
                i = t % 2
                kx, kb, kh, kp, kt = "nx%d" % i, "nb%d" % i, "nh%d" % i, "nps%d" % i, "nt%d" % i
                nss = nss_all[:, 4 * i:4 * i + 4]; kss = "nss%d" % i; ksq = "nsq%d" % i
                S.dma("sp", lambda e, i=i, t=t: e.dma_start(out=nx[i][:], in_=self.xres[t * 128:(t + 1) * 128, :]), reads=["xres"], writes=[kx])
                S.op("act", lambda e, i=i, nss=nss: e.activation(out=nsq[:, i, :], in_=nx[i][:], func=AF.Square, accum_out=nss[:, 0:1]), reads=[kx], writes=[ksq, kss])
                S.op("dve", lambda e, nss=nss: e.tensor_scalar(out=nss[:, 1:2], in0=nss[:, 0:1], scalar1=1.0 / D, scalar2=EPS, op0=ALU.mult, op1=ALU.add), reads=[kss], writes=[kss])
                S.op("act", lambda e, nss=nss: e.activation(out=nss[:, 2:3], in_=nss[:, 1:2], func=AF.Sqrt), reads=[kss], writes=[kss])
                S.op("dve", lambda e, nss=nss: e.reciprocal(out=nss[:, 3:4], in_=nss[:, 2:3]), reads=[kss], writes=[kss])
                S.op("dve", lambda e, i=i, nss=nss: e.tensor_scalar(out=nb[i][:], in0=nx[i][:], scalar1=nss[:, 3:4], scalar2=None, op0=ALU.mult), reads=[kx, kss], writes=[kb])
                for c in range(8):
                    S.op("pe", lambda e, i=i, c=c: e.transpose(nps[i][:, c, :], nb[i][:, c * 128:(c + 1) * 128], self.ident_b()), reads=[kb, "cstb"], writes=[kp])
                for c in range(8):
                    S.op("act", lambda e, i=i, c=c: e.activation(out=nh[i][:, c, :], in_=nps[i][:, c, :], func=AF.Identity,
                                                                 scale=nG[:, c:c + 1], bias=self.modc(l, p_shift, c)), reads=[kp, "nG", "modT"], writes=[kh])
                S.dma("sp", lambda e, i=i, t=t: e.dma_start(out=self.hT_d[:, :, t * 128:(t + 1) * 128].rearrange("c p t -> p c t"), in_=nh[i][:]), reads=[kh], writes=["hT_d"])
                if which == 1:
                    pass
            if which == 1:
                self._h2_tokmajor(l, nx, nss_all[:, 0:4], nG, nt, npt0, p_shift)

    def _h2_tokmajor(self, l, nx, nss, nG, nt, npt0, p_shift):
        nc, S = self.nc, self.S
        with (self.sbt("dg", [128, 128], F32) as dg,
              self.sbt("Gbc", [128, D], F32) as Gbc, self.sbt("Sbc", [128, D], F32) as Sbc):
            for which_v, dst in ((0, Gbc), (1, Sbc)):
                for c in range(8):
                    col = nG[:, c:c + 1] if which_v == 0 else self.modc(l, p_shift, c)
                    S.op("dve", lambda e, col=col: e.tensor_scalar(out=dg[:], in0=self.ident_f(), scalar1=col, scalar2=None, op0=ALU.mult), reads=["cst", "nG", "modT"], writes=["dg"])
                    S.op("pe", lambda e, c=c: e.matmul(npt0[:, c, :], lhsT=self.ones_f(), rhs=dg[:], start=True, stop=True), reads=["cst", "dg"], writes=["npt0"])
                S.op("act", lambda e, dst=dst: e.activation(out=dst[:], in_=npt0[:].rearrange("p c t -> p (c t)"), func=AF.Copy), reads=["npt0"], writes=["bc%d" % which_v])
            for t in range(NT):
                i = t % 2
                kx, kt = "nx%d" % i, "nt%d" % i
                S.dma("sp", lambda e, i=i, t=t: e.dma_start(out=nx[i][:], in_=self.xres[t * 128:(t + 1) * 128, :]), reads=["xres"], writes=[kx])
                S.op("act", lambda e, i=i: e.activation(out=nt[i][:], in_=nx[i][:], func=AF.Square, accum_out=nss[:, 0:1]), reads=[kx], writes=[kt, "nss0"])
                S.op("dve", lambda e: e.tensor_scalar(out=nss[:, 1:2], in0=nss[:, 0:1], scalar1=1.0 / D, scalar2=EPS, op0=ALU.mult, op1=ALU.add), reads=["nss0"], writes=["nss0"])
                S.op("act", lambda e: e.activation(out=nss[:, 2:3], in_=nss[:, 1:2], func=AF.Sqrt), reads=["nss0"], writes=["nss0"])
                S.op("dve", lambda e: e.reciprocal(out=nss[:, 3:4], in_=nss[:, 2:3]), reads=["nss0"], writes=["nss0"])
                S.op("dve", lambda e, i=i: e.scalar_tensor_tensor(out=nt[i][:], in0=nx[i][:], scalar=nss[:, 3:4], in1=Gbc[:], op0=ALU.mult, op1=ALU.mult), reads=[kx, "nss0", "bc0"], writes=[kt])
                S.op("dve", lambda e, i=i: e.tensor_tensor(out=nt[i][:], in0=nt[i][:], in1=Sbc[:], op=ALU.add), reads=[kt, "bc1"], writes=[kt])
                S.dma("sp", lambda e, i=i, t=t: e.dma_start(out=self.h2_d[t * 128:(t + 1) * 128, :], in_=nt[i][:]), reads=[kt], writes=["h2_d"])

    def stage_proj(self, l):
        nc, S = self.nc, self.S
        fm = []
        for c in range(4):
            fm.append((0 + c * 128, 128, self.qT_d[c * 128:(c + 1) * 128, :], AF.Copy, 0.125, BF16))
        for c in range(4):
            fm.append((512 + c * 128, 128, self.kT_d[c * 128:(c + 1) * 128, :], AF.Copy, 1.0, BF16))
        for c in range(4):
            fm.append((1536 + c * 128, 128, self.hqT_d[c * 128:(c + 1) * 128, :], AF.Copy, 1.0, F32))
        for c in range(4):
            fm.append((2048 + c * 128, 128, self.hfT_d[c * 128:(c + 1) * 128, :], AF.Copy, 1.0, F32))
        for c in range(4):
            fm.append((3072 + c * 128, 128, self.hgT_d[c * 128:(c + 1) * 128, :], AF.Silu, 1.0, F32))
        for c in range(8):
            fm.append((3584 + c * 128, 128, self.mqkT_d[c * 128:(c + 1) * 128, :], AF.Copy, 1.0, F32))
        for c in range(4):
            fm.append((5120 + c * 128, 128, self.moT_d[c * 128:(c + 1) * 128, :], AF.Sigmoid, 1.0, F32))
        fm.append((5632, 4, self.gT_d[0, :, :], AF.Copy, 1.0, F32))
        fm.append((5636, 4, self.gT_d[1, :, :], AF.Copy, 1.0, F32))
        for c in range(24):
            fm.append((5640 + c * 128, 128, self.bgT_d[c * 128:(c + 1) * 128, :], AF.Sigmoid, 1.0, F32))
        tm = [(1024, self.v_d, AF.Copy), (2560, self.hi_d, AF.Silu), (4608, self.mv_d, AF.Copy)]
        with (self.sbt("hT", [128, 8, S_LEN], BF16) as hT,
              self.sbt("pw0", [128, 8, 512], BF16) as pw0, self.sbt("pw1", [128, 8, 512], BF16) as pw1,
              self.sbt("pof0", [128, S_LEN], F32) as pof0, self.sbt("pof1", [128, S_LEN], F32) as pof1,
              self.sbt("pob0", [128, S_LEN], BF16) as pob0, self.sbt("pob1", [128, S_LEN], BF16) as pob1,
              self.sbt("pot0", [128, 512], BF16) as pot0, self.sbt("pot1", [128, 512], BF16) as pot1,
              self.pst("pp0", [128, 512], F32) as pp0, self.pst("pp1", [128, 512], F32) as pp1,
              self.pst("pp2", [128, 512], F32) as pp2, self.pst("pp3", [128, 512], F32) as pp3):
            for c in range(8):
                S.dma("sp", lambda e, c=c: e.dma_start(out=hT[:, c, :], in_=self.hT_d[c, :, :]), reads=["hT_d"], writes=["hT"])
            pw, pof, pob, pot, pp = [pw0, pw1], [pof0, pof1], [pob0, pob1], [pot0, pot1], [pp0, pp1, pp2, pp3]
            side = self.uvcast_gen(l, self._uvc_bufs) if (self.want("peer") and PEER_IMPLEMENTED) else None
            wi = 0
            pi = 0
            for ji, (col0, ncol, dest, func, scale, dt) in enumerate(fm):
                w = pw[wi % 2]; kw = "pw%d" % (wi % 2); wi += 1
                src = self.w_in[l, :, col0:col0 + ncol].rearrange("(kc p) c -> p kc c", p=128)
                S.dma("pool", lambda e, w=w, src=src, ncol=ncol: e.dma_start(out=w[:, :, 0:ncol], in_=src), writes=[kw])
                ob = (pof if dt == F32 else pob)[ji % 2]
                ko = ("pof%d" if dt == F32 else "pob%d") % (ji % 2)
                for tb in range(4):
                    ps = pp[pi % 4]; kp = "pp%d" % (pi % 4); pi += 1
                    for kc in range(8):
                        S.op("pe", lambda e, ps=ps, w=w, kc=kc, tb=tb, ncol=ncol: e.matmul(
                            ps[0:ncol, :], lhsT=w[:, kc, 0:ncol], rhs=hT[:, kc, tb * 512:(tb + 1) * 512],
                            start=(kc == 0), stop=(kc == 7)), reads=[kw, "hT"], writes=[kp])
                    S.op("act", lambda e, ps=ps, ob=ob, tb=tb, ncol=ncol, func=func, scale=scale: e.activation(
                        out=ob[0:ncol, tb * 512:(tb + 1) * 512], in_=ps[0:ncol, :], func=func, scale=scale), reads=[kp], writes=[ko])
                S.dma("sp", lambda e, ob=ob, dest=dest, ncol=ncol: e.dma_start(out=dest, in_=ob[0:ncol, :]), reads=[ko], writes=["projout"])
                if side is not None:
                    next(side, None)
            for (col0, dest, func) in tm:
                w = pw[wi % 2]; kw = "pw%d" % (wi % 2); wi += 1
                src = self.w_in[l, :, col0:col0 + 512].rearrange("(kc p) c -> p kc c", p=128)
                S.dma("pool", lambda e, w=w, src=src: e.dma_start(out=w[:], in_=src), writes=[kw])
                for t in range(NT):
                    ps = pp[pi % 4]; kp = "pp%d" % (pi % 4); pi += 1
                    for kc in range(8):
                        S.op("pe", lambda e, ps=ps, w=w, kc=kc, t=t: e.matmul(
                            ps[:], lhsT=hT[:, kc, t * 128:(t + 1) * 128], rhs=w[:, kc, :],
                            start=(kc == 0), stop=(kc == 7)), reads=[kw, "hT"], writes=[kp])
                    ot = pot[t % 2]; kt = "pot%d" % (t % 2)
                    S.op("act", lambda e, ps=ps, ot=ot, func=func: e.activation(out=ot[:], in_=ps[:], func=func), reads=[kp], writes=[kt])
                    S.dma("sp", lambda e, ot=ot, dest=dest, t=t: e.dma_start(out=dest[t * 128:(t + 1) * 128, :], in_=ot[:]), reads=[kt], writes=["projout"])
            if side is not None:
                for _ in side:
                    pass

    def stage_attn(self, l):
        nc, S = self.nc, self.S
        NBUF = 3
        with ExitStack() as es:
            sb = lambda n, sh, dt: es.enter_context(self.sbt(n, sh, dt))
            pt = lambda n, sh, dt: es.enter_context(self.pst(n, sh, dt))
            aq = [sb("aq%d" % i, [64, S_LEN], BF16) for i in range(2)]
            ak = [sb("ak%d" % i, [64, S_LEN], BF16) for i in range(2)]
            av = [sb("av%d" % i, [128, NT, 64], BF16) for i in range(2)]
            azs = [sb("azs%d" % i, [128, 512], F32) for i in range(NBUF)]
            asp = [sb("asp%d" % i, [128, 512], F32) for i in range(NBUF)]
            asb = [sb("asb%d" % i, [128, 512], BF16) for i in range(NBUF)]
            alw = [sb("alw%d" % i, [128, 512], F32) for i in range(NBUF)]
            awt = [sb("awt%d" % i, [128, 512], BF16) for i in range(NBUF)]
            ayo = [sb("ayo%d" % i, [64, 512], BF16) for i in range(2)]
            apz = [pt("apz%d" % i, [128, 512], F32) for i in range(2)]
            apc = [pt("apc%d" % i, [128, 512], F32) for i in range(2)]
            apy = [pt("apy%d" % i, [64, 512], F32) for i in range(2)]
            apr = [pt("apr%d" % i, [128, 512], F32) for i in range(2)]
            tri_b = self.cstb[:, 256:384]
            steps = []
            yi = 0
            for h in range(8):
                for qb in range(4):
                    nkb = 4 * (qb + 1)
                    for jn, j in enumerate(reversed(range(nkb))):
                        steps.append(dict(h=h, qb=qb, nkb=nkb, jn=jn, j=j, yi=yi, i=len(steps)))
                    yi += 1
            loaded = set()

            def load_head(h):
                if h in loaded or h >= 8:
                    return
                loaded.add(h)
                hb = h % 2
                S.dma("sp", lambda e: e.dma_start(out=aq[hb][:], in_=self.qT_d[h * 64:(h + 1) * 64, :]), reads=["projout"], writes=["aq%d" % hb])
                S.dma("sp", lambda e: e.dma_start(out=ak[hb][:], in_=self.kT_d[h * 64:(h + 1) * 64, :]), reads=["projout"], writes=["ak%d" % hb])
                S.dma("sp", lambda e: e.dma_start(out=av[hb][:], in_=self.v_d[:, h * 64:(h + 1) * 64].rearrange("(j p) d -> p j d", p=128)), reads=["projout"], writes=["av%d" % hb])

            def phaseA(st):
                h, qb, j, i = st["h"], st["qb"], st["j"], st["i"]
                load_head(h)
                hb = h % 2
                q, k = aq[hb], ak[hb]
                b2, b3 = i % 2, i % NBUF
                pz, zs, sp, sbb = apz[b2], azs[b3], asp[b3], asb[b3]
                kz, kzs, ksp, ksb = "apz%d" % b2, "azs%d" % b3, "asp%d" % b3, "asb%d" % b3
                diag = j >= 4 * qb
                base = qb * 512 - j * 128
                S.op("pe", lambda e: e.matmul(pz[:], lhsT=k[:, j * 128:(j + 1) * 128], rhs=q[:, qb * 512:(qb + 1) * 512], start=True, stop=True), reads=["aq%d" % hb, "ak%d" % hb], writes=[kz])
                S.op("dve", lambda e: e.tensor_copy(out=zs[:], in_=pz[:]), reads=[kz], writes=[kzs])
                S.op("act", lambda e: e.activation(out=sp[:], in_=zs[:], func=AF.Exp), reads=[kzs], writes=[ksp])
                S.op("act", lambda e: e.activation(out=sbb[:], in_=sp[:], func=AF.Ln, bias=1.0), reads=[ksp], writes=[ksb])
                if diag:
                    S.op("pool", lambda e: e.affine_select(out=sbb[:], in_=sbb[:], pattern=[[1, 512]], compare_op=ALU.is_gt, fill=0.0, base=base, channel_multiplier=-1), reads=[ksb], writes=[ksb])

            def phaseB(st):
                qb, j, jn, i = st["qb"], st["j"], st["jn"], st["i"]
                b2, b3 = i % 2, i % NBUF
                pc, zs, sbb, lw, wt = apc[b2], azs[b3], asb[b3], alw[b3], awt[b3]
                kc_, kzs, ksb, klw, kwt = "apc%d" % b2, "azs%d" % b3, "asb%d" % b3, "alw%d" % b3, "awt%d" % b3
                pr = apr[st["yi"] % 2]; kpr = "apr%d" % (st["yi"] % 2)
                diag = j >= 4 * qb
                base = qb * 512 - j * 128
                S.op("pe", lambda e: e.matmul(pc[:], lhsT=tri_b, rhs=sbb[:], start=True, stop=True), reads=["cstb", ksb], writes=[kc_])
                S.op("dve", lambda e: e.tensor_tensor(out=lw[:], in0=zs[:], in1=pc[:], op=ALU.subtract), reads=[kzs, kc_], writes=[klw])
                if jn > 0:
                    S.op("dve", lambda e: e.tensor_tensor(out=lw[:], in0=lw[:], in1=pr[:], op=ALU.subtract), reads=[klw, kpr], writes=[klw])
                S.op("act", lambda e: e.activation(out=wt[:], in_=lw[:], func=AF.Exp), reads=[klw], writes=[kwt])
                if diag:
                    S.op("pool", lambda e: e.affine_select(out=wt[:], in_=wt[:], pattern=[[1, 512]], compare_op=ALU.is_gt, fill=0.0, base=base, channel_multiplier=-1), reads=[kwt], writes=[kwt])

            def phaseC(st):
                h, qb, j, jn, nkb, i = st["h"], st["qb"], st["j"], st["jn"], st["nkb"], st["i"]
                hb = h % 2
                v = av[hb]
                b3 = i % NBUF
                sbb, wt = asb[b3], awt[b3]
                ksb, kwt = "asb%d" % b3, "awt%d" % b3
                y2 = st["yi"] % 2
                py, pr, yo = apy[y2], apr[y2], ayo[y2]
                kpy, kpr, kyo = "apy%d" % y2, "apr%d" % y2, "ayo%d" % y2
                S.op("pe", lambda e: e.matmul(py[:], lhsT=v[:, j, :], rhs=wt[:], start=(jn == 0), stop=(jn == nkb - 1)), reads=["av%d" % hb, kwt], writes=[kpy])
                if jn < nkb - 1:
                    S.op("pe", lambda e: e.matmul(pr[:], lhsT=self.ones_b(), rhs=sbb[:], start=(jn == 0), stop=(jn == nkb - 2)), reads=["cstb", ksb], writes=[kpr])
                else:
                    S.op("act", lambda e: e.activation(out=yo[:], in_=py[:], func=AF.Copy), reads=[kpy], writes=[kyo])
                    S.dma("sp", lambda e: e.dma_start(out=self.yT_d[0, h * 64:(h + 1) * 64, qb * 512:(qb + 1) * 512], in_=yo[:]), reads=[kyo], writes=["yT_d"])

            n = len(steps)
            for s_ in range(n + 2):
                if 0 <= s_ - 2 < n:
                    phaseC(steps[s_ - 2])
                if 0 <= s_ - 1 < n:
                    phaseB(steps[s_ - 1])
                if s_ < n:
                    phaseA(steps[s_])

    def _zero_branch(self, g):
        nc, S = self.nc, self.S
        with ExitStack() as es:
            z = es.enter_context(self.sbt("zb", [128, S_LEN], BF16))
            S.op("dve", lambda e: e.memset(z[:], 0.0), writes=["zb"])
            for kc in range(4):
                S.dma("sp", lambda e, kc=kc: e.dma_start(out=self.yT_d[g, kc * 128:(kc + 1) * 128, :], in_=z[:]), reads=["zb"], writes=["yT_d"])

    def stage_hgrn(self, l):
        nc, S = self.nc, self.S
        with ExitStack() as es:
            sb = lambda n, sh, dt: es.enter_context(self.sbt(n, sh, dt))
            pt = lambda n, sh, dt: es.enter_context(self.pst(n, sh, dt))
            q = sb("hq", [128, S_LEN], F32)
            f = sb("hf", [128, S_LEN], F32)
            lf = sb("hlf", [128, S_LEN], F32)
            kin = sb("hkin", [128, S_LEN], F32)
            Bg = sb("hBg", [128, S_LEN], F32)
            Dd = sb("hD", [128, S_LEN], F32)
            Ee = sb("hE", [128, S_LEN], F32)
            Q1, K1 = sb("hQ1", [128, S_LEN], BF16), sb("hK1", [128, S_LEN], BF16)
            Q2, K2 = sb("hQ2", [128, S_LEN], BF16), sb("hK2", [128, S_LEN], BF16)
            hi = sb("hhi", [128, NT, 128], BF16)
            k2t = sb("hk2t", [128, NT, 128], BF16)
            hg = sb("hhg", [128, S_LEN], F32)
            oT = sb("hoT", [128, S_LEN], F32)
            Bst, Bmid, Bend, dec = sb("hBst", [128, 32], F32), sb("hBmid", [128, 32], F32), sb("hBend", [128, 32], F32), sb("hdec", [128, 32], F32)
            oml = sb("homl", [128, 1], F32)
            St, Stb = sb("hSt", [128, 128], F32), sb("hStb", [128, 128], BF16)
            pTs = [sb("hpT%d" % i, [128, 64], BF16) for i in range(2)]
            sq = sb("hsq", [128, 512], F32)
            rs = sb("hrs", [128, 512], F32)
            yb = sb("hyb", [128, 512], F32)
            ybb = sb("hybb", [128, 512], BF16)
            pss = [pt("hpss%d" % i, [128, 512], F32) for i in range(2)]
            pso = [pt("hpso%d" % i, [128, 512], F32) for i in range(2)]
            psst = [pt("hpsst%d" % i, [128, 512], F32) for i in range(2)]
            ptr = pt("hptr", [128, 1024], BF16)
            psn = pt("hpsn", [128, 512], F32)
            mask = self.cst[:, C_M64:C_M64 + 64]
            Bg3 = Bg[:].rearrange("p (c t) -> p c t", t=64)
            for h in range(4):
                lb = self.lbT[:, l * 4 + h: l * 4 + h + 1]
                rows = slice(h * 128, (h + 1) * 128)
                S.dma("sp", lambda e, rows=rows: e.dma_start(out=q[:], in_=self.hqT_d[rows, :]), reads=["projout"], writes=["hq"])
                S.dma("sp", lambda e, rows=rows: e.dma_start(out=f[:], in_=self.hfT_d[rows, :]), reads=["projout"], writes=["hf"])
                S.dma("sp", lambda e, rows=rows: e.dma_start(out=hg[:], in_=self.hgT_d[rows, :]), reads=["projout"], writes=["hhg"])
                S.dma("sp", lambda e, rows=rows: e.dma_start(out=hi[:], in_=self.hi_d[:, rows].rearrange("(j p) v -> p j v", p=128)), reads=["projout"], writes=["hhi"])
                S.op("dve", lambda e, lb=lb: e.tensor_scalar(out=oml[:], in0=lb, scalar1=-1.0, scalar2=1.0, op0=ALU.mult, op1=ALU.add), reads=["lbT"], writes=["homl"])
                S.op("act", lambda e: e.activation(out=f[:], in_=f[:], func=AF.Sigmoid), reads=["hf"], writes=["hf"])
                S.op("dve", lambda e, lb=lb: e.tensor_scalar(out=f[:], in0=f[:], scalar1=oml[:, 0:1], scalar2=lb, op0=ALU.mult, op1=ALU.add), reads=["hf", "homl", "lbT"], writes=["hf"])
                S.op("act", lambda e: e.activation(out=lf[:], in_=f[:], func=AF.Ln), reads=["hf"], writes=["hlf"])
                S.op("dve", lambda e: e.tensor_scalar(out=kin[:], in0=f[:], scalar1=-1.0, scalar2=1.0, op0=ALU.mult, op1=ALU.add), reads=["hf"], writes=["hkin"])
                S.op("dve", lambda e: e.tensor_tensor_scan(out=Bg[:], data0=lf[:], data1=lf[:], initial=0.0, op0=ALU.add, op1=ALU.bypass), reads=["hlf"], writes=["hBg"])
                S.op("dve", lambda e: e.memset(Bst[:, 0:1], 0.0), writes=["hBst"])
                S.op("dve", lambda e: e.tensor_copy(out=Bst[:, 1:32], in_=Bg3[:, 0:31, 63]), reads=["hBg"], writes=["hBst"])
                S.op("dve", lambda e: e.tensor_copy(out=Bmid[:], in_=Bg3[:, :, 31]), reads=["hBg"], writes=["hBmid"])
                S.op("dve", lambda e: e.tensor_copy(out=Bend[:], in_=Bg3[:, :, 63]), reads=["hBg"], writes=["hBend"])
                S.op("dve", lambda e: e.tensor_tensor(out=dec[:], in0=Bend[:], in1=Bst[:], op=ALU.subtract), reads=["hBend", "hBst"], writes=["hdec"])
                S.op("act", lambda e: e.activation(out=dec[:], in_=dec[:], func=AF.Exp), reads=["hdec"], writes=["hdec"])

                def sub_cols(col, key):
                    for c in range(32):
                        S.op("dve", lambda e, c=c: e.tensor_scalar(out=Dd[:, c * 64:(c + 1) * 64], in0=Bg[:, c * 64:(c + 1) * 64], scalar1=col[:, c:c + 1], scalar2=None, op0=ALU.subtract),
                             reads=["hBg", key], writes=["hD"])
                sub_cols(Bmid, "hBmid")
                S.op("act", lambda e: e.activation(out=Ee[:], in_=Dd[:], func=AF.Exp), reads=["hD"], writes=["hE"])
                S.op("dve", lambda e: e.tensor_tensor(out=Q1[:], in0=q[:], in1=Ee[:], op=ALU.mult), reads=["hq", "hE"], writes=["hQ1"])
                S.op("act", lambda e: e.activation(out=Ee[:], in_=Dd[:], func=AF.Exp, scale=-1.0), reads=["hD", "hQ1"], writes=["hE"])
                S.op("dve", lambda e: e.tensor_tensor(out=K1[:], in0=kin[:], in1=Ee[:], op=ALU.mult), reads=["hkin", "hE"], writes=["hK1"])
                sub_cols(Bst, "hBst")
                S.op("act", lambda e: e.activation(out=Ee[:], in_=Dd[:], func=AF.Exp), reads=["hD", "hK1"], writes=["hE"])
                S.op("dve", lambda e: e.tensor_tensor(out=Q2[:], in0=q[:], in1=Ee[:], op=ALU.mult), reads=["hq", "hE"], writes=["hQ2"])
                sub_cols(Bend, "hBend")
                S.op("act", lambda e: e.activation(out=Ee[:], in_=Dd[:], func=AF.Exp, scale=-1.0), reads=["hD", "hQ2"], writes=["hE"])
                S.op("dve", lambda e: e.tensor_tensor(out=K2[:], in0=kin[:], in1=Ee[:], op=ALU.mult), reads=["hkin", "hE"], writes=["hK2"])
                for j in range(NT):
                    S.op("pe", lambda e, j=j: e.transpose(ptr[:, 0:128], K2[:, j * 128:(j + 1) * 128], self.ident_b()), reads=["hK2", "cstb"], writes=["hptr"])
                    S.op("act", lambda e, j=j: e.activation(out=k2t[:, j, :], in_=ptr[:, 0:128], func=AF.Copy), reads=["hptr"], writes=["hk2t"])
                def pre(c):
                    r0 = (c % 2) * 64
                    cs = slice(c * 64, (c + 1) * 64)
                    ps_s = pss[c % 2]; kps = "hpss%d" % (c % 2)
                    pT = pTs[c % 2]; kpT = "hpT%d" % (c % 2)
                    S.op("pe", lambda e: e.matmul(ps_s[r0:r0 + 64, 0:64], lhsT=K1[:, cs], rhs=Q1[:, cs], start=True, stop=True), reads=["hK1", "hQ1"], writes=[kps])
                    S.op("dve", lambda e: e.tensor_copy(out=pT[r0:r0 + 64, :], in_=ps_s[r0:r0 + 64, 0:64]), reads=[kps], writes=[kpT])
                    S.op("pool", lambda e: e.affine_select(out=pT[r0:r0 + 64, :], in_=pT[r0:r0 + 64, :], pattern=[[1, 64]], compare_op=ALU.is_ge, fill=0.0, base=0, channel_multiplier=-1),
                         reads=[kpT], writes=[kpT])

                pre(0)
                for c in range(32):
                    j, half = c // 2, c % 2
                    r0 = half * 64
                    cs = slice(c * 64, (c + 1) * 64)
                    pT = pTs[c % 2]; kpT = "hpT%d" % (c % 2)
                    po = pso[(c // 8) % 2]; kpo = "hpso%d" % ((c // 8) % 2)
                    ocs = slice((c % 8) * 64, (c % 8 + 1) * 64)
                    pst_ = psst[c % 2]; kpst = "hpsst%d" % (c % 2)
                    S.op("pe", lambda e, po=po, pT=pT, r0=r0, j=j, ocs=ocs, c=c: e.matmul(po[:, ocs], lhsT=hi[r0:r0 + 64, j, :], rhs=pT[r0:r0 + 64, :], start=True, stop=(c == 0)), reads=["hhi", kpT], writes=[kpo])
                    if c + 1 < 32:
                        pre(c + 1)
                    if c > 0:
                        S.op("pe", lambda e, po=po, ocs=ocs, cs=cs: e.matmul(po[:, ocs], lhsT=Stb[:], rhs=Q2[:, cs], start=False, stop=True), reads=["hStb", "hQ2"], writes=[kpo])
                    if c < 31:
                        S.op("pe", lambda e, pst_=pst_, r0=r0, j=j: e.matmul(pst_[:, 0:128], lhsT=k2t[r0:r0 + 64, j, :], rhs=hi[r0:r0 + 64, j, :], start=True, stop=True), reads=["hk2t", "hhi"], writes=[kpst])
                        if c == 0:
                            S.op("dve", lambda e, pst_=pst_: e.tensor_copy(out=St[:], in_=pst_[:, 0:128]), reads=[kpst], writes=["hSt"])
                        else:
                            S.op("dve", lambda e, pst_=pst_, c=c: e.scalar_tensor_tensor(out=St[:], in0=St[:], scalar=dec[:, c:c + 1], in1=pst_[:, 0:128], op0=ALU.mult, op1=ALU.add), reads=[kpst, "hSt", "hdec"], writes=["hSt"])
                        S.op("act", lambda e: e.activation(out=Stb[:], in_=St[:], func=AF.Copy), reads=["hSt"], writes=["hStb"])
                    if c % 8 == 7:
                        tb = c // 8
                        S.op("act", lambda e, po=po, tb=tb: e.activation(out=oT[:, tb * 512:(tb + 1) * 512], in_=po[:], func=AF.Copy), reads=[kpo], writes=["hoT"])
                gcol = self.smc("hgn", l, h)
                for tb in range(4):
                    bs = slice(tb * 512, (tb + 1) * 512)
                    S.op("act", lambda e, bs=bs: e.activation(out=sq[:], in_=oT[:, bs], func=AF.Square), reads=["hoT"], writes=["hsq"])
                    S.op("pe", lambda e: e.matmul(psn[:], lhsT=self.ones_f(), rhs=sq[:], start=True, stop=True), reads=["cst", "hsq"], writes=["hpsn"])
                    S.op("dve", lambda e: e.tensor_scalar(out=rs[:], in0=psn[:], scalar1=1.0 / 128, scalar2=EPS, op0=ALU.mult, op1=ALU.add), reads=["hpsn"], writes=["hrs"])
                    S.op("act", lambda e: e.activation(out=rs[:], in_=rs[:], func=AF.Sqrt), reads=["hrs"], writes=["hrs"])
                    S.op("dve", lambda e: e.reciprocal(out=rs[:], in_=rs[:]), reads=["hrs"], writes=["hrs"])
                    S.op("dve", lambda e, bs=bs: e.tensor_tensor(out=yb[:], in0=oT[:, bs], in1=rs[:], op=ALU.mult), reads=["hoT", "hrs"], writes=["hyb"])
                    S.op("dve", lambda e, bs=bs, gcol=gcol: e.scalar_tensor_tensor(out=ybb[:], in0=yb[:], scalar=gcol, in1=hg[:, bs], op0=ALU.mult, op1=ALU.mult), reads=["hyb", "sm", "hhg"], writes=["hybb"])
                    S.dma("sp", lambda e, bs=bs, rows=rows: e.dma_start(out=self.yT_d[1, rows, bs], in_=ybb[:]), reads=["hybb"], writes=["yT_d"])

    def stage_mlstm(self, l):
        nc, S = self.nc, self.S
        with ExitStack() as es:
            sb = lambda n, sh, dt: es.enter_context(self.sbt(n, sh, dt))
            pt = lambda n, sh, dt: es.enter_context(self.pst(n, sh, dt))
            gi, gf = sb("mgi", [4, S_LEN], F32), sb("mgf", [4, S_LEN], F32)
            Bc, aa, AA = sb("mB", [4, S_LEN], F32), sb("ma", [4, S_LEN], F32), sb("mA", [4, S_LEN], F32)
            em, Dr = sb("mem", [4, S_LEN], F32), sb("mDr", [4, S_LEN], F32)
            uu, ww, it = sb("mu", [4, S_LEN], F32), sb("mw", [4, S_LEN], F32), sb("mit", [4, S_LEN], F32)
            Aend, Aprev, decr = sb("mAend", [4, 32], F32), sb("mAprev", [4, 32], F32), sb("mdecr", [4, 32], F32)
            nbf = sb("mnbf", [4, 1], F32)
            og, _ = SM_OFF["gateb"]
            bi = self.sm[0:4, og + l * 2: og + l * 2 + 1]
            bf = self.sm[0:4, og + l * 2 + 1: og + l * 2 + 2]
            S.dma("sp", lambda e: e.dma_start(out=gi[:], in_=self.gT_d[0, :, :]), reads=["projout"], writes=["mgi"])
            S.dma("sp", lambda e: e.dma_start(out=gf[:], in_=self.gT_d[1, :, :]), reads=["projout"], writes=["mgf"])
            S.op("dve", lambda e: e.tensor_scalar(out=gi[:], in0=gi[:], scalar1=bi, scalar2=None, op0=ALU.add), reads=["mgi", "sm"], writes=["mgi"])
            S.op("dve", lambda e: e.tensor_scalar(out=nbf[:], in0=bf, scalar1=-1.0, scalar2=None, op0=ALU.mult), reads=["sm"], writes=["mnbf"])
            S.op("act", lambda e: e.activation(out=gf[:], in_=gf[:], func=AF.Exp, scale=-1.0, bias=nbf[:, 0:1]), reads=["mgf", "mnbf"], writes=["mgf"])
            S.op("act", lambda e: e.activation(out=gf[:], in_=gf[:], func=AF.Ln, bias=1.0), reads=["mgf"], writes=["mgf"])
            S.op("dve", lambda e: e.tensor_scalar(out=gf[:], in0=gf[:], scalar1=-1.0, scalar2=None, op0=ALU.mult), reads=["mgf"], writes=["mgf"])
            S.op("dve", lambda e: e.tensor_tensor_scan(out=Bc[:], data0=gf[:], data1=gf[:], initial=0.0, op0=ALU.add, op1=ALU.bypass), reads=["mgf"], writes=["mB"])
            S.op("dve", lambda e: e.tensor_tensor(out=aa[:], in0=gi[:], in1=Bc[:], op=ALU.subtract), reads=["mgi", "mB"], writes=["ma"])
            S.op("dve", lambda e: e.tensor_tensor_scan(out=AA[:], data0=aa[:], data1=aa[:], initial=0.0, op0=ALU.max, op1=ALU.bypass), reads=["ma"], writes=["mA"])
            S.op("dve", lambda e: e.tensor_tensor(out=em[:], in0=Bc[:], in1=AA[:], op=ALU.add), reads=["mB", "mA"], writes=["mem"])
            S.op("act", lambda e: e.activation(out=em[:], in_=em[:], func=AF.Exp, scale=-1.0), reads=["mem"], writes=["mem"])
            A3 = AA[:].rearrange("p (c t) -> p c t", t=64)
            a3 = aa[:].rearrange("p (c t) -> p c t", t=64)
            D3 = Dr[:].rearrange("p (c t) -> p c t", t=64)
            S.op("dve", lambda e: e.tensor_copy(out=Aend[:], in_=A3[:, :, 63]), reads=["mA"], writes=["mAend"])
            S.op("dve", lambda e: e.memset(Aprev[:, 0:1], 0.0), writes=["mAprev"])
            S.op("dve", lambda e: e.tensor_copy(out=Aprev[:, 1:32], in_=A3[:, 0:31, 63]), reads=["mA"], writes=["mAprev"])
            S.op("dve", lambda e: e.tensor_tensor(out=decr[:], in0=Aprev[:], in1=Aend[:], op=ALU.subtract), reads=["mAprev", "mAend"], writes=["mdecr"])
            S.op("act", lambda e: e.activation(out=decr[:], in_=decr[:], func=AF.Exp), reads=["mdecr"], writes=["mdecr"])
            bc = lambda col: col[:].unsqueeze(2).to_broadcast([4, 32, 64])
            S.op("dve", lambda e: e.tensor_tensor(out=D3, in0=A3, in1=bc(Aend), op=ALU.subtract), reads=["mA", "mAend"], writes=["mDr"])
            S.op("act", lambda e: e.activation(out=uu[:], in_=Dr[:], func=AF.Exp, scale=-1.0), reads=["mDr"], writes=["mu"])
            S.op("dve", lambda e: e.tensor_tensor(out=D3, in0=a3, in1=bc(Aend), op=ALU.subtract), reads=["ma", "mAend", "mu"], writes=["mDr"])
            S.op("act", lambda e: e.activation(out=ww[:], in_=Dr[:], func=AF.Exp), reads=["mDr"], writes=["mw"])
            S.op("dve", lambda e: e.tensor_tensor(out=D3, in0=A3, in1=bc(Aprev), op=ALU.subtract), reads=["mA", "mAprev", "mw"], writes=["mDr"])
            S.op("act", lambda e: e.activation(out=it[:], in_=Dr[:], func=AF.Exp, scale=-1.0), reads=["mDr"], writes=["mit"])
            xr, acc = sb("mxr", [128, S_LEN], F32), sb("macc2", [128, S_LEN], F32)
            Q1, Q2, K1 = sb("mQ1", [128, S_LEN], BF16), sb("mQ2", [128, S_LEN], BF16), sb("mK1", [128, S_LEN], BF16)
            vv = sb("mvv", [128, NT, 128], BF16)
            k2t = sb("mk2t", [128, NT, 128], BF16)
            mo = sb("mmo", [128, S_LEN], F32)
            hT = sb("mhT", [128, S_LEN], F32)
            dec = sb("mdec", [128, 32], F32)
            Cs, Csb = sb("mCs", [128, 128], F32), sb("mCsb", [128, 128], BF16)
            Ns, Nsb = sb("mNs", [128, 128], F32), sb("mNsb", [128, 128], BF16)
            pTs = [sb("mpT%d" % i, [128, 64], BF16) for i in range(2)]
            numT, embc, dmx = sb("mnumT", [128, 512], F32), sb("membc", [128, 512], F32), sb("mdmx", [128, 512], F32)
            sq, rs, yb, ybb = sb("msq", [128, 512], F32), sb("mrs", [128, 512], F32), sb("myb", [128, 512], F32), sb("mybb", [128, 512], BF16)
            pss = pt("mpss", [128, 512], F32)
            pso = [pt("mpso%d" % i, [128, 512], F32) for i in range(2)]
            psd = [pt("mpsd%d" % i, [128, 512], F32) for i in range(2)]
            pstC, pstN = pt("mpstC", [128, 512], F32), pt("mpstN", [128, 512], F32)
            pmisc = pt("mpmisc", [128, 512], F32)
            pmisc_b = pmisc[:].bitcast(BF16)
            KM = "mpmisc"
            ones_b = self.ones_b()

            def bcast_rows(rows, h, tb):
                S.op("pe", lambda e: e.matmul(pmisc[:], lhsT=self.cst[0:4, C_SEL + h * 128: C_SEL + (h + 1) * 128], rhs=rows[0:4, tb * 512:(tb + 1) * 512], start=True, stop=True),
                     reads=["cst", "mu", "mw", "mit", "mem"], writes=[KM])

            def conv_silu(chunk):
                cw = lambda tap: self.smc("convw", l, tap * 8 + chunk)
                S.op("dve", lambda e: e.tensor_scalar(out=acc[:], in0=xr[:], scalar1=cw(3), scalar2=self.smc("convb", l, chunk), op0=ALU.mult, op1=ALU.add), reads=["mxr", "sm"], writes=["macc2"])
                for sh in (1, 2, 3):
                    S.op("dve", lambda e, sh=sh: e.scalar_tensor_tensor(out=acc[:, sh:S_LEN], in0=xr[:, 0:S_LEN - sh], scalar=cw(3 - sh), in1=acc[:, sh:S_LEN], op0=ALU.mult, op1=ALU.add),
                         reads=["mxr", "sm", "macc2"], writes=["macc2"])
                S.op("act", lambda e: e.activation(out=acc[:], in_=acc[:], func=AF.Silu), reads=["macc2"], writes=["macc2"])

            for h in range(4):
                rows = slice(h * 128, (h + 1) * 128)
                S.dma("sp", lambda e, rows=rows: e.dma_start(out=xr[:], in_=self.mqkT_d[rows, :]), reads=["projout"], writes=["mxr"])
                S.dma("sp", lambda e, rows=rows: e.dma_start(out=mo[:], in_=self.moT_d[rows, :]), reads=["projout"], writes=["mmo"])
                S.dma("sp", lambda e, rows=rows: e.dma_start(out=vv[:], in_=self.mv_d[:, rows].rearrange("(j p) v -> p j v", p=128)), reads=["projout"], writes=["mvv"])
                conv_silu(h)
                for tb in range(4):
                    bs = slice(tb * 512, (tb + 1) * 512)
                    bcast_rows(uu, h, tb)
                    S.op("dve", lambda e, bs=bs: e.tensor_tensor(out=Q1[:, bs], in0=acc[:, bs], in1=pmisc[:], op=ALU.mult), reads=["macc2", KM], writes=["mQ1"])
                    bcast_rows(it, h, tb)
                    S.op("dve", lambda e, bs=bs: e.tensor_tensor(out=Q2[:, bs], in0=acc[:, bs], in1=pmisc[:], op=ALU.mult), reads=["macc2", KM], writes=["mQ2"])
                S.dma("sp", lambda e, h=h: e.dma_start(out=xr[:], in_=self.mqkT_d[512 + h * 128: 512 + (h + 1) * 128, :]), reads=["projout"], writes=["mxr"])
                conv_silu(4 + h)
                for tb in range(4):
                    bs = slice(tb * 512, (tb + 1) * 512)
                    bcast_rows(ww, h, tb)
                    S.op("dve", lambda e, bs=bs: e.scalar_tensor_tensor(out=K1[:, bs], in0=acc[:, bs], scalar=128.0 ** -0.5, in1=pmisc[:], op0=ALU.mult, op1=ALU.mult), reads=["macc2", KM], writes=["mK1"])
                S.op("pe", lambda e, h=h: e.matmul(pmisc[:, 0:32], lhsT=self.cst[0:4, C_SEL + h * 128: C_SEL + (h + 1) * 128], rhs=decr[0:4, :], start=True, stop=True), reads=["cst", "mdecr"], writes=[KM])
                S.op("act", lambda e: e.activation(out=dec[:], in_=pmisc[:, 0:32], func=AF.Copy), reads=[KM], writes=["mdec"])
                for j in range(NT):
                    S.op("pe", lambda e, j=j: e.transpose(pmisc_b[:, 0:128], K1[:, j * 128:(j + 1) * 128], self.ident_b()), reads=["mK1", "cstb"], writes=[KM])
                    S.op("act", lambda e, j=j: e.activation(out=k2t[:, j, :], in_=pmisc_b[:, 0:128], func=AF.Copy), reads=[KM], writes=["mk2t"])
                def pre(c):
                    r0 = (c % 2) * 64
                    cs = slice(c * 64, (c + 1) * 64)
                    pT = pTs[c % 2]; kpT = "mpT%d" % (c % 2)
                    S.op("pe", lambda e: e.matmul(pss[r0:r0 + 64, 0:64], lhsT=K1[:, cs], rhs=Q1[:, cs], start=True, stop=True), reads=["mK1", "mQ1"], writes=["mpss"])
                    S.op("dve", lambda e: e.tensor_copy(out=pT[r0:r0 + 64, :], in_=pss[r0:r0 + 64, 0:64]), reads=["mpss"], writes=[kpT])
                    S.op("pool", lambda e: e.affine_select(out=pT[r0:r0 + 64, :], in_=pT[r0:r0 + 64, :], pattern=[[1, 64]], compare_op=ALU.is_ge, fill=0.0, base=0, channel_multiplier=-1),
                         reads=[kpT], writes=[kpT])

                pre(0)
                for c in range(32):
                    j, half = c // 2, c % 2
                    r0 = half * 64
                    cs = slice(c * 64, (c + 1) * 64)
                    pT = pTs[c % 2]; kpT = "mpT%d" % (c % 2)
                    po = pso[(c // 8) % 2]; kpo = "mpso%d" % ((c // 8) % 2)
                    pd = psd[(c // 8) % 2]; kpd = "mpsd%d" % ((c // 8) % 2)
                    ocs = slice((c % 8) * 64, (c % 8 + 1) * 64)
                    S.op("pe", lambda e, po=po, pT=pT, r0=r0, j=j, ocs=ocs, c=c: e.matmul(po[:, ocs], lhsT=vv[r0:r0 + 64, j, :], rhs=pT[r0:r0 + 64, :], start=True, stop=(c == 0)), reads=["mvv", kpT], writes=[kpo])
                    if c > 0:
                        S.op("pe", lambda e, po=po, ocs=ocs, cs=cs: e.matmul(po[:, ocs], lhsT=Csb[:], rhs=Q2[:, cs], start=False, stop=True), reads=["mCsb", "mQ2"], writes=[kpo])
                    S.op("pe", lambda e, pd=pd, pT=pT, r0=r0, ocs=ocs, c=c: e.matmul(pd[:, ocs], lhsT=ones_b[r0:r0 + 64, :], rhs=pT[r0:r0 + 64, :], start=True, stop=(c == 0)), reads=["cstb", kpT], writes=[kpd])
                    if c + 1 < 32:
                        pre(c + 1)
                    if c > 0:
                        S.op("pe", lambda e, pd=pd, ocs=ocs, cs=cs: e.matmul(pd[:, ocs], lhsT=Nsb[:], rhs=Q2[:, cs], start=False, stop=True), reads=["mNsb", "mQ2"], writes=[kpd])
                    if c < 31:
                        S.op("pe", lambda e, r0=r0, j=j: e.matmul(pstC[:, 0:128], lhsT=k2t[r0:r0 + 64, j, :], rhs=vv[r0:r0 + 64, j, :], start=True, stop=True), reads=["mk2t", "mvv"], writes=["mpstC"])
                        S.op("pe", lambda e, r0=r0, j=j: e.matmul(pstN[:, 0:128], lhsT=k2t[r0:r0 + 64, j, :], rhs=ones_b[r0:r0 + 64, :], start=True, stop=True), reads=["mk2t", "cstb"], writes=["mpstN"])
                        if c == 0:
                            S.op("dve", lambda e: e.tensor_copy(out=Cs[:], in_=pstC[:, 0:128]), reads=["mpstC"], writes=["mCs"])
                            S.op("dve", lambda e: e.tensor_copy(out=Ns[:], in_=pstN[:, 0:128]), reads=["mpstN"], writes=["mNs"])
                        else:
                            S.op("dve", lambda e, c=c: e.scalar_tensor_tensor(out=Cs[:], in0=Cs[:], scalar=dec[:, c:c + 1], in1=pstC[:, 0:128], op0=ALU.mult, op1=ALU.add), reads=["mpstC", "mCs", "mdec"], writes=["mCs"])
                            S.op("dve", lambda e, c=c: e.scalar_tensor_tensor(out=Ns[:], in0=Ns[:], scalar=dec[:, c:c + 1], in1=pstN[:, 0:128], op0=ALU.mult, op1=ALU.add), reads=["mpstN", "mNs", "mdec"], writes=["mNs"])
                        S.op("act", lambda e: e.activation(out=Csb[:], in_=Cs[:], func=AF.Copy), reads=["mCs"], writes=["mCsb"])
                        S.op("act", lambda e: e.activation(out=Nsb[:], in_=Ns[:], func=AF.Copy), reads=["mNs"], writes=["mNsb"])
                    if c % 8 == 7:
                        tb = c // 8
                        bs = slice(tb * 512, (tb + 1) * 512)
                        S.op("act", lambda e, po=po: e.activation(out=numT[:], in_=po[:], func=AF.Copy), reads=[kpo], writes=["mnumT"])
                        bcast_rows(em, h, tb)
                        S.op("act", lambda e: e.activation(out=embc[:], in_=pmisc[:], func=AF.Copy), reads=[KM], writes=["membc"])
                        S.op("act", lambda e, pd=pd: e.activation(out=dmx[:], in_=pd[:], func=AF.Abs), reads=[kpd], writes=["mdmx"])
                        S.op("dve", lambda e: e.tensor_tensor(out=dmx[:], in0=dmx[:], in1=embc[:], op=ALU.max), reads=["mdmx", "membc"], writes=["mdmx"])
                        S.op("dve", lambda e: e.reciprocal(out=dmx[:], in_=dmx[:]), reads=["mdmx"], writes=["mdmx"])
                        S.op("dve", lambda e, bs=bs: e.tensor_tensor(out=hT[:, bs], in0=numT[:], in1=dmx[:], op=ALU.mult), reads=["mnumT", "mdmx"], writes=["mhT"])
                gcol = self.smc("mln", l, h)
                for tb in range(4):
                    bs = slice(tb * 512, (tb + 1) * 512)
                    S.op("act", lambda e, bs=bs: e.activation(out=sq[:], in_=hT[:, bs], func=AF.Square), reads=["mhT"], writes=["msq"])
                    S.op("pe", lambda e: e.matmul(pmisc[:], lhsT=self.ones_f(), rhs=sq[:], start=True, stop=True), reads=["cst", "msq"], writes=[KM])
                    S.op("dve", lambda e: e.tensor_scalar(out=rs[:], in0=pmisc[:], scalar1=1.0 / 128, scalar2=EPS, op0=ALU.mult, op1=ALU.add), reads=[KM], writes=["mrs"])
                    S.op("act", lambda e: e.activation(out=rs[:], in_=rs[:], func=AF.Sqrt), reads=["mrs"], writes=["mrs"])
                    S.op("dve", lambda e: e.reciprocal(out=rs[:], in_=rs[:]), reads=["mrs"], writes=["mrs"])
                    S.op("dve", lambda e, bs=bs: e.tensor_tensor(out=yb[:], in0=hT[:, bs], in1=rs[:], op=ALU.mult), reads=["mhT", "mrs"], writes=["myb"])
                    S.op("dve", lambda e, bs=bs, gcol=gcol: e.scalar_tensor_tensor(out=ybb[:], in0=yb[:], scalar=gcol, in1=mo[:, bs], op0=ALU.mult, op1=ALU.mult), reads=["myb", "sm", "mmo"], writes=["mybb"])
                    S.dma("sp", lambda e, bs=bs, rows=rows: e.dma_start(out=self.yT_d[2, rows, bs], in_=ybb[:]), reads=["mybb"], writes=["yT_d"])

    def _bcast_tile(self, es, name, colfn, ps_ap=None, ps_key=None):
        nc, S = self.nc, self.S
        dg = es.enter_context(self.sbt(name + "dg", [128, 128], F32))
        dst = es.enter_context(self.sbt(name, [128, D], F32))
        if ps_ap is None:
            ps = es.enter_context(self.pst(name + "ps", [128, D], F32))[:]
            ps_key = name + "ps"
        else:
            ps = ps_ap
        for c in range(8):
            S.op("dve", lambda e, c=c: e.tensor_scalar(out=dg[:], in0=self.ident_f(), scalar1=colfn(c), scalar2=None, op0=ALU.mult),
                 reads=["cst", "modT", "sm"], writes=[name + "dg"])
            S.op("pe", lambda e, c=c: e.matmul(ps[:, c * 128:(c + 1) * 128], lhsT=self.ones_f(), rhs=dg[:], start=True, stop=True), reads=["cst", name + "dg"], writes=[ps_key])
        for hh in range(2):
            S.op("act", lambda e, hh=hh: e.activation(out=dst[:, hh * 512:(hh + 1) * 512], in_=ps[:, hh * 512:(hh + 1) * 512], func=AF.Copy), reads=[ps_key], writes=[name])
        return dst

    def stage_merge(self, l):
        nc, S = self.nc, self.S
        with ExitStack() as es:
            sb = lambda n, sh, dt: es.enter_context(self.sbt(n, sh, dt))
            pt = lambda n, sh, dt: es.enter_context(self.pst(n, sh, dt))
            g1bc = self._bcast_tile(es, "g1bc", lambda c: self.modc(l, 2, c))
            wb = sb("mwb", [128, 3, 4, D], BF16)
            wo = sb("mwo", [128, 8, D], BF16)
            yt = sb("myt", [128, 3, 4, 512], BF16)
            gts = [sb("mgt%d" % i, [128, 512], F32) for i in range(3)]
            tmps = [sb("mtmp%d" % i, [128, 512], F32) for i in range(2)]
            macc = sb("macc", [128, 512], F32)
            mT = sb("mT", [128, 8, 512], BF16)
            xts = [sb("mxt%d" % i, [128, D], F32) for i in range(2)]
            ytmp = sb("mytmp", [128, 512], F32)
            psa = [pt("mpsa%d" % i, [128, 512], F32) for i in range(2)]
            psy = [pt("mpsy%d" % i, [128, 512], F32) for i in range(2)]
            for g in range(3):
                S.dma("pool", lambda e, g=g: e.dma_start(out=wb[:, g, :, :], in_=self.w_branch[l, g, :, :].rearrange("(kc p) d -> p kc d", p=128)), writes=["mwb"])
            S.dma("pool", lambda e: e.dma_start(out=wo[:], in_=self.w_out[l, :, :].rearrange("(c p) d -> p c d", p=128)), writes=["mwo"])
            ai = 0
            gi = 0
            yi = 0
            for tb in range(4):
                for g in range(3):
                    S.dma("sp", lambda e, g=g, tb=tb: e.dma_start(out=yt[:, g, :, :], in_=self.yT_d[g, :, tb * 512:(tb + 1) * 512].rearrange("(kc p) t -> p kc t", p=128)),
                          reads=["yT_d"], writes=["myt"])
                for dc in range(8):
                    for g in range(3):
                        ps = psa[ai % 2]; kps = "mpsa%d" % (ai % 2); ai += 1
                        gt = gts[gi % 3]; kgt = "mgt%d" % (gi % 3); gi += 1
                        S.dma("sp", lambda e, gt=gt, g=g, dc=dc, tb=tb: e.dma_start(out=gt[:], in_=self.bgT_d[g * 1024 + dc * 128: g * 1024 + (dc + 1) * 128, tb * 512:(tb + 1) * 512]),
                              reads=["projout"], writes=[kgt])
                        for kc in range(4):
                            S.op("pe", lambda e, ps=ps, g=g, kc=kc, dc=dc: e.matmul(ps[:], lhsT=wb[:, g, kc, dc * 128:(dc + 1) * 128], rhs=yt[:, g, kc, :], start=(kc == 0), stop=(kc == 3)),
                                 reads=["mwb", "myt"], writes=[kps])
                        if g == 0:
                            S.op("dve", lambda e, ps=ps, gt=gt: e.tensor_tensor(out=macc[:], in0=ps[:], in1=gt[:], op=ALU.mult), reads=[kps, kgt], writes=["macc"])
                        else:
                            tmp = tmps[g % 2]; ktmp = "mtmp%d" % (g % 2)
                            S.op("dve", lambda e, ps=ps, gt=gt, tmp=tmp: e.tensor_tensor(out=tmp[:], in0=ps[:], in1=gt[:], op=ALU.mult), reads=[kps, kgt], writes=[ktmp])
                            if g == 1:
                                S.op("pool", lambda e, tmp=tmp: e.tensor_tensor(out=macc[:], in0=macc[:], in1=tmp[:], op=ALU.add), reads=[ktmp, "macc"], writes=["macc"])
                            else:
                                S.op("pool", lambda e, tmp=tmp, dc=dc: e.tensor_tensor(out=mT[:, dc, :], in0=macc[:], in1=tmp[:], op=ALU.add), reads=[ktmp, "macc"], writes=["mT"])
                for tt in range(4):
                    t = tb * 4 + tt
                    xt = xts[t % 2]; kxt = "mxt%d" % (t % 2)
                    S.dma("sp", lambda e, xt=xt, t=t: e.dma_start(out=xt[:], in_=self.xres[t * 128:(t + 1) * 128, :]), reads=["xres"], writes=[kxt])
                    for dh in range(2):
                        ps = psy[yi % 2]; kps = "mpsy%d" % (yi % 2); yi += 1
                        for c in range(8):
                            S.op("pe", lambda e, ps=ps, c=c, tt=tt, dh=dh: e.matmul(ps[:], lhsT=mT[:, c, tt * 128:(tt + 1) * 128], rhs=wo[:, c, dh * 512:(dh + 1) * 512], start=(c == 0), stop=(c == 7)),
                                 reads=["mT", "mwo"], writes=[kps])
                        S.op("dve", lambda e, ps=ps, dh=dh: e.tensor_tensor(out=ytmp[:], in0=ps[:], in1=g1bc[:, dh * 512:(dh + 1) * 512], op=ALU.mult), reads=[kps, "g1bc"], writes=["mytmp"])
                        S.op("dve", lambda e, xt=xt, dh=dh: e.tensor_tensor(out=xt[:, dh * 512:(dh + 1) * 512], in0=xt[:, dh * 512:(dh + 1) * 512], in1=ytmp[:], op=ALU.add), reads=["mytmp", kxt], writes=[kxt])
                    S.dma("sp", lambda e, xt=xt, t=t: e.dma_start(out=self.xres[t * 128:(t + 1) * 128, :], in_=xt[:]), reads=[kxt], writes=["xres"])

    def uvcast_gen(self, l, cbs):
        nc, S = self.nc, self.S
        for k in range(32):
            cb = cbs[k % 3]; kcb = "uvc%d" % (k % 3)
            src = self.pk_uv[l, k * 512:(k + 1) * 512, :].rearrange("(p r) d -> p r d", p=128)
            dst = self.uvb[k * 512:(k + 1) * 512, :].rearrange("(p r) d -> p r d", p=128)
            S.dma("pool", lambda e, cb=cb, src=src: e.dma_start(out=cb[:], in_=src), writes=[kcb])
            S.dma("sp", lambda e, cb=cb, dst=dst: e.dma_start(out=dst, in_=cb[:]), reads=[kcb], writes=["uvb"])
            yield

    def stage_peer(self, l):
        nc, S = self.nc, self.S
        NB = 6
        with ExitStack() as es:
            sb = lambda n, sh, dt: es.enter_context(self.sbt(n, sh, dt))
            pt = lambda n, sh, dt: es.enter_context(self.pst(n, sh, dt))
            two = lambda n, sh, dt: [sb("%s%d" % (n, i), sh, dt) for i in range(2)]
            hTs = two("phT", [128, 8, 128], BF16)
            wq = sb("pwq", [128, 8, D], BF16)
            kbd = sb("pkbd", [128, 8, 256], F32)
            qTts = two("pqTt", [128, 8, 128], F32)
            scs, s2s, cands = two("psc", [128, 2048], F32), two("ps2", [128, 2048], F32), two("pcand", [128, 2048], F32)
            mxs, mis, sifs = two("pmx", [128, 256], F32), two("pmi", [128, 256], U32), two("psif", [128, 256], F32)
            tvs, tps, tpfs = two("ptv", [128, 128], F32), two("ptp", [128, 128], U32), two("ptpf", [128, 128], F32)
            afs, bfs = two("paf", [128, 128], F32), two("pbf", [128, 128], F32)
            i1s, i2s = two("pi1", [128, 128], F32), two("pi2", [128, 128], F32)
            idxis = two("pidxi", [128, 128], I32)
            ees, ggs = two("pee", [128, 128], F32), two("pgg", [128, 128], F32)
            ssums = two("pssum", [128, 8], F32)
            aa, ga, gl = sb("paa", [128, 128], F32), sb("pga", [128, 128], F32), sb("pgl", [128, 128], F32)
            h2ts = two("ph2t", [128, D], F32)
            xts = two("pxt", [128, D], F32)
            ubs = [sb("pub%d" % i, [128, 2 * D], BF16) for i in range(NB)]
            junks = two("pjunk", [128, D], F32)
            acc = sb("pacc", [128, D], F32)
            tmpbs = [sb("ptmpb%d" % i, [128, D], BF16) for i in range(3)]
            psq = pt("ppsq", [128, 8, 128], F32)
            pssc = pt("ppssc", [128, 2048], F32)
            pacc = pt("ppacc", [128, D], F32)
            g2bc = self._bcast_tile(es, "g2bc", lambda c: self.modc(l, 5, c), ps_ap=pssc[:, 0:D], ps_key="ppssc")
            UV2 = self.uvb
            iota16 = self.cst[:, C_IOTA:C_IOTA + 16]
            thr15 = self.cst[:, C_THR:C_THR + 15]
            S.dma("pool", lambda e: e.dma_start(out=wq[:], in_=self.pk_wq[l, :, :].rearrange("(kc p) d -> p kc d", p=128)), writes=["pwq"])
            S.dma("sp", lambda e: e.dma_start(out=kbd[:], in_=self.keysbd[l, :, :, :].rearrange("h p n -> p h n")), writes=["pkbd"])
            B4 = [128, 8, 16, 16]

            def prep(t):
                p = t % 2
                K = lambda n: "%s%d" % (n, p)
                ts = slice(t * 128, (t + 1) * 128)
                h2t, xt, hT, qTt = h2ts[p], xts[p], hTs[p], qTts[p]
                sc, s2, cand, mx, mi, sif = scs[p], s2s[p], cands[p], mxs[p], mis[p], sifs[p]
                tv, tp, tpf, af, bf_, i1, i2, idxi, ee, gg, ssum = tvs[p], tps[p], tpfs[p], afs[p], bfs[p], i1s[p], i2s[p], idxis[p], ees[p], ggs[p], ssums[p]
                sc3 = sc[:].rearrange("p (g n) -> p g n", n=128)
                s23 = s2[:].rearrange("p (g n) -> p g n", n=128)
                mx3 = mx[:].rearrange("p (g k) -> p g k", k=16)
                mi3 = mi[:].rearrange("p (g k) -> p g k", k=16)
                mx4 = mx[:].rearrange("p (h q k) -> p h q k", h=8, q=2)
                sif4 = sif[:].rearrange("p (h q k) -> p h q k", h=8, q=2)
                cand4 = cand[:].rearrange("p (h a b) -> p h a b", h=8, a=16)
                tv3 = tv[:].rearrange("p (h k) -> p h k", h=8)
                tp3 = tp[:].rearrange("p (h k) -> p h k", h=8)
                oh4 = s2[:].rearrange("p (h k a) -> p h k a", h=8, k=16)
                cmp3 = sc[:, 0:1920].rearrange("p (r j) -> p r j", j=15)
                S.dma("sp", lambda e: e.dma_start(out=h2t[:], in_=self.h2_d[ts, :]), reads=["h2_d"], writes=[K("ph2t")])
                S.dma("sp", lambda e: e.dma_start(out=xt[:], in_=self.xres[ts, :]), reads=["xres"], writes=[K("pxt")])
                S.dma("sp", lambda e: e.dma_start(out=hT[:], in_=self.hT_d[:, :, ts].rearrange("c p t -> p c t")), reads=["hT_d"], writes=[K("phT")])
                yield
                for h in range(8):
                    for kc in range(8):
                        S.op("pe", lambda e, h=h, kc=kc: e.matmul(psq[:, h, :], lhsT=wq[:, kc, h * 128:(h + 1) * 128], rhs=hT[:, kc, :], start=(kc == 0), stop=(kc == 7)),
                             reads=["pwq", K("phT")], writes=["ppsq"])
                    yield
                for hh in range(2):
                    S.op("act", lambda e, hh=hh: e.activation(out=qTt[:, hh * 4:(hh + 1) * 4, :], in_=psq[:, hh * 4:(hh + 1) * 4, :], func=AF.Copy), reads=["ppsq"], writes=[K("pqTt")])
                for h in range(8):
                    S.op("pe", lambda e, h=h: e.matmul(pssc[:, h * 256:(h + 1) * 256], lhsT=qTt[:, h, :], rhs=kbd[:, h, :], start=True, stop=True), reads=[K("pqTt"), "pkbd"], writes=["ppssc"])
                for qd in range(4):
                    S.op("act", lambda e, qd=qd: e.activation(out=sc[:, qd * 512:(qd + 1) * 512], in_=pssc[:, qd * 512:(qd + 1) * 512], func=AF.Copy), reads=["ppssc"], writes=[K("psc")])
                yield
                for g in range(16):
                    S.op("dve", lambda e, g=g: e.max(out=mx3[:, g, 0:8], in_=sc3[:, g, :]), reads=[K("psc")], writes=[K("pmx")])
                    S.op("dve", lambda e, g=g: e.max_index(out=mi3[:, g, 0:8], in_max=mx3[:, g, 0:8], in_values=sc3[:, g, :]), reads=[K("psc"), K("pmx")], writes=[K("pmi")])
                    yield
                    S.op("dve", lambda e, g=g: e.match_replace(out=s23[:, g, :], in_to_replace=mx3[:, g, 0:8], in_values=sc3[:, g, :], imm_value=-1e30), reads=[K("psc"), K("pmx")], writes=[K("ps2")])
                    S.op("dve", lambda e, g=g: e.max(out=mx3[:, g, 8:16], in_=s23[:, g, :]), reads=[K("ps2")], writes=[K("pmx")])
                    yield
                    S.op("dve", lambda e, g=g: e.max_index(out=mi3[:, g, 8:16], in_max=mx3[:, g, 8:16], in_values=s23[:, g, :]), reads=[K("ps2"), K("pmx")], writes=[K("pmi")])
                    yield
                S.op("dve", lambda e: e.tensor_copy(out=sif[:], in_=mi[:]), reads=[K("pmi")], writes=[K("psif")])
                S.op("dve", lambda e: e.tensor_tensor(out=cand4, in0=mx4[:, :, 0, :].unsqueeze(3).to_broadcast(B4), in1=mx4[:, :, 1, :].unsqueeze(2).to_broadcast(B4), op=ALU.add),
                     reads=[K("pmx")], writes=[K("pcand")])
                yield
                for h in range(8):
                    hs = slice(h * 256, (h + 1) * 256)
                    S.op("dve", lambda e, h=h, hs=hs: e.max(out=tv3[:, h, 0:8], in_=cand[:, hs]), reads=[K("pcand")], writes=[K("ptv")])
                    S.op("dve", lambda e, h=h, hs=hs: e.max_index(out=tp3[:, h, 0:8], in_max=tv3[:, h, 0:8], in_values=cand[:, hs]), reads=[K("pcand"), K("ptv")], writes=[K("ptp")])
                    yield
                    S.op("dve", lambda e, h=h, hs=hs: e.match_replace(out=s2[:, hs], in_to_replace=tv3[:, h, 0:8], in_values=cand[:, hs], imm_value=-1e30), reads=[K("pcand"), K("ptv")], writes=[K("ps2")])
                    S.op("dve", lambda e, h=h, hs=hs: e.max(out=tv3[:, h, 8:16], in_=s2[:, hs]), reads=[K("ps2")], writes=[K("ptv")])
                    yield
                    S.op("dve", lambda e, h=h, hs=hs: e.max_index(out=tp3[:, h, 8:16], in_max=tv3[:, h, 8:16], in_values=s2[:, hs]), reads=[K("ps2"), K("ptv")], writes=[K("ptp")])
                    yield
                S.op("dve", lambda e: e.tensor_copy(out=tpf[:], in_=tp[:]), reads=[K("ptp")], writes=[K("ptpf")])
                S.op("dve", lambda e: e.tensor_tensor(out=cmp3, in0=tpf[:].unsqueeze(2).to_broadcast([128, 128, 15]), in1=thr15.unsqueeze(1).to_broadcast([128, 128, 15]), op=ALU.is_ge),
                     reads=[K("ptpf"), "cst"], writes=[K("psc")])
                yield
                S.op("dve", lambda e: e.tensor_reduce(out=af[:], in_=cmp3, axis=AX.X, op=ALU.add), reads=[K("psc")], writes=[K("paf")])
                S.op("dve", lambda e: e.scalar_tensor_tensor(out=bf_[:], in0=af[:], scalar=-16.0, in1=tpf[:], op0=ALU.mult, op1=ALU.add), reads=[K("paf"), K("ptpf")], writes=[K("pbf")])
                yield
                for (src, q_, dst, kd) in ((af, 0, i1, K("pi1")), (bf_, 1, i2, K("pi2"))):
                    ksrc = K("paf") if q_ == 0 else K("pbf")
                    S.op("dve", lambda e, src=src: e.tensor_tensor(out=oh4, in0=src[:].rearrange("p (h k) -> p h k", h=8).unsqueeze(3).to_broadcast(B4),
                                                                   in1=iota16.unsqueeze(1).unsqueeze(1).to_broadcast(B4), op=ALU.is_equal), reads=[ksrc, "cst"], writes=[K("ps2")])
                    yield
                    S.op("dve", lambda e, q_=q_: e.tensor_tensor(out=oh4, in0=oh4, in1=sif4[:, :, q_, :].unsqueeze(2).to_broadcast(B4), op=ALU.mult), reads=[K("ps2"), K("psif")], writes=[K("ps2")])
                    yield
                    S.op("dve", lambda e, dst=dst: e.tensor_reduce(out=dst[:], in_=oh4, axis=AX.X, op=ALU.add), reads=[K("ps2")], writes=[kd])
                    yield
                S.op("dve", lambda e: e.scalar_tensor_tensor(out=i1[:], in0=i1[:], scalar=128.0, in1=i2[:], op0=ALU.mult, op1=ALU.add), reads=[K("pi1"), K("pi2")], writes=[K("pi1")])
                S.op("dve", lambda e: e.tensor_copy(out=idxi[:], in_=i1[:]), reads=[K("pi1")], writes=[K("pidxi")])
                yield
                ee3 = ee[:].rearrange("p (h k) -> p h k", h=8)
                S.op("dve", lambda e: e.tensor_tensor(out=ee3, in0=tv3, in1=tv3[:, :, 0:1].to_broadcast([128, 8, 16]), op=ALU.subtract), reads=[K("ptv")], writes=[K("pee")])
                S.op("act", lambda e: e.activation(out=ee[:], in_=ee[:], func=AF.Exp), reads=[K("pee")], writes=[K("pee")])
                S.op("dve", lambda e: e.tensor_reduce(out=ssum[:], in_=ee3, axis=AX.X, op=ALU.add), reads=[K("pee")], writes=[K("pssum")])
                yield
                S.op("dve", lambda e: e.reciprocal(out=ssum[:], in_=ssum[:]), reads=[K("pssum")], writes=[K("pssum")])
                S.op("dve", lambda e: e.tensor_tensor(out=gg[:].rearrange("p (h k) -> p h k", h=8), in0=ee3, in1=ssum[:].unsqueeze(2).to_broadcast([128, 8, 16]), op=ALU.mult),
                     reads=[K("pee"), K("pssum")], writes=[K("pgg")])
                yield

            def exhaust(g):
                if g is not None:
                    for _ in g:
                        pass

            def advance(g, n):
                if g is None:
                    return
                for _ in range(n):
                    try:
                        next(g)
                    except StopIteration:
                        return

            exhaust(prep(0))
            ui = 0
            for t in range(NT):
                p = t % 2
                K = lambda n: "%s%d" % (n, p)
                ts = slice(t * 128, (t + 1) * 128)
                h2t, xt, idxi, gg = h2ts[p], xts[p], idxis[p], ggs[p]
                nxt = prep(t + 1) if t + 1 < NT else None
                for r in range(128):
                    ub = ubs[ui % NB]; kub = "pub%d" % (ui % NB)
                    jk = junks[ui % 2]; kjk = "pjunk%d" % (ui % 2)
                    tb_ = tmpbs[ui % 3]; ktb = "ptmpb%d" % (ui % 3)
                    ui += 1
                    ka, kg, kga = "paa%d" % r, "pgl%d" % r, "pga%d" % r
                    S.dma("pool", lambda e, ub=ub, r=r, idxi=idxi: e.indirect_dma_start(out=ub[:], out_offset=None, in_=UV2[:, :], in_offset=bass.IndirectOffsetOnAxis(ap=idxi[:, r:r + 1], axis=0)),
                          reads=[K("pidxi"), "uvb"], writes=[kub])
                    S.op("dve", lambda e, ub=ub, r=r, h2t=h2t, jk=jk: e.scalar_tensor_tensor(out=jk[:], in0=ub[:, 0:D], scalar=1.0, in1=h2t[:], op0=ALU.mult, op1=ALU.mult, accum_out=aa[:, r:r + 1]),
                         reads=[kub, K("ph2t")], writes=[kjk, ka])
                    S.op("act", lambda e, r=r: e.activation(out=gl[:, r:r + 1], in_=aa[:, r:r + 1], func=AF.Gelu_apprx_tanh), reads=[ka], writes=[kg])
                    S.op("act", lambda e, r=r, gg=gg: e.activation(out=ga[:, r:r + 1], in_=gl[:, r:r + 1], func=AF.Identity, scale=gg[:, r:r + 1]), reads=[kg, K("pgg")], writes=[kga])
                    S.op("act", lambda e, ub=ub, r=r, tb_=tb_: e.activation(out=tb_[:], in_=ub[:, D:2 * D], func=AF.Identity, scale=ga[:, r:r + 1]), reads=[kub, kga], writes=[ktb])
                    for dh in range(2):
                        S.op("pe", lambda e, tb_=tb_, dh=dh, r=r: e.matmul(pacc[:, dh * 512:(dh + 1) * 512], lhsT=self.ident_b(), rhs=tb_[:, dh * 512:(dh + 1) * 512], start=(r == 0), stop=(r == 127)),
                             reads=["cstb", ktb], writes=["ppacc"])
                    advance(nxt, 1)
                for dh in range(2):
                    S.op("dve", lambda e, dh=dh: e.tensor_tensor(out=acc[:, dh * 512:(dh + 1) * 512], in0=pacc[:, dh * 512:(dh + 1) * 512], in1=g2bc[:, dh * 512:(dh + 1) * 512], op=ALU.mult),
                         reads=["ppacc", "g2bc"], writes=["pacc"])
                S.op("dve", lambda e, xt=xt: e.tensor_tensor(out=xt[:], in0=xt[:], in1=acc[:], op=ALU.add), reads=["pacc", K("pxt")], writes=[K("pxt")])
                S.dma("sp", lambda e, xt=xt, ts=ts: e.dma_start(out=self.xres[ts, :], in_=xt[:]), reads=[K("pxt")], writes=["xres"])
                exhaust(nxt)

    def stage_final(self):
        nc, S = self.nc, self.S
        with ExitStack() as es:
            sb = lambda n, sh, dt: es.enter_context(self.sbt(n, sh, dt))
            o, _ = SM_OFF["fg"]
            fgbc = self._bcast_tile(es, "fgbc", lambda c: self.sm[:, o + c:o + c + 1])
            xts = [sb("fxt%d" % i, [128, D], F32) for i in range(2)]
            sq = sb("fsq", [128, D], F32)
            ss = sb("fss", [128, 4], F32)
            for t in range(NT):
                xt = xts[t % 2]; kxt = "fxt%d" % (t % 2)
                S.dma("sp", lambda e, xt=xt, t=t: e.dma_start(out=xt[:], in_=self.xres[t * 128:(t + 1) * 128, :]), reads=["xres"], writes=[kxt])
                S.op("act", lambda e, xt=xt: e.activation(out=sq[:], in_=xt[:], func=AF.Square, accum_out=ss[:, 0:1]), reads=[kxt], writes=["fsq", "fss"])
                S.op("dve", lambda e: e.tensor_scalar(out=ss[:, 1:2], in0=ss[:, 0:1], scalar1=1.0 / D, scalar2=EPS, op0=ALU.mult, op1=ALU.add), reads=["fss"], writes=["fss"])
                S.op("act", lambda e: e.activation(out=ss[:, 2:3], in_=ss[:, 1:2], func=AF.Sqrt), reads=["fss"], writes=["fss"])
                S.op("dve", lambda e: e.reciprocal(out=ss[:, 3:4], in_=ss[:, 2:3]), reads=["fss"], writes=["fss"])
                S.op("dve", lambda e, xt=xt: e.scalar_tensor_tensor(out=xt[:], in0=xt[:], scalar=ss[:, 3:4], in1=fgbc[:], op0=ALU.mult, op1=ALU.mult), reads=[kxt, "fss", "fgbc"], writes=[kxt])
                S.dma("sp", lambda e, xt=xt, t=t: e.dma_start(out=self.out_d[t * 128:(t + 1) * 128, :], in_=xt[:]), reads=[kxt], writes=["out_d"])


def prep_inputs(inputs):
    inp = {k: np.ascontiguousarray(np.asarray(v)) for k, v in inputs.items()}
    consts = make_consts()
    keys = inp["pk_keys"]
    kbd = np.zeros((DEPTH, 8, 128, 256), np.float32)
    for p in range(2):
        kbd[:, :, p * 64:(p + 1) * 64, p * 128:(p + 1) * 128] = keys[:, :, p].transpose(0, 1, 3, 2)
    shared = dict(consts=consts, mod_w=inp["mod_w"], w_in=inp["w_in"], w_branch=inp["w_branch"], w_out=inp["w_out"],
                  pk_wq=inp["pk_wq"], keysbd=kbd,
                  pk_uv=np.concatenate([inp["pk_u"], inp["pk_v"]], axis=2))
    in_maps = []
    for b in range(8):
        m = dict(shared)
        m["x"] = inp["x"][b]
        m["small"] = make_small(inp, b)
        in_maps.append(m)
    return in_maps


_PROG_CACHE = {}


def kernel(**inputs):
    in_maps = prep_inputs(inputs)
    if "nc" not in _PROG_CACHE:
        _PROG_CACHE["nc"] = Prog().build()
    res = run_bass_kernel_spmd(_PROG_CACHE["nc"], in_maps, core_ids=list(range(8)))
    return np.stack([np.asarray(r["out"]) for r in res.results], axis=0).astype(np.float32)
```

```python
from contextlib import ExitStack
import numpy as np
import concourse.bass as bass
import concourse.mybir as mybir
from concourse.bass_utils import run_bass_kernel_spmd

F32 = mybir.dt.float32
BF16 = mybir.dt.bfloat16
U32 = mybir.dt.uint32
I32 = mybir.dt.int32
AF = mybir.ActivationFunctionType
ALU = mybir.AluOpType
AX = mybir.AxisListType

D = 1024
S_LEN = 2048
DEPTH = 4
NT = S_LEN // 128
INW = 8712
EPS = 1e-6
PEER_IMPLEMENTED = True


class Sched:
    ENG = ("pe", "act", "dve", "pool", "sp")

    def __init__(self, nc, dma_slots=None, same_engine_sync=True):
        self.nc = nc
        self.ops = {e: [] for e in self.ENG}
        self.count = {e: 0 for e in self.ENG}
        self.sem = {}
        self.last_w = {}
        self.readers = {}
        self.same_engine_sync = same_engine_sync
        self.dma_slots_n = dma_slots or {"sp": 8, "pool": 12, "act": 4}
        self.dma_sems = {}
        self.dma_rr = {}
        self.seen = {e: {} for e in self.ENG}
        self._ctx = []
        self.n_ops = 0

    def open(self):
        nc = self.nc
        for e in self.ENG:
            cm = nc.semaphore("s_" + e)
            self.sem[e] = cm.__enter__()
            self._ctx.append(cm)
        for q, n in self.dma_slots_n.items():
            self.dma_sems[q] = []
            self.dma_rr[q] = 0
            for i in range(n):
                cm = nc.semaphore("sd_%s%d" % (q, i))
                s = cm.__enter__()
                self._ctx.append(cm)
                self.dma_sems[q].append(dict(sem=s, count=0, name="d_%s%d" % (q, i)))

    def close(self):
        for cm in reversed(self._ctx):
            cm.__exit__(None, None, None)

    def _tok_wait(self, tok):
        if tok[0] == "c":
            return (self.sem[tok[1]], tok[1], tok[2])
        d = self.dma_sems[tok[1]][tok[2]]
        return (d["sem"], d["name"], tok[3])

    def _collect(self, eng, reads, writes):
        toks = []
        for k in reads:
            t = self.last_w.get(k)
            if t is not None:
                toks.append(t)
        for k in writes:
            t = self.last_w.get(k)
            if t is not None:
                toks.append(t)
            toks.extend(self.readers.get(k, ()))
        waits = {}
        for t in toks:
            if t[0] == "c" and t[1] == eng and not self.same_engine_sync:
                continue
            s, name, v = self._tok_wait(t)
            if self.seen[eng].get(name, 0) >= v:
                continue
            if name not in waits or waits[name][1] < v:
                waits[name] = (s, v)
        for name, (s, v) in waits.items():
            self.seen[eng][name] = v
        return list(waits.values())

    def _commit(self, tok, reads, writes):
        for k in reads:
            self.readers.setdefault(k, []).append(tok)
        for k in writes:
            self.last_w[k] = tok
            self.readers[k] = []

    def op(self, eng, fn, reads=(), writes=()):
        waits = self._collect(eng, reads, writes)
        self.count[eng] += 1
        tok = ("c", eng, self.count[eng])
        self.ops[eng].append((fn, waits, "c", None))
        self._commit(tok, reads, writes)
        self.n_ops += 1
        return tok

    def dma(self, eng, fn, reads=(), writes=()):
        slot = self.dma_rr[eng]
        self.dma_rr[eng] = (slot + 1) % len(self.dma_sems[eng])
        d = self.dma_sems[eng][slot]
        waits = self._collect(eng, reads, writes)
        if d["count"] > 0 and self.seen[eng].get(d["name"], 0) < d["count"]:
            waits.append((d["sem"], d["count"]))
            self.seen[eng][d["name"]] = d["count"]
        d["count"] += 16
        tok = ("d", eng, slot, d["count"])
        self.ops[eng].append((fn, waits, "d", d["sem"]))
        self._commit(tok, reads, writes)
        self.n_ops += 1
        return tok

    def barrier(self):
        for e in self.ENG:
            waits = []
            for e2 in self.ENG:
                v = self.count[e2]
                if v > 0 and self.seen[e].get(e2, 0) < v and e2 != e:
                    waits.append((self.sem[e2], v))
                    self.seen[e][e2] = v
            for q in self.dma_sems:
                for d in self.dma_sems[q]:
                    if d["count"] > 0 and self.seen[e].get(d["name"], 0) < d["count"]:
                        waits.append((d["sem"], d["count"]))
                        self.seen[e][d["name"]] = d["count"]
            if self.count[e] > 0:
                waits.append((self.sem[e], self.count[e]))
                self.seen[e][e] = self.count[e]
            self.ops[e].append((None, waits, "w", None))

    def emit(self):
        nc = self.nc
        sched = self
        ops = self.ops
        self.ops = {e: [] for e in self.ENG}
        with nc.Block() as block:
            def run(engname, e):
                for fn, waits, kind, dsem in ops[engname]:
                    for s, v in waits:
                        e.wait_ge(s, v)
                    if fn is None:
                        continue
                    ins = fn(e)
                    if kind == "c":
                        ins.then_inc(sched.sem[engname], 1)
                    else:
                        ins.then_inc(dsem, 16)

            @block.sync
            def _(e):
                run("sp", e)

            @block.tensor
            def _(e):
                run("pe", e)

            @block.scalar
            def _(e):
                run("act", e)

            @block.vector
            def _(e):
                run("dve", e)

            @block.gpsimd
            def _(e):
                run("pool", e)


def _small_layout():
    off = {}
    o = 0
    for name, n in (("mod_b", DEPTH * 48), ("nmg", DEPTH * 8), ("nfg", DEPTH * 8),
                    ("convw", DEPTH * 32), ("convb", DEPTH * 8), ("gateb", DEPTH * 2),
                    ("lbl", DEPTH * 4), ("hgn", DEPTH * 4), ("mln", DEPTH * 4), ("fg", 8), ("cT", 8)):
        off[name] = (o, n)
        o += n
    return off, o


SM_OFF, NS = _small_layout()
C_ID, C_ONES, C_TRI, C_M64, C_SEL, C_IOTA, C_THR = 0, 128, 256, 384, 448, 960, 976
NCONST = 992


def make_consts():
    c = np.zeros((128, NCONST), np.float32)
    c[:, C_ID:C_ID + 128] = np.eye(128, dtype=np.float32)
    c[:, C_ONES:C_ONES + 128] = 1.0
    sp = np.arange(128)[:, None]
    s = np.arange(128)[None, :]
    c[:, C_TRI:C_TRI + 128] = (sp >= s).astype(np.float32)
    t = np.arange(64)[None, :]
    c[:, C_M64:C_M64 + 64] = (t >= (sp % 64)).astype(np.float32)
    for h in range(4):
        c[h, C_SEL + h * 128:C_SEL + (h + 1) * 128] = 1.0
    c[:, C_IOTA:C_IOTA + 16] = np.arange(16, dtype=np.float32)[None, :]
    c[:, C_THR:C_THR + 15] = 16.0 * np.arange(1, 16, dtype=np.float32)[None, :]
    return c


def make_small(inp, b):
    sm = np.zeros((128, NS), np.float32)

    def put(name, arr):
        o, n = SM_OFF[name]
        arr = np.asarray(arr, np.float32).reshape(128, n)
        sm[:, o:o + n] = arr

    put("mod_b", inp["mod_b"].reshape(DEPTH, 48, 128).transpose(2, 0, 1))
    put("nmg", inp["norm_mix_g"].reshape(DEPTH, 8, 128).transpose(2, 0, 1))
    put("nfg", inp["norm_ffn_g"].reshape(DEPTH, 8, 128).transpose(2, 0, 1))
    put("convw", inp["ml_conv_w"].reshape(DEPTH, 4, 8, 128).transpose(3, 0, 1, 2))
    put("convb", inp["ml_conv_b"].reshape(DEPTH, 8, 128).transpose(2, 0, 1))
    gb = np.zeros((128, DEPTH, 2), np.float32)
    gb[0:4, :, 0] = inp["ml_gate_b"][:, 0:4].T
    gb[0:4, :, 1] = inp["ml_gate_b"][:, 4:8].T
    put("gateb", gb)
    put("lbl", inp["hg_lb_logits"].reshape(DEPTH, 4, 128).transpose(2, 0, 1))
    put("hgn", inp["hg_norm_g"].reshape(DEPTH, 4, 128).transpose(2, 0, 1))
    put("mln", inp["ml_norm_g"].reshape(DEPTH, 4, 128).transpose(2, 0, 1))
    put("fg", inp["final_g"].reshape(8, 128).T)
    put("cT", inp["c"][b].reshape(8, 128).T)
    return sm


class Prog:
    def __init__(self, n_layers=DEPTH, stages=None, debug=()):
        self.n_layers = n_layers
        self.stages = stages
        self.debug = set(debug)
        self.nc = bass.Bass("TRN2", target_bir_lowering=False)
        self.S = Sched(self.nc)
        self.uid = 0

    def sbt(self, name, shape, dt):
        self.uid += 1
        return self.nc.sbuf_tensor("%s_u%d" % (name, self.uid), shape, dt)

    def pst(self, name, shape, dt):
        self.uid += 1
        return self.nc.psum_tensor("%s_u%d" % (name, self.uid), shape, dt)

    def want(self, st):
        return self.stages is None or st in self.stages

    def dram(self, name, shape, dt, kind=None):
        if kind is None:
            kind = "ExternalOutput" if name in self.debug else "Internal"
        return self.nc.dram_tensor(name, list(shape), dt, kind=kind).ap()

    def build(self):
        nc, S = self.nc, self.S
        L = self.n_layers
        ext = lambda n, s, dt=F32: nc.dram_tensor(n, list(s), dt, kind="ExternalInput").ap()
        self.x_in = ext("x", [S_LEN, D])
        self.small_d = ext("small", [128, NS])
        self.consts_d = ext("consts", [128, NCONST])
        self.mod_w = ext("mod_w", [L, D, 6 * D])
        self.w_in = ext("w_in", [L, D, INW])
        self.input_names = ["x", "small", "consts", "mod_w", "w_in"]
        if self.want("merge"):
            self.w_branch = ext("w_branch", [L, 3, 512, D])
            self.w_out = ext("w_out", [L, D, D])
            self.input_names += ["w_branch", "w_out"]
        if self.want("peer") and PEER_IMPLEMENTED:
            self.pk_wq = ext("pk_wq", [L, D, D])
            self.keysbd = ext("keysbd", [L, 8, 128, 256])
            self.pk_uv = ext("pk_uv", [L, 16384, 2 * D])
            self.input_names += ["pk_wq", "keysbd", "pk_uv"]
        self.out_d = nc.dram_tensor("out", [S_LEN, D], F32, kind="ExternalOutput").ap()
        self.xres = self.dram("xres", [S_LEN, D], F32)
        self.hT_d = self.dram("hT", [8, 128, S_LEN], BF16)
        self.qT_d = self.dram("qT", [512, S_LEN], BF16)
        self.kT_d = self.dram("kT", [512, S_LEN], BF16)
        self.v_d = self.dram("v_tok", [S_LEN, 512], BF16)
        self.hqT_d = self.dram("hqT", [512, S_LEN], F32)
        self.hfT_d = self.dram("hfT", [512, S_LEN], F32)
        self.hgT_d = self.dram("hgT", [512, S_LEN], F32)
        self.hi_d = self.dram("hi_tok", [S_LEN, 512], BF16)
        self.mqkT_d = self.dram("mqkT", [1024, S_LEN], F32)
        self.mv_d = self.dram("mv_tok", [S_LEN, 512], BF16)
        self.moT_d = self.dram("moT", [512, S_LEN], F32)
        self.gT_d = self.dram("gT", [2, 4, S_LEN], F32)
        self.bgT_d = self.dram("bgT", [3072, S_LEN], F32)
        self.yT_d = self.dram("yT", [3, 512, S_LEN], BF16)
        self.h2_d = self.dram("h2_tok", [S_LEN, D], F32)
        self.uvb = self.dram("uvb", [16384, 2 * D], BF16)
        S.open()
        with (self.sbt("consts", [128, NCONST], F32) as cst,
              self.sbt("constb", [128, 384], BF16) as cstb,
              self.sbt("small", [128, NS], F32) as sm,
              self.sbt("modT", [128, DEPTH * 48], F32) as modT,
              self.sbt("lbT", [128, DEPTH * 4], F32) as lbT):
            self.cst, self.cstb, self.sm, self.modT, self.lbT = cst, cstb, sm, modT, lbT
            S.dma("sp", lambda e: e.dma_start(out=cst[:], in_=self.consts_d[:, :]), writes=["cst"])
            S.dma("sp", lambda e: e.dma_start(out=sm[:], in_=self.small_d[:, :]), writes=["sm"])
            S.op("dve", lambda e: e.tensor_copy(out=cstb[:], in_=cst[:, 0:384]), reads=["cst"], writes=["cstb"])
            S.dma("sp", lambda e: e.dma_start(out=self.xres[:, :], in_=self.x_in[:, :]), writes=["xres"])
            self.stage_mod()
            S.barrier(); S.emit()
            for l in range(L):
                if self.want("norm1"):
                    self.stage_norm(l, 0)
                    S.barrier(); S.emit()
                if self.want("proj"):
                    with ExitStack() as es2:
                        if self.want("peer") and PEER_IMPLEMENTED:
                            self._uvc_bufs = [es2.enter_context(self.sbt("uvc%d" % i, [128, 4, 2 * D], BF16)) for i in range(3)]
                        self.stage_proj(l)
                        S.barrier(); S.emit()
                if self.want("attn"):
                    self.stage_attn(l)
                    S.barrier(); S.emit()
                if self.want("hgrn"):
                    self.stage_hgrn(l)
                    S.barrier(); S.emit()
                if self.want("mlstm"):
                    self.stage_mlstm(l)
                    S.barrier(); S.emit()
                if self.want("merge"):
                    self.stage_merge(l)
                    S.barrier(); S.emit()
                if self.want("peer") and PEER_IMPLEMENTED:
                    self.stage_norm(l, 1)
                    S.barrier(); S.emit()
                    self.stage_peer(l)
                    S.barrier(); S.emit()
            self.stage_final()
            S.barrier(); S.emit()
        S.close()
        return nc

    def ident_f(self):
        return self.cst[:, C_ID:C_ID + 128]

    def ones_f(self):
        return self.cst[:, C_ONES:C_ONES + 128]

    def tri_f(self):
        return self.cst[:, C_TRI:C_TRI + 128]

    def ident_b(self):
        return self.cstb[:, 0:128]

    def ones_b(self):
        return self.cstb[:, 128:256]

    def smc(self, name, l, j, n=1):
        o, tot = SM_OFF[name]
        per = tot // DEPTH
        return self.sm[:, o + l * per + j: o + l * per + j + n]

    def modc(self, l, part, c):
        j = l * 48 + part * 8 + c
        return self.modT[:, j:j + 1]

    def stage_mod(self):
        nc, S = self.nc, self.S
        L = self.n_layers
        sm = self.sm
        with (self.sbt("condT", [128, 8], F32) as condT,
              self.sbt("mw0", [128, 8, 768], F32) as mw0,
              self.sbt("mw1", [128, 8, 768], F32) as mw1,
              self.sbt("lbe", [128, DEPTH * 4], F32) as lbe,
              self.sbt("lbs", [128, 4], F32) as lbs,
              self.sbt("lbm", [128, 4], F32) as lbm,
              self.pst("ps_mod", [128, DEPTH * 48], F32) as psm):
            o, _ = SM_OFF["cT"]
            S.op("act", lambda e: e.activation(out=condT[:], in_=sm[:, o:o + 8], func=AF.Silu), reads=["sm"], writes=["condT"])
            mws = [mw0, mw1]
            gi = 0
            for l in range(L):
                for g in range(8):
                    mw = mws[gi % 2]
                    key = "mw%d" % (gi % 2)
                    gi += 1
                    src = self.mod_w[l, :, g * 768:(g + 1) * 768].rearrange("(kc p) c -> p kc c", p=128)
                    S.dma("sp", lambda e, mw=mw, src=src: e.dma_start(out=mw[:], in_=src), writes=[key])
                    for cc in range(6):
                        j = l * 48 + g * 6 + cc
                        for kc in range(8):
                            S.op("pe", lambda e, mw=mw, cc=cc, kc=kc, j=j: e.matmul(
                                psm[:, j:j + 1], lhsT=mw[:, kc, cc * 128:(cc + 1) * 128], rhs=condT[:, kc:kc + 1],
                                start=(kc == 0), stop=(kc == 7)), reads=[key, "condT"], writes=["psm"])
            ob, _ = SM_OFF["mod_b"]
            S.op("dve", lambda e: e.tensor_tensor(out=self.modT[:, 0:L * 48], in0=psm[:, 0:L * 48], in1=sm[:, ob:ob + L * 48], op=ALU.add),
                 reads=["psm", "sm"], writes=["modT"])
            ol, _ = SM_OFF["lbl"]
            lg = lambda l: sm[:, ol + l * 4: ol + l * 4 + 4]
            S.op("dve", lambda e: e.tensor_tensor(out=lbm[:], in0=lg(0), in1=lg(1), op=ALU.max), reads=["sm"], writes=["lbm"])
            for l in (2, 3):
                S.op("dve", lambda e, l=l: e.tensor_tensor(out=lbm[:], in0=lbm[:], in1=lg(l), op=ALU.max), reads=["sm", "lbm"], writes=["lbm"])
            for l in range(DEPTH):
                S.op("dve", lambda e, l=l: e.tensor_tensor(out=lbe[:, l * 4:l * 4 + 4], in0=lg(l), in1=lbm[:], op=ALU.subtract), reads=["sm", "lbm"], writes=["lbe"])
            S.op("act", lambda e: e.activation(out=lbe[:], in_=lbe[:], func=AF.Exp), reads=["lbe"], writes=["lbe"])
            S.op("dve", lambda e: e.tensor_tensor(out=lbs[:], in0=lbe[:, 0:4], in1=lbe[:, 4:8], op=ALU.add), reads=["lbe"], writes=["lbs"])
            for l in (2, 3):
                S.op("dve", lambda e, l=l: e.tensor_tensor(out=lbs[:], in0=lbs[:], in1=lbe[:, l * 4:l * 4 + 4], op=ALU.add), reads=["lbe", "lbs"], writes=["lbs"])
            S.op("dve", lambda e: e.reciprocal(out=lbs[:], in_=lbs[:]), reads=["lbs"], writes=["lbs"])
            lbT = self.lbT
            S.op("dve", lambda e: e.memset(lbT[:, 0:4], 0.0), writes=["lbT"])
            for l in range(1, DEPTH):
                S.op("dve", lambda e, l=l: e.tensor_tensor(out=lbe[:, l * 4:l * 4 + 4], in0=lbe[:, l * 4:l * 4 + 4], in1=lbs[:], op=ALU.mult), reads=["lbe", "lbs"], writes=["lbe"])
                S.op("dve", lambda e, l=l: e.tensor_tensor(out=lbT[:, l * 4:l * 4 + 4], in0=lbT[:, (l - 1) * 4:l * 4], in1=lbe[:, l * 4:l * 4 + 4], op=ALU.add), reads=["lbe", "lbT"], writes=["lbT"])

    def stage_norm(self, l, which):
        nc, S = self.nc, self.S
        gname = "nmg" if which == 0 else "nfg"
        p_shift, p_scale = (0, 1) if which == 0 else (3, 4)
        with (self.sbt("nx0", [128, D], F32) as nx0, self.sbt("nx1", [128, D], F32) as nx1,
              self.sbt("nsq", [128, 2, D], BF16) as nsq,
              self.sbt("nb0", [128, D], BF16) as nb0, self.sbt("nb1", [128, D], BF16) as nb1,
              self.sbt("nh0", [128, 8, 128], BF16) as nh0, self.sbt("nh1", [128, 8, 128], BF16) as nh1,
              self.sbt("nt0", [128, D], F32) as nt0, self.sbt("nt1", [128, D], F32) as nt1,
              self.sbt("nss", [128, 8], F32) as nss_all,
              self.sbt("nG", [128, 8], F32) as nG,
              self.pst("nps0", [128, 8, 128], BF16) as nps0, self.pst("nps1", [128, 8, 128], BF16) as nps1,
              self.pst("npt0", [128, 8, 128], F32) as npt0):
            j0 = l * 48 + p_scale * 8
            S.op("dve", lambda e: e.scalar_tensor_tensor(out=nG[:], in0=self.modT[:, j0:j0 + 8], scalar=1.0, in1=self.smc(gname, l, 0, 8),
                                                         op0=ALU.add, op1=ALU.mult), reads=["modT", "sm"], writes=["nG"])
            nx, nb, nh, nps, nt = [nx0, nx1], [nb0, nb1], [nh0, nh1], [nps0, nps1], [nt0, nt1]
            for t in range(NT):
                i = t % 2
                kx, kb, kh, kp, kt = "nx%d" % i, "nb%d" % i, "nh%d" % i, "nps%d" % i, "nt%d" % i
                nss = nss_all[:, 4 * i:4 * i + 4]; kss = "nss%d" % i; ksq = "nsq%d" % i
                S.dma("sp", lambda e, i=i, t=t: e.dma_start(out=nx[i][:], in_=self.xres[t * 128:(t + 1) * 128, :]), reads=["xres"], writes=[kx])
                S.op("act", lambda e, i=i, nss=nss: e.activation(out=nsq[:, i, :], in_=nx[i][:], func=AF.Square, accum_out=nss[:, 0:1]), reads=[kx], writes=[ksq, kss])
                S.op("dve", lambda e, nss=nss: e.tensor_scalar(out=nss[:, 1:2], in0=nss[:, 0:1], scalar1=1.0 / D, scalar2=EPS, op0=ALU.mult, op1=ALU.add), reads=[kss], writes=[kss])
                S.op("act", lambda e, nss=nss: e.activation(out=nss[:, 2:3], in_=nss[:, 1:2], func=AF.Sqrt), reads=[kss], writes=[kss])
                S.op("dve", lambda e, nss=nss: e.reciprocal(out=nss[:, 3:4], in_=nss[:, 2:3]), reads=[kss], writes=[kss])
                S.op("dve", lambda e, i=i, nss=nss: e.tensor_scalar(out=nb[i][:], in0=nx[i][:], scalar1=nss[:, 3:4], scalar2=None, op0=ALU.mult), reads=[kx, kss], writes=[kb])
                for c in range(8):
                    S.op("pe", lambda e, i=i, c=c: e.transpose(nps[i][:, c, :], nb[i][:, c * 128:(c + 1) * 128], self.ident_b()), reads=[kb, "cstb"], writes=[kp])
                for c in range(8):
                    S.op("act", lambda e, i=i, c=c: e.activation(out=nh[i][:, c, :], in_=nps[i][:, c, :], func=AF.Identity,
                                                                 scale=nG[:, c:c + 1], bias=self.modc(l, p_shift, c)), reads=[kp, "nG", "modT"], writes=[kh])
                S.dma("sp", lambda e, i=i, t=t: e.dma_start(out=self.hT_d[:, :, t * 128:(t + 1) * 128].rearrange("c p t -> p c t"), in_=nh[i][:]), reads=[kh], writes=["hT_d"])
                if which == 1:
                    pass
            if which == 1:
                self._h2_tokmajor(l, nx, nss_all[:, 0:4], nG, nt, npt0, p_shift)

    def _h2_tokmajor(self, l, nx, nss, nG, nt, npt0, p_shift):
        nc, S = self.nc, self.S
        with (self.sbt("dg", [128, 128], F32) as dg,
              self.sbt("Gbc", [128, D], F32) as Gbc, self.sbt("Sbc", [128, D], F32) as Sbc):
            for which_v, dst in ((0, Gbc), (1, Sbc)):
                for c in range(8):
                    col = nG[:, c:c + 1] if which_v == 0 else self.modc(l, p_shift, c)
                    S.op("dve", lambda e, col=col: e.tensor_scalar(out=dg[:], in0=self.ident_f(), scalar1=col, scalar2=None, op0=ALU.mult), reads=["cst", "nG", "modT"], writes=["dg"])
                    S.op("pe", lambda e, c=c: e.matmul(npt0[:, c, :], lhsT=self.ones_f(), rhs=dg[:], start=True, stop=True), reads=["cst", "dg"], writes=["npt0"])
                S.op("act", lambda e, dst=dst: e.activation(out=dst[:], in_=npt0[:].rearrange("p c t -> p (c t)"), func=AF.Copy), reads=["npt0"], writes=["bc%d" % which_v])
            for t in range(NT):
                i = t % 2
                kx, kt = "nx%d" % i, "nt%d" % i
                S.dma("sp", lambda e, i=i, t=t: e.dma_start(out=nx[i][:], in_=self.xres[t * 128:(t + 1) * 128, :]), reads=["xres"], writes=[kx])
                S.op("act", lambda e, i=i: e.activation(out=nt[i][:], in_=nx[i][:], func=AF.Square, accum_out=nss[:, 0:1]), reads=[kx], writes=[kt, "nss0"])
                S.op("dve", lambda e: e.tensor_scalar(out=nss[:, 1:2], in0=nss[:, 0:1], scalar1=1.0 / D, scalar2=EPS, op0=ALU.mult, op1=ALU.add), reads=["nss0"], writes=["nss0"])
                S.op("act", lambda e: e.activation(out=nss[:, 2:3], in_=nss[:, 1:2], func=AF.Sqrt), reads=["nss0"], writes=["nss0"])
                S.op("dve", lambda e: e.reciprocal(out=nss[:, 3:4], in_=nss[:, 2:3]), reads=["nss0"], writes=["nss0"])
                S.op("dve", lambda e, i=i: e.scalar_tensor_tensor(out=nt[i][:], in0=nx[i][:], scalar=nss[:, 3:4], in1=Gbc[:], op0=ALU.mult, op1=ALU.mult), reads=[kx, "nss0", "bc0"], writes=[kt])
                S.op("dve", lambda e, i=i: e.tensor_tensor(out=nt[i][:], in0=nt[i][:], in1=Sbc[:], op=ALU.add), reads=[kt, "bc1"], writes=[kt])
                S.dma("sp", lambda e, i=i, t=t: e.dma_start(out=self.h2_d[t * 128:(t + 1) * 128, :], in_=nt[i][:]), reads=[kt], writes=["h2_d"])

    def stage_proj(self, l):
        nc, S = self.nc, self.S
        fm = []
        for c in range(4):
            fm.append((0 + c * 128, 128, self.qT_d[c * 128:(c + 1) * 128, :], AF.Copy, 0.125, BF16))
        for c in range(4):
            fm.append((512 + c * 128, 128, self.kT_d[c * 128:(c + 1) * 128, :], AF.Copy, 1.0, BF16))
        for c in range(4):
            fm.append((1536 + c * 128, 128, self.hqT_d[c * 128:(c + 1) * 128, :], AF.Copy, 1.0, F32))
        for c in range(4):
            fm.append((2048 + c * 128, 128, self.hfT_d[c * 128:(c + 1) * 128, :], AF.Copy, 1.0, F32))
        for c in range(4):
            fm.append((3072 + c * 128, 128, self.hgT_d[c * 128:(c + 1) * 128, :], AF.Silu, 1.0, F32))
        for c in range(8):
            fm.append((3584 + c * 128, 128, self.mqkT_d[c * 128:(c + 1) * 128, :], AF.Copy, 1.0, F32))
        for c in range(4):
            fm.append((5120 + c * 128, 128, self.moT_d[c * 128:(c + 1) * 128, :], AF.Sigmoid, 1.0, F32))
        fm.append((5632, 4, self.gT_d[0, :, :], AF.Copy, 1.0, F32))
        fm.append((5636, 4, self.gT_d[1, :, :], AF.Copy, 1.0, F32))
        for c in range(24):
            fm.append((5640 + c * 128, 128, self.bgT_d[c * 128:(c + 1) * 128, :], AF.Sigmoid, 1.0, F32))
        tm = [(1024, self.v_d, AF.Copy), (2560, self.hi_d, AF.Silu), (4608, self.mv_d, AF.Copy)]
        with (self.sbt("hT", [128, 8, S_LEN], BF16) as hT,
              self.sbt("pw0", [128, 8, 512], BF16) as pw0, self.sbt("pw1", [128, 8, 512], BF16) as pw1,
              self.sbt("pof0", [128, S_LEN], F32) as pof0, self.sbt("pof1", [128, S_LEN], F32) as pof1,
              self.sbt("pob0", [128, S_LEN], BF16) as pob0, self.sbt("pob1", [128, S_LEN], BF16) as pob1,
              self.sbt("pot0", [128, 512], BF16) as pot0, self.sbt("pot1", [128, 512], BF16) as pot1,
              self.pst("pp0", [128, 512], F32) as pp0, self.pst("pp1", [128, 512], F32) as pp1,
              self.pst("pp2", [128, 512], F32) as pp2, self.pst("pp3", [128, 512], F32) as pp3):
            for c in range(8):
                S.dma("sp", lambda e, c=c: e.dma_start(out=hT[:, c, :], in_=self.hT_d[c, :, :]), reads=["hT_d"], writes=["hT"])
            pw, pof, pob, pot, pp = [pw0, pw1], [pof0, pof1], [pob0, pob1], [pot0, pot1], [pp0, pp1, pp2, pp3]
            side = self.uvcast_gen(l, self._uvc_bufs) if (self.want("peer") and PEER_IMPLEMENTED) else None
            wi = 0
            pi = 0
            for ji, (col0, ncol, dest, func, scale, dt) in enumerate(fm):
                w = pw[wi % 2]; kw = "pw%d" % (wi % 2); wi += 1
                src = self.w_in[l, :, col0:col0 + ncol].rearrange("(kc p) c -> p kc c", p=128)
                S.dma("pool", lambda e, w=w, src=src, ncol=ncol: e.dma_start(out=w[:, :, 0:ncol], in_=src), writes=[kw])
                ob = (pof if dt == F32 else pob)[ji % 2]
                ko = ("pof%d" if dt == F32 else "pob%d") % (ji % 2)
                for tb in range(4):
                    ps = pp[pi % 4]; kp = "pp%d" % (pi % 4); pi += 1
                    for kc in range(8):
                        S.op("pe", lambda e, ps=ps, w=w, kc=kc, tb=tb, ncol=ncol: e.matmul(
                            ps[0:ncol, :], lhsT=w[:, kc, 0:ncol], rhs=hT[:, kc, tb * 512:(tb + 1) * 512],
                            start=(kc == 0), stop=(kc == 7)), reads=[kw, "hT"], writes=[kp])
                    S.op("act", lambda e, ps=ps, ob=ob, tb=tb, ncol=ncol, func=func, scale=scale: e.activation(
                        out=ob[0:ncol, tb * 512:(tb + 1) * 512], in_=ps[0:ncol, :], func=func, scale=scale), reads=[kp], writes=[ko])
                S.dma("sp", lambda e, ob=ob, dest=dest, ncol=ncol: e.dma_start(out=dest, in_=ob[0:ncol, :]), reads=[ko], writes=["projout"])
                if side is not None:
                    next(side, None)
            for (col0, dest, func) in tm:
                w = pw[wi % 2]; kw = "pw%d" % (wi % 2); wi += 1
                src = self.w_in[l, :, col0:col0 + 512].rearrange("(kc p) c -> p kc c", p=128)
                S.dma("pool", lambda e, w=w, src=src: e.dma_start(out=w[:], in_=src), writes=[kw])
                for t in range(NT):
                    ps = pp[pi % 4]; kp = "pp%d" % (pi % 4); pi += 1
                    for kc in range(8):
                        S.op("pe", lambda e, ps=ps, w=w, kc=kc, t=t: e.matmul(
                            ps[:], lhsT=hT[:, kc, t * 128:(t + 1) * 128], rhs=w[:, kc, :],
                            start=(kc == 0), stop=(kc == 7)), reads=[kw, "hT"], writes=[kp])
                    ot = pot[t % 2]; kt = "pot%d" % (t % 2)
                    S.op("act", lambda e, ps=ps, ot=ot, func=func: e.activation(out=ot[:], in_=ps[:], func=func), reads=[kp], writes=[kt])
                    S.dma("sp", lambda e, ot=ot, dest=dest, t=t: e.dma_start(out=dest[t * 128:(t + 1) * 128, :], in_=ot[:]), reads=[kt], writes=["projout"])
            if side is not None:
                for _ in side:
                    pass

    def stage_attn(self, l):
        nc, S = self.nc, self.S
        NBUF = 4
        with ExitStack() as es:
            sb = lambda n, sh, dt: es.enter_context(self.sbt(n, sh, dt))
            pt = lambda n, sh, dt: es.enter_context(self.pst(n, sh, dt))
            aq = [sb("aq%d" % i, [64, S_LEN], BF16) for i in range(2)]
            ak = [sb("ak%d" % i, [64, S_LEN], BF16) for i in range(2)]
            av = [sb("av%d" % i, [128, NT, 64], BF16) for i in range(2)]
            azs = [sb("azs%d" % i, [128, 512], F32) for i in range(NBUF)]
            asp = [sb("asp%d" % i, [128, 512], F32) for i in range(NBUF)]
            asb = [sb("asb%d" % i, [128, 512], BF16) for i in range(NBUF)]
            alw = [sb("alw%d" % i, [128, 512], F32) for i in range(NBUF)]
            awt = [sb("awt%d" % i, [128, 512], BF16) for i in range(NBUF)]
            ayo = [sb("ayo%d" % i, [64, 512], BF16) for i in range(2)]
            apz = [pt("apz%d" % i, [128, 512], F32) for i in range(2)]
            apc = [pt("apc%d" % i, [128, 512], F32) for i in range(2)]
            apy = [pt("apy%d" % i, [64, 512], F32) for i in range(2)]
            apr = [pt("apr%d" % i, [128, 512], F32) for i in range(2)]
            tri_b = self.cstb[:, 256:384]
            steps = []
            yi = 0
            for h in range(8):
                for qb in range(4):
                    nkb = 4 * (qb + 1)
                    for jn, j in enumerate(reversed(range(nkb))):
                        steps.append(dict(h=h, qb=qb, nkb=nkb, jn=jn, j=j, yi=yi, i=len(steps)))
                    yi += 1
            loaded = set()

            def load_head(h):
                if h in loaded or h >= 8:
                    return
                loaded.add(h)
                hb = h % 2
                S.dma("sp", lambda e: e.dma_start(out=aq[hb][:], in_=self.qT_d[h * 64:(h + 1) * 64, :]), reads=["projout"], writes=["aq%d" % hb])
                S.dma("sp", lambda e: e.dma_start(out=ak[hb][:], in_=self.kT_d[h * 64:(h + 1) * 64, :]), reads=["projout"], writes=["ak%d" % hb])
                S.dma("sp", lambda e: e.dma_start(out=av[hb][:], in_=self.v_d[:, h * 64:(h + 1) * 64].rearrange("(j p) d -> p j d", p=128)), reads=["projout"], writes=["av%d" % hb])

            def phaseA(st):
                h, qb, j, i = st["h"], st["qb"], st["j"], st["i"]
                load_head(h)
                hb = h % 2
                q, k = aq[hb], ak[hb]
                b2, b3 = i % 2, i % NBUF
                pz, zs, sp, sbb = apz[b2], azs[b3], asp[b3], asb[b3]
                kz, kzs, ksp, ksb = "apz%d" % b2, "azs%d" % b3, "asp%d" % b3, "asb%d" % b3
                diag = j >= 4 * qb
                base = qb * 512 - j * 128
                S.op("pe", lambda e: e.matmul(pz[:], lhsT=k[:, j * 128:(j + 1) * 128], rhs=q[:, qb * 512:(qb + 1) * 512], start=True, stop=True), reads=["aq%d" % hb, "ak%d" % hb], writes=[kz])
                S.op("dve", lambda e: e.tensor_copy(out=zs[:], in_=pz[:]), reads=[kz], writes=[kzs])
                S.op("act", lambda e: e.activation(out=sp[:], in_=zs[:], func=AF.Exp), reads=[kzs], writes=[ksp])
                S.op("act", lambda e: e.activation(out=sbb[:], in_=sp[:], func=AF.Ln, bias=1.0), reads=[ksp], writes=[ksb])
                if diag:
                    S.op("pool", lambda e: e.affine_select(out=sbb[:], in_=sbb[:], pattern=[[1, 512]], compare_op=ALU.is_gt, fill=0.0, base=base, channel_multiplier=-1), reads=[ksb], writes=[ksb])

            def phaseB(st):
                qb, j, jn, i = st["qb"], st["j"], st["jn"], st["i"]
                b2, b3 = i % 2, i % NBUF
                pc, zs, sbb, lw, wt = apc[b2], azs[b3], asb[b3], alw[b3], awt[b3]
                kc_, kzs, ksb, klw, kwt = "apc%d" % b2, "azs%d" % b3, "asb%d" % b3, "alw%d" % b3, "awt%d" % b3
                pr = apr[st["yi"] % 2]; kpr = "apr%d" % (st["yi"] % 2)
                diag = j >= 4 * qb
                base = qb * 512 - j * 128
                S.op("pe", lambda e: e.matmul(pc[:], lhsT=tri_b, rhs=sbb[:], start=True, stop=True), reads=["cstb", ksb], writes=[kc_])
                S.op("dve", lambda e: e.tensor_tensor(out=lw[:], in0=zs[:], in1=pc[:], op=ALU.subtract), reads=[kzs, kc_], writes=[klw])
                if jn > 0:
                    S.op("dve", lambda e: e.tensor_tensor(out=lw[:], in0=lw[:], in1=pr[:], op=ALU.subtract), reads=[klw, kpr], writes=[klw])
                S.op("act", lambda e: e.activation(out=wt[:], in_=lw[:], func=AF.Exp), reads=[klw], writes=[kwt])
                if diag:
                    S.op("pool", lambda e: e.affine_select(out=wt[:], in_=wt[:], pattern=[[1, 512]], compare_op=ALU.is_gt, fill=0.0, base=base, channel_multiplier=-1), reads=[kwt], writes=[kwt])

            def phaseC(st):
                h, qb, j, jn, nkb, i = st["h"], st["qb"], st["j"], st["jn"], st["nkb"], st["i"]
                hb = h % 2
                v = av[hb]
                b3 = i % NBUF
                sbb, wt = asb[b3], awt[b3]
                ksb, kwt = "asb%d" % b3, "awt%d" % b3
                y2 = st["yi"] % 2
                py, pr, yo = apy[y2], apr[y2], ayo[y2]
                kpy, kpr, kyo = "apy%d" % y2, "apr%d" % y2, "ayo%d" % y2
                S.op("pe", lambda e: e.matmul(py[:], lhsT=v[:, j, :], rhs=wt[:], start=(jn == 0), stop=(jn == nkb - 1)), reads=["av%d" % hb, kwt], writes=[kpy])
                if jn < nkb - 1:
                    S.op("pe", lambda e: e.matmul(pr[:], lhsT=self.ones_b(), rhs=sbb[:], start=(jn == 0), stop=(jn == nkb - 2)), reads=["cstb", ksb], writes=[kpr])
                else:
                    S.op("act", lambda e: e.activation(out=yo[:], in_=py[:], func=AF.Copy), reads=[kpy], writes=[kyo])
                    S.dma("sp", lambda e: e.dma_start(out=self.yT_d[0, h * 64:(h + 1) * 64, qb * 512:(qb + 1) * 512], in_=yo[:]), reads=[kyo], writes=["yT_d"])

            n = len(steps)
            for s_ in range(n + 2):
                if 0 <= s_ - 2 < n:
                    phaseC(steps[s_ - 2])
                if 0 <= s_ - 1 < n:
                    phaseB(steps[s_ - 1])
                if s_ < n:
                    phaseA(steps[s_])

    def _zero_branch(self, g):
        nc, S = self.nc, self.S
        with ExitStack() as es:
            z = es.enter_context(self.sbt("zb", [128, S_LEN], BF16))
            S.op("dve", lambda e: e.memset(z[:], 0.0), writes=["zb"])
            for kc in range(4):
                S.dma("sp", lambda e, kc=kc: e.dma_start(out=self.yT_d[g, kc * 128:(kc + 1) * 128, :], in_=z[:]), reads=["zb"], writes=["yT_d"])

    def stage_hgrn(self, l):
        nc, S = self.nc, self.S
        with ExitStack() as es:
            sb = lambda n, sh, dt: es.enter_context(self.sbt(n, sh, dt))
            pt = lambda n, sh, dt: es.enter_context(self.pst(n, sh, dt))
            q = sb("hq", [128, S_LEN], F32)
            f = sb("hf", [128, S_LEN], F32)
            lf = sb("hlf", [128, S_LEN], F32)
            kin = sb("hkin", [128, S_LEN], F32)
            Bg = sb("hBg", [128, S_LEN], F32)
            Dd = sb("hD", [128, S_LEN], F32)
            Ee = sb("hE", [128, S_LEN], F32)
            Q1, K1 = sb("hQ1", [128, S_LEN], BF16), sb("hK1", [128, S_LEN], BF16)
            Q2, K2 = sb("hQ2", [128, S_LEN], BF16), sb("hK2", [128, S_LEN], BF16)
            hi = sb("hhi", [128, NT, 128], BF16)
            k2t = sb("hk2t", [128, NT, 128], BF16)
            hg = sb("hhg", [128, S_LEN], F32)
            oT = sb("hoT", [128, S_LEN], F32)
            Bst, Bmid, Bend, dec = sb("hBst", [128, 32], F32), sb("hBmid", [128, 32], F32), sb("hBend", [128, 32], F32), sb("hdec", [128, 32], F32)
            oml = sb("homl", [128, 1], F32)
            St, Stb = sb("hSt", [128, 128], F32), sb("hStb", [128, 128], BF16)
            pTs = [sb("hpT%d" % i, [128, 64], BF16) for i in range(2)]
            sq = sb("hsq", [128, 512], F32)
            rs = sb("hrs", [128, 512], F32)
            yb = sb("hyb", [128, 512], F32)
            ybb = sb("hybb", [128, 512], BF16)
            pss = [pt("hpss%d" % i, [128, 512], F32) for i in range(2)]
            pso = [pt("hpso%d" % i, [128, 512], F32) for i in range(2)]
            psst = [pt("hpsst%d" % i, [128, 512], F32) for i in range(2)]
            ptr = pt("hptr", [128, 1024], BF16)
            psn = pt("hpsn", [128, 512], F32)
            mask = self.cst[:, C_M64:C_M64 + 64]
            Bg3 = Bg[:].rearrange("p (c t) -> p c t", t=64)
            for h in range(4):
                lb = self.lbT[:, l * 4 + h: l * 4 + h + 1]
                rows = slice(h * 128, (h + 1) * 128)
                S.dma("sp", lambda e, rows=rows: e.dma_start(out=q[:], in_=self.hqT_d[rows, :]), reads=["projout"], writes=["hq"])
                S.dma("sp", lambda e, rows=rows: e.dma_start(out=f[:], in_=self.hfT_d[rows, :]), reads=["projout"], writes=["hf"])
                S.dma("sp", lambda e, rows=rows: e.dma_start(out=hg[:], in_=self.hgT_d[rows, :]), reads=["projout"], writes=["hhg"])
                S.dma("sp", lambda e, rows=rows: e.dma_start(out=hi[:], in_=self.hi_d[:, rows].rearrange("(j p) v -> p j v", p=128)), reads=["projout"], writes=["hhi"])
                S.op("dve", lambda e, lb=lb: e.tensor_scalar(out=oml[:], in0=lb, scalar1=-1.0, scalar2=1.0, op0=ALU.mult, op1=ALU.add), reads=["lbT"], writes=["homl"])
                S.op("act", lambda e: e.activation(out=f[:], in_=f[:], func=AF.Sigmoid), reads=["hf"], writes=["hf"])
                S.op("dve", lambda e, lb=lb: e.tensor_scalar(out=f[:], in0=f[:], scalar1=oml[:, 0:1], scalar2=lb, op0=ALU.mult, op1=ALU.add), reads=["hf", "homl", "lbT"], writes=["hf"])
                S.op("act", lambda e: e.activation(out=lf[:], in_=f[:], func=AF.Ln), reads=["hf"], writes=["hlf"])
                S.op("dve", lambda e: e.tensor_scalar(out=kin[:], in0=f[:], scalar1=-1.0, scalar2=1.0, op0=ALU.mult, op1=ALU.add), reads=["hf"], writes=["hkin"])
                S.op("dve", lambda e: e.tensor_tensor_scan(out=Bg[:], data0=lf[:], data1=lf[:], initial=0.0, op0=ALU.add, op1=ALU.bypass), reads=["hlf"], writes=["hBg"])
                S.op("dve", lambda e: e.memset(Bst[:, 0:1], 0.0), writes=["hBst"])
                S.op("dve", lambda e: e.tensor_copy(out=Bst[:, 1:32], in_=Bg3[:, 0:31, 63]), reads=["hBg"], writes=["hBst"])
                S.op("dve", lambda e: e.tensor_copy(out=Bmid[:], in_=Bg3[:, :, 31]), reads=["hBg"], writes=["hBmid"])
                S.op("dve", lambda e: e.tensor_copy(out=Bend[:], in_=Bg3[:, :, 63]), reads=["hBg"], writes=["hBend"])
                S.op("dve", lambda e: e.tensor_tensor(out=dec[:], in0=Bend[:], in1=Bst[:], op=ALU.subtract), reads=["hBend", "hBst"], writes=["hdec"])
                S.op("act", lambda e: e.activation(out=dec[:], in_=dec[:], func=AF.Exp), reads=["hdec"], writes=["hdec"])

                def sub_cols(col, key):
                    for c in range(32):
                        S.op("dve", lambda e, c=c: e.tensor_scalar(out=Dd[:, c * 64:(c + 1) * 64], in0=Bg[:, c * 64:(c + 1) * 64], scalar1=col[:, c:c + 1], scalar2=None, op0=ALU.subtract),
                             reads=["hBg", key], writes=["hD"])
                sub_cols(Bmid, "hBmid")
                S.op("act", lambda e: e.activation(out=Ee[:], in_=Dd[:], func=AF.Exp), reads=["hD"], writes=["hE"])
                S.op("dve", lambda e: e.tensor_tensor(out=Q1[:], in0=q[:], in1=Ee[:], op=ALU.mult), reads=["hq", "hE"], writes=["hQ1"])
                S.op("act", lambda e: e.activation(out=Ee[:], in_=Dd[:], func=AF.Exp, scale=-1.0), reads=["hD", "hQ1"], writes=["hE"])
                S.op("dve", lambda e: e.tensor_tensor(out=K1[:], in0=kin[:], in1=Ee[:], op=ALU.mult), reads=["hkin", "hE"], writes=["hK1"])
                sub_cols(Bst, "hBst")
                S.op("act", lambda e: e.activation(out=Ee[:], in_=Dd[:], func=AF.Exp), reads=["hD", "hK1"], writes=["hE"])
                S.op("dve", lambda e: e.tensor_tensor(out=Q2[:], in0=q[:], in1=Ee[:], op=ALU.mult), reads=["hq", "hE"], writes=["hQ2"])
                sub_cols(Bend, "hBend")
                S.op("act", lambda e: e.activation(out=Ee[:], in_=Dd[:], func=AF.Exp, scale=-1.0), reads=["hD", "hQ2"], writes=["hE"])
                S.op("dve", lambda e: e.tensor_tensor(out=K2[:], in0=kin[:], in1=Ee[:], op=ALU.mult), reads=["hkin", "hE"], writes=["hK2"])
                for j in range(NT):
                    S.op("pe", lambda e, j=j: e.transpose(ptr[:, 0:128], K2[:, j * 128:(j + 1) * 128], self.ident_b()), reads=["hK2", "cstb"], writes=["hptr"])
                    S.op("act", lambda e, j=j: e.activation(out=k2t[:, j, :], in_=ptr[:, 0:128], func=AF.Copy), reads=["hptr"], writes=["hk2t"])
                def pre(c):
                    r0 = (c % 2) * 64
                    cs = slice(c * 64, (c + 1) * 64)
                    ps_s = pss[c % 2]; kps = "hpss%d" % (c % 2)
                    pT = pTs[c % 2]; kpT = "hpT%d" % (c % 2)
                    S.op("pe", lambda e: e.matmul(ps_s[r0:r0 + 64, 0:64], lhsT=K1[:, cs], rhs=Q1[:, cs], start=True, stop=True), reads=["hK1", "hQ1"], writes=[kps])
                    S.op("dve", lambda e: e.tensor_copy(out=pT[r0:r0 + 64, :], in_=ps_s[r0:r0 + 64, 0:64]), reads=[kps], writes=[kpT])
                    S.op("pool", lambda e: e.affine_select(out=pT[r0:r0 + 64, :], in_=pT[r0:r0 + 64, :], pattern=[[1, 64]], compare_op=ALU.is_ge, fill=0.0, base=0, channel_multiplier=-1),
                         reads=[kpT], writes=[kpT])

                pre(0)
                for c in range(32):
                    j, half = c // 2, c % 2
                    r0 = half * 64
                    cs = slice(c * 64, (c + 1) * 64)
                    pT = pTs[c % 2]; kpT = "hpT%d" % (c % 2)
                    po = pso[(c // 8) % 2]; kpo = "hpso%d" % ((c // 8) % 2)
                    ocs = slice((c % 8) * 64, (c % 8 + 1) * 64)
                    pst_ = psst[c % 2]; kpst = "hpsst%d" % (c % 2)
                    S.op("pe", lambda e, po=po, pT=pT, r0=r0, j=j, ocs=ocs, c=c: e.matmul(po[:, ocs], lhsT=hi[r0:r0 + 64, j, :], rhs=pT[r0:r0 + 64, :], start=True, stop=(c == 0)), reads=["hhi", kpT], writes=[kpo])
                    if c + 1 < 32:
                        pre(c + 1)
                    if c > 0:
                        S.op("pe", lambda e, po=po, ocs=ocs, cs=cs: e.matmul(po[:, ocs], lhsT=Stb[:], rhs=Q2[:, cs], start=False, stop=True), reads=["hStb", "hQ2"], writes=[kpo])
                    if c < 31:
                        S.op("pe", lambda e, pst_=pst_, r0=r0, j=j: e.matmul(pst_[:, 0:128], lhsT=k2t[r0:r0 + 64, j, :], rhs=hi[r0:r0 + 64, j, :], start=True, stop=True), reads=["hk2t", "hhi"], writes=[kpst])
                        if c == 0:
                            S.op("dve", lambda e, pst_=pst_: e.tensor_copy(out=St[:], in_=pst_[:, 0:128]), reads=[kpst], writes=["hSt"])
                        else:
                            S.op("dve", lambda e, pst_=pst_, c=c: e.scalar_tensor_tensor(out=St[:], in0=St[:], scalar=dec[:, c:c + 1], in1=pst_[:, 0:128], op0=ALU.mult, op1=ALU.add), reads=[kpst, "hSt", "hdec"], writes=["hSt"])
                        S.op("act", lambda e: e.activation(out=Stb[:], in_=St[:], func=AF.Copy), reads=["hSt"], writes=["hStb"])
                    if c % 8 == 7:
                        tb = c // 8
                        S.op("act", lambda e, po=po, tb=tb: e.activation(out=oT[:, tb * 512:(tb + 1) * 512], in_=po[:], func=AF.Copy), reads=[kpo], writes=["hoT"])
                gcol = self.smc("hgn", l, h)
                for tb in range(4):
                    bs = slice(tb * 512, (tb + 1) * 512)
                    S.op("act", lambda e, bs=bs: e.activation(out=sq[:], in_=oT[:, bs], func=AF.Square), reads=["hoT"], writes=["hsq"])
                    S.op("pe", lambda e: e.matmul(psn[:], lhsT=self.ones_f(), rhs=sq[:], start=True, stop=True), reads=["cst", "hsq"], writes=["hpsn"])
                    S.op("dve", lambda e: e.tensor_scalar(out=rs[:], in0=psn[:], scalar1=1.0 / 128, scalar2=EPS, op0=ALU.mult, op1=ALU.add), reads=["hpsn"], writes=["hrs"])
                    S.op("act", lambda e: e.activation(out=rs[:], in_=rs[:], func=AF.Sqrt), reads=["hrs"], writes=["hrs"])
                    S.op("dve", lambda e: e.reciprocal(out=rs[:], in_=rs[:]), reads=["hrs"], writes=["hrs"])
                    S.op("dve", lambda e, bs=bs: e.tensor_tensor(out=yb[:], in0=oT[:, bs], in1=rs[:], op=ALU.mult), reads=["hoT", "hrs"], writes=["hyb"])
                    S.op("dve", lambda e, bs=bs, gcol=gcol: e.scalar_tensor_tensor(out=ybb[:], in0=yb[:], scalar=gcol, in1=hg[:, bs], op0=ALU.mult, op1=ALU.mult), reads=["hyb", "sm", "hhg"], writes=["hybb"])
                    S.dma("sp", lambda e, bs=bs, rows=rows: e.dma_start(out=self.yT_d[1, rows, bs], in_=ybb[:]), reads=["hybb"], writes=["yT_d"])

    def stage_mlstm(self, l):
        nc, S = self.nc, self.S
        with ExitStack() as es:
            sb = lambda n, sh, dt: es.enter_context(self.sbt(n, sh, dt))
            pt = lambda n, sh, dt: es.enter_context(self.pst(n, sh, dt))
            gi, gf = sb("mgi", [4, S_LEN], F32), sb("mgf", [4, S_LEN], F32)
            Bc, aa, AA = sb("mB", [4, S_LEN], F32), sb("ma", [4, S_LEN], F32), sb("mA", [4, S_LEN], F32)
            em, Dr = sb("mem", [4, S_LEN], F32), sb("mDr", [4, S_LEN], F32)
            uu, ww, it = sb("mu", [4, S_LEN], F32), sb("mw", [4, S_LEN], F32), sb("mit", [4, S_LEN], F32)
            Aend, Aprev, decr = sb("mAend", [4, 32], F32), sb("mAprev", [4, 32], F32), sb("mdecr", [4, 32], F32)
            nbf = sb("mnbf", [4, 1], F32)
            og, _ = SM_OFF["gateb"]
            bi = self.sm[0:4, og + l * 2: og + l * 2 + 1]
            bf = self.sm[0:4, og + l * 2 + 1: og + l * 2 + 2]
            S.dma("sp", lambda e: e.dma_start(out=gi[:], in_=self.gT_d[0, :, :]), reads=["projout"], writes=["mgi"])
            S.dma("sp", lambda e: e.dma_start(out=gf[:], in_=self.gT_d[1, :, :]), reads=["projout"], writes=["mgf"])
            S.op("dve", lambda e: e.tensor_scalar(out=gi[:], in0=gi[:], scalar1=bi, scalar2=None, op0=ALU.add), reads=["mgi", "sm"], writes=["mgi"])
            S.op("dve", lambda e: e.tensor_scalar(out=nbf[:], in0=bf, scalar1=-1.0, scalar2=None, op0=ALU.mult), reads=["sm"], writes=["mnbf"])
            S.op("act", lambda e: e.activation(out=gf[:], in_=gf[:], func=AF.Exp, scale=-1.0, bias=nbf[:, 0:1]), reads=["mgf", "mnbf"], writes=["mgf"])
            S.op("act", lambda e: e.activation(out=gf[:], in_=gf[:], func=AF.Ln, bias=1.0), reads=["mgf"], writes=["mgf"])
            S.op("dve", lambda e: e.tensor_scalar(out=gf[:], in0=gf[:], scalar1=-1.0, scalar2=None, op0=ALU.mult), reads=["mgf"], writes=["mgf"])
            S.op("dve", lambda e: e.tensor_tensor_scan(out=Bc[:], data0=gf[:], data1=gf[:], initial=0.0, op0=ALU.add, op1=ALU.bypass), reads=["mgf"], writes=["mB"])
            S.op("dve", lambda e: e.tensor_tensor(out=aa[:], in0=gi[:], in1=Bc[:], op=ALU.subtract), reads=["mgi", "mB"], writes=["ma"])
            S.op("dve", lambda e: e.tensor_tensor_scan(out=AA[:], data0=aa[:], data1=aa[:], initial=0.0, op0=ALU.max, op1=ALU.bypass), reads=["ma"], writes=["mA"])
            S.op("dve", lambda e: e.tensor_tensor(out=em[:], in0=Bc[:], in1=AA[:], op=ALU.add), reads=["mB", "mA"], writes=["mem"])
            S.op("act", lambda e: e.activation(out=em[:], in_=em[:], func=AF.Exp, scale=-1.0), reads=["mem"], writes=["mem"])
            A3 = AA[:].rearrange("p (c t) -> p c t", t=64)
            a3 = aa[:].rearrange("p (c t) -> p c t", t=64)
            D3 = Dr[:].rearrange("p (c t) -> p c t", t=64)
            S.op("dve", lambda e: e.tensor_copy(out=Aend[:], in_=A3[:, :, 63]), reads=["mA"], writes=["mAend"])
            S.op("dve", lambda e: e.memset(Aprev[:, 0:1], 0.0), writes=["mAprev"])
            S.op("dve", lambda e: e.tensor_copy(out=Aprev[:, 1:32], in_=A3[:, 0:31, 63]), reads=["mA"], writes=["mAprev"])
            S.op("dve", lambda e: e.tensor_tensor(out=decr[:], in0=Aprev[:], in1=Aend[:], op=ALU.subtract), reads=["mAprev", "mAend"], writes=["mdecr"])
            S.op("act", lambda e: e.activation(out=decr[:], in_=decr[:], func=AF.Exp), reads=["mdecr"], writes=["mdecr"])
            bc = lambda col: col[:].unsqueeze(2).to_broadcast([4, 32, 64])
            S.op("dve", lambda e: e.tensor_tensor(out=D3, in0=A3, in1=bc(Aend), op=ALU.subtract), reads=["mA", "mAend"], writes=["mDr"])
            S.op("act", lambda e: e.activation(out=uu[:], in_=Dr[:], func=AF.Exp, scale=-1.0), reads=["mDr"], writes=["mu"])
            S.op("dve", lambda e: e.tensor_tensor(out=D3, in0=a3, in1=bc(Aend), op=ALU.subtract), reads=["ma", "mAend", "mu"], writes=["mDr"])
            S.op("act", lambda e: e.activation(out=ww[:], in_=Dr[:], func=AF.Exp), reads=["mDr"], writes=["mw"])
            S.op("dve", lambda e: e.tensor_tensor(out=D3, in0=A3, in1=bc(Aprev), op=ALU.subtract), reads=["mA", "mAprev", "mw"], writes=["mDr"])
            S.op("act", lambda e: e.activation(out=it[:], in_=Dr[:], func=AF.Exp, scale=-1.0), reads=["mDr"], writes=["mit"])
            xr, acc = sb("mxr", [128, S_LEN], F32), sb("macc2", [128, S_LEN], F32)
            Q1, Q2, K1 = sb("mQ1", [128, S_LEN], BF16), sb("mQ2", [128, S_LEN], BF16), sb("mK1", [128, S_LEN], BF16)
            vv = sb("mvv", [128, NT, 128], BF16)
            k2t = sb("mk2t", [128, NT, 128], BF16)
            mo = sb("mmo", [128, S_LEN], F32)
            hT = sb("mhT", [128, S_LEN], F32)
            dec = sb("mdec", [128, 32], F32)
            Cs, Csb = sb("mCs", [128, 128], F32), sb("mCsb", [128, 128], BF16)
            Ns, Nsb = sb("mNs", [128, 128], F32), sb("mNsb", [128, 128], BF16)
            pTs = [sb("mpT%d" % i, [128, 64], BF16) for i in range(2)]
            numT, embc, dmx = sb("mnumT", [128, 512], F32), sb("membc", [128, 512], F32), sb("mdmx", [128, 512], F32)
            sq, rs, yb, ybb = sb("msq", [128, 512], F32), sb("mrs", [128, 512], F32), sb("myb", [128, 512], F32), sb("mybb", [128, 512], BF16)
            pss = pt("mpss", [128, 512], F32)
            pso = [pt("mpso%d" % i, [128, 512], F32) for i in range(2)]
            psd = [pt("mpsd%d" % i, [128, 512], F32) for i in range(2)]
            pstC, pstN = pt("mpstC", [128, 512], F32), pt("mpstN", [128, 512], F32)
            pmisc = pt("mpmisc", [128, 512], F32)
            pmisc_b = pmisc[:].bitcast(BF16)
            KM = "mpmisc"
            ones_b = self.ones_b()

            def bcast_rows(rows, h, tb):
                S.op("pe", lambda e: e.matmul(pmisc[:], lhsT=self.cst[0:4, C_SEL + h * 128: C_SEL + (h + 1) * 128], rhs=rows[0:4, tb * 512:(tb + 1) * 512], start=True, stop=True),
                     reads=["cst", "mu", "mw", "mit", "mem"], writes=[KM])

            def conv_silu(chunk):
                cw = lambda tap: self.smc("convw", l, tap * 8 + chunk)
                S.op("dve", lambda e: e.tensor_scalar(out=acc[:], in0=xr[:], scalar1=cw(3), scalar2=self.smc("convb", l, chunk), op0=ALU.mult, op1=ALU.add), reads=["mxr", "sm"], writes=["macc2"])
                for sh in (1, 2, 3):
                    S.op("dve", lambda e, sh=sh: e.scalar_tensor_tensor(out=acc[:, sh:S_LEN], in0=xr[:, 0:S_LEN - sh], scalar=cw(3 - sh), in1=acc[:, sh:S_LEN], op0=ALU.mult, op1=ALU.add),
                         reads=["mxr", "sm", "macc2"], writes=["macc2"])
                S.op("act", lambda e: e.activation(out=acc[:], in_=acc[:], func=AF.Silu), reads=["macc2"], writes=["macc2"])

            for h in range(4):
                rows = slice(h * 128, (h + 1) * 128)
                S.dma("sp", lambda e, rows=rows: e.dma_start(out=xr[:], in_=self.mqkT_d[rows, :]), reads=["projout"], writes=["mxr"])
                S.dma("sp", lambda e, rows=rows: e.dma_start(out=mo[:], in_=self.moT_d[rows, :]), reads=["projout"], writes=["mmo"])
                S.dma("sp", lambda e, rows=rows: e.dma_start(out=vv[:], in_=self.mv_d[:, rows].rearrange("(j p) v -> p j v", p=128)), reads=["projout"], writes=["mvv"])
                conv_silu(h)
                for tb in range(4):
                    bs = slice(tb * 512, (tb + 1) * 512)
                    bcast_rows(uu, h, tb)
                    S.op("dve", lambda e, bs=bs: e.tensor_tensor(out=Q1[:, bs], in0=acc[:, bs], in1=pmisc[:], op=ALU.mult), reads=["macc2", KM], writes=["mQ1"])
                    bcast_rows(it, h, tb)
                    S.op("dve", lambda e, bs=bs: e.tensor_tensor(out=Q2[:, bs], in0=acc[:, bs], in1=pmisc[:], op=ALU.mult), reads=["macc2", KM], writes=["mQ2"])
                S.dma("sp", lambda e, h=h: e.dma_start(out=xr[:], in_=self.mqkT_d[512 + h * 128: 512 + (h + 1) * 128, :]), reads=["projout"], writes=["mxr"])
                conv_silu(4 + h)
                for tb in range(4):
                    bs = slice(tb * 512, (tb + 1) * 512)
                    bcast_rows(ww, h, tb)
                    S.op("dve", lambda e, bs=bs: e.scalar_tensor_tensor(out=K1[:, bs], in0=acc[:, bs], scalar=128.0 ** -0.5, in1=pmisc[:], op0=ALU.mult, op1=ALU.mult), reads=["macc2", KM], writes=["mK1"])
                S.op("pe", lambda e, h=h: e.matmul(pmisc[:, 0:32], lhsT=self.cst[0:4, C_SEL + h * 128: C_SEL + (h + 1) * 128], rhs=decr[0:4, :], start=True, stop=True), reads=["cst", "mdecr"], writes=[KM])
                S.op("act", lambda e: e.activation(out=dec[:], in_=pmisc[:, 0:32], func=AF.Copy), reads=[KM], writes=["mdec"])
                for j in range(NT):
                    S.op("pe", lambda e, j=j: e.transpose(pmisc_b[:, 0:128], K1[:, j * 128:(j + 1) * 128], self.ident_b()), reads=["mK1", "cstb"], writes=[KM])
                    S.op("act", lambda e, j=j: e.activation(out=k2t[:, j, :], in_=pmisc_b[:, 0:128], func=AF.Copy), reads=[KM], writes=["mk2t"])
                def pre(c):
                    r0 = (c % 2) * 64
                    cs = slice(c * 64, (c + 1) * 64)
                    pT = pTs[c % 2]; kpT = "mpT%d" % (c % 2)
                    S.op("pe", lambda e: e.matmul(pss[r0:r0 + 64, 0:64], lhsT=K1[:, cs], rhs=Q1[:, cs], start=True, stop=True), reads=["mK1", "mQ1"], writes=["mpss"])
                    S.op("dve", lambda e: e.tensor_copy(out=pT[r0:r0 + 64, :], in_=pss[r0:r0 + 64, 0:64]), reads=["mpss"], writes=[kpT])
                    S.op("pool", lambda e: e.affine_select(out=pT[r0:r0 + 64, :], in_=pT[r0:r0 + 64, :], pattern=[[1, 64]], compare_op=ALU.is_ge, fill=0.0, base=0, channel_multiplier=-1),
                         reads=[kpT], writes=[kpT])

                pre(0)
                for c in range(32):
                    j, half = c // 2, c % 2
                    r0 = half * 64
                    cs = slice(c * 64, (c + 1) * 64)
                    pT = pTs[c % 2]; kpT = "mpT%d" % (c % 2)
                    po = pso[(c // 8) % 2]; kpo = "mpso%d" % ((c // 8) % 2)
                    pd = psd[(c // 8) % 2]; kpd = "mpsd%d" % ((c // 8) % 2)
                    ocs = slice((c % 8) * 64, (c % 8 + 1) * 64)
                    S.op("pe", lambda e, po=po, pT=pT, r0=r0, j=j, ocs=ocs, c=c: e.matmul(po[:, ocs], lhsT=vv[r0:r0 + 64, j, :], rhs=pT[r0:r0 + 64, :], start=True, stop=(c == 0)), reads=["mvv", kpT], writes=[kpo])
                    if c > 0:
                        S.op("pe", lambda e, po=po, ocs=ocs, cs=cs: e.matmul(po[:, ocs], lhsT=Csb[:], rhs=Q2[:, cs], start=False, stop=True), reads=["mCsb", "mQ2"], writes=[kpo])
                    S.op("pe", lambda e, pd=pd, pT=pT, r0=r0, ocs=ocs, c=c: e.matmul(pd[:, ocs], lhsT=ones_b[r0:r0 + 64, :], rhs=pT[r0:r0 + 64, :], start=True, stop=(c == 0)), reads=["cstb", kpT], writes=[kpd])
                    if c + 1 < 32:
                        pre(c + 1)
                    if c > 0:
                        S.op("pe", lambda e, pd=pd, ocs=ocs, cs=cs: e.matmul(pd[:, ocs], lhsT=Nsb[:], rhs=Q2[:, cs], start=False, stop=True), reads=["mNsb", "mQ2"], writes=[kpd])
                    if c < 31:
                        S.op("pe", lambda e, r0=r0, j=j: e.matmul(pstC[:, 0:128], lhsT=k2t[r0:r0 + 64, j, :], rhs=vv[r0:r0 + 64, j, :], start=True, stop=True), reads=["mk2t", "mvv"], writes=["mpstC"])
                        S.op("pe", lambda e, r0=r0, j=j: e.matmul(pstN[:, 0:128], lhsT=k2t[r0:r0 + 64, j, :], rhs=ones_b[r0:r0 + 64, :], start=True, stop=True), reads=["mk2t", "cstb"], writes=["mpstN"])
                        if c == 0:
                            S.op("dve", lambda e: e.tensor_copy(out=Cs[:], in_=pstC[:, 0:128]), reads=["mpstC"], writes=["mCs"])
                            S.op("dve", lambda e: e.tensor_copy(out=Ns[:], in_=pstN[:, 0:128]), reads=["mpstN"], writes=["mNs"])
                        else:
                            S.op("dve", lambda e, c=c: e.scalar_tensor_tensor(out=Cs[:], in0=Cs[:], scalar=dec[:, c:c + 1], in1=pstC[:, 0:128], op0=ALU.mult, op1=ALU.add), reads=["mpstC", "mCs", "mdec"], writes=["mCs"])
                            S.op("dve", lambda e, c=c: e.scalar_tensor_tensor(out=Ns[:], in0=Ns[:], scalar=dec[:, c:c + 1], in1=pstN[:, 0:128], op0=ALU.mult, op1=ALU.add), reads=["mpstN", "mNs", "mdec"], writes=["mNs"])
                        S.op("act", lambda e: e.activation(out=Csb[:], in_=Cs[:], func=AF.Copy), reads=["mCs"], writes=["mCsb"])
                        S.op("act", lambda e: e.activation(out=Nsb[:], in_=Ns[:], func=AF.Copy), reads=["mNs"], writes=["mNsb"])
                    if c % 8 == 7:
                        tb = c // 8
                        bs = slice(tb * 512, (tb + 1) * 512)
                        S.op("act", lambda e, po=po: e.activation(out=numT[:], in_=po[:], func=AF.Copy), reads=[kpo], writes=["mnumT"])
                        bcast_rows(em, h, tb)
                        S.op("act", lambda e: e.activation(out=embc[:], in_=pmisc[:], func=AF.Copy), reads=[KM], writes=["membc"])
                        S.op("act", lambda e, pd=pd: e.activation(out=dmx[:], in_=pd[:], func=AF.Abs), reads=[kpd], writes=["mdmx"])
                        S.op("dve", lambda e: e.tensor_tensor(out=dmx[:], in0=dmx[:], in1=embc[:], op=ALU.max), reads=["mdmx", "membc"], writes=["mdmx"])
                        S.op("dve", lambda e: e.reciprocal(out=dmx[:], in_=dmx[:]), reads=["mdmx"], writes=["mdmx"])
                        S.op("dve", lambda e, bs=bs: e.tensor_tensor(out=hT[:, bs], in0=numT[:], in1=dmx[:], op=ALU.mult), reads=["mnumT", "mdmx"], writes=["mhT"])
                gcol = self.smc("mln", l, h)
                for tb in range(4):
                    bs = slice(tb * 512, (tb + 1) * 512)
                    S.op("act", lambda e, bs=bs: e.activation(out=sq[:], in_=hT[:, bs], func=AF.Square), reads=["mhT"], writes=["msq"])
                    S.op("pe", lambda e: e.matmul(pmisc[:], lhsT=self.ones_f(), rhs=sq[:], start=True, stop=True), reads=["cst", "msq"], writes=[KM])
                    S.op("dve", lambda e: e.tensor_scalar(out=rs[:], in0=pmisc[:], scalar1=1.0 / 128, scalar2=EPS, op0=ALU.mult, op1=ALU.add), reads=[KM], writes=["mrs"])
                    S.op("act", lambda e: e.activation(out=rs[:], in_=rs[:], func=AF.Sqrt), reads=["mrs"], writes=["mrs"])
                    S.op("dve", lambda e: e.reciprocal(out=rs[:], in_=rs[:]), reads=["mrs"], writes=["mrs"])
                    S.op("dve", lambda e, bs=bs: e.tensor_tensor(out=yb[:], in0=hT[:, bs], in1=rs[:], op=ALU.mult), reads=["mhT", "mrs"], writes=["myb"])
                    S.op("dve", lambda e, bs=bs, gcol=gcol: e.scalar_tensor_tensor(out=ybb[:], in0=yb[:], scalar=gcol, in1=mo[:, bs], op0=ALU.mult, op1=ALU.mult), reads=["myb", "sm", "mmo"], writes=["mybb"])
                    S.dma("sp", lambda e, bs=bs, rows=rows: e.dma_start(out=self.yT_d[2, rows, bs], in_=ybb[:]), reads=["mybb"], writes=["yT_d"])

    def _bcast_tile(self, es, name, colfn, ps_ap=None, ps_key=None):
        nc, S = self.nc, self.S
        dg = es.enter_context(self.sbt(name + "dg", [128, 128], F32))
        dst = es.enter_context(self.sbt(name, [128, D], F32))
        if ps_ap is None:
            ps = es.enter_context(self.pst(name + "ps", [128, D], F32))[:]
            ps_key = name + "ps"
        else:
            ps = ps_ap
        for c in range(8):
            S.op("dve", lambda e, c=c: e.tensor_scalar(out=dg[:], in0=self.ident_f(), scalar1=colfn(c), scalar2=None, op0=ALU.mult),
                 reads=["cst", "modT", "sm"], writes=[name + "dg"])
            S.op("pe", lambda e, c=c: e.matmul(ps[:, c * 128:(c + 1) * 128], lhsT=self.ones_f(), rhs=dg[:], start=True, stop=True), reads=["cst", name + "dg"], writes=[ps_key])
        for hh in range(2):
            S.op("act", lambda e, hh=hh: e.activation(out=dst[:, hh * 512:(hh + 1) * 512], in_=ps[:, hh * 512:(hh + 1) * 512], func=AF.Copy), reads=[ps_key], writes=[name])
        return dst

    def stage_merge(self, l):
        nc, S = self.nc, self.S
        with ExitStack() as es:
            sb = lambda n, sh, dt: es.enter_context(self.sbt(n, sh, dt))
            pt = lambda n, sh, dt: es.enter_context(self.pst(n, sh, dt))
            g1bc = self._bcast_tile(es, "g1bc", lambda c: self.modc(l, 2, c))
            wb = sb("mwb", [128, 3, 4, D], BF16)
            wo = sb("mwo", [128, 8, D], BF16)
            yt = sb("myt", [128, 3, 4, 512], BF16)
            gts = [sb("mgt%d" % i, [128, 512], F32) for i in range(3)]
            tmps = [sb("mtmp%d" % i, [128, 512], F32) for i in range(2)]
            macc = sb("macc", [128, 512], F32)
            mT = sb("mT", [128, 8, 512], BF16)
            xts = [sb("mxt%d" % i, [128, D], F32) for i in range(2)]
            ytmp = sb("mytmp", [128, 512], F32)
            psa = [pt("mpsa%d" % i, [128, 512], F32) for i in range(2)]
            psy = [pt("mpsy%d" % i, [128, 512], F32) for i in range(2)]
            for g in range(3):
                S.dma("pool", lambda e, g=g: e.dma_start(out=wb[:, g, :, :], in_=self.w_branch[l, g, :, :].rearrange("(kc p) d -> p kc d", p=128)), writes=["mwb"])
            S.dma("pool", lambda e: e.dma_start(out=wo[:], in_=self.w_out[l, :, :].rearrange("(c p) d -> p c d", p=128)), writes=["mwo"])
            ai = 0
            gi = 0
            yi = 0
            for tb in range(4):
                for g in range(3):
                    S.dma("sp", lambda e, g=g, tb=tb: e.dma_start(out=yt[:, g, :, :], in_=self.yT_d[g, :, tb * 512:(tb + 1) * 512].rearrange("(kc p) t -> p kc t", p=128)),
                          reads=["yT_d"], writes=["myt"])
                for dc in range(8):
                    for g in range(3):
                        ps = psa[ai % 2]; kps = "mpsa%d" % (ai % 2); ai += 1
                        gt = gts[gi % 3]; kgt = "mgt%d" % (gi % 3); gi += 1
                        S.dma("sp", lambda e, gt=gt, g=g, dc=dc, tb=tb: e.dma_start(out=gt[:], in_=self.bgT_d[g * 1024 + dc * 128: g * 1024 + (dc + 1) * 128, tb * 512:(tb + 1) * 512]),
                              reads=["projout"], writes=[kgt])
                        for kc in range(4):
                            S.op("pe", lambda e, ps=ps, g=g, kc=kc, dc=dc: e.matmul(ps[:], lhsT=wb[:, g, kc, dc * 128:(dc + 1) * 128], rhs=yt[:, g, kc, :], start=(kc == 0), stop=(kc == 3)),
                                 reads=["mwb", "myt"], writes=[kps])
                        if g == 0:
                            S.op("dve", lambda e, ps=ps, gt=gt: e.tensor_tensor(out=macc[:], in0=ps[:], in1=gt[:], op=ALU.mult), reads=[kps, kgt], writes=["macc"])
                        else:
                            tmp = tmps[g % 2]; ktmp = "mtmp%d" % (g % 2)
                            S.op("dve", lambda e, ps=ps, gt=gt, tmp=tmp: e.tensor_tensor(out=tmp[:], in0=ps[:], in1=gt[:], op=ALU.mult), reads=[kps, kgt], writes=[ktmp])
                            if g == 1:
                                S.op("pool", lambda e, tmp=tmp: e.tensor_tensor(out=macc[:], in0=macc[:], in1=tmp[:], op=ALU.add), reads=[ktmp, "macc"], writes=["macc"])
                            else:
                                S.op("pool", lambda e, tmp=tmp, dc=dc: e.tensor_tensor(out=mT[:, dc, :], in0=macc[:], in1=tmp[:], op=ALU.add), reads=[ktmp, "macc"], writes=["mT"])
                for tt in range(4):
                    t = tb * 4 + tt
                    xt = xts[t % 2]; kxt = "mxt%d" % (t % 2)
                    S.dma("sp", lambda e, xt=xt, t=t: e.dma_start(out=xt[:], in_=self.xres[t * 128:(t + 1) * 128, :]), reads=["xres"], writes=[kxt])
                    for dh in range(2):
                        ps = psy[yi % 2]; kps = "mpsy%d" % (yi % 2); yi += 1
                        for c in range(8):
                            S.op("pe", lambda e, ps=ps, c=c, tt=tt, dh=dh: e.matmul(ps[:], lhsT=mT[:, c, tt * 128:(tt + 1) * 128], rhs=wo[:, c, dh * 512:(dh + 1) * 512], start=(c == 0), stop=(c == 7)),
                                 reads=["mT", "mwo"], writes=[kps])
                        S.op("dve", lambda e, ps=ps, dh=dh: e.tensor_tensor(out=ytmp[:], in0=ps[:], in1=g1bc[:, dh * 512:(dh + 1) * 512], op=ALU.mult), reads=[kps, "g1bc"], writes=["mytmp"])
                        S.op("dve", lambda e, xt=xt, dh=dh: e.tensor_tensor(out=xt[:, dh * 512:(dh + 1) * 512], in0=xt[:, dh * 512:(dh + 1) * 512], in1=ytmp[:], op=ALU.add), reads=["mytmp", kxt], writes=[kxt])
                    S.dma("sp", lambda e, xt=xt, t=t: e.dma_start(out=self.xres[t * 128:(t + 1) * 128, :], in_=xt[:]), reads=[kxt], writes=["xres"])

    def uvcast_gen(self, l, cbs):
        nc, S = self.nc, self.S
        for k in range(32):
            cb = cbs[k % 3]; kcb = "uvc%d" % (k % 3)
            src = self.pk_uv[l, k * 512:(k + 1) * 512, :].rearrange("(p r) d -> p r d", p=128)
            dst = self.uvb[k * 512:(k + 1) * 512, :].rearrange("(p r) d -> p r d", p=128)
            S.dma("pool", lambda e, cb=cb, src=src: e.dma_start(out=cb[:], in_=src), writes=[kcb])
            S.dma("sp", lambda e, cb=cb, dst=dst: e.dma_start(out=dst, in_=cb[:]), reads=[kcb], writes=["uvb"])
            yield

    def stage_peer(self, l):
        nc, S = self.nc, self.S
        NB = 8
        with ExitStack() as es:
            sb = lambda n, sh, dt: es.enter_context(self.sbt(n, sh, dt))
            pt = lambda n, sh, dt: es.enter_context(self.pst(n, sh, dt))
            two = lambda n, sh, dt: [sb("%s%d" % (n, i), sh, dt) for i in range(2)]
            hTs = two("phT", [128, 8, 128], BF16)
            wq = sb("pwq", [128, 8, D], BF16)
            kbd = sb("pkbd", [128, 8, 256], F32)
            qTts = two("pqTt", [128, 8, 128], F32)
            scs, s2s, cands = two("psc", [128, 2048], F32), two("ps2", [128, 2048], F32), two("pcand", [128, 2048], F32)
            mxs, mis, sifs = two("pmx", [128, 256], F32), two("pmi", [128, 256], U32), two("psif", [128, 256], F32)
            tvs, tps, tpfs = two("ptv", [128, 128], F32), two("ptp", [128, 128], U32), two("ptpf", [128, 128], F32)
            afs, bfs = two("paf", [128, 128], F32), two("pbf", [128, 128], F32)
            i1s, i2s = two("pi1", [128, 128], F32), two("pi2", [128, 128], F32)
            idxis = two("pidxi", [128, 128], I32)
            ees, ggs = two("pee", [128, 128], F32), two("pgg", [128, 128], F32)
            ssums = two("pssum", [128, 8], F32)
            aa, ga, gl = sb("paa", [128, 128], F32), sb("pga", [128, 128], F32), sb("pgl", [128, 128], F32)
            h2ts = two("ph2t", [128, D], F32)
            xts = two("pxt", [128, D], F32)
            ubs = [sb("pub%d" % i, [128, 2 * D], BF16) for i in range(NB)]
            junks = two("pjunk", [128, D], F32)
            acc = sb("pacc", [128, D], F32)
            tmpbs = [sb("ptmpb%d" % i, [128, D], BF16) for i in range(4)]
            psq = pt("ppsq", [128, 8, 128], F32)
            pssc = pt("ppssc", [128, 2048], F32)
            pacc = pt("ppacc", [128, D], F32)
            g2bc = self._bcast_tile(es, "g2bc", lambda c: self.modc(l, 5, c), ps_ap=pssc[:, 0:D], ps_key="ppssc")
            UV2 = self.uvb
            iota16 = self.cst[:, C_IOTA:C_IOTA + 16]
            thr15 = self.cst[:, C_THR:C_THR + 15]
            S.dma("pool", lambda e: e.dma_start(out=wq[:], in_=self.pk_wq[l, :, :].rearrange("(kc p) d -> p kc d", p=128)), writes=["pwq"])
            S.dma("sp", lambda e: e.dma_start(out=kbd[:], in_=self.keysbd[l, :, :, :].rearrange("h p n -> p h n")), writes=["pkbd"])
            B4 = [128, 8, 16, 16]

            def prep(t):
                p = t % 2
                K = lambda n: "%s%d" % (n, p)
                ts = slice(t * 128, (t + 1) * 128)
                h2t, xt, hT, qTt = h2ts[p], xts[p], hTs[p], qTts[p]
                sc, s2, cand, mx, mi, sif = scs[p], s2s[p], cands[p], mxs[p], mis[p], sifs[p]
                tv, tp, tpf, af, bf_, i1, i2, idxi, ee, gg, ssum = tvs[p], tps[p], tpfs[p], afs[p], bfs[p], i1s[p], i2s[p], idxis[p], ees[p], ggs[p], ssums[p]
                sc3 = sc[:].rearrange("p (g n) -> p g n", n=128)
                s23 = s2[:].rearrange("p (g n) -> p g n", n=128)
                mx3 = mx[:].rearrange("p (g k) -> p g k", k=16)
                mi3 = mi[:].rearrange("p (g k) -> p g k", k=16)
                mx4 = mx[:].rearrange("p (h q k) -> p h q k", h=8, q=2)
                sif4 = sif[:].rearrange("p (h q k) -> p h q k", h=8, q=2)
                cand4 = cand[:].rearrange("p (h a b) -> p h a b", h=8, a=16)
                tv3 = tv[:].rearrange("p (h k) -> p h k", h=8)
                tp3 = tp[:].rearrange("p (h k) -> p h k", h=8)
                oh4 = s2[:].rearrange("p (h k a) -> p h k a", h=8, k=16)
                cmp3 = sc[:, 0:1920].rearrange("p (r j) -> p r j", j=15)
                S.dma("sp", lambda e: e.dma_start(out=h2t[:], in_=self.h2_d[ts, :]), reads=["h2_d"], writes=[K("ph2t")])
                S.dma("sp", lambda e: e.dma_start(out=xt[:], in_=self.xres[ts, :]), reads=["xres"], writes=[K("pxt")])
                S.dma("sp", lambda e: e.dma_start(out=hT[:], in_=self.hT_d[:, :, ts].rearrange("c p t -> p c t")), reads=["hT_d"], writes=[K("phT")])
                yield
                for h in range(8):
                    for kc in range(8):
                        S.op("pe", lambda e, h=h, kc=kc: e.matmul(psq[:, h, :], lhsT=wq[:, kc, h * 128:(h + 1) * 128], rhs=hT[:, kc, :], start=(kc == 0), stop=(kc == 7)),
                             reads=["pwq", K("phT")], writes=["ppsq"])
                    yield
                for hh in range(2):
                    S.op("act", lambda e, hh=hh: e.activation(out=qTt[:, hh * 4:(hh + 1) * 4, :], in_=psq[:, hh * 4:(hh + 1) * 4, :], func=AF.Copy), reads=["ppsq"], writes=[K("pqTt")])
                for h in range(8):
                    S.op("pe", lambda e, h=h: e.matmul(pssc[:, h * 256:(h + 1) * 256], lhsT=qTt[:, h, :], rhs=kbd[:, h, :], start=True, stop=True), reads=[K("pqTt"), "pkbd"], writes=["ppssc"])
                for qd in range(4):
                    S.op("act", lambda e, qd=qd: e.activation(out=sc[:, qd * 512:(qd + 1) * 512], in_=pssc[:, qd * 512:(qd + 1) * 512], func=AF.Copy), reads=["ppssc"], writes=[K("psc")])
                yield
                for g in range(16):
                    S.op("dve", lambda e, g=g: e.max(out=mx3[:, g, 0:8], in_=sc3[:, g, :]), reads=[K("psc")], writes=[K("pmx")])
                    S.op("dve", lambda e, g=g: e.max_index(out=mi3[:, g, 0:8], in_max=mx3[:, g, 0:8], in_values=sc3[:, g, :]), reads=[K("psc"), K("pmx")], writes=[K("pmi")])
                    yield
                    S.op("dve", lambda e, g=g: e.match_replace(out=s23[:, g, :], in_to_replace=mx3[:, g, 0:8], in_values=sc3[:, g, :], imm_value=-1e30), reads=[K("psc"), K("pmx")], writes=[K("ps2")])
                    S.op("dve", lambda e, g=g: e.max(out=mx3[:, g, 8:16], in_=s23[:, g, :]), reads=[K("ps2")], writes=[K("pmx")])
                    yield
                    S.op("dve", lambda e, g=g: e.max_index(out=mi3[:, g, 8:16], in_max=mx3[:, g, 8:16], in_values=s23[:, g, :]), reads=[K("ps2"), K("pmx")], writes=[K("pmi")])
                    yield
                S.op("dve", lambda e: e.tensor_copy(out=sif[:], in_=mi[:]), reads=[K("pmi")], writes=[K("psif")])
                S.op("dve", lambda e: e.tensor_tensor(out=cand4, in0=mx4[:, :, 0, :].unsqueeze(3).to_broadcast(B4), in1=mx4[:, :, 1, :].unsqueeze(2).to_broadcast(B4), op=ALU.add),
                     reads=[K("pmx")], writes=[K("pcand")])
                yield
                for h in range(8):
                    hs = slice(h * 256, (h + 1) * 256)
                    S.op("dve", lambda e, h=h, hs=hs: e.max(out=tv3[:, h, 0:8], in_=cand[:, hs]), reads=[K("pcand")], writes=[K("ptv")])
                    S.op("dve", lambda e, h=h, hs=hs: e.max_index(out=tp3[:, h, 0:8], in_max=tv3[:, h, 0:8], in_values=cand[:, hs]), reads=[K("pcand"), K("ptv")], writes=[K("ptp")])
                    yield
                    S.op("dve", lambda e, h=h, hs=hs: e.match_replace(out=s2[:, hs], in_to_replace=tv3[:, h, 0:8], in_values=cand[:, hs], imm_value=-1e30), reads=[K("pcand"), K("ptv")], writes=[K("ps2")])
                    S.op("dve", lambda e, h=h, hs=hs: e.max(out=tv3[:, h, 8:16], in_=s2[:, hs]), reads=[K("ps2")], writes=[K("ptv")])
                    yield
                    S.op("dve", lambda e, h=h, hs=hs: e.max_index(out=tp3[:, h, 8:16], in_max=tv3[:, h, 8:16], in_values=s2[:, hs]), reads=[K("ps2"), K("ptv")], writes=[K("ptp")])
                    yield
                S.op("dve", lambda e: e.tensor_copy(out=tpf[:], in_=tp[:]), reads=[K("ptp")], writes=[K("ptpf")])
                S.op("dve", lambda e: e.tensor_tensor(out=cmp3, in0=tpf[:].unsqueeze(2).to_broadcast([128, 128, 15]), in1=thr15.unsqueeze(1).to_broadcast([128, 128, 15]), op=ALU.is_ge),
                     reads=[K("ptpf"), "cst"], writes=[K("psc")])
                yield
                S.op("dve", lambda e: e.tensor_reduce(out=af[:], in_=cmp3, axis=AX.X, op=ALU.add), reads=[K("psc")], writes=[K("paf")])
                S.op("dve", lambda e: e.scalar_tensor_tensor(out=bf_[:], in0=af[:], scalar=-16.0, in1=tpf[:], op0=ALU.mult, op1=ALU.add), reads=[K("paf"), K("ptpf")], writes=[K("pbf")])
                yield
                for (src, q_, dst, kd) in ((af, 0, i1, K("pi1")), (bf_, 1, i2, K("pi2"))):
                    ksrc = K("paf") if q_ == 0 else K("pbf")
                    S.op("dve", lambda e, src=src: e.tensor_tensor(out=oh4, in0=src[:].rearrange("p (h k) -> p h k", h=8).unsqueeze(3).to_broadcast(B4),
                                                                   in1=iota16.unsqueeze(1).unsqueeze(1).to_broadcast(B4), op=ALU.is_equal), reads=[ksrc, "cst"], writes=[K("ps2")])
                    yield
                    S.op("dve", lambda e, q_=q_: e.tensor_tensor(out=oh4, in0=oh4, in1=sif4[:, :, q_, :].unsqueeze(2).to_broadcast(B4), op=ALU.mult), reads=[K("ps2"), K("psif")], writes=[K("ps2")])
                    yield
                    S.op("dve", lambda e, dst=dst: e.tensor_reduce(out=dst[:], in_=oh4, axis=AX.X, op=ALU.add), reads=[K("ps2")], writes=[kd])
                    yield
                S.op("dve", lambda e: e.scalar_tensor_tensor(out=i1[:], in0=i1[:], scalar=128.0, in1=i2[:], op0=ALU.mult, op1=ALU.add), reads=[K("pi1"), K("pi2")], writes=[K("pi1")])
                S.op("dve", lambda e: e.tensor_copy(out=idxi[:], in_=i1[:]), reads=[K("pi1")], writes=[K("pidxi")])
                yield
                ee3 = ee[:].rearrange("p (h k) -> p h k", h=8)
                S.op("dve", lambda e: e.tensor_tensor(out=ee3, in0=tv3, in1=tv3[:, :, 0:1].to_broadcast([128, 8, 16]), op=ALU.subtract), reads=[K("ptv")], writes=[K("pee")])
                S.op("act", lambda e: e.activation(out=ee[:], in_=ee[:], func=AF.Exp), reads=[K("pee")], writes=[K("pee")])
                S.op("dve", lambda e: e.tensor_reduce(out=ssum[:], in_=ee3, axis=AX.X, op=ALU.add), reads=[K("pee")], writes=[K("pssum")])
                yield
                S.op("dve", lambda e: e.reciprocal(out=ssum[:], in_=ssum[:]), reads=[K("pssum")], writes=[K("pssum")])
                S.op("dve", lambda e: e.tensor_tensor(out=gg[:].rearrange("p (h k) -> p h k", h=8), in0=ee3, in1=ssum[:].unsqueeze(2).to_broadcast([128, 8, 16]), op=ALU.mult),
                     reads=[K("pee"), K("pssum")], writes=[K("pgg")])
                yield

            def exhaust(g):
                if g is not None:
                    for _ in g:
                        pass

            def advance(g, n):
                if g is None:
                    return
                for _ in range(n):
                    try:
                        next(g)
                    except StopIteration:
                        return

            exhaust(prep(0))
            ui = 0
            for t in range(NT):
                p = t % 2
                K = lambda n: "%s%d" % (n, p)
                ts = slice(t * 128, (t + 1) * 128)
                h2t, xt, idxi, gg = h2ts[p], xts[p], idxis[p], ggs[p]
                nxt = prep(t + 1) if t + 1 < NT else None
                for r in range(128):
                    ub = ubs[ui % NB]; kub = "pub%d" % (ui % NB)
                    jk = junks[ui % 2]; kjk = "pjunk%d" % (ui % 2)
                    tb_ = tmpbs[ui % 4]; ktb = "ptmpb%d" % (ui % 4)
                    ui += 1
                    ka, kg, kga = "paa%d" % r, "pgl%d" % r, "pga%d" % r
                    S.dma("pool", lambda e, ub=ub, r=r, idxi=idxi: e.indirect_dma_start(out=ub[:], out_offset=None, in_=UV2[:, :], in_offset=bass.IndirectOffsetOnAxis(ap=idxi[:, r:r + 1], axis=0)),
                          reads=[K("pidxi"), "uvb"], writes=[kub])
                    S.op("dve", lambda e, ub=ub, r=r, h2t=h2t, jk=jk: e.scalar_tensor_tensor(out=jk[:], in0=ub[:, 0:D], scalar=1.0, in1=h2t[:], op0=ALU.mult, op1=ALU.mult, accum_out=aa[:, r:r + 1]),
                         reads=[kub, K("ph2t")], writes=[kjk, ka])
                    S.op("act", lambda e, r=r: e.activation(out=gl[:, r:r + 1], in_=aa[:, r:r + 1], func=AF.Gelu_apprx_tanh), reads=[ka], writes=[kg])
                    S.op("act", lambda e, r=r, gg=gg: e.activation(out=ga[:, r:r + 1], in_=gl[:, r:r + 1], func=AF.Identity, scale=gg[:, r:r + 1]), reads=[kg, K("pgg")], writes=[kga])
                    S.op("act", lambda e, ub=ub, r=r, tb_=tb_: e.activation(out=tb_[:], in_=ub[:, D:2 * D], func=AF.Identity, scale=ga[:, r:r + 1]), reads=[kub, kga], writes=[ktb])
                    for dh in range(2):
                        S.op("pe", lambda e, tb_=tb_, dh=dh, r=r: e.matmul(pacc[:, dh * 512:(dh + 1) * 512], lhsT=self.ident_b(), rhs=tb_[:, dh * 512:(dh + 1) * 512], start=(r == 0), stop=(r == 127)),
                             reads=["cstb", ktb], writes=["ppacc"])
                    advance(nxt, 1)
                for dh in range(2):
                    S.op("dve", lambda e, dh=dh: e.tensor_tensor(out=acc[:, dh * 512:(dh + 1) * 512], in0=pacc[:, dh * 512:(dh + 1) * 512], in1=g2bc[:, dh * 512:(dh + 1) * 512], op=ALU.mult),
                         reads=["ppacc", "g2bc"], writes=["pacc"])
                S.op("dve", lambda e, xt=xt: e.tensor_tensor(out=xt[:], in0=xt[:], in1=acc[:], op=ALU.add), reads=["pacc", K("pxt")], writes=[K("pxt")])
                S.dma("sp", lambda e, xt=xt, ts=ts: e.dma_start(out=self.xres[ts, :], in_=xt[:]), reads=[K("pxt")], writes=["xres"])
                exhaust(nxt)

    def stage_final(self):
        nc, S = self.nc, self.S
        with ExitStack() as es:
            sb = lambda n, sh, dt: es.enter_context(self.sbt(n, sh, dt))
            o, _ = SM_OFF["fg"]
            fgbc = self._bcast_tile(es, "fgbc", lambda c: self.sm[:, o + c:o + c + 1])
            xts = [sb("fxt%d" % i, [128, D], F32) for i in range(2)]
            sq = sb("fsq", [128, D], F32)
            ss = sb("fss", [128, 4], F32)
            for t in range(NT):
                xt = xts[t % 2]; kxt = "fxt%d" % (t % 2)
                S.dma("sp", lambda e, xt=xt, t=t: e.dma_start(out=xt[:], in_=self.xres[t * 128:(t + 1) * 128, :]), reads=["xres"], writes=[kxt])
                S.op("act", lambda e, xt=xt: e.activation(out=sq[:], in_=xt[:], func=AF.Square, accum_out=ss[:, 0:1]), reads=[kxt], writes=["fsq", "fss"])
                S.op("dve", lambda e: e.tensor_scalar(out=ss[:, 1:2], in0=ss[:, 0:1], scalar1=1.0 / D, scalar2=EPS, op0=ALU.mult, op1=ALU.add), reads=["fss"], writes=["fss"])
                S.op("act", lambda e: e.activation(out=ss[:, 2:3], in_=ss[:, 1:2], func=AF.Sqrt), reads=["fss"], writes=["fss"])
                S.op("dve", lambda e: e.reciprocal(out=ss[:, 3:4], in_=ss[:, 2:3]), reads=["fss"], writes=["fss"])
                S.op("dve", lambda e, xt=xt: e.scalar_tensor_tensor(out=xt[:], in0=xt[:], scalar=ss[:, 3:4], in1=fgbc[:], op0=ALU.mult, op1=ALU.mult), reads=[kxt, "fss", "fgbc"], writes=[kxt])
                S.dma("sp", lambda e, xt=xt, t=t: e.dma_start(out=self.out_d[t * 128:(t + 1) * 128, :], in_=xt[:]), reads=[kxt], writes=["out_d"])


def prep_inputs(inputs):
    inp = {k: np.ascontiguousarray(np.asarray(v)) for k, v in inputs.items()}
    consts = make_consts()
    keys = inp["pk_keys"]
    kbd = np.zeros((DEPTH, 8, 128, 256), np.float32)
    for p in range(2):
        kbd[:, :, p * 64:(p + 1) * 64, p * 128:(p + 1) * 128] = keys[:, :, p].transpose(0, 1, 3, 2)
    shared = dict(consts=consts, mod_w=inp["mod_w"], w_in=inp["w_in"], w_branch=inp["w_branch"], w_out=inp["w_out"],
                  pk_wq=inp["pk_wq"], keysbd=kbd,
                  pk_uv=np.concatenate([inp["pk_u"], inp["pk_v"]], axis=2))
    in_maps = []
    for b in range(8):
        m = dict(shared)
        m["x"] = inp["x"][b]
        m["small"] = make_small(inp, b)
        in_maps.append(m)
    return in_maps


_PROG_CACHE = {}


def kernel(**inputs):
    in_maps = prep_inputs(inputs)
    if "nc" not in _PROG_CACHE:
        _PROG_CACHE["nc"] = Prog().build()
    res = run_bass_kernel_spmd(_PROG_CACHE["nc"], in_maps, core_ids=list(range(8)))
    return np.stack([np.asarray(r["out"]) for r in res.results], axis=0).astype(np.float32)
```

```python
from contextlib import ExitStack
import numpy as np
import concourse.bass as bass
import concourse.mybir as mybir
from concourse.bass_utils import run_bass_kernel_spmd

F32 = mybir.dt.float32
BF16 = mybir.dt.bfloat16
U32 = mybir.dt.uint32
I32 = mybir.dt.int32
AF = mybir.ActivationFunctionType
ALU = mybir.AluOpType
AX = mybir.AxisListType

D = 1024
S_LEN = 2048
DEPTH = 4
NT = S_LEN // 128
INW = 8712
EPS = 1e-6
PEER_IMPLEMENTED = True


class Sched:
    ENG = ("pe", "act", "dve", "pool", "sp")

    def __init__(self, nc, dma_slots=None, same_engine_sync=True):
        self.nc = nc
        self.ops = {e: [] for e in self.ENG}
        self.count = {e: 0 for e in self.ENG}
        self.sem = {}
        self.last_w = {}
        self.readers = {}
        self.same_engine_sync = same_engine_sync
        self.dma_slots_n = dma_slots or {"sp": 8, "pool": 12, "act": 4}
        self.dma_sems = {}
        self.dma_rr = {}
        self.seen = {e: {} for e in self.ENG}
        self._ctx = []
        self.n_ops = 0

    def open(self):
        nc = self.nc
        for e in self.ENG:
            cm = nc.semaphore("s_" + e)
            self.sem[e] = cm.__enter__()
            self._ctx.append(cm)
        for q, n in self.dma_slots_n.items():
            self.dma_sems[q] = []
            self.dma_rr[q] = 0
            for i in range(n):
                cm = nc.semaphore("sd_%s%d" % (q, i))
                s = cm.__enter__()
                self._ctx.append(cm)
                self.dma_sems[q].append(dict(sem=s, count=0, name="d_%s%d" % (q, i)))

    def close(self):
        for cm in reversed(self._ctx):
            cm.__exit__(None, None, None)

    def _tok_wait(self, tok):
        if tok[0] == "c":
            return (self.sem[tok[1]], tok[1], tok[2])
        d = self.dma_sems[tok[1]][tok[2]]
        return (d["sem"], d["name"], tok[3])

    def _collect(self, eng, reads, writes):
        toks = []
        for k in reads:
            t = self.last_w.get(k)
            if t is not None:
                toks.append(t)
        for k in writes:
            t = self.last_w.get(k)
            if t is not None:
                toks.append(t)
            toks.extend(self.readers.get(k, ()))
        waits = {}
        for t in toks:
            if t[0] == "c" and t[1] == eng and not self.same_engine_sync:
                continue
            s, name, v = self._tok_wait(t)
            if self.seen[eng].get(name, 0) >= v:
                continue
            if name not in waits or waits[name][1] < v:
                waits[name] = (s, v)
        for name, (s, v) in waits.items():
            self.seen[eng][name] = v
        return list(waits.values())

    def _commit(self, tok, reads, writes):
        for k in reads:
            self.readers.setdefault(k, []).append(tok)
        for k in writes:
            self.last_w[k] = tok
            self.readers[k] = []

    def op(self, eng, fn, reads=(), writes=()):
        waits = self._collect(eng, reads, writes)
        self.count[eng] += 1
        tok = ("c", eng, self.count[eng])
        self.ops[eng].append((fn, waits, "c", None))
        self._commit(tok, reads, writes)
        self.n_ops += 1
        return tok

    def dma(self, eng, fn, reads=(), writes=()):
        slot = self.dma_rr[eng]
        self.dma_rr[eng] = (slot + 1) % len(self.dma_sems[eng])
        d = self.dma_sems[eng][slot]
        waits = self._collect(eng, reads, writes)
        if d["count"] > 0 and self.seen[eng].get(d["name"], 0) < d["count"]:
            waits.append((d["sem"], d["count"]))
            self.seen[eng][d["name"]] = d["count"]
        d["count"] += 16
        tok = ("d", eng, slot, d["count"])
        self.ops[eng].append((fn, waits, "d", d["sem"]))
        self._commit(tok, reads, writes)
        self.n_ops += 1
        return tok

    def barrier(self):
        for e in self.ENG:
            waits = []
            for e2 in self.ENG:
                v = self.count[e2]
                if v > 0 and self.seen[e].get(e2, 0) < v and e2 != e:
                    waits.append((self.sem[e2], v))
                    self.seen[e][e2] = v
            for q in self.dma_sems:
                for d in self.dma_sems[q]:
                    if d["count"] > 0 and self.seen[e].get(d["name"], 0) < d["count"]:
                        waits.append((d["sem"], d["count"]))
                        self.seen[e][d["name"]] = d["count"]
            if self.count[e] > 0:
                waits.append((self.sem[e], self.count[e]))
                self.seen[e][e] = self.count[e]
            self.ops[e].append((None, waits, "w", None))

    def emit(self):
        nc = self.nc
        sched = self
        ops = self.ops
        self.ops = {e: [] for e in self.ENG}
        with nc.Block() as block:
            def run(engname, e):
                for fn, waits, kind, dsem in ops[engname]:
                    for s, v in waits:
                        e.wait_ge(s, v)
                    if fn is None:
                        continue
                    ins = fn(e)
                    if kind == "c":
                        ins.then_inc(sched.sem[engname], 1)
                    else:
                        ins.then_inc(dsem, 16)

            @block.sync
            def _(e):
                run("sp", e)

            @block.tensor
            def _(e):
                run("pe", e)

            @block.scalar
            def _(e):
                run("act", e)

            @block.vector
            def _(e):
                run("dve", e)

            @block.gpsimd
            def _(e):
                run("pool", e)


def _small_layout():
    off = {}
    o = 0
    for name, n in (("mod_b", DEPTH * 48), ("nmg", DEPTH * 8), ("nfg", DEPTH * 8),
                    ("convw", DEPTH * 32), ("convb", DEPTH * 8), ("gateb", DEPTH * 2),
                    ("lbl", DEPTH * 4), ("hgn", DEPTH * 4), ("mln", DEPTH * 4), ("fg", 8), ("cT", 8)):
        off[name] = (o, n)
        o += n
    return off, o


SM_OFF, NS = _small_layout()
C_ID, C_ONES, C_TRI, C_M64, C_SEL, C_IOTA, C_THR = 0, 128, 256, 384, 448, 960, 976
NCONST = 992


def make_consts():
    c = np.zeros((128, NCONST), np.float32)
    c[:, C_ID:C_ID + 128] = np.eye(128, dtype=np.float32)
    c[:, C_ONES:C_ONES + 128] = 1.0
    sp = np.arange(128)[:, None]
    s = np.arange(128)[None, :]
    c[:, C_TRI:C_TRI + 128] = (sp >= s).astype(np.float32)
    t = np.arange(64)[None, :]
    c[:, C_M64:C_M64 + 64] = (t >= (sp % 64)).astype(np.float32)
    for h in range(4):
        c[h, C_SEL + h * 128:C_SEL + (h + 1) * 128] = 1.0
    c[:, C_IOTA:C_IOTA + 16] = np.arange(16, dtype=np.float32)[None, :]
    c[:, C_THR:C_THR + 15] = 16.0 * np.arange(1, 16, dtype=np.float32)[None, :]
    return c


def make_small(inp, b):
    sm = np.zeros((128, NS), np.float32)

    def put(name, arr):
        o, n = SM_OFF[name]
        arr = np.asarray(arr, np.float32).reshape(128, n)
        sm[:, o:o + n] = arr

    put("mod_b", inp["mod_b"].reshape(DEPTH, 48, 128).transpose(2, 0, 1))
    put("nmg", inp["norm_mix_g"].reshape(DEPTH, 8, 128).transpose(2, 0, 1))
    put("nfg", inp["norm_ffn_g"].reshape(DEPTH, 8, 128).transpose(2, 0, 1))
    put("convw", inp["ml_conv_w"].reshape(DEPTH, 4, 8, 128).transpose(3, 0, 1, 2))
    put("convb", inp["ml_conv_b"].reshape(DEPTH, 8, 128).transpose(2, 0, 1))
    gb = np.zeros((128, DEPTH, 2), np.float32)
    gb[0:4, :, 0] = inp["ml_gate_b"][:, 0:4].T
    gb[0:4, :, 1] = inp["ml_gate_b"][:, 4:8].T
    put("gateb", gb)
    put("lbl", inp["hg_lb_logits"].reshape(DEPTH, 4, 128).transpose(2, 0, 1))
    put("hgn", inp["hg_norm_g"].reshape(DEPTH, 4, 128).transpose(2, 0, 1))
    put("mln", inp["ml_norm_g"].reshape(DEPTH, 4, 128).transpose(2, 0, 1))
    put("fg", inp["final_g"].reshape(8, 128).T)
    put("cT", inp["c"][b].reshape(8, 128).T)
    return sm


class Prog:
    def __init__(self, n_layers=DEPTH, stages=None, debug=()):
        self.n_layers = n_layers
        self.stages = stages
        self.debug = set(debug)
        self.nc = bass.Bass("TRN2", target_bir_lowering=False)
        self.S = Sched(self.nc)
        self.uid = 0

    def sbt(self, name, shape, dt):
        self.uid += 1
        return self.nc.sbuf_tensor("%s_u%d" % (name, self.uid), shape, dt)

    def pst(self, name, shape, dt):
        self.uid += 1
        return self.nc.psum_tensor("%s_u%d" % (name, self.uid), shape, dt)

    def want(self, st):
        return self.stages is None or st in self.stages

    def dram(self, name, shape, dt, kind=None):
        if kind is None:
            kind = "ExternalOutput" if name in self.debug else "Internal"
        return self.nc.dram_tensor(name, list(shape), dt, kind=kind).ap()

    def build(self):
        nc, S = self.nc, self.S
        L = self.n_layers
        ext = lambda n, s, dt=F32: nc.dram_tensor(n, list(s), dt, kind="ExternalInput").ap()
        self.x_in = ext("x", [S_LEN, D])
        self.small_d = ext("small", [128, NS])
        self.consts_d = ext("consts", [128, NCONST])
        self.mod_w = ext("mod_w", [L, D, 6 * D])
        self.w_in = ext("w_in", [L, D, INW])
        self.input_names = ["x", "small", "consts", "mod_w", "w_in"]
        if self.want("merge"):
            self.w_branch = ext("w_branch", [L, 3, 512, D])
            self.w_out = ext("w_out", [L, D, D])
            self.input_names += ["w_branch", "w_out"]
        if self.want("peer") and PEER_IMPLEMENTED:
            self.pk_wq = ext("pk_wq", [L, D, D])
            self.keysbd = ext("keysbd", [L, 8, 128, 256])
            self.pk_uv = ext("pk_uv", [L, 16384, 2 * D])
            self.input_names += ["pk_wq", "keysbd", "pk_uv"]
        self.out_d = nc.dram_tensor("out", [S_LEN, D], F32, kind="ExternalOutput").ap()
        self.xres = self.dram("xres", [S_LEN, D], F32)
        self.hT_d = self.dram("hT", [8, 128, S_LEN], BF16)
        self.qT_d = self.dram("qT", [512, S_LEN], BF16)
        self.kT_d = self.dram("kT", [512, S_LEN], BF16)
        self.v_d = self.dram("v_tok", [S_LEN, 512], BF16)
        self.hqT_d = self.dram("hqT", [512, S_LEN], F32)
        self.hfT_d = self.dram("hfT", [512, S_LEN], F32)
        self.hgT_d = self.dram("hgT", [512, S_LEN], F32)
        self.hi_d = self.dram("hi_tok", [S_LEN, 512], BF16)
        self.mqkT_d = self.dram("mqkT", [1024, S_LEN], F32)
        self.mv_d = self.dram("mv_tok", [S_LEN, 512], BF16)
        self.moT_d = self.dram("moT", [512, S_LEN], F32)
        self.gT_d = self.dram("gT", [2, 4, S_LEN], F32)
        self.bgT_d = self.dram("bgT", [3072, S_LEN], F32)
        self.yT_d = self.dram("yT", [3, 512, S_LEN], BF16)
        self.h2_d = self.dram("h2_tok", [S_LEN, D], F32)
        self.uvb = self.dram("uvb", [16384, 2 * D], BF16)
        S.open()
        with (self.sbt("consts", [128, NCONST], F32) as cst,
              self.sbt("constb", [128, 384], BF16) as cstb,
              self.sbt("small", [128, NS], F32) as sm,
              self.sbt("modT", [128, DEPTH * 48], F32) as modT,
              self.sbt("lbT", [128, DEPTH * 4], F32) as lbT):
            self.cst, self.cstb, self.sm, self.modT, self.lbT = cst, cstb, sm, modT, lbT
            S.dma("sp", lambda e: e.dma_start(out=cst[:], in_=self.consts_d[:, :]), writes=["cst"])
            S.dma("sp", lambda e: e.dma_start(out=sm[:], in_=self.small_d[:, :]), writes=["sm"])
            S.op("dve", lambda e: e.tensor_copy(out=cstb[:], in_=cst[:, 0:384]), reads=["cst"], writes=["cstb"])
            S.dma("sp", lambda e: e.dma_start(out=self.xres[:, :], in_=self.x_in[:, :]), writes=["xres"])
            self.stage_mod()
            S.barrier(); S.emit()
            for l in range(L):
                if self.want("norm1"):
                    self.stage_norm(l, 0)
                    S.barrier(); S.emit()
                if self.want("proj"):
                    with ExitStack() as es2:
                        if self.want("peer") and PEER_IMPLEMENTED:
                            self._uvc_bufs = [es2.enter_context(self.sbt("uvc%d" % i, [128, 4, 2 * D], BF16)) for i in range(3)]
                        self.stage_proj(l)
                        S.barrier(); S.emit()
                if self.want("attn"):
                    self.stage_attn(l)
                    S.barrier(); S.emit()
                if self.want("hgrn"):
                    self.stage_hgrn(l)
                    S.barrier(); S.emit()
                if self.want("mlstm"):
                    self.stage_mlstm(l)
                    S.barrier(); S.emit()
                if self.want("merge"):
                    self.stage_merge(l)
                    S.barrier(); S.emit()
                if self.want("peer") and PEER_IMPLEMENTED:
                    self.stage_norm(l, 1)
                    S.barrier(); S.emit()
                    self.stage_peer(l)
                    S.barrier(); S.emit()
            self.stage_final()
            S.barrier(); S.emit()
        S.close()
        return nc

    def ident_f(self):
        return self.cst[:, C_ID:C_ID + 128]

    def ones_f(self):
        return self.cst[:, C_ONES:C_ONES + 128]

    def tri_f(self):
        return self.cst[:, C_TRI:C_TRI + 128]

    def ident_b(self):
        return self.cstb[:, 0:128]

    def ones_b(self):
        return self.cstb[:, 128:256]

    def smc(self, name, l, j, n=1):
        o, tot = SM_OFF[name]
        per = tot // DEPTH
        return self.sm[:, o + l * per + j: o + l * per + j + n]

    def modc(self, l, part, c):
        j = l * 48 + part * 8 + c
        return self.modT[:, j:j + 1]

    def stage_mod(self):
        nc, S = self.nc, self.S
        L = self.n_layers
        sm = self.sm
        with (self.sbt("condT", [128, 8], F32) as condT,
              self.sbt("mw0", [128, 8, 768], F32) as mw0,
              self.sbt("mw1", [128, 8, 768], F32) as mw1,
              self.sbt("lbe", [128, DEPTH * 4], F32) as lbe,
              self.sbt("lbs", [128, 4], F32) as lbs,
              self.sbt("lbm", [128, 4], F32) as lbm,
              self.pst("ps_mod", [128, DEPTH * 48], F32) as psm):
            o, _ = SM_OFF["cT"]
            S.op("act", lambda e: e.activation(out=condT[:], in_=sm[:, o:o + 8], func=AF.Silu), reads=["sm"], writes=["condT"])
            mws = [mw0, mw1]
            gi = 0
            for l in range(L):
                for g in range(8):
                    mw = mws[gi % 2]
                    key = "mw%d" % (gi % 2)
                    gi += 1
                    src = self.mod_w[l, :, g * 768:(g + 1) * 768].rearrange("(kc p) c -> p kc c", p=128)
                    S.dma("sp", lambda e, mw=mw, src=src: e.dma_start(out=mw[:], in_=src), writes=[key])
                    for cc in range(6):
                        j = l * 48 + g * 6 + cc
                        for kc in range(8):
                            S.op("pe", lambda e, mw=mw, cc=cc, kc=kc, j=j: e.matmul(
                                psm[:, j:j + 1], lhsT=mw[:, kc, cc * 128:(cc + 1) * 128], rhs=condT[:, kc:kc + 1],
                                start=(kc == 0), stop=(kc == 7)), reads=[key, "condT"], writes=["psm"])
            ob, _ = SM_OFF["mod_b"]
            S.op("dve", lambda e: e.tensor_tensor(out=self.modT[:, 0:L * 48], in0=psm[:, 0:L * 48], in1=sm[:, ob:ob + L * 48], op=ALU.add),
                 reads=["psm", "sm"], writes=["modT"])
            ol, _ = SM_OFF["lbl"]
            lg = lambda l: sm[:, ol + l * 4: ol + l * 4 + 4]
            S.op("dve", lambda e: e.tensor_tensor(out=lbm[:], in0=lg(0), in1=lg(1), op=ALU.max), reads=["sm"], writes=["lbm"])
            for l in (2, 3):
                S.op("dve", lambda e, l=l: e.tensor_tensor(out=lbm[:], in0=lbm[:], in1=lg(l), op=ALU.max), reads=["sm", "lbm"], writes=["lbm"])
            for l in range(DEPTH):
                S.op("dve", lambda e, l=l: e.tensor_tensor(out=lbe[:, l * 4:l * 4 + 4], in0=lg(l), in1=lbm[:], op=ALU.subtract), reads=["sm", "lbm"], writes=["lbe"])
            S.op("act", lambda e: e.activation(out=lbe[:], in_=lbe[:], func=AF.Exp), reads=["lbe"], writes=["lbe"])
            S.op("dve", lambda e: e.tensor_tensor(out=lbs[:], in0=lbe[:, 0:4], in1=lbe[:, 4:8], op=ALU.add), reads=["lbe"], writes=["lbs"])
            for l in (2, 3):
                S.op("dve", lambda e, l=l: e.tensor_tensor(out=lbs[:], in0=lbs[:], in1=lbe[:, l * 4:l * 4 + 4], op=ALU.add), reads=["lbe", "lbs"], writes=["lbs"])
            S.op("dve", lambda e: e.reciprocal(out=lbs[:], in_=lbs[:]), reads=["lbs"], writes=["lbs"])
            lbT = self.lbT
            S.op("dve", lambda e: e.memset(lbT[:, 0:4], 0.0), writes=["lbT"])
            for l in range(1, DEPTH):
                S.op("dve", lambda e, l=l: e.tensor_tensor(out=lbe[:, l * 4:l * 4 + 4], in0=lbe[:, l * 4:l * 4 + 4], in1=lbs[:], op=ALU.mult), reads=["lbe", "lbs"], writes=["lbe"])
                S.op("dve", lambda e, l=l: e.tensor_tensor(out=lbT[:, l * 4:l * 4 + 4], in0=lbT[:, (l - 1) * 4:l * 4], in1=lbe[:, l * 4:l * 4 + 4], op=ALU.add), reads=["lbe", "lbT"], writes=["lbT"])

    def stage_norm(self, l, which):
        nc, S = self.nc, self.S
        gname = "nmg" if which == 0 else "nfg"
        p_shift, p_scale = (0, 1) if which == 0 else (3, 4)
        with (self.sbt("nx0", [128, D], F32) as nx0, self.sbt("nx1", [128, D], F32) as nx1,
              self.sbt("nsq", [128, 2, D], BF16) as nsq,
              self.sbt("nb0", [128, D], BF16) as nb0, self.sbt("nb1", [128, D], BF16) as nb1,
              self.sbt("nh0", [128, 8, 128], BF16) as nh0, self.sbt("nh1", [128, 8, 128], BF16) as nh1,
              self.sbt("nt0", [128, D], F32) as nt0, self.sbt("nt1", [128, D], F32) as nt1,
              self.sbt("nss", [128, 8], F32) as nss_all,
              self.sbt("nG", [128, 8], F32) as nG,
              self.pst("nps0", [128, 8, 128], BF16) as nps0, self.pst("nps1", [128, 8, 128], BF16) as nps1,
              self.pst("npt0", [128, 8, 128], F32) as npt0):
            j0 = l * 48 + p_scale * 8
            S.op("dve", lambda e: e.scalar_tensor_tensor(out=nG[:], in0=self.modT[:, j0:j0 + 8], scalar=1.0, in1=self.smc(gname, l, 0, 8),
                                                         op0=ALU.add, op1=ALU.mult), reads=["modT", "sm"], writes=["nG"])
            nx, nb, nh, nps, nt = [nx0, nx1], [nb0, nb1], [nh0, nh1], [nps0, nps1], [nt0, nt1]
            for t in range(NT):
                i = t % 2
                kx, kb, kh, kp, kt = "nx%d" % i, "nb%d" % i, "nh%d" % i, "nps%d" % i, "nt%d" % i
                nss = nss_all[:, 4 * i:4 * i + 4]; kss = "nss%d" % i; ksq = "nsq%d" % i
                S.dma("sp", lambda e, i=i, t=t: e.dma_start(out=nx[i][:], in_=self.xres[t * 128:(t + 1) * 128, :]), reads=["xres"], writes=[kx])
                S.op("act", lambda e, i=i, nss=nss: e.activation(out=nsq[:, i, :], in_=nx[i][:], func=AF.Square, accum_out=nss[:, 0:1]), reads=[kx], writes=[ksq, kss])
                S.op("dve", lambda e, nss=nss: e.tensor_scalar(out=nss[:, 1:2], in0=nss[:, 0:1], scalar1=1.0 / D, scalar2=EPS, op0=ALU.mult, op1=ALU.add), reads=[kss], writes=[kss])
                S.op("act", lambda e, nss=nss: e.activation(out=nss[:, 2:3], in_=nss[:, 1:2], func=AF.Sqrt), reads=[kss], writes=[kss])
                S.op("dve", lambda e, nss=nss: e.reciprocal(out=nss[:, 3:4], in_=nss[:, 2:3]), reads=[kss], writes=[kss])
                S.op("dve", lambda e, i=i, nss=nss: e.tensor_scalar(out=nb[i][:], in0=nx[i][:], scalar1=nss[:, 3:4], scalar2=None, op0=ALU.mult), reads=[kx, kss], writes=[kb])
                for c in range(8):
                    S.op("pe", lambda e, i=i, c=c: e.transpose(nps[i][:, c, :], nb[i][:, c * 128:(c + 1) * 128], self.ident_b()), reads=[kb, "cstb"], writes=[kp])
                for c in range(8):
                    S.op("act", lambda e, i=i, c=c: e.activation(out=nh[i][:, c, :], in_=nps[i][:, c, :], func=AF.Identity,
                                                                 scale=nG[:, c:c + 1], bias=self.modc(l, p_shift, c)), reads=[kp, "nG", "modT"], writes=[kh])
                S.dma("sp", lambda e, i=i, t=t: e.dma_start(out=self.hT_d[:, :, t * 128:(t + 1) * 128].rearrange("c p t -> p c t"), in_=nh[i][:]), reads=[kh], writes=["hT_d"])
                if which == 1:
                    pass
            if which == 1:
                self._h2_tokmajor(l, nx, nss_all[:, 0:4], nG, nt, npt0, p_shift)

    def _h2_tokmajor(self, l, nx, nss, nG, nt, npt0, p_shift):
        nc, S = self.nc, self.S
        with (self.sbt("dg", [128, 128], F32) as dg,
              self.sbt("Gbc", [128, D], F32) as Gbc, self.sbt("Sbc", [128, D], F32) as Sbc):
            for which_v, dst in ((0, Gbc), (1, Sbc)):
                for c in range(8):
                    col = nG[:, c:c + 1] if which_v == 0 else self.modc(l, p_shift, c)
                    S.op("dve", lambda e, col=col: e.tensor_scalar(out=dg[:], in0=self.ident_f(), scalar1=col, scalar2=None, op0=ALU.mult), reads=["cst", "nG", "modT"], writes=["dg"])
                    S.op("pe", lambda e, c=c: e.matmul(npt0[:, c, :], lhsT=self.ones_f(), rhs=dg[:], start=True, stop=True), reads=["cst", "dg"], writes=["npt0"])
                S.op("act", lambda e, dst=dst: e.activation(out=dst[:], in_=npt0[:].rearrange("p c t -> p (c t)"), func=AF.Copy), reads=["npt0"], writes=["bc%d" % which_v])
            for t in range(NT):
                i = t % 2
                kx, kt = "nx%d" % i, "nt%d" % i
                S.dma("sp", lambda e, i=i, t=t: e.dma_start(out=nx[i][:], in_=self.xres[t * 128:(t + 1) * 128, :]), reads=["xres"], writes=[kx])
                S.op("act", lambda e, i=i: e.activation(out=nt[i][:], in_=nx[i][:], func=AF.Square, accum_out=nss[:, 0:1]), reads=[kx], writes=[kt, "nss0"])
                S.op("dve", lambda e: e.tensor_scalar(out=nss[:, 1:2], in0=nss[:, 0:1], scalar1=1.0 / D, scalar2=EPS, op0=ALU.mult, op1=ALU.add), reads=["nss0"], writes=["nss0"])
                S.op("act", lambda e: e.activation(out=nss[:, 2:3], in_=nss[:, 1:2], func=AF.Sqrt), reads=["nss0"], writes=["nss0"])
                S.op("dve", lambda e: e.reciprocal(out=nss[:, 3:4], in_=nss[:, 2:3]), reads=["nss0"], writes=["nss0"])
                S.op("dve", lambda e, i=i: e.scalar_tensor_tensor(out=nt[i][:], in0=nx[i][:], scalar=nss[:, 3:4], in1=Gbc[:], op0=ALU.mult, op1=ALU.mult), reads=[kx, "nss0", "bc0"], writes=[kt])
                S.op("dve", lambda e, i=i: e.tensor_tensor(out=nt[i][:], in0=nt[i][:], in1=Sbc[:], op=ALU.add), reads=[kt, "bc1"], writes=[kt])
                S.dma("sp", lambda e, i=i, t=t: e.dma_start(out=self.h2_d[t * 128:(t + 1) * 128, :], in_=nt[i][:]), reads=[kt], writes=["h2_d"])

    def stage_proj(self, l):
        nc, S = self.nc, self.S
        fm = []
        for c in range(4):
            fm.append((0 + c * 128, 128, self.qT_d[c * 128:(c + 1) * 128, :], AF.Copy, 0.125, BF16))
        for c in range(4):
            fm.append((512 + c * 128, 128, self.kT_d[c * 128:(c + 1) * 128, :], AF.Copy, 1.0, BF16))
        for c in range(4):
            fm.append((1536 + c * 128, 128, self.hqT_d[c * 128:(c + 1) * 128, :], AF.Copy, 1.0, F32))
        for c in range(4):
            fm.append((2048 + c * 128, 128, self.hfT_d[c * 128:(c + 1) * 128, :], AF.Copy, 1.0, F32))
        for c in range(4):
            fm.append((3072 + c * 128, 128, self.hgT_d[c * 128:(c + 1) * 128, :], AF.Silu, 1.0, F32))
        for c in range(8):
            fm.append((3584 + c * 128, 128, self.mqkT_d[c * 128:(c + 1) * 128, :], AF.Copy, 1.0, F32))
        for c in range(4):
            fm.append((5120 + c * 128, 128, self.moT_d[c * 128:(c + 1) * 128, :], AF.Sigmoid, 1.0, F32))
        fm.append((5632, 4, self.gT_d[0, :, :], AF.Copy, 1.0, F32))
        fm.append((5636, 4, self.gT_d[1, :, :], AF.Copy, 1.0, F32))
        for c in range(24):
            fm.append((5640 + c * 128, 128, self.bgT_d[c * 128:(c + 1) * 128, :], AF.Sigmoid, 1.0, F32))
        tm = [(1024, self.v_d, AF.Copy), (2560, self.hi_d, AF.Silu), (4608, self.mv_d, AF.Copy)]
        with (self.sbt("hT", [128, 8, S_LEN], BF16) as hT,
              self.sbt("pw0", [128, 8, 512], BF16) as pw0, self.sbt("pw1", [128, 8, 512], BF16) as pw1,
              self.sbt("pof0", [128, S_LEN], F32) as pof0, self.sbt("pof1", [128, S_LEN], F32) as pof1,
              self.sbt("pob0", [128, S_LEN], BF16) as pob0, self.sbt("pob1", [128, S_LEN], BF16) as pob1,
              self.sbt("pot0", [128, 512], BF16) as pot0, self.sbt("pot1", [128, 512], BF16) as pot1,
              self.pst("pp0", [128, 512], F32) as pp0, self.pst("pp1", [128, 512], F32) as pp1,
              self.pst("pp2", [128, 512], F32) as pp2, self.pst("pp3", [128, 512], F32) as pp3):
            for c in range(8):
                S.dma("sp", lambda e, c=c: e.dma_start(out=hT[:, c, :], in_=self.hT_d[c, :, :]), reads=["hT_d"], writes=["hT"])
            pw, pof, pob, pot, pp = [pw0, pw1], [pof0, pof1], [pob0, pob1], [pot0, pot1], [pp0, pp1, pp2, pp3]
            side = self.uvcast_gen(l, self._uvc_bufs) if (self.want("peer") and PEER_IMPLEMENTED) else None
            wi = 0
            pi = 0
            for ji, (col0, ncol, dest, func, scale, dt) in enumerate(fm):
                w = pw[wi % 2]; kw = "pw%d" % (wi % 2); wi += 1
                src = self.w_in[l, :, col0:col0 + ncol].rearrange("(kc p) c -> p kc c", p=128)
                S.dma("pool", lambda e, w=w, src=src, ncol=ncol: e.dma_start(out=w[:, :, 0:ncol], in_=src), writes=[kw])
                ob = (pof if dt == F32 else pob)[ji % 2]
                ko = ("pof%d" if dt == F32 else "pob%d") % (ji % 2)
                for tb in range(4):
                    ps = pp[pi % 4]; kp = "pp%d" % (pi % 4); pi += 1
                    for kc in range(8):
                        S.op("pe", lambda e, ps=ps, w=w, kc=kc, tb=tb, ncol=ncol: e.matmul(
                            ps[0:ncol, :], lhsT=w[:, kc, 0:ncol], rhs=hT[:, kc, tb * 512:(tb + 1) * 512],
                            start=(kc == 0), stop=(kc == 7)), reads=[kw, "hT"], writes=[kp])
                    S.op("act", lambda e, ps=ps, ob=ob, tb=tb, ncol=ncol, func=func, scale=scale: e.activation(
                        out=ob[0:ncol, tb * 512:(tb + 1) * 512], in_=ps[0:ncol, :], func=func, scale=scale), reads=[kp], writes=[ko])
                S.dma("sp", lambda e, ob=ob, dest=dest, ncol=ncol: e.dma_start(out=dest, in_=ob[0:ncol, :]), reads=[ko], writes=["projout"])
                if side is not None:
                    next(side, None)
            for (col0, dest, func) in tm:
                w = pw[wi % 2]; kw = "pw%d" % (wi % 2); wi += 1
                src = self.w_in[l, :, col0:col0 + 512].rearrange("(kc p) c -> p kc c", p=128)
                S.dma("pool", lambda e, w=w, src=src: e.dma_start(out=w[:], in_=src), writes=[kw])
                for t in range(NT):
                    ps = pp[pi % 4]; kp = "pp%d" % (pi % 4); pi += 1
                    for kc in range(8):
                        S.op("pe", lambda e, ps=ps, w=w, kc=kc, t=t: e.matmul(
                            ps[:], lhsT=hT[:, kc, t * 128:(t + 1) * 128], rhs=w[:, kc, :],
                            start=(kc == 0), stop=(kc == 7)), reads=[kw, "hT"], writes=[kp])
                    ot = pot[t % 2]; kt = "pot%d" % (t % 2)
                    S.op("act", lambda e, ps=ps, ot=ot, func=func: e.activation(out=ot[:], in_=ps[:], func=func), reads=[kp], writes=[kt])
                    S.dma("sp", lambda e, ot=ot, dest=dest, t=t: e.dma_start(out=dest[t * 128:(t + 1) * 128, :], in_=ot[:]), reads=[kt], writes=["projout"])
            if side is not None:
                for _ in side:
                    pass

    def stage_attn(self, l):
        nc, S = self.nc, self.S
        NBUF = 4
        with ExitStack() as es:
            sb = lambda n, sh, dt: es.enter_context(self.sbt(n, sh, dt))
            pt = lambda n, sh, dt: es.enter_context(self.pst(n, sh, dt))
            aq = [sb("aq%d" % i, [64, S_LEN], BF16) for i in range(2)]
            ak = [sb("ak%d" % i, [64, S_LEN], BF16) for i in range(2)]
            av = [sb("av%d" % i, [128, NT, 64], BF16) for i in range(2)]
            azs = [sb("azs%d" % i, [128, 512], F32) for i in range(NBUF)]
            asp = [sb("asp%d" % i, [128, 512], F32) for i in range(NBUF)]
            asb = [sb("asb%d" % i, [128, 512], BF16) for i in range(NBUF)]
            alw = [sb("alw%d" % i, [128, 512], F32) for i in range(NBUF)]
            awt = [sb("awt%d" % i, [128, 512], BF16) for i in range(NBUF)]
            ayo = [sb("ayo%d" % i, [64, 512], BF16) for i in range(2)]
            apz = [pt("apz%d" % i, [128, 512], F32) for i in range(2)]
            apc = [pt("apc%d" % i, [128, 512], F32) for i in range(2)]
            apy = [pt("apy%d" % i, [64, 512], F32) for i in range(2)]
            apr = [pt("apr%d" % i, [128, 512], F32) for i in range(2)]
            tri_b = self.cstb[:, 256:384]
            steps = []
            yi = 0
            for h in range(8):
                for qb in range(4):
                    nkb = 4 * (qb + 1)
                    for jn, j in enumerate(reversed(range(nkb))):
                        steps.append(dict(h=h, qb=qb, nkb=nkb, jn=jn, j=j, yi=yi, i=len(steps)))
                    yi += 1
            loaded = set()

            def load_head(h):
                if h in loaded or h >= 8:
                    return
                loaded.add(h)
                hb = h % 2
                S.dma("sp", lambda e: e.dma_start(out=aq[hb][:], in_=self.qT_d[h * 64:(h + 1) * 64, :]), reads=["projout"], writes=["aq%d" % hb])
                S.dma("sp", lambda e: e.dma_start(out=ak[hb][:], in_=self.kT_d[h * 64:(h + 1) * 64, :]), reads=["projout"], writes=["ak%d" % hb])
                S.dma("sp", lambda e: e.dma_start(out=av[hb][:], in_=self.v_d[:, h * 64:(h + 1) * 64].rearrange("(j p) d -> p j d", p=128)), reads=["projout"], writes=["av%d" % hb])

            def phaseA(st):
                h, qb, j, i = st["h"], st["qb"], st["j"], st["i"]
                load_head(h)
                hb = h % 2
                q, k = aq[hb], ak[hb]
                b2, b3 = i % 2, i % NBUF
                pz, zs, sp, sbb = apz[b2], azs[b3], asp[b3], asb[b3]
                kz, kzs, ksp, ksb = "apz%d" % b2, "azs%d" % b3, "asp%d" % b3, "asb%d" % b3
                diag = j >= 4 * qb
                base = qb * 512 - j * 128
                S.op("pe", lambda e: e.matmul(pz[:], lhsT=k[:, j * 128:(j + 1) * 128], rhs=q[:, qb * 512:(qb + 1) * 512], start=True, stop=True), reads=["aq%d" % hb, "ak%d" % hb], writes=[kz])
                S.op("dve", lambda e: e.tensor_copy(out=zs[:], in_=pz[:]), reads=[kz], writes=[kzs])
                S.op("act", lambda e: e.activation(out=sp[:], in_=zs[:], func=AF.Exp), reads=[kzs], writes=[ksp])
                S.op("act", lambda e: e.activation(out=sbb[:], in_=sp[:], func=AF.Ln, bias=1.0), reads=[ksp], writes=[ksb])
                if diag:
                    S.op("pool", lambda e: e.affine_select(out=sbb[:], in_=sbb[:], pattern=[[1, 512]], compare_op=ALU.is_gt, fill=0.0, base=base, channel_multiplier=-1), reads=[ksb], writes=[ksb])

            def phaseB(st):
                qb, j, jn, i = st["qb"], st["j"], st["jn"], st["i"]
                b2, b3 = i % 2, i % NBUF
                pc, zs, sbb, lw, wt = apc[b2], azs[b3], asb[b3], alw[b3], awt[b3]
                kc_, kzs, ksb, klw, kwt = "apc%d" % b2, "azs%d" % b3, "asb%d" % b3, "alw%d" % b3, "awt%d" % b3
                pr = apr[st["yi"] % 2]; kpr = "apr%d" % (st["yi"] % 2)
                diag = j >= 4 * qb
                base = qb * 512 - j * 128
                S.op("pe", lambda e: e.matmul(pc[:], lhsT=tri_b, rhs=sbb[:], start=True, stop=True), reads=["cstb", ksb], writes=[kc_])
                S.op("dve", lambda e: e.tensor_tensor(out=lw[:], in0=zs[:], in1=pc[:], op=ALU.subtract), reads=[kzs, kc_], writes=[klw])
                if jn > 0:
                    S.op("dve", lambda e: e.tensor_tensor(out=lw[:], in0=lw[:], in1=pr[:], op=ALU.subtract), reads=[klw, kpr], writes=[klw])
                S.op("act", lambda e: e.activation(out=wt[:], in_=lw[:], func=AF.Exp), reads=[klw], writes=[kwt])
                if diag:
                    S.op("pool", lambda e: e.affine_select(out=wt[:], in_=wt[:], pattern=[[1, 512]], compare_op=ALU.is_gt, fill=0.0, base=base, channel_multiplier=-1), reads=[kwt], writes=[kwt])

            def phaseC(st):
                h, qb, j, jn, nkb, i = st["h"], st["qb"], st["j"], st["jn"], st["nkb"], st["i"]
                hb = h % 2
                v = av[hb]
                b3 = i % NBUF
                sbb, wt = asb[b3], awt[b3]
                ksb, kwt = "asb%d" % b3, "awt%d" % b3
                y2 = st["yi"] % 2
                py, pr, yo = apy[y2], apr[y2], ayo[y2]
                kpy, kpr, kyo = "apy%d" % y2, "apr%d" % y2, "ayo%d" % y2
                S.op("pe", lambda e: e.matmul(py[:], lhsT=v[:, j, :], rhs=wt[:], start=(jn == 0), stop=(jn == nkb - 1)), reads=["av%d" % hb, kwt], writes=[kpy])
                if jn < nkb - 1:
                    S.op("pe", lambda e: e.matmul(pr[:], lhsT=self.ones_b(), rhs=sbb[:], start=(jn == 0), stop=(jn == nkb - 2)), reads=["cstb", ksb], writes=[kpr])
                else:
                    S.op("act", lambda e: e.activation(out=yo[:], in_=py[:], func=AF.Copy), reads=[kpy], writes=[kyo])
                    S.dma("sp", lambda e: e.dma_start(out=self.yT_d[0, h * 64:(h + 1) * 64, qb * 512:(qb + 1) * 512], in_=yo[:]), reads=[kyo], writes=["yT_d"])

            n = len(steps)
            for s_ in range(n + 2):
                if 0 <= s_ - 2 < n:
                    phaseC(steps[s_ - 2])
                if 0 <= s_ - 1 < n:
                    phaseB(steps[s_ - 1])
                if s_ < n:
                    phaseA(steps[s_])

    def _zero_branch(self, g):
        nc, S = self.nc, self.S
        with ExitStack() as es:
            z = es.enter_context(self.sbt("zb", [128, S_LEN], BF16))
            S.op("dve", lambda e: e.memset(z[:], 0.0), writes=["zb"])
            for kc in range(4):
                S.dma("sp", lambda e, kc=kc: e.dma_start(out=self.yT_d[g, kc * 128:(kc + 1) * 128, :], in_=z[:]), reads=["zb"], writes=["yT_d"])

    def stage_hgrn(self, l):
        nc, S = self.nc, self.S
        with ExitStack() as es:
            sb = lambda n, sh, dt: es.enter_context(self.sbt(n, sh, dt))
            pt = lambda n, sh, dt: es.enter_context(self.pst(n, sh, dt))
            q = sb("hq", [128, S_LEN], F32)
            f = sb("hf", [128, S_LEN], F32)
            lf = sb("hlf", [128, S_LEN], F32)
            kin = sb("hkin", [128, S_LEN], F32)
            Bg = sb("hBg", [128, S_LEN], F32)
            Dd = sb("hD", [128, S_LEN], F32)
            Ee = sb("hE", [128, S_LEN], F32)
            Q1, K1 = sb("hQ1", [128, S_LEN], BF16), sb("hK1", [128, S_LEN], BF16)
            Q2, K2 = sb("hQ2", [128, S_LEN], BF16), sb("hK2", [128, S_LEN], BF16)
            hi = sb("hhi", [128, NT, 128], BF16)
            k2t = sb("hk2t", [128, NT, 128], BF16)
            hg = sb("hhg", [128, S_LEN], F32)
            oT = sb("hoT", [128, S_LEN], F32)
            Bst, Bmid, Bend, dec = sb("hBst", [128, 32], F32), sb("hBmid", [128, 32], F32), sb("hBend", [128, 32], F32), sb("hdec", [128, 32], F32)
            oml = sb("homl", [128, 1], F32)
            St, Stb = sb("hSt", [128, 128], F32), sb("hStb", [128, 128], BF16)
            pTs = [sb("hpT%d" % i, [128, 64], BF16) for i in range(2)]
            sq = sb("hsq", [128, 512], F32)
            rs = sb("hrs", [128, 512], F32)
            yb = sb("hyb", [128, 512], F32)
            ybb = sb("hybb", [128, 512], BF16)
            pss = [pt("hpss%d" % i, [128, 512], F32) for i in range(2)]
            pso = [pt("hpso%d" % i, [128, 512], F32) for i in range(2)]
            psst = [pt("hpsst%d" % i, [128, 512], F32) for i in range(2)]
            ptr = pt("hptr", [128, 1024], BF16)
            psn = pt("hpsn", [128, 512], F32)
            mask = self.cst[:, C_M64:C_M64 + 64]
            Bg3 = Bg[:].rearrange("p (c t) -> p c t", t=64)
            for h in range(4):
                lb = self.lbT[:, l * 4 + h: l * 4 + h + 1]
                rows = slice(h * 128, (h + 1) * 128)
                S.dma("sp", lambda e, rows=rows: e.dma_start(out=q[:], in_=self.hqT_d[rows, :]), reads=["projout"], writes=["hq"])
                S.dma("sp", lambda e, rows=rows: e.dma_start(out=f[:], in_=self.hfT_d[rows, :]), reads=["projout"], writes=["hf"])
                S.dma("sp", lambda e, rows=rows: e.dma_start(out=hg[:], in_=self.hgT_d[rows, :]), reads=["projout"], writes=["hhg"])
                S.dma("sp", lambda e, rows=rows: e.dma_start(out=hi[:], in_=self.hi_d[:, rows].rearrange("(j p) v -> p j v", p=128)), reads=["projout"], writes=["hhi"])
                S.op("dve", lambda e, lb=lb: e.tensor_scalar(out=oml[:], in0=lb, scalar1=-1.0, scalar2=1.0, op0=ALU.mult, op1=ALU.add), reads=["lbT"], writes=["homl"])
                S.op("act", lambda e: e.activation(out=f[:], in_=f[:], func=AF.Sigmoid), reads=["hf"], writes=["hf"])
                S.op("dve", lambda e, lb=lb: e.tensor_scalar(out=f[:], in0=f[:], scalar1=oml[:, 0:1], scalar2=lb, op0=ALU.mult, op1=ALU.add), reads=["hf", "homl", "lbT"], writes=["hf"])
                S.op("act", lambda e: e.activation(out=lf[:], in_=f[:], func=AF.Ln), reads=["hf"], writes=["hlf"])
                S.op("dve", lambda e: e.tensor_scalar(out=kin[:], in0=f[:], scalar1=-1.0, scalar2=1.0, op0=ALU.mult, op1=ALU.add), reads=["hf"], writes=["hkin"])
                S.op("dve", lambda e: e.tensor_tensor_scan(out=Bg[:], data0=lf[:], data1=lf[:], initial=0.0, op0=ALU.add, op1=ALU.bypass), reads=["hlf"], writes=["hBg"])
                S.op("dve", lambda e: e.memset(Bst[:, 0:1], 0.0), writes=["hBst"])
                S.op("dve", lambda e: e.tensor_copy(out=Bst[:, 1:32], in_=Bg3[:, 0:31, 63]), reads=["hBg"], writes=["hBst"])
                S.op("dve", lambda e: e.tensor_copy(out=Bmid[:], in_=Bg3[:, :, 31]), reads=["hBg"], writes=["hBmid"])
                S.op("dve", lambda e: e.tensor_copy(out=Bend[:], in_=Bg3[:, :, 63]), reads=["hBg"], writes=["hBend"])
                S.op("dve", lambda e: e.tensor_tensor(out=dec[:], in0=Bend[:], in1=Bst[:], op=ALU.subtract), reads=["hBend", "hBst"], writes=["hdec"])
                S.op("act", lambda e: e.activation(out=dec[:], in_=dec[:], func=AF.Exp), reads=["hdec"], writes=["hdec"])

                def sub_cols(col, key):
                    S.op("dve", lambda e: e.tensor_tensor(out=Dd[:].rearrange("p (c t) -> p c t", t=64), in0=Bg3, in1=col[:].unsqueeze(2).to_broadcast([128, 32, 64]), op=ALU.subtract),
                         reads=["hBg", key], writes=["hD"])
                sub_cols(Bmid, "hBmid")
                S.op("act", lambda e: e.activation(out=Ee[:], in_=Dd[:], func=AF.Exp), reads=["hD"], writes=["hE"])
                S.op("dve", lambda e: e.tensor_tensor(out=Q1[:], in0=q[:], in1=Ee[:], op=ALU.mult), reads=["hq", "hE"], writes=["hQ1"])
                S.op("act", lambda e: e.activation(out=Ee[:], in_=Dd[:], func=AF.Exp, scale=-1.0), reads=["hD", "hQ1"], writes=["hE"])
                S.op("dve", lambda e: e.tensor_tensor(out=K1[:], in0=kin[:], in1=Ee[:], op=ALU.mult), reads=["hkin", "hE"], writes=["hK1"])
                sub_cols(Bst, "hBst")
                S.op("act", lambda e: e.activation(out=Ee[:], in_=Dd[:], func=AF.Exp), reads=["hD", "hK1"], writes=["hE"])
                S.op("dve", lambda e: e.tensor_tensor(out=Q2[:], in0=q[:], in1=Ee[:], op=ALU.mult), reads=["hq", "hE"], writes=["hQ2"])
                sub_cols(Bend, "hBend")
                S.op("act", lambda e: e.activation(out=Ee[:], in_=Dd[:], func=AF.Exp, scale=-1.0), reads=["hD", "hQ2"], writes=["hE"])
                S.op("dve", lambda e: e.tensor_tensor(out=K2[:], in0=kin[:], in1=Ee[:], op=ALU.mult), reads=["hkin", "hE"], writes=["hK2"])
                for j in range(NT):
                    S.op("pe", lambda e, j=j: e.transpose(ptr[:, 0:128], K2[:, j * 128:(j + 1) * 128], self.ident_b()), reads=["hK2", "cstb"], writes=["hptr"])
                    S.op("act", lambda e, j=j: e.activation(out=k2t[:, j, :], in_=ptr[:, 0:128], func=AF.Copy), reads=["hptr"], writes=["hk2t"])
                def pre(c):
                    r0 = (c % 2) * 64
                    cs = slice(c * 64, (c + 1) * 64)
                    ps_s = pss[c % 2]; kps = "hpss%d" % (c % 2)
                    pT = pTs[c % 2]; kpT = "hpT%d" % (c % 2)
                    S.op("pe", lambda e: e.matmul(ps_s[r0:r0 + 64, 0:64], lhsT=K1[:, cs], rhs=Q1[:, cs], start=True, stop=True), reads=["hK1", "hQ1"], writes=[kps])
                    S.op("dve", lambda e: e.tensor_copy(out=pT[r0:r0 + 64, :], in_=ps_s[r0:r0 + 64, 0:64]), reads=[kps], writes=[kpT])
                    S.op("pool", lambda e: e.affine_select(out=pT[r0:r0 + 64, :], in_=pT[r0:r0 + 64, :], pattern=[[1, 64]], compare_op=ALU.is_ge, fill=0.0, base=0, channel_multiplier=-1),
                         reads=[kpT], writes=[kpT])

                pre(0)
                for c in range(32):
                    j, half = c // 2, c % 2
                    r0 = half * 64
                    cs = slice(c * 64, (c + 1) * 64)
                    pT = pTs[c % 2]; kpT = "hpT%d" % (c % 2)
                    po = pso[(c // 8) % 2]; kpo = "hpso%d" % ((c // 8) % 2)
                    ocs = slice((c % 8) * 64, (c % 8 + 1) * 64)
                    pst_ = psst[c % 2]; kpst = "hpsst%d" % (c % 2)
                    S.op("pe", lambda e, po=po, pT=pT, r0=r0, j=j, ocs=ocs, c=c: e.matmul(po[:, ocs], lhsT=hi[r0:r0 + 64, j, :], rhs=pT[r0:r0 + 64, :], start=True, stop=(c == 0)), reads=["hhi", kpT], writes=[kpo])
                    if c + 1 < 32:
                        pre(c + 1)
                    if c > 0:
                        S.op("pe", lambda e, po=po, ocs=ocs, cs=cs: e.matmul(po[:, ocs], lhsT=Stb[:], rhs=Q2[:, cs], start=False, stop=True), reads=["hStb", "hQ2"], writes=[kpo])
                    if c < 31:
                        S.op("pe", lambda e, pst_=pst_, r0=r0, j=j: e.matmul(pst_[:, 0:128], lhsT=k2t[r0:r0 + 64, j, :], rhs=hi[r0:r0 + 64, j, :], start=True, stop=True), reads=["hk2t", "hhi"], writes=[kpst])
                        if c == 0:
                            S.op("dve", lambda e, pst_=pst_: e.tensor_copy(out=St[:], in_=pst_[:, 0:128]), reads=[kpst], writes=["hSt"])
                        else:
                            S.op("dve", lambda e, pst_=pst_, c=c: e.scalar_tensor_tensor(out=St[:], in0=St[:], scalar=dec[:, c:c + 1], in1=pst_[:, 0:128], op0=ALU.mult, op1=ALU.add), reads=[kpst, "hSt", "hdec"], writes=["hSt"])
                        S.op("act", lambda e: e.activation(out=Stb[:], in_=St[:], func=AF.Copy), reads=["hSt"], writes=["hStb"])
                    if c % 8 == 7:
                        tb = c // 8
                        S.op("act", lambda e, po=po, tb=tb: e.activation(out=oT[:, tb * 512:(tb + 1) * 512], in_=po[:], func=AF.Copy), reads=[kpo], writes=["hoT"])
                gcol = self.smc("hgn", l, h)
                for tb in range(4):
                    bs = slice(tb * 512, (tb + 1) * 512)
                    S.op("act", lambda e, bs=bs: e.activation(out=sq[:], in_=oT[:, bs], func=AF.Square), reads=["hoT"], writes=["hsq"])
                    S.op("pe", lambda e: e.matmul(psn[:], lhsT=self.ones_f(), rhs=sq[:], start=True, stop=True), reads=["cst", "hsq"], writes=["hpsn"])
                    S.op("dve", lambda e: e.tensor_scalar(out=rs[:], in0=psn[:], scalar1=1.0 / 128, scalar2=EPS, op0=ALU.mult, op1=ALU.add), reads=["hpsn"], writes=["hrs"])
                    S.op("act", lambda e: e.activation(out=rs[:], in_=rs[:], func=AF.Sqrt), reads=["hrs"], writes=["hrs"])
                    S.op("dve", lambda e: e.reciprocal(out=rs[:], in_=rs[:]), reads=["hrs"], writes=["hrs"])
                    S.op("dve", lambda e, bs=bs: e.tensor_tensor(out=yb[:], in0=oT[:, bs], in1=rs[:], op=ALU.mult), reads=["hoT", "hrs"], writes=["hyb"])
                    S.op("dve", lambda e, bs=bs, gcol=gcol: e.scalar_tensor_tensor(out=ybb[:], in0=yb[:], scalar=gcol, in1=hg[:, bs], op0=ALU.mult, op1=ALU.mult), reads=["hyb", "sm", "hhg"], writes=["hybb"])
                    S.dma("sp", lambda e, bs=bs, rows=rows: e.dma_start(out=self.yT_d[1, rows, bs], in_=ybb[:]), reads=["hybb"], writes=["yT_d"])

    def stage_mlstm(self, l):
        nc, S = self.nc, self.S
        with ExitStack() as es:
            sb = lambda n, sh, dt: es.enter_context(self.sbt(n, sh, dt))
            pt = lambda n, sh, dt: es.enter_context(self.pst(n, sh, dt))
            gi, gf = sb("mgi", [4, S_LEN], F32), sb("mgf", [4, S_LEN], F32)
            Bc, aa, AA = sb("mB", [4, S_LEN], F32), sb("ma", [4, S_LEN], F32), sb("mA", [4, S_LEN], F32)
            em, Dr = sb("mem", [4, S_LEN], F32), sb("mDr", [4, S_LEN], F32)
            uu, ww, it = sb("mu", [4, S_LEN], F32), sb("mw", [4, S_LEN], F32), sb("mit", [4, S_LEN], F32)
            Aend, Aprev, decr = sb("mAend", [4, 32], F32), sb("mAprev", [4, 32], F32), sb("mdecr", [4, 32], F32)
            nbf = sb("mnbf", [4, 1], F32)
            og, _ = SM_OFF["gateb"]
            bi = self.sm[0:4, og + l * 2: og + l * 2 + 1]
            bf = self.sm[0:4, og + l * 2 + 1: og + l * 2 + 2]
            S.dma("sp", lambda e: e.dma_start(out=gi[:], in_=self.gT_d[0, :, :]), reads=["projout"], writes=["mgi"])
            S.dma("sp", lambda e: e.dma_start(out=gf[:], in_=self.gT_d[1, :, :]), reads=["projout"], writes=["mgf"])
            S.op("dve", lambda e: e.tensor_scalar(out=gi[:], in0=gi[:], scalar1=bi, scalar2=None, op0=ALU.add), reads=["mgi", "sm"], writes=["mgi"])
            S.op("dve", lambda e: e.tensor_scalar(out=nbf[:], in0=bf, scalar1=-1.0, scalar2=None, op0=ALU.mult), reads=["sm"], writes=["mnbf"])
            S.op("act", lambda e: e.activation(out=gf[:], in_=gf[:], func=AF.Exp, scale=-1.0, bias=nbf[:, 0:1]), reads=["mgf", "mnbf"], writes=["mgf"])
            S.op("act", lambda e: e.activation(out=gf[:], in_=gf[:], func=AF.Ln, bias=1.0), reads=["mgf"], writes=["mgf"])
            S.op("dve", lambda e: e.tensor_scalar(out=gf[:], in0=gf[:], scalar1=-1.0, scalar2=None, op0=ALU.mult), reads=["mgf"], writes=["mgf"])
            S.op("dve", lambda e: e.tensor_tensor_scan(out=Bc[:], data0=gf[:], data1=gf[:], initial=0.0, op0=ALU.add, op1=ALU.bypass), reads=["mgf"], writes=["mB"])
            S.op("dve", lambda e: e.tensor_tensor(out=aa[:], in0=gi[:], in1=Bc[:], op=ALU.subtract), reads=["mgi", "mB"], writes=["ma"])
            S.op("dve", lambda e: e.tensor_tensor_scan(out=AA[:], data0=aa[:], data1=aa[:], initial=0.0, op0=ALU.max, op1=ALU.bypass), reads=["ma"], writes=["mA"])
            S.op("dve", lambda e: e.tensor_tensor(out=em[:], in0=Bc[:], in1=AA[:], op=ALU.add), reads=["mB", "mA"], writes=["mem"])
            S.op("act", lambda e: e.activation(out=em[:], in_=em[:], func=AF.Exp, scale=-1.0), reads=["mem"], writes=["mem"])
            A3 = AA[:].rearrange("p (c t) -> p c t", t=64)
            a3 = aa[:].rearrange("p (c t) -> p c t", t=64)
            D3 = Dr[:].rearrange("p (c t) -> p c t", t=64)
            S.op("dve", lambda e: e.tensor_copy(out=Aend[:], in_=A3[:, :, 63]), reads=["mA"], writes=["mAend"])
            S.op("dve", lambda e: e.memset(Aprev[:, 0:1], 0.0), writes=["mAprev"])
            S.op("dve", lambda e: e.tensor_copy(out=Aprev[:, 1:32], in_=A3[:, 0:31, 63]), reads=["mA"], writes=["mAprev"])
            S.op("dve", lambda e: e.tensor_tensor(out=decr[:], in0=Aprev[:], in1=Aend[:], op=ALU.subtract), reads=["mAprev", "mAend"], writes=["mdecr"])
            S.op("act", lambda e: e.activation(out=decr[:], in_=decr[:], func=AF.Exp), reads=["mdecr"], writes=["mdecr"])
            bc = lambda col: col[:].unsqueeze(2).to_broadcast([4, 32, 64])
            S.op("dve", lambda e: e.tensor_tensor(out=D3, in0=A3, in1=bc(Aend), op=ALU.subtract), reads=["mA", "mAend"], writes=["mDr"])
            S.op("act", lambda e: e.activation(out=uu[:], in_=Dr[:], func=AF.Exp, scale=-1.0), reads=["mDr"], writes=["mu"])
            S.op("dve", lambda e: e.tensor_tensor(out=D3, in0=a3, in1=bc(Aend), op=ALU.subtract), reads=["ma", "mAend", "mu"], writes=["mDr"])
            S.op("act", lambda e: e.activation(out=ww[:], in_=Dr[:], func=AF.Exp), reads=["mDr"], writes=["mw"])
            S.op("dve", lambda e: e.tensor_tensor(out=D3, in0=A3, in1=bc(Aprev), op=ALU.subtract), reads=["mA", "mAprev", "mw"], writes=["mDr"])
            S.op("act", lambda e: e.activation(out=it[:], in_=Dr[:], func=AF.Exp, scale=-1.0), reads=["mDr"], writes=["mit"])
            xr, acc = sb("mxr", [128, S_LEN], F32), sb("macc2", [128, S_LEN], F32)
            Q1, Q2, K1 = sb("mQ1", [128, S_LEN], BF16), sb("mQ2", [128, S_LEN], BF16), sb("mK1", [128, S_LEN], BF16)
            vv = sb("mvv", [128, NT, 128], BF16)
            k2t = sb("mk2t", [128, NT, 128], BF16)
            mo = sb("mmo", [128, S_LEN], F32)
            hT = sb("mhT", [128, S_LEN], F32)
            dec = sb("mdec", [128, 32], F32)
            Cs, Csb = sb("mCs", [128, 128], F32), sb("mCsb", [128, 128], BF16)
            Ns, Nsb = sb("mNs", [128, 128], F32), sb("mNsb", [128, 128], BF16)
            pTs = [sb("mpT%d" % i, [128, 64], BF16) for i in range(2)]
            numT, embc, dmx = sb("mnumT", [128, 512], F32), sb("membc", [128, 512], F32), sb("mdmx", [128, 512], F32)
            sq, rs, yb, ybb = sb("msq", [128, 512], F32), sb("mrs", [128, 512], F32), sb("myb", [128, 512], F32), sb("mybb", [128, 512], BF16)
            pss = pt("mpss", [128, 512], F32)
            pso = [pt("mpso%d" % i, [128, 512], F32) for i in range(2)]
            psd = [pt("mpsd%d" % i, [128, 512], F32) for i in range(2)]
            pstC, pstN = pt("mpstC", [128, 512], F32), pt("mpstN", [128, 512], F32)
            pmisc = pt("mpmisc", [128, 512], F32)
            pmisc_b = pmisc[:].bitcast(BF16)
            KM = "mpmisc"
            ones_b = self.ones_b()

            def bcast_rows(rows, h, tb):
                S.op("pe", lambda e: e.matmul(pmisc[:], lhsT=self.cst[0:4, C_SEL + h * 128: C_SEL + (h + 1) * 128], rhs=rows[0:4, tb * 512:(tb + 1) * 512], start=True, stop=True),
                     reads=["cst", "mu", "mw", "mit", "mem"], writes=[KM])

            def conv_silu(chunk):
                cw = lambda tap: self.smc("convw", l, tap * 8 + chunk)
                S.op("dve", lambda e: e.tensor_scalar(out=acc[:], in0=xr[:], scalar1=cw(3), scalar2=self.smc("convb", l, chunk), op0=ALU.mult, op1=ALU.add), reads=["mxr", "sm"], writes=["macc2"])
                for sh in (1, 2, 3):
                    S.op("dve", lambda e, sh=sh: e.scalar_tensor_tensor(out=acc[:, sh:S_LEN], in0=xr[:, 0:S_LEN - sh], scalar=cw(3 - sh), in1=acc[:, sh:S_LEN], op0=ALU.mult, op1=ALU.add),
                         reads=["mxr", "sm", "macc2"], writes=["macc2"])
                S.op("act", lambda e: e.activation(out=acc[:], in_=acc[:], func=AF.Silu), reads=["macc2"], writes=["macc2"])

            for h in range(4):
                rows = slice(h * 128, (h + 1) * 128)
                S.dma("sp", lambda e, rows=rows: e.dma_start(out=xr[:], in_=self.mqkT_d[rows, :]), reads=["projout"], writes=["mxr"])
                S.dma("sp", lambda e, rows=rows: e.dma_start(out=mo[:], in_=self.moT_d[rows, :]), reads=["projout"], writes=["mmo"])
                S.dma("sp", lambda e, rows=rows: e.dma_start(out=vv[:], in_=self.mv_d[:, rows].rearrange("(j p) v -> p j v", p=128)), reads=["projout"], writes=["mvv"])
                conv_silu(h)
                for tb in range(4):
                    bs = slice(tb * 512, (tb + 1) * 512)
                    bcast_rows(uu, h, tb)
                    S.op("dve", lambda e, bs=bs: e.tensor_tensor(out=Q1[:, bs], in0=acc[:, bs], in1=pmisc[:], op=ALU.mult), reads=["macc2", KM], writes=["mQ1"])
                    bcast_rows(it, h, tb)
                    S.op("dve", lambda e, bs=bs: e.tensor_tensor(out=Q2[:, bs], in0=acc[:, bs], in1=pmisc[:], op=ALU.mult), reads=["macc2", KM], writes=["mQ2"])
                S.dma("sp", lambda e, h=h: e.dma_start(out=xr[:], in_=self.mqkT_d[512 + h * 128: 512 + (h + 1) * 128, :]), reads=["projout"], writes=["mxr"])
                conv_silu(4 + h)
                for tb in range(4):
                    bs = slice(tb * 512, (tb + 1) * 512)
                    bcast_rows(ww, h, tb)
                    S.op("dve", lambda e, bs=bs: e.scalar_tensor_tensor(out=K1[:, bs], in0=acc[:, bs], scalar=128.0 ** -0.5, in1=pmisc[:], op0=ALU.mult, op1=ALU.mult), reads=["macc2", KM], writes=["mK1"])
                S.op("pe", lambda e, h=h: e.matmul(pmisc[:, 0:32], lhsT=self.cst[0:4, C_SEL + h * 128: C_SEL + (h + 1) * 128], rhs=decr[0:4, :], start=True, stop=True), reads=["cst", "mdecr"], writes=[KM])
                S.op("act", lambda e: e.activation(out=dec[:], in_=pmisc[:, 0:32], func=AF.Copy), reads=[KM], writes=["mdec"])
                for j in range(NT):
                    S.op("pe", lambda e, j=j: e.transpose(pmisc_b[:, 0:128], K1[:, j * 128:(j + 1) * 128], self.ident_b()), reads=["mK1", "cstb"], writes=[KM])
                    S.op("act", lambda e, j=j: e.activation(out=k2t[:, j, :], in_=pmisc_b[:, 0:128], func=AF.Copy), reads=[KM], writes=["mk2t"])
                def pre(c):
                    r0 = (c % 2) * 64
                    cs = slice(c * 64, (c + 1) * 64)
                    pT = pTs[c % 2]; kpT = "mpT%d" % (c % 2)
                    S.op("pe", lambda e: e.matmul(pss[r0:r0 + 64, 0:64], lhsT=K1[:, cs], rhs=Q1[:, cs], start=True, stop=True), reads=["mK1", "mQ1"], writes=["mpss"])
                    S.op("dve", lambda e: e.tensor_copy(out=pT[r0:r0 + 64, :], in_=pss[r0:r0 + 64, 0:64]), reads=["mpss"], writes=[kpT])
                    S.op("pool", lambda e: e.affine_select(out=pT[r0:r0 + 64, :], in_=pT[r0:r0 + 64, :], pattern=[[1, 64]], compare_op=ALU.is_ge, fill=0.0, base=0, channel_multiplier=-1),
                         reads=[kpT], writes=[kpT])

                pre(0)
                for c in range(32):
                    j, half = c // 2, c % 2
                    r0 = half * 64
                    cs = slice(c * 64, (c + 1) * 64)
                    pT = pTs[c % 2]; kpT = "mpT%d" % (c % 2)
                    po = pso[(c // 8) % 2]; kpo = "mpso%d" % ((c // 8) % 2)
                    pd = psd[(c // 8) % 2]; kpd = "mpsd%d" % ((c // 8) % 2)
                    ocs = slice((c % 8) * 64, (c % 8 + 1) * 64)
                    S.op("pe", lambda e, po=po, pT=pT, r0=r0, j=j, ocs=ocs, c=c: e.matmul(po[:, ocs], lhsT=vv[r0:r0 + 64, j, :], rhs=pT[r0:r0 + 64, :], start=True, stop=(c == 0)), reads=["mvv", kpT], writes=[kpo])
                    if c > 0:
                        S.op("pe", lambda e, po=po, ocs=ocs, cs=cs: e.matmul(po[:, ocs], lhsT=Csb[:], rhs=Q2[:, cs], start=False, stop=True), reads=["mCsb", "mQ2"], writes=[kpo])
                    S.op("pe", lambda e, pd=pd, pT=pT, r0=r0, ocs=ocs, c=c: e.matmul(pd[:, ocs], lhsT=ones_b[r0:r0 + 64, :], rhs=pT[r0:r0 + 64, :], start=True, stop=(c == 0)), reads=["cstb", kpT], writes=[kpd])
                    if c + 1 < 32:
                        pre(c + 1)
                    if c > 0:
                        S.op("pe", lambda e, pd=pd, ocs=ocs, cs=cs: e.matmul(pd[:, ocs], lhsT=Nsb[:], rhs=Q2[:, cs], start=False, stop=True), reads=["mNsb", "mQ2"], writes=[kpd])
                    if c < 31:
                        S.op("pe", lambda e, r0=r0, j=j: e.matmul(pstC[:, 0:128], lhsT=k2t[r0:r0 + 64, j, :], rhs=vv[r0:r0 + 64, j, :], start=True, stop=True), reads=["mk2t", "mvv"], writes=["mpstC"])
                        S.op("pe", lambda e, r0=r0, j=j: e.matmul(pstN[:, 0:128], lhsT=k2t[r0:r0 + 64, j, :], rhs=ones_b[r0:r0 + 64, :], start=True, stop=True), reads=["mk2t", "cstb"], writes=["mpstN"])
                        if c == 0:
                            S.op("dve", lambda e: e.tensor_copy(out=Cs[:], in_=pstC[:, 0:128]), reads=["mpstC"], writes=["mCs"])
                            S.op("dve", lambda e: e.tensor_copy(out=Ns[:], in_=pstN[:, 0:128]), reads=["mpstN"], writes=["mNs"])
                        else:
                            S.op("dve", lambda e, c=c: e.scalar_tensor_tensor(out=Cs[:], in0=Cs[:], scalar=dec[:, c:c + 1], in1=pstC[:, 0:128], op0=ALU.mult, op1=ALU.add), reads=["mpstC", "mCs", "mdec"], writes=["mCs"])
                            S.op("dve", lambda e, c=c: e.scalar_tensor_tensor(out=Ns[:], in0=Ns[:], scalar=dec[:, c:c + 1], in1=pstN[:, 0:128], op0=ALU.mult, op1=ALU.add), reads=["mpstN", "mNs", "mdec"], writes=["mNs"])
                        S.op("act", lambda e: e.activation(out=Csb[:], in_=Cs[:], func=AF.Copy), reads=["mCs"], writes=["mCsb"])
                        S.op("act", lambda e: e.activation(out=Nsb[:], in_=Ns[:], func=AF.Copy), reads=["mNs"], writes=["mNsb"])
                    if c % 8 == 7:
                        tb = c // 8
                        bs = slice(tb * 512, (tb + 1) * 512)
                        S.op("act", lambda e, po=po: e.activation(out=numT[:], in_=po[:], func=AF.Copy), reads=[kpo], writes=["mnumT"])
                        bcast_rows(em, h, tb)
                        S.op("act", lambda e: e.activation(out=embc[:], in_=pmisc[:], func=AF.Copy), reads=[KM], writes=["membc"])
                        S.op("act", lambda e, pd=pd: e.activation(out=dmx[:], in_=pd[:], func=AF.Abs), reads=[kpd], writes=["mdmx"])
                        S.op("dve", lambda e: e.tensor_tensor(out=dmx[:], in0=dmx[:], in1=embc[:], op=ALU.max), reads=["mdmx", "membc"], writes=["mdmx"])
                        S.op("dve", lambda e: e.reciprocal(out=dmx[:], in_=dmx[:]), reads=["mdmx"], writes=["mdmx"])
                        S.op("dve", lambda e, bs=bs: e.tensor_tensor(out=hT[:, bs], in0=numT[:], in1=dmx[:], op=ALU.mult), reads=["mnumT", "mdmx"], writes=["mhT"])
                gcol = self.smc("mln", l, h)
                for tb in range(4):
                    bs = slice(tb * 512, (tb + 1) * 512)
                    S.op("act", lambda e, bs=bs: e.activation(out=sq[:], in_=hT[:, bs], func=AF.Square), reads=["mhT"], writes=["msq"])
                    S.op("pe", lambda e: e.matmul(pmisc[:], lhsT=self.ones_f(), rhs=sq[:], start=True, stop=True), reads=["cst", "msq"], writes=[KM])
                    S.op("dve", lambda e: e.tensor_scalar(out=rs[:], in0=pmisc[:], scalar1=1.0 / 128, scalar2=EPS, op0=ALU.mult, op1=ALU.add), reads=[KM], writes=["mrs"])
                    S.op("act", lambda e: e.activation(out=rs[:], in_=rs[:], func=AF.Sqrt), reads=["mrs"], writes=["mrs"])
                    S.op("dve", lambda e: e.reciprocal(out=rs[:], in_=rs[:]), reads=["mrs"], writes=["mrs"])
                    S.op("dve", lambda e, bs=bs: e.tensor_tensor(out=yb[:], in0=hT[:, bs], in1=rs[:], op=ALU.mult), reads=["mhT", "mrs"], writes=["myb"])
                    S.op("dve", lambda e, bs=bs, gcol=gcol: e.scalar_tensor_tensor(out=ybb[:], in0=yb[:], scalar=gcol, in1=mo[:, bs], op0=ALU.mult, op1=ALU.mult), reads=["myb", "sm", "mmo"], writes=["mybb"])
                    S.dma("sp", lambda e, bs=bs, rows=rows: e.dma_start(out=self.yT_d[2, rows, bs], in_=ybb[:]), reads=["mybb"], writes=["yT_d"])

    def _bcast_tile(self, es, name, colfn, ps_ap=None, ps_key=None):
        nc, S = self.nc, self.S
        dg = es.enter_context(self.sbt(name + "dg", [128, 128], F32))
        dst = es.enter_context(self.sbt(name, [128, D], F32))
        if ps_ap is None:
            ps = es.enter_context(self.pst(name + "ps", [128, D], F32))[:]
            ps_key = name + "ps"
        else:
            ps = ps_ap
        for c in range(8):
            S.op("dve", lambda e, c=c: e.tensor_scalar(out=dg[:], in0=self.ident_f(), scalar1=colfn(c), scalar2=None, op0=ALU.mult),
                 reads=["cst", "modT", "sm"], writes=[name + "dg"])
            S.op("pe", lambda e, c=c: e.matmul(ps[:, c * 128:(c + 1) * 128], lhsT=self.ones_f(), rhs=dg[:], start=True, stop=True), reads=["cst", name + "dg"], writes=[ps_key])
        for hh in range(2):
            S.op("act", lambda e, hh=hh: e.activation(out=dst[:, hh * 512:(hh + 1) * 512], in_=ps[:, hh * 512:(hh + 1) * 512], func=AF.Copy), reads=[ps_key], writes=[name])
        return dst

    def stage_merge(self, l):
        nc, S = self.nc, self.S
        with ExitStack() as es:
            sb = lambda n, sh, dt: es.enter_context(self.sbt(n, sh, dt))
            pt = lambda n, sh, dt: es.enter_context(self.pst(n, sh, dt))
            g1bc = self._bcast_tile(es, "g1bc", lambda c: self.modc(l, 2, c))
            wb = sb("mwb", [128, 3, 4, D], BF16)
            wo = sb("mwo", [128, 8, D], BF16)
            yt = sb("myt", [128, 3, 4, 512], BF16)
            gts = [sb("mgt%d" % i, [128, 512], F32) for i in range(3)]
            tmps = [sb("mtmp%d" % i, [128, 512], F32) for i in range(2)]
            macc = sb("macc", [128, 512], F32)
            mT = sb("mT", [128, 8, 512], BF16)
            xts = [sb("mxt%d" % i, [128, D], F32) for i in range(2)]
            ytmp = sb("mytmp", [128, 512], F32)
            psa = [pt("mpsa%d" % i, [128, 512], F32) for i in range(2)]
            psy = [pt("mpsy%d" % i, [128, 512], F32) for i in range(2)]
            for g in range(3):
                S.dma("pool", lambda e, g=g: e.dma_start(out=wb[:, g, :, :], in_=self.w_branch[l, g, :, :].rearrange("(kc p) d -> p kc d", p=128)), writes=["mwb"])
            S.dma("pool", lambda e: e.dma_start(out=wo[:], in_=self.w_out[l, :, :].rearrange("(c p) d -> p c d", p=128)), writes=["mwo"])
            ai = 0
            gi = 0
            yi = 0
            for tb in range(4):
                for g in range(3):
                    S.dma("sp", lambda e, g=g, tb=tb: e.dma_start(out=yt[:, g, :, :], in_=self.yT_d[g, :, tb * 512:(tb + 1) * 512].rearrange("(kc p) t -> p kc t", p=128)),
                          reads=["yT_d"], writes=["myt"])
                for dc in range(8):
                    for g in range(3):
                        ps = psa[ai % 2]; kps = "mpsa%d" % (ai % 2); ai += 1
                        gt = gts[gi % 3]; kgt = "mgt%d" % (gi % 3); gi += 1
                        S.dma("sp", lambda e, gt=gt, g=g, dc=dc, tb=tb: e.dma_start(out=gt[:], in_=self.bgT_d[g * 1024 + dc * 128: g * 1024 + (dc + 1) * 128, tb * 512:(tb + 1) * 512]),
                              reads=["projout"], writes=[kgt])
                        for kc in range(4):
                            S.op("pe", lambda e, ps=ps, g=g, kc=kc, dc=dc: e.matmul(ps[:], lhsT=wb[:, g, kc, dc * 128:(dc + 1) * 128], rhs=yt[:, g, kc, :], start=(kc == 0), stop=(kc == 3)),
                                 reads=["mwb", "myt"], writes=[kps])
                        if g == 0:
                            S.op("dve", lambda e, ps=ps, gt=gt: e.tensor_tensor(out=macc[:], in0=ps[:], in1=gt[:], op=ALU.mult), reads=[kps, kgt], writes=["macc"])
                        else:
                            tmp = tmps[g % 2]; ktmp = "mtmp%d" % (g % 2)
                            S.op("dve", lambda e, ps=ps, gt=gt, tmp=tmp: e.tensor_tensor(out=tmp[:], in0=ps[:], in1=gt[:], op=ALU.mult), reads=[kps, kgt], writes=[ktmp])
                            if g == 1:
                                S.op("pool", lambda e, tmp=tmp: e.tensor_tensor(out=macc[:], in0=macc[:], in1=tmp[:], op=ALU.add), reads=[ktmp, "macc"], writes=["macc"])
                            else:
                                S.op("pool", lambda e, tmp=tmp, dc=dc: e.tensor_tensor(out=mT[:, dc, :], in0=macc[:], in1=tmp[:], op=ALU.add), reads=[ktmp, "macc"], writes=["mT"])
                for tt in range(4):
                    t = tb * 4 + tt
                    xt = xts[t % 2]; kxt = "mxt%d" % (t % 2)
                    S.dma("sp", lambda e, xt=xt, t=t: e.dma_start(out=xt[:], in_=self.xres[t * 128:(t + 1) * 128, :]), reads=["xres"], writes=[kxt])
                    for dh in range(2):
                        ps = psy[yi % 2]; kps = "mpsy%d" % (yi % 2); yi += 1
                        for c in range(8):
                            S.op("pe", lambda e, ps=ps, c=c, tt=tt, dh=dh: e.matmul(ps[:], lhsT=mT[:, c, tt * 128:(tt + 1) * 128], rhs=wo[:, c, dh * 512:(dh + 1) * 512], start=(c == 0), stop=(c == 7)),
                                 reads=["mT", "mwo"], writes=[kps])
                        S.op("dve", lambda e, ps=ps, dh=dh: e.tensor_tensor(out=ytmp[:], in0=ps[:], in1=g1bc[:, dh * 512:(dh + 1) * 512], op=ALU.mult), reads=[kps, "g1bc"], writes=["mytmp"])
                        S.op("dve", lambda e, xt=xt, dh=dh: e.tensor_tensor(out=xt[:, dh * 512:(dh + 1) * 512], in0=xt[:, dh * 512:(dh + 1) * 512], in1=ytmp[:], op=ALU.add), reads=["mytmp", kxt], writes=[kxt])
                    S.dma("sp", lambda e, xt=xt, t=t: e.dma_start(out=self.xres[t * 128:(t + 1) * 128, :], in_=xt[:]), reads=[kxt], writes=["xres"])

    def uvcast_gen(self, l, cbs):
        nc, S = self.nc, self.S
        for k in range(32):
            cb = cbs[k % 3]; kcb = "uvc%d" % (k % 3)
            src = self.pk_uv[l, k * 512:(k + 1) * 512, :].rearrange("(p r) d -> p r d", p=128)
            dst = self.uvb[k * 512:(k + 1) * 512, :].rearrange("(p r) d -> p r d", p=128)
            S.dma("pool", lambda e, cb=cb, src=src: e.dma_start(out=cb[:], in_=src), writes=[kcb])
            S.dma("sp", lambda e, cb=cb, dst=dst: e.dma_start(out=dst, in_=cb[:]), reads=[kcb], writes=["uvb"])
            yield

    def stage_peer(self, l):
        nc, S = self.nc, self.S
        NB = 8
        with ExitStack() as es:
            sb = lambda n, sh, dt: es.enter_context(self.sbt(n, sh, dt))
            pt = lambda n, sh, dt: es.enter_context(self.pst(n, sh, dt))
            two = lambda n, sh, dt: [sb("%s%d" % (n, i), sh, dt) for i in range(2)]
            hTs = two("phT", [128, 8, 128], BF16)
            wq = sb("pwq", [128, 8, D], BF16)
            kbd = sb("pkbd", [128, 8, 256], F32)
            qTts = two("pqTt", [128, 8, 128], F32)
            scs, s2s, cands = two("psc", [128, 2048], F32), two("ps2", [128, 2048], F32), two("pcand", [128, 2048], F32)
            mxs, mis, sifs = two("pmx", [128, 256], F32), two("pmi", [128, 256], U32), two("psif", [128, 256], F32)
            tvs, tps, tpfs = two("ptv", [128, 128], F32), two("ptp", [128, 128], U32), two("ptpf", [128, 128], F32)
            afs, bfs = two("paf", [128, 128], F32), two("pbf", [128, 128], F32)
            i1s, i2s = two("pi1", [128, 128], F32), two("pi2", [128, 128], F32)
            idxis = two("pidxi", [128, 128], I32)
            ees, ggs = two("pee", [128, 128], F32), two("pgg", [128, 128], F32)
            ssums = two("pssum", [128, 8], F32)
            aa, ga, gl = sb("paa", [128, 128], F32), sb("pga", [128, 128], F32), sb("pgl", [128, 128], F32)
            h2ts = two("ph2t", [128, D], F32)
            xts = two("pxt", [128, D], F32)
            ubs = [sb("pub%d" % i, [128, 2 * D], BF16) for i in range(NB)]
            junks = two("pjunk", [128, D], F32)
            acc = sb("pacc", [128, D], F32)
            tmpbs = [sb("ptmpb%d" % i, [128, D], BF16) for i in range(4)]
            psq = pt("ppsq", [128, 8, 128], F32)
            pssc = pt("ppssc", [128, 2048], F32)
            pacc = pt("ppacc", [128, D], F32)
            g2bc = self._bcast_tile(es, "g2bc", lambda c: self.modc(l, 5, c), ps_ap=pssc[:, 0:D], ps_key="ppssc")
            UV2 = self.uvb
            iota16 = self.cst[:, C_IOTA:C_IOTA + 16]
            thr15 = self.cst[:, C_THR:C_THR + 15]
            S.dma("pool", lambda e: e.dma_start(out=wq[:], in_=self.pk_wq[l, :, :].rearrange("(kc p) d -> p kc d", p=128)), writes=["pwq"])
            S.dma("sp", lambda e: e.dma_start(out=kbd[:], in_=self.keysbd[l, :, :, :].rearrange("h p n -> p h n")), writes=["pkbd"])
            B4 = [128, 8, 16, 16]

            def prep(t):
                p = t % 2
                K = lambda n: "%s%d" % (n, p)
                ts = slice(t * 128, (t + 1) * 128)
                h2t, xt, hT, qTt = h2ts[p], xts[p], hTs[p], qTts[p]
                sc, s2, cand, mx, mi, sif = scs[p], s2s[p], cands[p], mxs[p], mis[p], sifs[p]
                tv, tp, tpf, af, bf_, i1, i2, idxi, ee, gg, ssum = tvs[p], tps[p], tpfs[p], afs[p], bfs[p], i1s[p], i2s[p], idxis[p], ees[p], ggs[p], ssums[p]
                sc3 = sc[:].rearrange("p (g n) -> p g n", n=128)
                s23 = s2[:].rearrange("p (g n) -> p g n", n=128)
                mx3 = mx[:].rearrange("p (g k) -> p g k", k=16)
                mi3 = mi[:].rearrange("p (g k) -> p g k", k=16)
                mx4 = mx[:].rearrange("p (h q k) -> p h q k", h=8, q=2)
                sif4 = sif[:].rearrange("p (h q k) -> p h q k", h=8, q=2)
                cand4 = cand[:].rearrange("p (h a b) -> p h a b", h=8, a=16)
                tv3 = tv[:].rearrange("p (h k) -> p h k", h=8)
                tp3 = tp[:].rearrange("p (h k) -> p h k", h=8)
                oh4 = s2[:].rearrange("p (h k a) -> p h k a", h=8, k=16)
                cmp3 = sc[:, 0:1920].rearrange("p (r j) -> p r j", j=15)
                S.dma("sp", lambda e: e.dma_start(out=h2t[:], in_=self.h2_d[ts, :]), reads=["h2_d"], writes=[K("ph2t")])
                S.dma("sp", lambda e: e.dma_start(out=xt[:], in_=self.xres[ts, :]), reads=["xres"], writes=[K("pxt")])
                S.dma("sp", lambda e: e.dma_start(out=hT[:], in_=self.hT_d[:, :, ts].rearrange("c p t -> p c t")), reads=["hT_d"], writes=[K("phT")])
                yield
                for h in range(8):
                    for kc in range(8):
                        S.op("pe", lambda e, h=h, kc=kc: e.matmul(psq[:, h, :], lhsT=wq[:, kc, h * 128:(h + 1) * 128], rhs=hT[:, kc, :], start=(kc == 0), stop=(kc == 7)),
                             reads=["pwq", K("phT")], writes=["ppsq"])
                    yield
                for hh in range(2):
                    S.op("act", lambda e, hh=hh: e.activation(out=qTt[:, hh * 4:(hh + 1) * 4, :], in_=psq[:, hh * 4:(hh + 1) * 4, :], func=AF.Copy), reads=["ppsq"], writes=[K("pqTt")])
                for h in range(8):
                    S.op("pe", lambda e, h=h: e.matmul(pssc[:, h * 256:(h + 1) * 256], lhsT=qTt[:, h, :], rhs=kbd[:, h, :], start=True, stop=True), reads=[K("pqTt"), "pkbd"], writes=["ppssc"])
                for qd in range(4):
                    S.op("act", lambda e, qd=qd: e.activation(out=sc[:, qd * 512:(qd + 1) * 512], in_=pssc[:, qd * 512:(qd + 1) * 512], func=AF.Copy), reads=["ppssc"], writes=[K("psc")])
                yield
                for g in range(16):
                    S.op("dve", lambda e, g=g: e.max(out=mx3[:, g, 0:8], in_=sc3[:, g, :]), reads=[K("psc")], writes=[K("pmx")])
                    S.op("dve", lambda e, g=g: e.max_index(out=mi3[:, g, 0:8], in_max=mx3[:, g, 0:8], in_values=sc3[:, g, :]), reads=[K("psc"), K("pmx")], writes=[K("pmi")])
                    yield
                    S.op("dve", lambda e, g=g: e.match_replace(out=s23[:, g, :], in_to_replace=mx3[:, g, 0:8], in_values=sc3[:, g, :], imm_value=-1e30), reads=[K("psc"), K("pmx")], writes=[K("ps2")])
                    S.op("dve", lambda e, g=g: e.max(out=mx3[:, g, 8:16], in_=s23[:, g, :]), reads=[K("ps2")], writes=[K("pmx")])
                    yield
                    S.op("dve", lambda e, g=g: e.max_index(out=mi3[:, g, 8:16], in_max=mx3[:, g, 8:16], in_values=s23[:, g, :]), reads=[K("ps2"), K("pmx")], writes=[K("pmi")])
                    yield
                S.op("dve", lambda e: e.tensor_copy(out=sif[:], in_=mi[:]), reads=[K("pmi")], writes=[K("psif")])
                S.op("dve", lambda e: e.tensor_tensor(out=cand4, in0=mx4[:, :, 0, :].unsqueeze(3).to_broadcast(B4), in1=mx4[:, :, 1, :].unsqueeze(2).to_broadcast(B4), op=ALU.add),
                     reads=[K("pmx")], writes=[K("pcand")])
                yield
                for h in range(8):
                    hs = slice(h * 256, (h + 1) * 256)
                    S.op("dve", lambda e, h=h, hs=hs: e.max(out=tv3[:, h, 0:8], in_=cand[:, hs]), reads=[K("pcand")], writes=[K("ptv")])
                    S.op("dve", lambda e, h=h, hs=hs: e.max_index(out=tp3[:, h, 0:8], in_max=tv3[:, h, 0:8], in_values=cand[:, hs]), reads=[K("pcand"), K("ptv")], writes=[K("ptp")])
                    yield
                    S.op("dve", lambda e, h=h, hs=hs: e.match_replace(out=s2[:, hs], in_to_replace=tv3[:, h, 0:8], in_values=cand[:, hs], imm_value=-1e30), reads=[K("pcand"), K("ptv")], writes=[K("ps2")])
                    S.op("dve", lambda e, h=h, hs=hs: e.max(out=tv3[:, h, 8:16], in_=s2[:, hs]), reads=[K("ps2")], writes=[K("ptv")])
                    yield
                    S.op("dve", lambda e, h=h, hs=hs: e.max_index(out=tp3[:, h, 8:16], in_max=tv3[:, h, 8:16], in_values=s2[:, hs]), reads=[K("ps2"), K("ptv")], writes=[K("ptp")])
                    yield
                S.op("dve", lambda e: e.tensor_copy(out=tpf[:], in_=tp[:]), reads=[K("ptp")], writes=[K("ptpf")])
                S.op("dve", lambda e: e.tensor_tensor(out=cmp3, in0=tpf[:].unsqueeze(2).to_broadcast([128, 128, 15]), in1=thr15.unsqueeze(1).to_broadcast([128, 128, 15]), op=ALU.is_ge),
                     reads=[K("ptpf"), "cst"], writes=[K("psc")])
                yield
                S.op("dve", lambda e: e.tensor_reduce(out=af[:], in_=cmp3, axis=AX.X, op=ALU.add), reads=[K("psc")], writes=[K("paf")])
                S.op("dve", lambda e: e.scalar_tensor_tensor(out=bf_[:], in0=af[:], scalar=-16.0, in1=tpf[:], op0=ALU.mult, op1=ALU.add), reads=[K("paf"), K("ptpf")], writes=[K("pbf")])
                yield
                for (src, q_, dst, kd) in ((af, 0, i1, K("pi1")), (bf_, 1, i2, K("pi2"))):
                    ksrc = K("paf") if q_ == 0 else K("pbf")
                    S.op("dve", lambda e, src=src: e.tensor_tensor(out=oh4, in0=src[:].rearrange("p (h k) -> p h k", h=8).unsqueeze(3).to_broadcast(B4),
                                                                   in1=iota16.unsqueeze(1).unsqueeze(1).to_broadcast(B4), op=ALU.is_equal), reads=[ksrc, "cst"], writes=[K("ps2")])
                    yield
                    S.op("dve", lambda e, q_=q_: e.tensor_tensor(out=oh4, in0=oh4, in1=sif4[:, :, q_, :].unsqueeze(2).to_broadcast(B4), op=ALU.mult), reads=[K("ps2"), K("psif")], writes=[K("ps2")])
                    yield
                    S.op("dve", lambda e, dst=dst: e.tensor_reduce(out=dst[:], in_=oh4, axis=AX.X, op=ALU.add), reads=[K("ps2")], writes=[kd])
                    yield
                S.op("dve", lambda e: e.scalar_tensor_tensor(out=i1[:], in0=i1[:], scalar=128.0, in1=i2[:], op0=ALU.mult, op1=ALU.add), reads=[K("pi1"), K("pi2")], writes=[K("pi1")])
                S.op("dve", lambda e: e.tensor_copy(out=idxi[:], in_=i1[:]), reads=[K("pi1")], writes=[K("pidxi")])
                yield
                ee3 = ee[:].rearrange("p (h k) -> p h k", h=8)
                S.op("dve", lambda e: e.tensor_tensor(out=ee3, in0=tv3, in1=tv3[:, :, 0:1].to_broadcast([128, 8, 16]), op=ALU.subtract), reads=[K("ptv")], writes=[K("pee")])
                S.op("act", lambda e: e.activation(out=ee[:], in_=ee[:], func=AF.Exp), reads=[K("pee")], writes=[K("pee")])
                S.op("dve", lambda e: e.tensor_reduce(out=ssum[:], in_=ee3, axis=AX.X, op=ALU.add), reads=[K("pee")], writes=[K("pssum")])
                yield
                S.op("dve", lambda e: e.reciprocal(out=ssum[:], in_=ssum[:]), reads=[K("pssum")], writes=[K("pssum")])
                S.op("dve", lambda e: e.tensor_tensor(out=gg[:].rearrange("p (h k) -> p h k", h=8), in0=ee3, in1=ssum[:].unsqueeze(2).to_broadcast([128, 8, 16]), op=ALU.mult),
                     reads=[K("pee"), K("pssum")], writes=[K("pgg")])
                yield

            def exhaust(g):
                if g is not None:
                    for _ in g:
                        pass

            def advance(g, n):
                if g is None:
                    return
                for _ in range(n):
                    try:
                        next(g)
                    except StopIteration:
                        return

            exhaust(prep(0))
            ui = 0
            for t in range(NT):
                p = t % 2
                K = lambda n: "%s%d" % (n, p)
                ts = slice(t * 128, (t + 1) * 128)
                h2t, xt, idxi, gg = h2ts[p], xts[p], idxis[p], ggs[p]
                nxt = prep(t + 1) if t + 1 < NT else None
                for r in range(128):
                    ub = ubs[ui % NB]; kub = "pub%d" % (ui % NB)
                    jk = junks[ui % 2]; kjk = "pjunk%d" % (ui % 2)
                    tb_ = tmpbs[ui % 4]; ktb = "ptmpb%d" % (ui % 4)
                    ui += 1
                    ka, kg, kga = "paa%d" % r, "pgl%d" % r, "pga%d" % r
                    S.dma("pool", lambda e, ub=ub, r=r, idxi=idxi: e.indirect_dma_start(out=ub[:], out_offset=None, in_=UV2[:, :], in_offset=bass.IndirectOffsetOnAxis(ap=idxi[:, r:r + 1], axis=0)),
                          reads=[K("pidxi"), "uvb"], writes=[kub])
                    S.op("dve", lambda e, ub=ub, r=r, h2t=h2t, jk=jk: e.scalar_tensor_tensor(out=jk[:], in0=ub[:, 0:D], scalar=1.0, in1=h2t[:], op0=ALU.mult, op1=ALU.mult, accum_out=aa[:, r:r + 1]),
                         reads=[kub, K("ph2t")], writes=[kjk, ka])
                    S.op("act", lambda e, r=r: e.activation(out=gl[:, r:r + 1], in_=aa[:, r:r + 1], func=AF.Gelu_apprx_tanh), reads=[ka], writes=[kg])
                    S.op("act", lambda e, r=r, gg=gg: e.activation(out=ga[:, r:r + 1], in_=gl[:, r:r + 1], func=AF.Identity, scale=gg[:, r:r + 1]), reads=[kg, K("pgg")], writes=[kga])
                    S.op("act", lambda e, ub=ub, r=r, tb_=tb_: e.activation(out=tb_[:], in_=ub[:, D:2 * D], func=AF.Identity, scale=ga[:, r:r + 1]), reads=[kub, kga], writes=[ktb])
                    for dh in range(2):
                        S.op("pe", lambda e, tb_=tb_, dh=dh, r=r: e.matmul(pacc[:, dh * 512:(dh + 1) * 512], lhsT=self.ident_b(), rhs=tb_[:, dh * 512:(dh + 1) * 512], start=(r == 0), stop=(r == 127)),
                             reads=["cstb", ktb], writes=["ppacc"])
                    advance(nxt, 1)
                for dh in range(2):
                    S.op("dve", lambda e, dh=dh: e.tensor_tensor(out=acc[:, dh * 512:(dh + 1) * 512], in0=pacc[:, dh * 512:(dh + 1) * 512], in1=g2bc[:, dh * 512:(dh + 1) * 512], op=ALU.mult),
                         reads=["ppacc", "g2bc"], writes=["pacc"])
                S.op("dve", lambda e, xt=xt: e.tensor_tensor(out=xt[:], in0=xt[:], in1=acc[:], op=ALU.add), reads=["pacc", K("pxt")], writes=[K("pxt")])
                S.dma("sp", lambda e, xt=xt, ts=ts: e.dma_start(out=self.xres[ts, :], in_=xt[:]), reads=[K("pxt")], writes=["xres"])
                exhaust(nxt)

    def stage_final(self):
        nc, S = self.nc, self.S
        with ExitStack() as es:
            sb = lambda n, sh, dt: es.enter_context(self.sbt(n, sh, dt))
            o, _ = SM_OFF["fg"]
            fgbc = self._bcast_tile(es, "fgbc", lambda c: self.sm[:, o + c:o + c + 1])
            xts = [sb("fxt%d" % i, [128, D], F32) for i in range(2)]
            sq = sb("fsq", [128, D], F32)
            ss = sb("fss", [128, 4], F32)
            for t in range(NT):
                xt = xts[t % 2]; kxt = "fxt%d" % (t % 2)
                S.dma("sp", lambda e, xt=xt, t=t: e.dma_start(out=xt[:], in_=self.xres[t * 128:(t + 1) * 128, :]), reads=["xres"], writes=[kxt])
                S.op("act", lambda e, xt=xt: e.activation(out=sq[:], in_=xt[:], func=AF.Square, accum_out=ss[:, 0:1]), reads=[kxt], writes=["fsq", "fss"])
                S.op("dve", lambda e: e.tensor_scalar(out=ss[:, 1:2], in0=ss[:, 0:1], scalar1=1.0 / D, scalar2=EPS, op0=ALU.mult, op1=ALU.add), reads=["fss"], writes=["fss"])
                S.op("act", lambda e: e.activation(out=ss[:, 2:3], in_=ss[:, 1:2], func=AF.Sqrt), reads=["fss"], writes=["fss"])
                S.op("dve", lambda e: e.reciprocal(out=ss[:, 3:4], in_=ss[:, 2:3]), reads=["fss"], writes=["fss"])
                S.op("dve", lambda e, xt=xt: e.scalar_tensor_tensor(out=xt[:], in0=xt[:], scalar=ss[:, 3:4], in1=fgbc[:], op0=ALU.mult, op1=ALU.mult), reads=[kxt, "fss", "fgbc"], writes=[kxt])
                S.dma("sp", lambda e, xt=xt, t=t: e.dma_start(out=self.out_d[t * 128:(t + 1) * 128, :], in_=xt[:]), reads=[kxt], writes=["out_d"])


def prep_inputs(inputs):
    inp = {k: np.ascontiguousarray(np.asarray(v)) for k, v in inputs.items()}
    consts = make_consts()
    keys = inp["pk_keys"]
    kbd = np.zeros((DEPTH, 8, 128, 256), np.float32)
    for p in range(2):
        kbd[:, :, p * 64:(p + 1) * 64, p * 128:(p + 1) * 128] = keys[:, :, p].transpose(0, 1, 3, 2)
    shared = dict(consts=consts, mod_w=inp["mod_w"], w_in=inp["w_in"], w_branch=inp["w_branch"], w_out=inp["w_out"],
                  pk_wq=inp["pk_wq"], keysbd=kbd,
                  pk_uv=np.concatenate([inp["pk_u"], inp["pk_v"]], axis=2))
    in_maps = []
    for b in range(8):
        m = dict(shared)
        m["x"] = inp["x"][b]
        m["small"] = make_small(inp, b)
        in_maps.append(m)
    return in_maps


_PROG_CACHE = {}


def kernel(**inputs):
    in_maps = prep_inputs(inputs)
    if "nc" not in _PROG_CACHE:
        _PROG_CACHE["nc"] = Prog().build()
    res = run_bass_kernel_spmd(_PROG_CACHE["nc"], in_maps, core_ids=list(range(8)))
    return np.stack([np.asarray(r["out"]) for r in res.results], axis=0).astype(np.float32)
```
